# Optimizing a Trainium2 kernel written in Bass

```python
import jax, jax.numpy as jnp
from jax import lax
import numpy as np

D_MODEL = 1024
BATCH = 8
SEQ = 4096
DEPTH = 1

CHUNK = 64
LEFT_CHUNKS = 8
BAND = (LEFT_CHUNKS + 1) * CHUNK
N_HEADS = 8
HEAD_DIM = 64
ATTN_W = N_HEADS * HEAD_DIM
CONV_W = D_MODEL // 2
CONV_K = 3
MAX_REL_PAST = 256
NUM_REL = (CHUNK - 1) + MAX_REL_PAST + 1
PLE_DIM = 256
N_GROUPS = 4
EXPERTS_PER_GROUP = 8
N_EXPERTS = N_GROUPS * EXPERTS_PER_GROUP
TOP_K = 2
D_EXPERT = 512
BLK = 128
EPS = 1e-6

IN_WIDTHS = [ATTN_W, ATTN_W, ATTN_W, CONV_W, CONV_W, CONV_W, D_MODEL, D_MODEL]
IN_TOTAL = sum(IN_WIDTHS)
SPLITS = [int(v) for v in np.cumsum(IN_WIDTHS)[:-1]]

kernel_name = "hybrid_chunked_attn_shortconv_hmoe_block"


def rms_norm(x, g):
    xf = x.astype(jnp.float32)
    y = xf * lax.rsqrt(jnp.mean(xf * xf, axis=-1, keepdims=True) + EPS)
    return (y * g.astype(jnp.float32)).astype(x.dtype)


def chunked_rel_attention(q, k, v, g_q, g_k, rel_bias):
    b, s, h, dh = q.shape
    nc = s // CHUNK
    pad = LEFT_CHUNKS * CHUNK
    q = rms_norm(q, g_q)
    k = rms_norm(k, g_k)
    kp = jnp.pad(k, ((0, 0), (pad, 0), (0, 0), (0, 0)))
    vp = jnp.pad(v, ((0, 0), (pad, 0), (0, 0), (0, 0)))
    dist = jnp.arange(CHUNK)[:, None] - jnp.arange(BAND)[None, :] + pad
    idx = jnp.clip(dist, -(CHUNK - 1), MAX_REL_PAST) + (CHUNK - 1)
    bias = rel_bias[:, idx].astype(jnp.float32)
    qc = q.reshape(b, nc, CHUNK, h, dh).transpose(1, 0, 2, 3, 4)
    scale = dh ** -0.5

    def one_chunk(args):
        q_blk, c = args
        start = c * CHUNK
        kb = lax.dynamic_slice_in_dim(kp, start, BAND, axis=1)
        vb = lax.dynamic_slice_in_dim(vp, start, BAND, axis=1)
        sc = jnp.einsum('bqhd,bkhd->bhqk', q_blk, kb).astype(jnp.float32) * scale + bias
        valid = (start - pad + jnp.arange(BAND)) >= 0
        sc = jnp.where(valid[None, None, None, :], sc, -1e30)
        pr = jax.nn.softmax(sc, axis=-1).astype(vb.dtype)
        return jnp.einsum('bhqk,bkhd->bqhd', pr, vb)

    out = lax.map(one_chunk, (qc, jnp.arange(nc)))
    return out.transpose(1, 0, 2, 3, 4).reshape(b, s, h * dh)


def short_conv(u, bg, cg, w, bias):
    s = u.shape[1]
    zp = jnp.pad(cg * u, ((0, 0), (CONV_K - 1, 0), (0, 0)))
    y = bias
    for j in range(CONV_K):
        y = y + w[j] * zp[:, j:j + s]
    return bg * y


def hier_moe(xn, w_group, b_group, w_router, b_router, w1, w3, w2):
    t, d = xn.shape
    gl = (xn @ w_group).astype(jnp.float32) + b_group
    grp = jnp.argmax(gl, axis=-1)
    p_grp = jnp.take_along_axis(jax.nn.softmax(gl, axis=-1), grp[:, None], axis=1)
    el = ((xn @ w_router).astype(jnp.float32) + b_router).reshape(t, N_GROUPS, EXPERTS_PER_GROUP)
    el = jnp.take_along_axis(el, grp[:, None, None], axis=1)[:, 0]
    top_l, top_i = lax.top_k(el, TOP_K)
    wgt = (p_grp * jax.nn.softmax(top_l, axis=-1)).reshape(-1)
    eid = (grp[:, None] * EXPERTS_PER_GROUP + top_i).reshape(-1).astype(jnp.int32)
    tok = jnp.repeat(jnp.arange(t, dtype=jnp.int32), TOP_K)
    a = t * TOP_K
    order = jnp.argsort(eid)
    se = eid[order]
    counts = jnp.bincount(eid, length=N_EXPERTS)
    pcounts = (counts + BLK - 1) // BLK * BLK
    pends = jnp.cumsum(pcounts)
    pstarts = pends - pcounts
    starts = jnp.cumsum(counts) - counts
    dest = pstarts[se] + jnp.arange(a) - starts[se]
    n_rows = (a + BLK - 1) // BLK * BLK + N_EXPERTS * BLK
    n_blk = n_rows // BLK
    row_tok = jnp.zeros((n_rows,), jnp.int32).at[dest].set(tok[order])
    row_w = jnp.zeros((n_rows,), wgt.dtype).at[dest].set(wgt[order])
    blk_e = jnp.minimum(jnp.searchsorted(pends, jnp.arange(n_blk) * BLK, side='right'), N_EXPERTS - 1)
    xs = xn[row_tok].reshape(n_blk, BLK, d)

    def expert_block(args):
        xb, e = args
        hdn = jax.nn.silu(xb @ w1[e]) * (xb @ w3[e])
        return hdn @ w2[e]

    ys = lax.map(expert_block, (xs, blk_e)).reshape(n_rows, d)
    return jnp.zeros((t, d), xn.dtype).at[row_tok].add(ys * row_w[:, None].astype(ys.dtype))


def setup_inputs(seed: int = 0) -> dict:
    key = jax.random.key(seed)
    ks = jax.random.split(key, 32)
    nrm = lambda k, shape, sc: jax.random.normal(k, shape, jnp.float32) * sc
    L = DEPTH
    return {
        "x": nrm(ks[0], (BATCH, SEQ, D_MODEL), 1.0),
        "p": nrm(ks[1], (DEPTH, BATCH, SEQ, PLE_DIM), 1.0),
        "g_mix": 1.0 + nrm(ks[2], (L, D_MODEL), 0.02),
        "w_in": nrm(ks[3], (L, D_MODEL, IN_TOTAL), D_MODEL ** -0.5),
        "b_in": nrm(ks[4], (L, IN_TOTAL), 0.02),
        "g_q": 1.0 + nrm(ks[5], (L, HEAD_DIM), 0.02),
        "g_k": 1.0 + nrm(ks[6], (L, HEAD_DIM), 0.02),
        "rel_bias": nrm(ks[7], (L, N_HEADS, NUM_REL), 0.1),
        "conv_w": nrm(ks[8], (L, CONV_K, CONV_W), CONV_K ** -0.5),
        "conv_b": nrm(ks[9], (L, CONV_W), 0.02),
        "w_pa": nrm(ks[10], (L, ATTN_W, D_MODEL), ATTN_W ** -0.5),
        "w_pc": nrm(ks[11], (L, CONV_W, D_MODEL), CONV_W ** -0.5),
        "w_o": nrm(ks[12], (L, D_MODEL, D_MODEL), D_MODEL ** -0.5),
        "g_ffn": 1.0 + nrm(ks[13], (L, D_MODEL), 0.02),
        "w_group": nrm(ks[14], (L, D_MODEL, N_GROUPS), D_MODEL ** -0.5),
        "b_group": nrm(ks[15], (L, N_GROUPS), 0.01),
        "w_router": nrm(ks[16], (L, D_MODEL, N_EXPERTS), D_MODEL ** -0.5),
        "b_router": nrm(ks[17], (L, N_EXPERTS), 0.01),
        "w1": nrm(ks[18], (L, N_EXPERTS, D_MODEL, D_EXPERT), D_MODEL ** -0.5),
        "w3": nrm(ks[19], (L, N_EXPERTS, D_MODEL, D_EXPERT), D_MODEL ** -0.5),
        "w2": nrm(ks[20], (L, N_EXPERTS, D_EXPERT, D_MODEL), D_EXPERT ** -0.5),
        "g_ple": 1.0 + nrm(ks[21], (L, D_MODEL), 0.02),
        "w_ple_gate": nrm(ks[22], (L, D_MODEL, D_MODEL), D_MODEL ** -0.5),
        "b_ple_gate": nrm(ks[23], (L, D_MODEL), 0.02),
        "w_ple_proj": nrm(ks[24], (L, PLE_DIM, D_MODEL), PLE_DIM ** -0.5),
    }


def reference(x, p, g_mix, w_in, b_in, g_q, g_k, rel_bias, conv_w, conv_b, w_pa, w_pc, w_o,
              g_ffn, w_group, b_group, w_router, b_router, w1, w3, w2,
              g_ple, w_ple_gate, b_ple_gate, w_ple_proj):
    b, s, d = x.shape
    h = x
    for i in range(DEPTH):
        n = rms_norm(h, g_mix[i])
        z = n @ w_in[i] + b_in[i]
        q, k, v, u, bg, cg, ga, gc = jnp.split(z, SPLITS, axis=-1)
        ya = chunked_rel_attention(q.reshape(b, s, N_HEADS, HEAD_DIM),
                                   k.reshape(b, s, N_HEADS, HEAD_DIM),
                                   v.reshape(b, s, N_HEADS, HEAD_DIM),
                                   g_q[i], g_k[i], rel_bias[i])
        yc = short_conv(u, bg, cg, conv_w[i], conv_b[i])
        m = jax.nn.sigmoid(ga) * (ya @ w_pa[i]) + jax.nn.sigmoid(gc) * (yc @ w_pc[i])
        h = h + m @ w_o[i]
        n2 = rms_norm(h, g_ffn[i]).reshape(b * s, d)
        h = h + hier_moe(n2, w_group[i], b_group[i], w_router[i], b_router[i],
                         w1[i], w3[i], w2[i]).reshape(b, s, d)
        gate = jax.nn.sigmoid(rms_norm(h, g_ple[i]) @ w_ple_gate[i] + b_ple_gate[i])
        h = h + gate * (p[i] @ w_ple_proj[i])
    return h
```

```python
import numpy as np
import ml_dtypes
import concourse.bass as bass
import concourse.mybir as mybir
from concourse.bass_utils import run_bass_kernel_spmd

F32 = mybir.dt.float32
BF16 = mybir.dt.bfloat16
I32 = mybir.dt.int32
ALU = mybir.AluOpType
AF = mybir.ActivationFunctionType
AX = mybir.AxisListType

NCORES = 8
S = 4096
D = 1024
NT = S // 128
BT = 512
NB = S // BT
NH = 8
DH = 64
NE = 32
CAP = 384
NSLOT = NE * CAP
KR = 12
EPS = 1e-6

ENGS = ("sync", "scalar", "vector", "gpsimd", "tensor")
NDMASEM = 24
SAME_ENG_WINDOW = 10 ** 9


class Op:
    __slots__ = ("idx", "eng", "fn", "reads", "writes", "dma", "deps", "sig",
                 "sem", "val", "clock", "epos", "barrier")


class Prog:
    def __init__(self, nc):
        self.nc = nc
        self.ops = []
        self.last_w = {}
        self.readers = {}
        self.out_ops = []
        self.last_barrier = None
        self.since_barrier = []

    def op(self, eng, fn, reads=(), writes=(), dma=False, is_out=False):
        o = Op()
        o.idx = len(self.ops)
        o.eng = eng
        o.fn = fn
        o.dma = dma
        o.barrier = False
        o.reads = tuple(reads)
        o.writes = tuple(writes)
        deps = set()
        for k in o.reads:
            w = self.last_w.get(k)
            if w is not None:
                deps.add(w)
        for k in o.writes:
            w = self.last_w.get(k)
            if w is not None:
                deps.add(w)
            for r in self.readers.get(k, ()):
                deps.add(r)
        for k in o.writes:
            self.last_w[k] = o.idx
            self.readers[k] = []
        for k in o.reads:
            if k not in o.writes:
                self.readers.setdefault(k, []).append(o.idx)
        if self.last_barrier is not None:
            deps.add(self.last_barrier)
        deps.discard(o.idx)
        o.deps = sorted(deps)
        o.sig = False
        self.ops.append(o)
        self.since_barrier.append(o.idx)
        if is_out:
            self.out_ops.append(o.idx)
        return o.idx

    def barrier(self):
        o = Op()
        o.idx = len(self.ops)
        o.eng = "sync"
        o.fn = "BARRIER"
        o.dma = False
        o.barrier = True
        o.reads = ()
        o.writes = ()
        last = {}
        deps = []
        for i in self.since_barrier:
            p = self.ops[i]
            if p.dma:
                deps.append(i)
            else:
                last[p.eng] = i
        deps.extend(last.values())
        if self.last_barrier is not None:
            deps.append(self.last_barrier)
        o.deps = sorted(set(deps))
        o.sig = True
        self.ops.append(o)
        self.last_barrier = o.idx
        self.since_barrier = []
        self.last_w = {}
        self.readers = {}

    def emit(self):
        nc = self.nc
        ops = self.ops
        epos = {e: 0 for e in ENGS}
        for o in ops:
            o.epos = epos[o.eng]
            epos[o.eng] += 1
        fin = Op()
        fin.idx = len(ops)
        fin.eng = "sync"
        fin.fn = None
        fin.dma = False
        fin.barrier = False
        fin.reads = ()
        fin.writes = ()
        fin.deps = list(self.out_ops)
        fin.sig = False
        fin.epos = epos["sync"]
        ops = ops + [fin]
        for o in ops:
            nd = []
            for d in o.deps:
                do = ops[d]
                if do.eng == o.eng and not do.dma and not o.barrier:
                    if o.eng == "tensor" and not o.dma:
                        continue
                    if o.dma:
                        pass
                    elif o.epos - do.epos > SAME_ENG_WINDOW:
                        continue
                nd.append(d)
            o.deps = nd
            for d in nd:
                ops[d].sig = True
        sems = {}
        dma_engs = set(o.eng for o in ops if o.dma)
        for e in ENGS:
            sems[("c", e)] = nc.alloc_semaphore("c_" + e)
            if e in dma_engs:
                for i in range(NDMASEM):
                    sems[("d", e, i)] = nc.alloc_semaphore("d_%s_%d" % (e, i))
        ccount = {e: 0 for e in ENGS}
        dcount = {e: 0 for e in ENGS}
        dma_prev = {}
        for o in ops:
            if o.dma:
                k = dcount[o.eng]
                dcount[o.eng] += 1
                slot = k % NDMASEM
                o.sem = ("d", o.eng, slot)
                o.val = 16 * (k // NDMASEM + 1)
                prev = dma_prev.get((o.eng, slot))
                if prev is not None and prev not in o.deps:
                    o.deps.append(prev)
                dma_prev[(o.eng, slot)] = o.idx
            elif o.sig:
                ccount[o.eng] += 1
                o.sem = ("c", o.eng)
                o.val = ccount[o.eng]
            else:
                o.sem = None
                o.val = 0
        known = {e: {} for e in ENGS}
        streams = {e: [] for e in ENGS}
        for o in ops:
            kn = known[o.eng]
            wm = {}
            for d in sorted(o.deps, reverse=True):
                do = ops[d]
                if kn.get(do.sem, 0) >= do.val:
                    continue
                if wm.get(do.sem, 0) < do.val:
                    wm[do.sem] = do.val
                for s, v in do.clock.items():
                    if kn.get(s, 0) < v:
                        kn[s] = v
            o.clock = dict(kn)
            if o.sem is not None:
                o.clock[o.sem] = o.val
            streams[o.eng].append((o, list(wm.items())))
        self.n_waits = sum(len(w) for st in streams.values() for _, w in st)
        self.counts = (dict(ccount), dict(dcount))

        def run_stream(eng_name):
            def body(eng):
                for o, waits in streams[eng_name]:
                    for s, v in waits:
                        eng.wait_ge(sems[s], v)
                    if o.fn is None:
                        continue
                    if o.barrier:
                        eng.sem_inc(sems[o.sem], 1)
                        continue
                    ins = o.fn(eng)
                    if o.sem is not None:
                        ins.then_inc(sems[o.sem], 16 if o.dma else 1)
            return body

        with nc.Block() as block:
            for e in ENGS:
                if streams[e]:
                    getattr(block, e)(run_stream(e))


class SBAlloc:
    LO = 16512
    HI = 229344

    def __init__(self, nc):
        self.nc = nc
        self.cur = self.LO
        self.n = 0

    def alloc(self, name, shape, dt):
        esz = {F32: 4, BF16: 2, I32: 4}[dt]
        nbytes = esz
        for s in shape[1:]:
            nbytes *= s
        off = (self.cur + 31) // 32 * 32
        assert off + nbytes <= self.HI, "SBUF overflow at %s: need %d have %d" % (name, nbytes, self.HI - off)
        self.n += 1
        t = self.nc.alloc_sbuf_tensor_at("%s_%d" % (name, self.n), list(shape), dt, offset=off)
        self.cur = off + nbytes
        return t

    def mark(self):
        return self.cur

    def reset(self, m):
        self.cur = m


def bc_last(ap, n):
    shp = list(ap.shape)
    return ap.unsqueeze(len(shp)).broadcast_to(shp + [n])


def bc_mid(ap, n):
    shp = list(ap.shape)
    return ap.unsqueeze(1).broadcast_to([shp[0], n] + shp[1:])


def build(debug=False, phases=(1, 2, 3, 4)):
    nc = bass.Bass("TRN2", target_bir_lowering=False)
    P = Prog(nc)
    sb = SBAlloc(nc)

    def din(name, shape, dt=F32):
        return nc.dram_tensor(name, list(shape), dt, kind="ExternalInput")

    def dscr(name, shape, dt):
        return nc.dram_tensor(name, list(shape), dt, kind="ExternalOutput" if debug else "Internal")

    x_d = din("x", [S, D])
    p_d = din("p", [S, 256])
    gmix_d = din("g_mix", [1, D])
    win_d = din("w_in", [D, 5120])
    bcol_d = din("bcol", [128, 40])
    gqk_d = din("gqk", [128, 2])
    bv_d = din("bv", [1, 512])
    rbT_d = din("rbT", [128, NH * 5 * 128])
    maskT_d = din("maskT", [128, 5 * 128])
    cw_d = din("cw", [128, 12])
    cb_d = din("cb", [128, 4])
    wpa_d = din("w_pa", [512, D])
    wpc_d = din("w_pc", [512, D])
    wo_d = din("w_o", [D, D])
    gffn_d = din("g_ffn", [1, D])
    wrg_d = din("w_rg", [D, 36])
    brg_d = din("b_rg", [1, 36])
    w1_d = din("w1", [NE, D, 512])
    w3_d = din("w3", [NE, D, 512])
    w2_d = din("w2", [NE, 512, D])
    gple_d = din("g_ple", [1, D])
    wpg_d = din("w_pg", [D, D])
    bpg_d = din("b_pg", [1, D])
    wpp_d = din("w_pp", [256, D])
    ident_d = din("ident", [128, 128], BF16)
    utri_d = din("utri", [128, 128], BF16)
    ones_d = din("ones", [128, 128], BF16)
    bdiag_d = din("bdiag", [128, 128], BF16)
    ecap_d = din("ecap", [128, NE])
    out_d = nc.dram_tensor("out", [S, D], F32, kind="ExternalOutput")

    nT_d = dscr("nT_s", [D, S], BF16)
    yaT_d = dscr("yaT_s", [512, S], BF16)
    ycT_d = dscr("ycT_s", [512, S], BF16)
    h_d = dscr("h_s", [S, D], F32)
    xs_d = dscr("xs_s", [NSLOT + 128, D], BF16)
    ys_d = dscr("ys_s", [NSLOT + 128, D], BF16)
    if debug:
        dtab_o = nc.dram_tensor("dtab_o", [128, NT * 2], I32, kind="ExternalOutput")
        wtab_o = nc.dram_tensor("wtab_o", [128, NT * 2], F32, kind="ExternalOutput")

    pT = nc.alloc_psum_tensor("pT", [128, 8, 128], BF16)
    A0 = nc.alloc_psum_tensor("A0", [128, 512], F32)
    A1 = nc.alloc_psum_tensor("A1", [128, 512], F32)
    A2 = nc.alloc_psum_tensor("A2", [128, 512], F32)
    B0 = nc.alloc_psum_tensor("B0", [128, 1024], F32)
    B1 = nc.alloc_psum_tensor("B1", [128, 1024], F32)

    ident = sb.alloc("ident", [128, 128], BF16)
    utri = sb.alloc("utri", [128, 128], BF16)
    ones = sb.alloc("ones", [128, 128], BF16)
    bdiag = sb.alloc("bdiag", [128, 128], BF16)
    ecap = sb.alloc("ecap", [128, NE], F32)
    bcol = sb.alloc("bcol", [128, 40], F32)
    hbcol = sb.alloc("hbcol", [128, 40], F32)
    mhalf = sb.alloc("mhalf", [128, 8], F32)
    epsc = sb.alloc("epsc", [128, 8], F32)
    dtab = sb.alloc("dtab", [128, NT, 2], I32)
    wtab = sb.alloc("wtab", [128, NT, 2], F32)
    cnt = sb.alloc("cnt", [128, NE], F32)
    ss = sb.alloc("ss", [128, 8], F32)
    rs = sb.alloc("rs", [128, 8], F32)
    junk = sb.alloc("junk", [128, D], BF16)

    def ld(eng, dst, src, key):
        P.op(eng, lambda e: e.dma_start(out=dst, in_=src), writes=[key], dma=True)

    ld("sync", ident[:], ident_d.ap(), "ident")
    ld("sync", utri[:], utri_d.ap(), "utri")
    ld("sync", ones[:], ones_d.ap(), "ones")
    ld("sync", bdiag[:], bdiag_d.ap(), "bdiag")
    ld("sync", ecap[:], ecap_d.ap(), "ecap")
    ld("sync", bcol[:], bcol_d.ap(), "bcol")
    P.op("vector", lambda e: e.tensor_scalar(out=hbcol[:], in0=bcol[:], scalar1=0.5, scalar2=None, op0=ALU.mult),
         reads=["bcol"], writes=["hbcol"])
    P.op("gpsimd", lambda e: e.memset(mhalf[:], -0.5), writes=["mhalf"])
    P.op("gpsimd", lambda e: e.memset(epsc[:], EPS), writes=["epsc"])
    P.op("gpsimd", lambda e: e.memset(cnt[:], 0.0), writes=["cnt"])
    P.op("gpsimd", lambda e: e.memset(wtab[:], 0.0), writes=["wtab"])

    nrm_ctr = [0]

    def rmsnorm_tile(src, g_bc, dst_bf, src_key, g_key, dst_key):
        i = nrm_ctr[0] % 8
        nrm_ctr[0] += 1
        ssk, rsk = ("ss", i), ("rs", i)
        P.op("scalar", lambda e: e.activation(out=junk[:], in_=src, func=AF.Square, accum_out=ss[:, i:i + 1]),
             reads=[src_key], writes=["junk", ssk])
        P.op("vector", lambda e: e.tensor_scalar(out=rs[:, i:i + 1], in0=ss[:, i:i + 1], scalar1=1.0 / D, scalar2=EPS,
                                                  op0=ALU.mult, op1=ALU.add), reads=[ssk], writes=[rsk])
        P.op("gpsimd", lambda e: e.tensor_tensor(out=rs[:, i:i + 1], in0=rs[:, i:i + 1], in1=mhalf[:, 0:1], op=ALU.pow),
             reads=[rsk, "mhalf"], writes=[rsk])
        P.op("vector", lambda e: e.scalar_tensor_tensor(out=dst_bf, in0=src, scalar=rs[:, i:i + 1], in1=g_bc,
                                                         op0=ALU.mult, op1=ALU.mult),
             reads=[src_key, rsk, g_key], writes=[dst_key])

    def transpose_tile(src_bf, nchunk, dst, src_key, dst_key, evac="scalar", interleave=0):
        for c in range(nchunk):
            if interleave:
                src_c = src_bf.rearrange("t (p c) -> t c p", c=interleave)[:, c, :]
            else:
                src_c = src_bf[:, c * 128:(c + 1) * 128]
            P.op("tensor", lambda e, c=c, src_c=src_c: e.transpose(out=pT[:, c, :], in_=src_c, identity=ident[:]),
                 reads=(list(src_key) if isinstance(src_key, list) else [src_key]) + ["ident"], writes=[("pT", c)])
        if evac == "scalar":
            P.op("scalar", lambda e: e.activation(out=dst, in_=pT[:, 0:nchunk, :], func=AF.Copy),
                 reads=[("pT", c) for c in range(nchunk)], writes=[dst_key])
        else:
            P.op("vector", lambda e: e.tensor_copy(out=dst, in_=pT[:, 0:nchunk, :]),
                 reads=[("pT", c) for c in range(nchunk)], writes=[dst_key])

    _breg = {}

    def breg(e):
        if "r" not in _breg:
            _breg["r"] = e.to_reg(NSLOT - 1)
        return _breg["r"]

    base_mark = sb.mark()

    def phase1():
        Wa = sb.alloc("Wa", [128, 8, 3072], BF16)
        gmix = sb.alloc("gmix", [128, D], F32)
        gqk = sb.alloc("gqk", [128, 2], F32)
        bvb = sb.alloc("bvb", [128, 512], F32)
        cw = sb.alloc("cw", [128, 12], F32)
        cb = sb.alloc("cb", [128, 4], F32)
        expB = sb.alloc("expB", [128, NH, 5, 128], BF16)
        maskT = sb.alloc("maskT", [128, 5, 128], F32)
        kring = sb.alloc("kring", [128, 4, KR * 128], BF16)
        vring = sb.alloc("vring", [128, KR, NH, 65], BF16)
        xt = [sb.alloc("xt%d" % i, [128, D], F32) for i in range(2)]
        nb = [sb.alloc("nb%d" % i, [128, D], BF16) for i in range(2)]
        nTb = [sb.alloc("nTb%d" % i, [128, 8, BT], BF16) for i in range(2)]
        qT = [sb.alloc("qT%d" % i, [128, 4, BT], BF16) for i in range(2)]
        zq = [sb.alloc("zq%d" % i, [128, BT], F32) for i in range(3)]
        sq = [sb.alloc("sq%d" % i, [128, BT], BF16) for i in range(3)]
        r1 = [sb.alloc("r1%d" % i, [128, BT], F32) for i in range(3)]
        gw = sb.alloc("gw", [128, 1], F32)
        us = [sb.alloc("us%d" % i, [128, BT], F32) for i in range(2)]
        t1 = [sb.alloc("t1%d" % i, [128, BT], F32) for i in range(2)]
        cu = sb.alloc("cu", [128, 4, BT + 2], F32)
        ycT = [sb.alloc("ycT%d" % i, [128, 4, BT], BF16) for i in range(2)]
        yaT = [sb.alloc("yaT%d" % i, [128, 4, BT], BF16) for i in range(2)]
        pt = [sb.alloc("pt%d" % i, [128, 4, 128], BF16) for i in range(4)]
        rden = [sb.alloc("rden%d" % i, [128, 4], F32) for i in range(2)]
        ya = sb.alloc("ya", [128, 4, 512], BF16)

        win_v = win_d.ap().rearrange("(c p) n -> p c n", p=128)
        for (c0_, c1_) in ((0, 1024), (1024, 1536), (1536, 3072)):
            for c in range(0, 8, 2):
                P.op("gpsimd", lambda e, c=c, c0_=c0_, c1_=c1_: e.dma_start(out=Wa[:, c:c + 2, c0_:c1_], in_=win_v[:, c:c + 2, c0_:c1_]),
                     writes=[("Wa", c, c0_), ("Wa", c + 1, c0_)], dma=True)
        zt = sb.alloc("zt", [128, 8 * D], BF16)
        P.op("gpsimd", lambda e: e.memset(zt[:], 0.0), writes=["zt"])
        NR = (NSLOT + 128) // 128
        xs_z = xs_d.ap().rearrange("(p r) d -> p (r d)", p=128)
        zchunks = [(r0, min(r0 + 8, NR)) for r0 in range(0, NR, 8)]

        def zero_fill(k):
            for (r0, r1_) in zchunks[k::NB]:
                P.op("sync", lambda e, r0=r0, r1_=r1_: e.dma_start(out=xs_z[:, r0 * D:r1_ * D], in_=zt[:, 0:(r1_ - r0) * D]),
                     reads=["zt"], writes=[("xs_zero", r0)], dma=True)

        ld("sync", gmix[:], gmix_d.ap().partition_broadcast(128), "gmix")
        ld("sync", gqk[:], gqk_d.ap(), "gqk")
        ld("sync", bvb[:], bv_d.ap().partition_broadcast(128), "bvb")
        P.op("vector", lambda e: e.tensor_tensor(out=gw[:], in0=gqk[:, 0:1], in1=gqk[:, 1:2], op=ALU.mult), reads=["gqk"], writes=["gw"])
        ld("sync", cw[:], cw_d.ap(), "cw")
        ld("sync", cb[:], cb_d.ap(), "cb")
        ld("sync", maskT[:], maskT_d.ap().rearrange("p (j q) -> p j q", j=5), "maskT")
        rb_v = rbT_d.ap().rearrange("p (h n) -> p h n", h=NH)
        stg = [sb.alloc("stg%d" % i, [128, 640], F32) for i in range(2)]
        for h in range(NH):
            st = stg[h % 2]
            P.op("sync", lambda e, h=h, st=st: e.dma_start(out=st[:], in_=rb_v[:, h, :]),
                 writes=[("stg", h % 2)], dma=True)
            P.op("scalar", lambda e, st=st: e.activation(out=st[:], in_=st[:], func=AF.Exp),
                 reads=[("stg", h % 2)], writes=[("stg", h % 2)])
            P.op("vector", lambda e, h=h, st=st: e.tensor_tensor(
                out=expB[:, h, :, :], in0=st[:].rearrange("p (j q) -> p j q", j=5), in1=maskT[:], op=ALU.mult),
                reads=[("stg", h % 2), "maskT"], writes=[("expB", h)])
        P.op("gpsimd", lambda e: e.memset(vring[:], 1.0), writes=[("v", s_) for s_ in range(KR)])
        P.op("gpsimd", lambda e: e.memset(cu[:], 0.0), writes=[("cu", ct) for ct in range(4)] + [("cuh", ct) for ct in range(4)])

        nT_v = nT_d.ap().rearrange("(c p) t -> p c t", p=128)
        yaT_v = yaT_d.ap().rearrange("(c p) t -> p c t", p=128)
        ycT_v = ycT_d.ap().rearrange("(c p) t -> p c t", p=128)
        acc_rot = [0]
        ACC = [(A0, "A0"), (A1, "A1")]

        def next_acc():
            a = ACC[acc_rot[0] % 2]
            acc_rot[0] += 1
            return a

        def A_norm(b, tl):
            t = 4 * b + tl
            s2 = t % 2
            P.op("sync", lambda e, t=t, s2=s2: e.dma_start(out=xt[s2][:], in_=x_d.ap()[t * 128:(t + 1) * 128, :]),
                 writes=[("xt", s2)], dma=True)
            rmsnorm_tile(xt[s2][:], gmix[:], nb[s2][:], ("xt", s2), "gmix", ("nb", s2))

        def A_tr(b, tl):
            t = 4 * b + tl
            s2 = t % 2
            bb = b % 2
            transpose_tile(nb[s2], 8, nTb[bb][:, :, tl * 128:(tl + 1) * 128], ("nb", s2), ("nTb", bb, tl))
            if tl == 3:
                P.op("sync", lambda e, bb=bb, b=b: e.dma_start(out=nT_v[:, :, b * BT:(b + 1) * BT], in_=nTb[bb][:]),
                     reads=[("nTb", bb, q_) for q_ in range(4)], writes=[("nT_d", b)], dma=True)

        def secC(b):
            bb = b % 2
            tok0 = b * BT
            nkeys = [("nTb", bb, tl) for tl in range(4)]
            PACC = [(A0[:], "A0"), (A1[:], "A1"), (B0[:, 0:512], ("B0", 0))]

            def proj(j):
                acc, akey = PACC[j % 3]
                for c in range(8):
                    P.op("tensor", lambda e, j=j, c=c, acc=acc: e.matmul(
                        acc, lhsT=Wa[:, c, j * 128:(j + 1) * 128], rhs=nTb[bb][:, c, :], start=(c == 0), stop=(c == 7)),
                        reads=[("Wa", c, 0)] + nkeys, writes=[akey])
                z = j % 3
                P.op("scalar", lambda e, j=j, z=z, acc=acc: e.activation(out=zq[z][:], in_=acc, func=AF.Identity, bias=bcol[:, j:j + 1]),
                     reads=[akey, "bcol"], writes=[("zq", z)])
                P.op("gpsimd", lambda e, z=z: e.tensor_tensor(out=sq[z][:], in0=zq[z][:], in1=zq[z][:], op=ALU.mult),
                     reads=[("zq", z)], writes=[("sq", z)])

            def post(j):
                z = j % 3
                P.op("tensor", lambda e, z=z: e.matmul(A2[:], lhsT=bdiag[:], rhs=sq[z][:], start=True, stop=True),
                     reads=[("sq", z), "bdiag"], writes=["A2"])
                P.op("scalar", lambda e, z=z: e.activation(out=r1[z][:], in_=A2[:], func=AF.Sqrt, bias=epsc[:, 0:1]),
                     reads=["A2", "epsc"], writes=[("r1", z)])
                P.op("vector", lambda e, z=z: e.reciprocal(out=r1[z][:], in_=r1[z][:]), reads=[("r1", z)], writes=[("r1", z)])
                if j < 4:
                    P.op("gpsimd", lambda e, j=j, z=z: e.tensor_tensor(out=qT[bb][:, j, :], in0=zq[z][:], in1=r1[z][:], op=ALU.mult),
                         reads=[("zq", z), ("r1", z)], writes=[("qT", bb, j)])
                else:
                    hp = j - 4
                    sl0 = (4 * b) % KR
                    P.op("vector", lambda e, hp=hp, z=z, sl0=sl0: e.scalar_tensor_tensor(
                        out=kring[:, hp, sl0 * 128:(sl0 + 4) * 128], in0=zq[z][:], scalar=gw[:, 0:1], in1=r1[z][:], op0=ALU.mult, op1=ALU.mult),
                        reads=[("zq", z), ("r1", z), "gw"], writes=[("k", hp, sl0 + q_) for q_ in range(4)])

            proj(0)
            proj(1)
            for j in range(8):
                if j + 2 < 8:
                    proj(j + 2)
                post(j)
            for tl in range(4):
                t = 4 * b + tl
                sl = t % KR
                acc, akey = next_acc()
                for c in range(8):
                    P.op("tensor", lambda e, c=c, tl=tl, acc=acc: e.matmul(
                        acc[:], lhsT=nTb[bb][:, c, tl * 128:(tl + 1) * 128], rhs=Wa[:, c, 1024:1536], start=(c == 0), stop=(c == 7)),
                        reads=[("Wa", c, 1024), ("nTb", bb, tl)], writes=[akey])
                P.op("vector", lambda e, sl=sl, acc=acc: e.tensor_tensor(
                    out=vring[:, sl, :, 0:64], in0=acc[:].rearrange("p (h d) -> p h d", h=NH),
                    in1=bvb[:].rearrange("p (h d) -> p h d", h=NH), op=ALU.add),
                    reads=[akey, "bvb"], writes=[("v", sl)])
            SETS = [((A0[:], ["A0"]), (A1[:], ["A1"]), (A2[:], ["A2"])),
                    ((B0[:, 0:512], [("B0", 0)]), (B0[:, 512:1024], [("B0", 4)]), (B1[:, 0:512], [("B1h", 0)]))]
            for ct in range(4):
                z = ct % 2
                (pu, ku), (pb, kb), (pc, kc) = SETS[ct % 2]
                for (dst, dkey, col0) in ((pu, ku, 1536), (pb, kb, 2048), (pc, kc, 2560)):
                    for c in range(8):
                        P.op("tensor", lambda e, c=c, dst=dst, col0=col0, ct=ct: e.matmul(
                            dst, lhsT=Wa[:, c, col0 + ct * 128:col0 + (ct + 1) * 128], rhs=nTb[bb][:, c, :],
                            start=(c == 0), stop=(c == 7)),
                            reads=[("Wa", c, 1536)] + nkeys, writes=dkey)
                ju, jb, jc = 12 + ct, 16 + ct, 20 + ct
                P.op("scalar", lambda e, z=z, ju=ju, pu=pu: e.activation(out=us[z][:], in_=pu, func=AF.Identity, bias=bcol[:, ju:ju + 1]),
                     reads=ku + ["bcol"], writes=[("us", z)])
                P.op("vector", lambda e, z=z, jc=jc, ct=ct, pc=pc: e.scalar_tensor_tensor(
                    out=cu[:, ct, 2:BT + 2], in0=pc, scalar=bcol[:, jc:jc + 1], in1=us[z][:], op0=ALU.add, op1=ALU.mult),
                    reads=kc + ["bcol", ("us", z)], writes=[("cu", ct)])
                P.op("scalar", lambda e, z=z, ct=ct: e.activation(out=t1[z][:], in_=cu[:, ct, 2:BT + 2], func=AF.Identity,
                                                               scale=cw[:, ct * 3 + 2:ct * 3 + 3], bias=cb[:, ct:ct + 1]),
                     reads=[("cu", ct), "cw", "cb"], writes=[("t1", z)])
                P.op("vector", lambda e, z=z, ct=ct: e.scalar_tensor_tensor(
                    out=t1[z][:], in0=cu[:, ct, 1:BT + 1], scalar=cw[:, ct * 3 + 1:ct * 3 + 2], in1=t1[z][:], op0=ALU.mult, op1=ALU.add),
                    reads=[("cu", ct), ("cuh", ct), "cw", ("t1", z)], writes=[("t1", z)])
                P.op("vector", lambda e, z=z, ct=ct: e.scalar_tensor_tensor(
                    out=t1[z][:], in0=cu[:, ct, 0:BT], scalar=cw[:, ct * 3:ct * 3 + 1], in1=t1[z][:], op0=ALU.mult, op1=ALU.add),
                    reads=[("cu", ct), ("cuh", ct), "cw", ("t1", z)], writes=[("t1", z)])
                P.op("vector", lambda e, z=z, jb=jb, ct=ct, pb=pb: e.scalar_tensor_tensor(
                    out=ycT[bb][:, ct, :], in0=pb, scalar=bcol[:, jb:jb + 1], in1=t1[z][:], op0=ALU.add, op1=ALU.mult),
                    reads=kb + ["bcol", ("t1", z)], writes=[("ycT", bb, ct)])
                P.op("vector", lambda e, ct=ct: e.tensor_copy(out=cu[:, ct, 0:2], in_=cu[:, ct, BT:BT + 2]),
                     reads=[("cu", ct)], writes=[("cuh", ct)])
            P.op("sync", lambda e, tok0=tok0: e.dma_start(out=ycT_v[:, :, tok0:tok0 + BT], in_=ycT[bb][:]),
                 reads=[("ycT", bb, ct) for ct in range(4)], writes=[("ycT_d", b)], dma=True)

        def secD(b):
            bb = b % 2
            tok0 = b * BT
            nxt = b + 1 < NB
            units = []
            for h in range(NH):
                for m in range(8):
                    kt = 4 * b - 4 + m
                    if kt < 0:
                        continue
                    units.append((h, m, kt, max(m - 4, 0), min(m, 3)))
            SPS = [(A0[:], "A0"), (A1[:], "A1"), (B0[:, 0:512], ("B0", 0))]

            def QKEXP(u):
                h, m, kt, tlo, thi = units[u]
                hp, r0 = h // 2, (h % 2) * 64
                nq = thi - tlo + 1
                sp, sk = SPS[u % 3]
                pz = u % 4
                sl = kt % KR
                P.op("tensor", lambda e, hp=hp, r0=r0, sl=sl, sp=sp, tlo=tlo, thi=thi: e.matmul(
                    sp[:, 0:(thi - tlo + 1) * 128], lhsT=kring[r0:r0 + 64, hp, sl * 128:(sl + 1) * 128],
                    rhs=qT[bb][r0:r0 + 64, hp, tlo * 128:(thi + 1) * 128], start=True, stop=True),
                    reads=[("k", hp, sl), ("qT", bb, hp)], writes=[sk])
                P.op("scalar", lambda e, pz=pz, sp=sp, nq=nq: e.activation(
                    out=pt[pz][:, 0:nq, :], in_=sp[:, 0:nq * 128].rearrange("p (j q) -> p j q", q=128), func=AF.Exp, scale=DH ** -0.5),
                    reads=[sk], writes=[("pt", pz)])
                rlo = 4 - m + tlo
                P.op("vector", lambda e, pz=pz, nq=nq, h=h, rlo=rlo: e.tensor_tensor(
                    out=pt[pz][:, 0:nq, :], in0=pt[pz][:, 0:nq, :], in1=expB[:, h, rlo:rlo + nq, :], op=ALU.mult),
                    reads=[("pt", pz), ("expB", h)], writes=[("pt", pz)])

            def PV(u):
                h, m, kt, tlo, thi = units[u]
                pz = u % 4
                sl = kt % KR
                hb2 = h % 2
                first = (u == 0) or units[u - 1][0] != h
                last_u = (u + 1 == len(units)) or units[u + 1][0] != h
                if first:
                    P.op("tensor", lambda e, hb2=hb2: e.matmul(
                        B1[:, hb2 * 512:hb2 * 512 + 260], lhsT=zt[:, 0:128], rhs=zt[:, 0:260], start=True, stop=False),
                        reads=["zt"], writes=[("B1h", hb2)])
                for tl in range(tlo, thi + 1):
                    c0 = hb2 * 512 + tl * 65
                    P.op("tensor", lambda e, h=h, tl=tl, tlo=tlo, sl=sl, pz=pz, c0=c0, fin=(last_u and tl == thi): e.matmul(
                        B1[:, c0:c0 + 65], lhsT=pt[pz][:, tl - tlo, :], rhs=vring[:, sl, h, :],
                        start=False, stop=fin),
                        reads=[("pt", pz), ("v", sl)], writes=[("B1h", hb2)])

            def FINH(h):
                hb2 = h % 2
                Bv = B1[:, hb2 * 512:hb2 * 512 + 260].rearrange("p (t d) -> p t d", d=65)
                P.op("vector", lambda e, hb2=hb2, Bv=Bv: e.reciprocal(out=rden[hb2][:], in_=Bv[:, :, 64]),
                     reads=[("B1h", hb2)], writes=[("rden", hb2)])
                P.op("vector", lambda e, hb2=hb2, Bv=Bv, h=h: e.tensor_tensor(
                    out=ya[:, :, h * 64:(h + 1) * 64], in0=Bv[:, :, 0:64], in1=bc_last(rden[hb2][:], 64), op=ALU.mult),
                    reads=[("B1h", hb2), ("rden", hb2)], writes=[("ya", h)])

            if nxt:
                A_norm(b + 1, 0)
            QKEXP(0)
            if len(units) > 1:
                QKEXP(1)
            for u in range(len(units)):
                if u + 2 < len(units):
                    QKEXP(u + 2)
                PV(u)
                h = units[u][0]
                if u + 1 == len(units) or units[u + 1][0] != h:
                    FINH(h)
                    if nxt and h % 2 == 1:
                        tl = h // 2
                        A_tr(b + 1, tl)
                        if tl + 1 < 4:
                            A_norm(b + 1, tl + 1)
            for tl in range(4):
                transpose_tile(ya[:, tl, :], 4, yaT[bb][:, :, tl * 128:(tl + 1) * 128], [("ya", h) for h in range(NH)], ("yaT", bb, tl))
            P.op("sync", lambda e, tok0=tok0: e.dma_start(out=yaT_v[:, :, tok0:tok0 + BT], in_=yaT[bb][:]),
                 reads=[("yaT", bb, tl) for tl in range(4)], writes=[("yaT_d", b)], dma=True)

        for tl in range(4):
            A_norm(0, tl)
            A_tr(0, tl)
        for b in range(NB):
            secC(b)
            zero_fill(b)
            secD(b)
        P.barrier()
    if 1 in phases:
        phase1()
    sb.reset(base_mark)

    def phase2():
        Wg = sb.alloc("Wg", [128, 8, 2048], BF16)
        wpa = sb.alloc("wpa", [128, 4, D], BF16)
        wpc = sb.alloc("wpc", [128, 4, D], BF16)
        wo = sb.alloc("wo", [128, 8, D], BF16)
        wrg = sb.alloc("wrg", [128, 8, 36], BF16)
        brg = sb.alloc("brg", [128, 36], F32)
        gffn = sb.alloc("gffn", [128, D], F32)
        nTb = [sb.alloc("nTb%d" % i, [128, 8, BT], BF16) for i in range(2)]
        yaT = [sb.alloc("yaT%d" % i, [128, 4, BT], BF16) for i in range(2)]
        ycT = [sb.alloc("ycT%d" % i, [128, 4, BT], BF16) for i in range(2)]
        xt = [sb.alloc("xt%d" % i, [128, D], F32) for i in range(4)]
        tA = [sb.alloc("tA%d" % i, [128, BT], F32) for i in range(2)]
        tC = [sb.alloc("tC%d" % i, [128, BT], F32) for i in range(2)]
        mA = [sb.alloc("mA%d" % i, [128, BT], F32) for i in range(2)]
        mC = [sb.alloc("mC%d" % i, [128, BT], F32) for i in range(2)]
        mT = [sb.alloc("mT%d" % i, [128, 8, BT], BF16) for i in range(2)]
        ht = [sb.alloc("ht%d" % i, [128, D], F32) for i in range(3)]
        n2 = [sb.alloc("n2%d" % i, [128, 4, D], BF16) for i in range(2)]
        n2T = [sb.alloc("n2T%d" % i, [128, 8, 128], BF16) for i in range(2)]
        lg = sb.alloc("lg", [128, 4, 36], F32)
        gmax = sb.alloc("gmax", [128, 4], F32)
        gmask = sb.alloc("gmask", [128, 4, 4], F32)
        gex = sb.alloc("gex", [128, 4, 4], F32)
        gse = sb.alloc("gse", [128, 4], F32)
        pen = sb.alloc("pen", [128, 4, 4], F32)
        elm = sb.alloc("elm", [128, 4, 32], F32)
        elm2 = sb.alloc("elm2", [128, 4, 32], F32)
        m1 = sb.alloc("m1", [128, 4], F32)
        m2 = sb.alloc("m2", [128, 4], F32)
        mk1 = sb.alloc("mk1", [128, 4, 32], F32)
        mk2 = sb.alloc("mk2", [128, 4, 32], F32)
        Mb = sb.alloc("Mb", [128, 4, 32], BF16)
        dd = sb.alloc("dd", [128, 4], F32)
        ee = sb.alloc("ee", [128, 4], F32)
        rr = sb.alloc("rr", [128, 4], F32)
        wA = sb.alloc("wA", [128, 4], F32)
        wB = sb.alloc("wB", [128, 4], F32)
        pos = sb.alloc("pos", [128, 4, 32], F32)
        okm = sb.alloc("okm", [128, 4, 32], F32)
        slot = sb.alloc("slot", [128, 4, 32], F32)
        tmp = sb.alloc("tmp", [128, 4, 32], F32)
        dsel = sb.alloc("dsel", [128, 4, 2], F32)
        oksel = sb.alloc("oksel", [128, 4, 2], F32)

        win_v = win_d.ap().rearrange("(c p) n -> p c n", p=128)
        def wg_load(q4):
            for g0 in (0, 1024):
                c0_ = g0 + q4 * 256
                P.op("gpsimd", lambda e, c0_=c0_: e.dma_start(out=Wg[:, :, c0_:c0_ + 256], in_=win_v[:, :, 3072 + c0_:3072 + c0_ + 256]),
                     writes=[("Wg", c0_)], dma=True)

        wg_load(0)
        P.op("gpsimd", lambda e: e.dma_start(out=wpa[:], in_=wpa_d.ap().rearrange("(c p) n -> p c n", p=128)), writes=["wpa"], dma=True)
        P.op("gpsimd", lambda e: e.dma_start(out=wpc[:], in_=wpc_d.ap().rearrange("(c p) n -> p c n", p=128)), writes=["wpc"], dma=True)
        for q4 in range(1, 4):
            wg_load(q4)
        wo_v = wo_d.ap().rearrange("(c p) n -> p c n", p=128)
        for c in range(0, 8, 4):
            P.op("gpsimd", lambda e, c=c: e.dma_start(out=wo[:, c:c + 4, :], in_=wo_v[:, c:c + 4, :]), writes=[("wo", c)], dma=True)
        P.op("gpsimd", lambda e: e.dma_start(out=wrg[:], in_=wrg_d.ap().rearrange("(c p) n -> p c n", p=128)), writes=["wrg"], dma=True)
        ld("sync", brg[:], brg_d.ap().partition_broadcast(128), "brg")
        ld("sync", gffn[:], gffn_d.ap().partition_broadcast(128), "gffn")

        nT_v = nT_d.ap().rearrange("(c p) t -> p c t", p=128)
        yaT_v = yaT_d.ap().rearrange("(c p) t -> p c t", p=128)
        ycT_v = ycT_d.ap().rearrange("(c p) t -> p c t", p=128)
        wokeys = [("wo", 0), ("wo", 4)]
        xctr = [0]
        hctr = [0]

        def loads2(b):
            bb = b % 2
            tok0 = b * BT
            P.op("sync", lambda e, bb=bb, tok0=tok0: e.dma_start(out=nTb[bb][:], in_=nT_v[:, :, tok0:tok0 + BT]), writes=[("nTb", bb)], dma=True)
            P.op("sync", lambda e, bb=bb, tok0=tok0: e.dma_start(out=yaT[bb][:], in_=yaT_v[:, :, tok0:tok0 + BT]), writes=[("yaT", bb)], dma=True)
            P.op("sync", lambda e, bb=bb, tok0=tok0: e.dma_start(out=ycT[bb][:], in_=ycT_v[:, :, tok0:tok0 + BT]), writes=[("ycT", bb)], dma=True)

        def xload(t):
            P.op("sync", lambda e, t=t: e.dma_start(out=xt[t % 4][:], in_=x_d.ap()[t * 128:(t + 1) * 128, :]), writes=[("xt", t % 4)], dma=True)

        loads2(0)
        for t_ in range(3):
            xload(t_)
        def gates2(b):
            bb = b % 2
            tok0 = b * BT
            if b + 1 < NB:
                loads2(b + 1)
            for j in range(8):
                z = j % 2
                for (dst, dkey, col0) in ((A0, "A0", 0), (A1, "A1", 1024)):
                    for c in range(8):
                        P.op("tensor", lambda e, c=c, dst=dst, col0=col0, j=j, bb=bb: e.matmul(
                            dst[:], lhsT=Wg[:, c, col0 + j * 128:col0 + (j + 1) * 128], rhs=nTb[bb][:, c, :], start=(c == 0), stop=(c == 7)),
                            reads=[("Wg", col0 + (j // 2) * 256), ("nTb", bb)], writes=[dkey])
                for (dst, dkey, wsrc, wkey, asrc, akey) in ((A2, "A2", wpa, "wpa", yaT, "yaT"), (B0, ("B0", 0), wpc, "wpc", ycT, "ycT")):
                    for c in range(4):
                        P.op("tensor", lambda e, c=c, dst=dst, wsrc=wsrc, asrc=asrc, j=j, bb=bb: e.matmul(
                            dst[:, 0:512], lhsT=wsrc[:, c, j * 128:(j + 1) * 128], rhs=asrc[bb][:, c, :], start=(c == 0), stop=(c == 3)),
                            reads=[wkey, (akey, bb)], writes=[dkey])
                P.op("scalar", lambda e, z=z, j=j: e.activation(out=tA[z][:], in_=A0[:], func=AF.Tanh, scale=0.5, bias=hbcol[:, 24 + j:25 + j]),
                     reads=["A0", "hbcol"], writes=[("tA", z)])
                P.op("scalar", lambda e, z=z, j=j: e.activation(out=tC[z][:], in_=A1[:], func=AF.Tanh, scale=0.5, bias=hbcol[:, 32 + j:33 + j]),
                     reads=["A1", "hbcol"], writes=[("tC", z)])
                P.op("vector", lambda e, z=z: e.scalar_tensor_tensor(out=mA[z][:], in0=tA[z][:], scalar=1.0, in1=A2[:], op0=ALU.add, op1=ALU.mult),
                     reads=[("tA", z), "A2"], writes=[("mA", z)])
                P.op("vector", lambda e, z=z: e.scalar_tensor_tensor(out=mC[z][:], in0=tC[z][:], scalar=1.0, in1=B0[:, 0:512], op0=ALU.add, op1=ALU.mult),
                     reads=[("tC", z), ("B0", 0)], writes=[("mC", z)])
                P.op("gpsimd", lambda e, z=z, j=j, bb=bb: e.tensor_tensor(out=mT[bb][:, j, :], in0=mA[z][:], in1=mC[z][:], op=ALU.add),
                     reads=[("mA", z), ("mC", z)], writes=[("mT", bb, j)])

        def hsec2(b):
            bb = b % 2
            tok0 = b * BT
            mkeys = [("mT", bb, j) for j in range(8)]
            def hmm(tl):
                t = 4 * b + tl
                xs_ = t % 4
                hs_ = t % 3
                if t + 3 < NT:
                    xload(t + 3)
                for half in range(2):
                    for j in range(8):
                        P.op("tensor", lambda e, j=j, half=half, tl=tl, bb=bb: e.matmul(
                            B1[:, half * 512:(half + 1) * 512], lhsT=mT[bb][:, j, tl * 128:(tl + 1) * 128], rhs=wo[:, j, half * 512:(half + 1) * 512],
                            start=(j == 0), stop=(j == 7)),
                            reads=mkeys + wokeys, writes=[("B1", half)])
                    P.op("vector", lambda e, half=half, xs_=xs_, hs_=hs_: e.scalar_tensor_tensor(
                        out=ht[hs_][:, half * 512:(half + 1) * 512], in0=B1[:, half * 512:(half + 1) * 512], scalar=0.5,
                        in1=xt[xs_][:, half * 512:(half + 1) * 512], op0=ALU.mult, op1=ALU.add),
                        reads=[("B1", half), ("xt", xs_)] + ([("ht", hs_)] if half == 1 else []), writes=[("ht", hs_)])
                P.op("sync", lambda e, t=t, hs_=hs_: e.dma_start(out=h_d.ap()[t * 128:(t + 1) * 128, :], in_=ht[hs_][:]),
                     reads=[("ht", hs_)], writes=[("h_d", t)], dma=True)

            def hnorm(tl):
                t = 4 * b + tl
                hs_ = t % 3
                rmsnorm_tile(ht[hs_][:], gffn[:], n2[bb][:, tl, :], ("ht", hs_), "gffn", ("n2", bb, tl))

            def htr(tl):
                t = 4 * b + tl
                z2 = t % 2
                transpose_tile(n2[bb][:, tl, :], 8, n2T[z2][:], ("n2", bb, tl), ("n2T", z2))
                for c in range(8):
                    P.op("tensor", lambda e, c=c, tl=tl, z2=z2: e.matmul(
                        B0[:, 512 + tl * 36:512 + (tl + 1) * 36], lhsT=n2T[z2][:, c, :], rhs=wrg[:, c, :], start=(c == 0), stop=(c == 7)),
                        reads=[("n2T", z2), "wrg"], writes=[("lgp", tl)])

            hmm(0)
            hnorm(0)
            for tl in range(4):
                if tl + 1 < 4:
                    hmm(tl + 1)
                htr(tl)
                if tl + 1 < 4:
                    hnorm(tl + 1)

        def rout2(b):
            bb = b % 2
            tok0 = b * BT
            lgp = B0[:, 512:512 + 144].rearrange("p (t n) -> p t n", t=4)
            R = []

            def V(fn, reads, writes):
                P.op("vector", fn, reads=reads, writes=writes)

            V(lambda e: e.tensor_tensor(out=lg[:], in0=lgp, in1=bc_mid(brg[:], 4), op=ALU.add),
              [("lgp", tl) for tl in range(4)] + ["brg"], ["lg"])
            V(lambda e: e.tensor_reduce(out=gmax[:], in_=lg[:, :, 0:4], axis=AX.X, op=ALU.max), ["lg"], ["gmax"])
            V(lambda e: e.tensor_tensor(out=gmask[:], in0=lg[:, :, 0:4], in1=bc_last(gmax[:], 4), op=ALU.is_equal), ["lg", "gmax"], ["gmask"])
            V(lambda e: e.tensor_tensor(out=gex[:], in0=lg[:, :, 0:4], in1=bc_last(gmax[:], 4), op=ALU.subtract), ["lg", "gmax"], ["gex"])
            P.op("scalar", lambda e: e.activation(out=gex[:], in_=gex[:], func=AF.Exp), reads=["gex"], writes=["gex"])
            V(lambda e: e.tensor_reduce(out=gse[:], in_=gex[:], axis=AX.X, op=ALU.add), ["gex"], ["gse"])
            V(lambda e: e.reciprocal(out=gse[:], in_=gse[:]), ["gse"], ["gse"])
            V(lambda e: e.tensor_scalar(out=pen[:], in0=gmask[:], scalar1=1.0, scalar2=1e30, op0=ALU.subtract, op1=ALU.mult), ["gmask"], ["pen"])
            V(lambda e: e.tensor_tensor(out=elm[:].rearrange("p t (g k) -> p t g k", g=4),
                                        in0=lg[:, :, 4:36].rearrange("p t (g k) -> p t g k", g=4),
                                        in1=bc_last(pen[:], 8), op=ALU.add), ["lg", "pen"], ["elm"])
            V(lambda e: e.tensor_reduce(out=m1[:], in_=elm[:], axis=AX.X, op=ALU.max), ["elm"], ["m1"])
            V(lambda e: e.tensor_tensor(out=mk1[:], in0=elm[:], in1=bc_last(m1[:], 32), op=ALU.is_equal), ["elm", "m1"], ["mk1"])
            V(lambda e: e.scalar_tensor_tensor(out=elm2[:], in0=mk1[:], scalar=-1e30, in1=elm[:], op0=ALU.mult, op1=ALU.add), ["mk1", "elm"], ["elm2"])
            V(lambda e: e.tensor_reduce(out=m2[:], in_=elm2[:], axis=AX.X, op=ALU.max), ["elm2"], ["m2"])
            V(lambda e: e.tensor_tensor(out=mk2[:], in0=elm2[:], in1=bc_last(m2[:], 32), op=ALU.is_equal), ["elm2", "m2"], ["mk2"])
            V(lambda e: e.tensor_tensor(out=dd[:], in0=m2[:], in1=m1[:], op=ALU.subtract), ["m1", "m2"], ["dd"])
            P.op("scalar", lambda e: e.activation(out=ee[:], in_=dd[:], func=AF.Exp), reads=["dd"], writes=["ee"])
            V(lambda e: e.tensor_scalar(out=rr[:], in0=ee[:], scalar1=1.0, scalar2=None, op0=ALU.add), ["ee"], ["rr"])
            V(lambda e: e.reciprocal(out=rr[:], in_=rr[:]), ["rr"], ["rr"])
            V(lambda e: e.tensor_tensor(out=wA[:], in0=gse[:], in1=rr[:], op=ALU.mult), ["gse", "rr"], ["wA"])
            V(lambda e: e.tensor_tensor(out=wB[:], in0=wA[:], in1=ee[:], op=ALU.mult), ["wA", "ee"], ["wB"])
            V(lambda e: e.tensor_tensor(out=Mb[:], in0=mk1[:], in1=mk2[:], op=ALU.add), ["mk1", "mk2"], ["Mb"])
            for tl in range(4):
                P.op("tensor", lambda e, tl=tl: e.matmul(A0[:, tl * 32:(tl + 1) * 32], lhsT=utri[:], rhs=Mb[:, tl, :], start=True, stop=(tl == 0)),
                     reads=["utri", "Mb"], writes=["A0"])
                for t2 in range(tl):
                    P.op("tensor", lambda e, tl=tl, t2=t2: e.matmul(A0[:, tl * 32:(tl + 1) * 32], lhsT=ones[:], rhs=Mb[:, t2, :], start=False, stop=(t2 == tl - 1)),
                         reads=["ones", "Mb"], writes=["A0"])
            for tl in range(4):
                P.op("tensor", lambda e, tl=tl: e.matmul(A1[:, 0:32], lhsT=ones[:], rhs=Mb[:, tl, :], start=(tl == 0), stop=(tl == 3)),
                     reads=["ones", "Mb"], writes=["A1"])
            V(lambda e: e.tensor_tensor(out=pos[:], in0=A0[:, 0:128].rearrange("p (t n) -> p t n", t=4), in1=bc_mid(cnt[:], 4), op=ALU.add),
              ["A0", "cnt"], ["pos"])
            V(lambda e: e.tensor_tensor(out=cnt[:], in0=cnt[:], in1=A1[:, 0:32], op=ALU.add), ["A1", "cnt", "pos"], ["cnt"])
            V(lambda e: e.tensor_scalar(out=okm[:], in0=pos[:], scalar1=float(CAP), scalar2=None, op0=ALU.is_lt), ["pos"], ["okm"])
            V(lambda e: e.tensor_tensor(out=slot[:], in0=pos[:], in1=bc_mid(ecap[:], 4), op=ALU.add), ["pos", "ecap"], ["slot"])
            V(lambda e: e.tensor_scalar(out=tmp[:], in0=okm[:], scalar1=-1.0e6, scalar2=1.0e6, op0=ALU.mult, op1=ALU.add), ["okm"], ["tmp"])
            V(lambda e: e.tensor_tensor(out=slot[:], in0=slot[:], in1=tmp[:], op=ALU.add), ["slot", "tmp"], ["slot"])
            V(lambda e: e.tensor_scalar(out=slot[:], in0=slot[:], scalar1=float(NSLOT), scalar2=None, op0=ALU.min), ["slot"], ["slot"])
            for k, mk in ((0, mk1), (1, mk2)):
                V(lambda e, mk=mk: e.tensor_tensor(out=tmp[:], in0=mk[:], in1=slot[:], op=ALU.mult), ["mk1", "mk2", "slot"], ["tmp"])
                V(lambda e, k=k: e.tensor_reduce(out=dsel[:, :, k], in_=tmp[:], axis=AX.X, op=ALU.add), ["tmp"], [("dsel", k)])
                V(lambda e, mk=mk: e.tensor_tensor(out=tmp[:], in0=mk[:], in1=okm[:], op=ALU.mult), ["mk1", "mk2", "okm", ("dsel", k)], ["tmp"])
                V(lambda e, k=k: e.tensor_reduce(out=oksel[:, :, k], in_=tmp[:], axis=AX.X, op=ALU.add), ["tmp"], [("oksel", k)])
            tb = 4 * b
            V(lambda e, tb=tb: e.tensor_copy(out=dtab[:, tb:tb + 4, :], in_=dsel[:]), [("dsel", 0), ("dsel", 1)], [("dtab", b)])
            V(lambda e, tb=tb: e.tensor_tensor(out=wtab[:, tb:tb + 4, 0], in0=wA[:], in1=oksel[:, :, 0], op=ALU.mult), ["wA", ("oksel", 0)], [("wtab", b, 0)])
            V(lambda e, tb=tb: e.tensor_tensor(out=wtab[:, tb:tb + 4, 1], in0=wB[:], in1=oksel[:, :, 1], op=ALU.mult), ["wB", ("oksel", 1)], [("wtab", b, 1)])
            for tl in range(4):
                t = 4 * b + tl
                for k in range(2):
                    P.op("gpsimd", lambda e, t=t, k=k, tl=tl, bb=bb: e.indirect_dma_start(
                        out=xs_d[:, :], out_offset=bass.IndirectOffsetOnAxis(ap=dtab[:, t, k:k + 1], axis=0),
                        in_=n2[bb][:, tl, :], in_offset=None),
                        reads=[("n2", bb, tl), ("dtab", b)], writes=[("xs_d", t, k)], dma=True)

        gates2(0)
        for b in range(NB):
            hsec2(b)
            if b + 1 < NB:
                gates2(b + 1)
            rout2(b)
        if debug:
            P.op("sync", lambda e: e.dma_start(out=dtab_o.ap(), in_=dtab[:].rearrange("p t k -> p (t k)")),
                 reads=[("dtab", b) for b in range(NB)], dma=True, is_out=True)
            P.op("sync", lambda e: e.dma_start(out=wtab_o.ap(), in_=wtab[:].rearrange("p t k -> p (t k)")),
                 reads=[("wtab", b, k) for b in range(NB) for k in range(2)], dma=True, is_out=True)
        P.barrier()
    if 2 in phases:
        phase2()
    sb.reset(base_mark)

    TOP = SBAlloc.HI - 20 * 1024
    p4w = {"wpg": nc.alloc_sbuf_tensor_at("wpg_top", [128, 8, D], BF16, offset=TOP),
           "wpp": nc.alloc_sbuf_tensor_at("wpp_top", [128, 2, D], BF16, offset=TOP + 16 * 1024)}

    def p4_weight_loads():
        wpg_v = wpg_d.ap().rearrange("(c p) n -> p c n", p=128)
        for c in range(0, 8, 4):
            P.op("gpsimd", lambda e, c=c: e.dma_start(out=p4w["wpg"][:, c:c + 4, :], in_=wpg_v[:, c:c + 4, :]), writes=[("wpg", c)], dma=True)
        P.op("gpsimd", lambda e: e.dma_start(out=p4w["wpp"][:], in_=wpp_d.ap().rearrange("(c p) n -> p c n", p=128)), writes=["wpp"], dma=True)

    def phase3():
        NWB = 3
        w1b = [sb.alloc("w1b%d" % i, [128, 8, 512], BF16) for i in range(NWB)]
        w3b = [sb.alloc("w3b%d" % i, [128, 8, 512], BF16) for i in range(NWB)]
        w2b = [sb.alloc("w2b%d" % i, [128, 4, D], BF16) for i in range(NWB)]
        xr = [sb.alloc("xr%d" % i, [128, 3, D], BF16) for i in range(3)]
        xsT = [sb.alloc("xsT%d" % i, [128, 8, CAP], BF16) for i in range(2)]
        s1 = [sb.alloc("s1%d" % i, [128, CAP], F32) for i in range(2)]
        hdn = [sb.alloc("hdn%d" % i, [128, 4, CAP], BF16) for i in range(2)]
        yb = [sb.alloc("yb%d" % i, [128, D], BF16) for i in range(3)]
        HACC = [(A0, "A0", A1, "A1"), (A2, "A2", B0, ("B0", 0))]
        yctr = [0]
        P.op("gpsimd", lambda e: e.memset(yb[0][:], 0.0), writes=[("yb", 0, 0), ("yb", 0, 1)])
        P.op("sync", lambda e: e.dma_start(out=ys_d.ap()[NSLOT:NSLOT + 128, :], in_=yb[0][:]),
             reads=[("yb", 0, 0), ("yb", 0, 1)], writes=["ys_trash"], dma=True)
        def wload(ex, after=()):
            wb_ = ex % NWB
            if after:
                P.op("gpsimd", lambda e: e.memset(junk[:, 0:8], 0.0), reads=list(after), writes=["junk_g"])
            P.op("gpsimd", lambda e, ex=ex, wb_=wb_: e.dma_start(out=w1b[wb_][:], in_=w1_d.ap()[ex].rearrange("(p c) f -> p c f", c=8)),
                 writes=[("w1b", wb_)], dma=True)
            P.op("gpsimd", lambda e, ex=ex, wb_=wb_: e.dma_start(out=w3b[wb_][:], in_=w3_d.ap()[ex].rearrange("(p c) f -> p c f", c=8)),
                 writes=[("w3b", wb_)], dma=True)
            P.op("gpsimd", lambda e, ex=ex, wb_=wb_: e.dma_start(out=w2b[wb_][:], in_=w2_d.ap()[ex].rearrange("(c p) f -> p c f", p=128)),
                 writes=[("w2b", wb_)], dma=True)

        def xsload(ex):
            e3 = ex % 3
            P.op("sync", lambda e, ex=ex, e3=e3: e.dma_start(
                out=xr[e3][:], in_=xs_d.ap()[ex * CAP:(ex + 1) * CAP, :].rearrange("(r p) d -> p r d", p=128)),
                writes=[("xr", e3)], dma=True)

        def xsT_group(ex, r):
            eb = ex % 2
            e3 = ex % 3
            transpose_tile(xr[e3][:, r, :], 8, xsT[eb][:, :, r * 128:(r + 1) * 128], ("xr", e3), ("xsT", eb, r),
                           evac=("scalar" if r % 2 == 0 else "vector"), interleave=8)

        xsload(0)
        wload(0)
        xsload(1)
        wload(1, after=[("w1b", 0), ("w3b", 0), ("w2b", 0)])
        for r in range(3):
            xsT_group(0, r)
        for ex in range(NE):
            eb = ex % 2
            wb_ = ex % NWB
            if ex + 2 < NE:
                wload(ex + 2)
                xsload(ex + 2)
            if ex == 2:
                p4_weight_loads()
            xk = [("xsT", eb, r) for r in range(3)]
            for f in range(4):
                a1, k1, a3, k3 = HACC[f % 2]
                z = f % 2
                for (dst, dkey, wsrc, wkey) in ((a1, k1, w1b, "w1b"), (a3, k3, w3b, "w3b")):
                    for c in range(8):
                        P.op("tensor", lambda e, c=c, dst=dst, wsrc=wsrc, f=f, eb=eb, wb_=wb_: e.matmul(
                            dst[:, 0:CAP], lhsT=wsrc[wb_][:, c, f * 128:(f + 1) * 128], rhs=xsT[eb][:, c, :], start=(c == 0), stop=(c == 7)),
                            reads=[(wkey, wb_)] + xk, writes=[dkey])
                P.op("scalar", lambda e, a1=a1, z=z: e.activation(out=s1[z][:], in_=a1[:, 0:CAP], func=AF.Silu), reads=[k1], writes=[("s1", z)])
                P.op("vector", lambda e, a3=a3, z=z, f=f, eb=eb: e.tensor_tensor(out=hdn[eb][:, f, :], in0=s1[z][:], in1=a3[:, 0:CAP], op=ALU.mult),
                     reads=[("s1", z), k3], writes=[("hdn", eb, f)])
            hk_ = [("hdn", eb, f) for f in range(4)]
            for r in range(3):
                if ex + 1 < NE:
                    xsT_group(ex + 1, r)
                ys_ = yctr[0] % 3
                yctr[0] += 1
                for half in range(2):
                    for f in range(4):
                        P.op("tensor", lambda e, f=f, half=half, r=r, eb=eb, wb_=wb_: e.matmul(
                            B1[:, half * 512:(half + 1) * 512], lhsT=hdn[eb][:, f, r * 128:(r + 1) * 128], rhs=w2b[wb_][:, f, half * 512:(half + 1) * 512],
                            start=(f == 0), stop=(f == 3)),
                            reads=hk_ + [("w2b", wb_)], writes=[("B1", half)])
                    if half == 0:
                        P.op("scalar", lambda e, ys_=ys_: e.activation(out=yb[ys_][:, 0:512], in_=B1[:, 0:512], func=AF.Copy),
                             reads=[("B1", 0)], writes=[("yb", ys_, 0)])
                    else:
                        P.op("vector", lambda e, ys_=ys_: e.tensor_copy(out=yb[ys_][:, 512:1024], in_=B1[:, 512:1024]),
                             reads=[("B1", 1)], writes=[("yb", ys_, 1)])
                row0 = ex * CAP + r * 128
                P.op("sync", lambda e, row0=row0, ys_=ys_: e.dma_start(out=ys_d.ap()[row0:row0 + 128, :], in_=yb[ys_][:]),
                     reads=[("yb", ys_, 0), ("yb", ys_, 1)], writes=[("ys_d", ex, r)], dma=True)
        P.barrier()
    if 3 in phases:
        phase3()
    sb.reset(base_mark)

    def phase4():
        wpg, wpp = p4w["wpg"], p4w["wpp"]
        gple = sb.alloc("gple", [128, D], F32)
        bpg = sb.alloc("bpg", [128, D], F32)
        hb = [sb.alloc("hb%d" % i, [128, D], F32) for i in range(3)]
        y1 = [sb.alloc("y1%d" % i, [128, D], BF16) for i in range(3)]
        y2 = [sb.alloc("y2%d" % i, [128, D], BF16) for i in range(3)]
        pin = [sb.alloc("pin%d" % i, [128, 256], F32) for i in range(3)]
        pbf = [sb.alloc("pbf%d" % i, [128, 256], BF16) for i in range(2)]
        ppT = [sb.alloc("ppT%d" % i, [128, 2, 128], BF16) for i in range(2)]
        n3 = [sb.alloc("n3%d" % i, [128, D], BF16) for i in range(2)]
        n3T = [sb.alloc("n3T%d" % i, [128, 8, 128], BF16) for i in range(2)]
        gz = [sb.alloc("gz%d" % i, [128, D], F32) for i in range(2)]
        ob = [sb.alloc("ob%d" % i, [128, D], F32) for i in range(2)]
        ld("sync", gple[:], gple_d.ap().partition_broadcast(128), "gple")
        ld("sync", bpg[:], bpg_d.ap().partition_broadcast(128), "bpg")
        def loads4(t):
            h3 = t % 3
            P.op("sync", lambda e, t=t, h3=h3: e.dma_start(out=hb[h3][:], in_=h_d.ap()[t * 128:(t + 1) * 128, :]), writes=[("hb", h3)], dma=True)
            P.op("sync", lambda e, t=t, h3=h3: e.dma_start(out=pin[h3][:], in_=p_d.ap()[t * 128:(t + 1) * 128, :]), writes=[("pin", h3)], dma=True)
            for (yy, ykey, k) in ((y1, "y1", 0), (y2, "y2", 1)):
                P.op("gpsimd", lambda e, yy=yy, k=k, t=t, h3=h3: e.indirect_dma_start(
                    out=yy[h3][:, :], out_offset=None, in_=ys_d[:, :], in_offset=bass.IndirectOffsetOnAxis(ap=dtab[:, t, k:k + 1], axis=0)), reads=["dtab_all"], writes=[(ykey, h3)], dma=True)

        def S1a(t):
            h3 = t % 3
            z = t % 2
            P.op("vector", lambda e, h3=h3, t=t: e.scalar_tensor_tensor(out=hb[h3][:], in0=y1[h3][:], scalar=wtab[:, t, 0:1], in1=hb[h3][:],
                                                                       op0=ALU.mult, op1=ALU.add), reads=[("y1", h3), ("hb", h3)], writes=[("hb", h3)])
            P.op("vector", lambda e, h3=h3, t=t: e.scalar_tensor_tensor(out=hb[h3][:], in0=y2[h3][:], scalar=wtab[:, t, 1:2], in1=hb[h3][:],
                                                                       op0=ALU.mult, op1=ALU.add), reads=[("y2", h3), ("hb", h3)], writes=[("hb", h3)])
            rmsnorm_tile(hb[h3][:], gple[:], n3[z][:], ("hb", h3), "gple", ("n3", z))
            P.op("scalar", lambda e, z=z, h3=h3: e.activation(out=pbf[z][:], in_=pin[h3][:], func=AF.Copy), reads=[("pin", h3)], writes=[("pbf", z)])

        def S1b(t):
            z = t % 2
            transpose_tile(n3[z], 8, n3T[z][:], ("n3", z), ("n3T", z))
            transpose_tile(pbf[z], 2, ppT[z][:], ("pbf", z), ("ppT", z))

        GACC = [((A0, "A0"), (A2, "A2")), ((A1, "A1"), (B0, ("B0", 0)))]

        def S2mm(t, half):
            z = t % 2
            (ga, gk), (pa_, pk) = GACC[half]
            for c in range(8):
                P.op("tensor", lambda e, c=c, half=half, ga=ga, z=z: e.matmul(
                    ga[:], lhsT=n3T[z][:, c, :], rhs=wpg[:, c, half * 512:(half + 1) * 512], start=(c == 0), stop=(c == 7)),
                    reads=[("n3T", z), ("wpg", 0), ("wpg", 4)], writes=[gk])
            for c in range(2):
                P.op("tensor", lambda e, c=c, half=half, pa_=pa_, z=z: e.matmul(
                    pa_[:, 0:512], lhsT=ppT[z][:, c, :], rhs=wpp[:, c, half * 512:(half + 1) * 512], start=(c == 0), stop=(c == 1)),
                    reads=[("ppT", z), "wpp"], writes=[pk])

        def S2tail(t):
            z = t % 2
            h3 = t % 3
            hsl = [slice(0, 512), slice(512, 1024)]
            for half in range(2):
                (ga, gk), (pa_, pk) = GACC[half]
                hs = hsl[half]
                P.op("vector", lambda e, ga=ga, z=z, hs=hs: e.tensor_tensor(out=gz[z][:, hs], in0=ga[:], in1=bpg[:, hs], op=ALU.add),
                     reads=[gk, "bpg"], writes=[("gz", z, half)])
                P.op("scalar", lambda e, z=z, hs=hs: e.activation(out=gz[z][:, hs], in_=gz[z][:, hs], func=AF.Tanh, scale=0.5),
                     reads=[("gz", z, half)], writes=[("gz", z, half)])
            for half in range(2):
                (ga, gk), (pa_, pk) = GACC[half]
                hs = hsl[half]
                P.op("vector", lambda e, pa_=pa_, z=z, hs=hs: e.scalar_tensor_tensor(out=gz[z][:, hs], in0=gz[z][:, hs], scalar=1.0, in1=pa_[:, 0:512],
                                                                                    op0=ALU.add, op1=ALU.mult), reads=[("gz", z, half), pk], writes=[("gz", z, half)])
                P.op("vector", lambda e, z=z, hs=hs, h3=h3: e.scalar_tensor_tensor(out=ob[z][:, hs], in0=gz[z][:, hs], scalar=0.5, in1=hb[h3][:, hs],
                                                                                  op0=ALU.mult, op1=ALU.add), reads=[("gz", z, half), ("hb", h3)], writes=[("ob", z, half)])

        loads4(0)
        loads4(1)
        S1a(0)
        S1b(0)
        for t in range(NT):
            z = t % 2
            if t + 2 < NT:
                loads4(t + 2)
            if t + 1 < NT:
                S1a(t + 1)
            S2mm(t, 0)
            S2mm(t, 1)
            S2tail(t)
            if t + 1 < NT:
                S1b(t + 1)
            P.op("sync", lambda e, t=t, z=z: e.dma_start(out=out_d.ap()[t * 128:(t + 1) * 128, :], in_=ob[z][:]),
                 reads=[("ob", z, 0), ("ob", z, 1)], dma=True, is_out=True)
    if 4 in phases:
        phase4()
    P.emit()
    return nc, P


def _host_layout(inp):
    f = lambda a: np.ascontiguousarray(np.asarray(a, dtype=np.float32))
    bf = ml_dtypes.bfloat16
    b_in = f(inp["b_in"])[0]
    rel = f(inp["rel_bias"])[0]
    jj = np.arange(5)[::-1][:, None, None]
    kk = np.arange(128)[None, :, None]
    qq = np.arange(128)[None, None, :]
    dist = qq - kk + 128 * (4 - jj)
    idx = np.clip(dist, -63, 256) + 63
    cdiff = (qq // 64) - (kk // 64) + 2 * (4 - jj)
    mask = ((cdiff >= 0) & (cdiff <= 8)).astype(np.float32)
    rbT = rel[:, idx]
    rbT = np.ascontiguousarray(rbT.transpose(2, 0, 1, 3)).reshape(128, NH * 5 * 128)
    maskT = np.ascontiguousarray(mask.transpose(1, 0, 2)).reshape(128, 5 * 128)
    cwv = f(inp["conv_w"])[0]
    cw = np.ascontiguousarray(cwv.reshape(3, 4, 128).transpose(2, 1, 0)).reshape(128, 12)
    cb = np.ascontiguousarray(f(inp["conv_b"])[0].reshape(4, 128).T)
    gq = f(inp["g_q"])[0]
    gk = f(inp["g_k"])[0]
    shared = {
        "g_mix": f(inp["g_mix"]),
        "w_in": f(inp["w_in"])[0],
        "bcol": np.ascontiguousarray(b_in.reshape(40, 128).T),
        "gqk": np.ascontiguousarray(np.stack([np.tile(gq, 2), np.tile(gk, 2)], axis=1)),
        "bv": np.ascontiguousarray(b_in[1024:1536].reshape(1, 512)),
        "rbT": rbT, "maskT": maskT, "cw": cw, "cb": cb,
        "w_pa": f(inp["w_pa"])[0], "w_pc": f(inp["w_pc"])[0], "w_o": f(inp["w_o"])[0],
        "g_ffn": f(inp["g_ffn"]),
        "w_rg": np.ascontiguousarray(np.concatenate([f(inp["w_group"])[0], f(inp["w_router"])[0]], axis=1)),
        "b_rg": np.ascontiguousarray(np.concatenate([f(inp["b_group"])[0], f(inp["b_router"])[0]])[None, :]),
        "w1": f(inp["w1"])[0], "w3": f(inp["w3"])[0], "w2": f(inp["w2"])[0],
        "g_ple": f(inp["g_ple"]), "w_pg": f(inp["w_ple_gate"])[0], "b_pg": f(inp["b_ple_gate"]),
        "w_pp": f(inp["w_ple_proj"])[0],
        "ident": np.eye(128, dtype=np.float32).astype(bf),
        "utri": np.triu(np.ones((128, 128), np.float32), 1).astype(bf),
        "ones": np.ones((128, 128), np.float32).astype(bf),
        "bdiag": (np.kron(np.eye(2, dtype=np.float32), np.ones((64, 64), np.float32)) / 64.0).astype(bf),
        "ecap": np.ascontiguousarray(np.broadcast_to((np.arange(NE, dtype=np.float32) * CAP)[None, :], (128, NE))),
    }
    x = f(inp["x"])
    p = f(inp["p"])[0]
    maps = []
    for c in range(NCORES):
        m = dict(shared)
        m["x"] = x[c]
        m["p"] = p[c]
        maps.append(m)
    return maps


_CACHE = {}


def kernel(**inputs):
    if "nc" not in _CACHE:
        _CACHE["nc"] = build(debug=False)[0]
    nc = _CACHE["nc"]
    maps = _host_layout(inputs)
    res = run_bass_kernel_spmd(nc, maps, core_ids=list(range(NCORES)))
    out = np.stack([np.asarray(res.results[c]["out"], dtype=np.float32) for c in range(NCORES)], axis=0)
    return out
```

```python
import numpy as np
import ml_dtypes
import concourse.bass as bass
import concourse.mybir as mybir
from concourse.bass_utils import run_bass_kernel_spmd

F32 = mybir.dt.float32
BF16 = mybir.dt.bfloat16
I32 = mybir.dt.int32
ALU = mybir.AluOpType
AF = mybir.ActivationFunctionType
AX = mybir.AxisListType

NCORES = 8
S = 4096
D = 1024
NT = S // 128
BT = 512
NB = S // BT
NH = 8
DH = 64
NE = 32
CAP = 384
NSLOT = NE * CAP
KR = 12
EPS = 1e-6

ENGS = ("sync", "scalar", "vector", "gpsimd", "tensor")
NDMASEM = 24
SAME_ENG_WINDOW = 10 ** 9


class Op:
    __slots__ = ("idx", "eng", "fn", "reads", "writes", "dma", "deps", "sig",
                 "sem", "val", "clock", "epos", "barrier")


class Prog:
    def __init__(self, nc):
        self.nc = nc
        self.ops = []
        self.last_w = {}
        self.readers = {}
        self.out_ops = []
        self.last_barrier = None
        self.since_barrier = []

    def op(self, eng, fn, reads=(), writes=(), dma=False, is_out=False):
        o = Op()
        o.idx = len(self.ops)
        o.eng = eng
        o.fn = fn
        o.dma = dma
        o.barrier = False
        o.reads = tuple(reads)
        o.writes = tuple(writes)
        deps = set()
        for k in o.reads:
            w = self.last_w.get(k)
            if w is not None:
                deps.add(w)
        for k in o.writes:
            w = self.last_w.get(k)
            if w is not None:
                deps.add(w)
            for r in self.readers.get(k, ()):
                deps.add(r)
        for k in o.writes:
            self.last_w[k] = o.idx
            self.readers[k] = []
        for k in o.reads:
            if k not in o.writes:
                self.readers.setdefault(k, []).append(o.idx)
        if self.last_barrier is not None:
            deps.add(self.last_barrier)
        deps.discard(o.idx)
        o.deps = sorted(deps)
        o.sig = False
        self.ops.append(o)
        self.since_barrier.append(o.idx)
        if is_out:
            self.out_ops.append(o.idx)
        return o.idx

    def barrier(self):
        o = Op()
        o.idx = len(self.ops)
        o.eng = "sync"
        o.fn = "BARRIER"
        o.dma = False
        o.barrier = True
        o.reads = ()
        o.writes = ()
        last = {}
        deps = []
        for i in self.since_barrier:
            p = self.ops[i]
            if p.dma:
                deps.append(i)
            else:
                last[p.eng] = i
        deps.extend(last.values())
        if self.last_barrier is not None:
            deps.append(self.last_barrier)
        o.deps = sorted(set(deps))
        o.sig = True
        self.ops.append(o)
        self.last_barrier = o.idx
        self.since_barrier = []
        self.last_w = {}
        self.readers = {}

    def emit(self):
        nc = self.nc
        ops = self.ops
        epos = {e: 0 for e in ENGS}
        for o in ops:
            o.epos = epos[o.eng]
            epos[o.eng] += 1
        fin = Op()
        fin.idx = len(ops)
        fin.eng = "sync"
        fin.fn = None
        fin.dma = False
        fin.barrier = False
        fin.reads = ()
        fin.writes = ()
        fin.deps = list(self.out_ops)
        fin.sig = False
        fin.epos = epos["sync"]
        ops = ops + [fin]
        for o in ops:
            nd = []
            for d in o.deps:
                do = ops[d]
                if do.eng == o.eng and not do.dma and not o.barrier:
                    if o.eng == "tensor" and not o.dma:
                        continue
                    if o.dma:
                        pass
                    elif o.epos - do.epos > SAME_ENG_WINDOW:
                        continue
                nd.append(d)
            o.deps = nd
            for d in nd:
                ops[d].sig = True
        sems = {}
        dma_engs = set(o.eng for o in ops if o.dma)
        for e in ENGS:
            sems[("c", e)] = nc.alloc_semaphore("c_" + e)
            if e in dma_engs:
                for i in range(NDMASEM):
                    sems[("d", e, i)] = nc.alloc_semaphore("d_%s_%d" % (e, i))
        ccount = {e: 0 for e in ENGS}
        dcount = {e: 0 for e in ENGS}
        dma_prev = {}
        for o in ops:
            if o.dma:
                k = dcount[o.eng]
                dcount[o.eng] += 1
                slot = k % NDMASEM
                o.sem = ("d", o.eng, slot)
                o.val = 16 * (k // NDMASEM + 1)
                prev = dma_prev.get((o.eng, slot))
                if prev is not None and prev not in o.deps:
                    o.deps.append(prev)
                dma_prev[(o.eng, slot)] = o.idx
            elif o.sig:
                ccount[o.eng] += 1
                o.sem = ("c", o.eng)
                o.val = ccount[o.eng]
            else:
                o.sem = None
                o.val = 0
        known = {e: {} for e in ENGS}
        streams = {e: [] for e in ENGS}
        for o in ops:
            kn = known[o.eng]
            wm = {}
            for d in sorted(o.deps, reverse=True):
                do = ops[d]
                if kn.get(do.sem, 0) >= do.val:
                    continue
                if wm.get(do.sem, 0) < do.val:
                    wm[do.sem] = do.val
                for s, v in do.clock.items():
                    if kn.get(s, 0) < v:
                        kn[s] = v
            o.clock = dict(kn)
            if o.sem is not None:
                o.clock[o.sem] = o.val
            streams[o.eng].append((o, list(wm.items())))
        self.n_waits = sum(len(w) for st in streams.values() for _, w in st)
        self.counts = (dict(ccount), dict(dcount))

        def run_stream(eng_name):
            def body(eng):
                for o, waits in streams[eng_name]:
                    for s, v in waits:
                        eng.wait_ge(sems[s], v)
                    if o.fn is None:
                        continue
                    if o.barrier:
                        eng.sem_inc(sems[o.sem], 1)
                        continue
                    ins = o.fn(eng)
                    if o.sem is not None:
                        ins.then_inc(sems[o.sem], 16 if o.dma else 1)
            return body

        with nc.Block() as block:
            for e in ENGS:
                if streams[e]:
                    getattr(block, e)(run_stream(e))


class SBAlloc:
    LO = 16512
    HI = 229344

    def __init__(self, nc):
        self.nc = nc
        self.cur = self.LO
        self.n = 0

    def alloc(self, name, shape, dt):
        esz = {F32: 4, BF16: 2, I32: 4}[dt]
        nbytes = esz
        for s in shape[1:]:
            nbytes *= s
        off = (self.cur + 31) // 32 * 32
        assert off + nbytes <= self.HI, "SBUF overflow at %s: need %d have %d" % (name, nbytes, self.HI - off)
        self.n += 1
        t = self.nc.alloc_sbuf_tensor_at("%s_%d" % (name, self.n), list(shape), dt, offset=off)
        self.cur = off + nbytes
        return t

    def mark(self):
        return self.cur

    def reset(self, m):
        self.cur = m


def bc_last(ap, n):
    shp = list(ap.shape)
    return ap.unsqueeze(len(shp)).broadcast_to(shp + [n])


def bc_mid(ap, n):
    shp = list(ap.shape)
    return ap.unsqueeze(1).broadcast_to([shp[0], n] + shp[1:])


def build(debug=False, phases=(1, 2, 3, 4)):
    nc = bass.Bass("TRN2", target_bir_lowering=False)
    P = Prog(nc)
    sb = SBAlloc(nc)

    def din(name, shape, dt=F32):
        return nc.dram_tensor(name, list(shape), dt, kind="ExternalInput")

    def dscr(name, shape, dt):
        return nc.dram_tensor(name, list(shape), dt, kind="ExternalOutput" if debug else "Internal")

    x_d = din("x", [S, D])
    p_d = din("p", [S, 256])
    gmix_d = din("g_mix", [1, D])
    win_d = din("w_in", [D, 5120])
    bcol_d = din("bcol", [128, 40])
    gqk_d = din("gqk", [128, 2])
    bv_d = din("bv", [1, 512])
    rbT_d = din("rbT", [128, NH * 5 * 128])
    maskT_d = din("maskT", [128, 5 * 128])
    cw_d = din("cw", [128, 12])
    cb_d = din("cb", [128, 4])
    wpa_d = din("w_pa", [512, D])
    wpc_d = din("w_pc", [512, D])
    wo_d = din("w_o", [D, D])
    gffn_d = din("g_ffn", [1, D])
    wrg_d = din("w_rg", [D, 36])
    brg_d = din("b_rg", [1, 36])
    w1_d = din("w1", [NE, D, 512])
    w3_d = din("w3", [NE, D, 512])
    w2_d = din("w2", [NE, 512, D])
    gple_d = din("g_ple", [1, D])
    wpg_d = din("w_pg", [D, D])
    bpg_d = din("b_pg", [1, D])
    wpp_d = din("w_pp", [256, D])
    ident_d = din("ident", [128, 128], BF16)
    utri_d = din("utri", [128, 128], BF16)
    ones_d = din("ones", [128, 128], BF16)
    bdiag_d = din("bdiag", [128, 128], BF16)
    ecap_d = din("ecap", [128, NE])
    selA_d = din("selA", [128, 8 * 128], BF16)
    selB_d = din("selB", [128, 8 * 128], BF16)
    out_d = nc.dram_tensor("out", [S, D], F32, kind="ExternalOutput")

    nT_d = dscr("nT_s", [D, S], BF16)
    yaT_d = dscr("yaT_s", [512, S], BF16)
    ycT_d = dscr("ycT_s", [512, S], BF16)
    h_d = dscr("h_s", [S, D], F32)
    xs_d = dscr("xs_s", [NSLOT + 128, D], BF16)
    ys_d = dscr("ys_s", [NSLOT + 128, D], BF16)
    if debug:
        dtab_o = nc.dram_tensor("dtab_o", [128, NT * 2], I32, kind="ExternalOutput")
        wtab_o = nc.dram_tensor("wtab_o", [128, NT * 2], F32, kind="ExternalOutput")

    pT = nc.alloc_psum_tensor("pT", [128, 8, 128], BF16)
    A0 = nc.alloc_psum_tensor("A0", [128, 512], F32)
    A1 = nc.alloc_psum_tensor("A1", [128, 512], F32)
    A2 = nc.alloc_psum_tensor("A2", [128, 512], F32)
    B0 = nc.alloc_psum_tensor("B0", [128, 1024], F32)
    B1 = nc.alloc_psum_tensor("B1", [128, 1024], F32)

    ident = sb.alloc("ident", [128, 128], BF16)
    utri = sb.alloc("utri", [128, 128], BF16)
    ones = sb.alloc("ones", [128, 128], BF16)
    bdiag = sb.alloc("bdiag", [128, 128], BF16)
    ecap = sb.alloc("ecap", [128, NE], F32)
    bcol = sb.alloc("bcol", [128, 40], F32)
    hbcol = sb.alloc("hbcol", [128, 40], F32)
    mhalf = sb.alloc("mhalf", [128, 8], F32)
    epsc = sb.alloc("epsc", [128, 8], F32)
    dtab = sb.alloc("dtab", [128, NT, 2], I32)
    wtab = sb.alloc("wtab", [128, NT, 2], F32)
    cnt = sb.alloc("cnt", [128, NE], F32)
    ss = sb.alloc("ss", [128, 8], F32)
    rs = sb.alloc("rs", [128, 8], F32)
    junk = sb.alloc("junk", [128, D], BF16)

    def ld(eng, dst, src, key):
        P.op(eng, lambda e: e.dma_start(out=dst, in_=src), writes=[key], dma=True)

    ld("sync", ident[:], ident_d.ap(), "ident")
    ld("sync", utri[:], utri_d.ap(), "utri")
    ld("sync", ones[:], ones_d.ap(), "ones")
    ld("sync", bdiag[:], bdiag_d.ap(), "bdiag")
    ld("sync", ecap[:], ecap_d.ap(), "ecap")
    ld("sync", bcol[:], bcol_d.ap(), "bcol")
    P.op("vector", lambda e: e.tensor_scalar(out=hbcol[:], in0=bcol[:], scalar1=0.5, scalar2=None, op0=ALU.mult),
         reads=["bcol"], writes=["hbcol"])
    P.op("gpsimd", lambda e: e.memset(mhalf[:], -0.5), writes=["mhalf"])
    P.op("gpsimd", lambda e: e.memset(epsc[:], EPS), writes=["epsc"])
    P.op("gpsimd", lambda e: e.memset(cnt[:], 0.0), writes=["cnt"])
    P.op("gpsimd", lambda e: e.memset(wtab[:], 0.0), writes=["wtab"])

    nrm_ctr = [0]

    def rmsnorm_tile(src, g_bc, dst_bf, src_key, g_key, dst_key):
        i = nrm_ctr[0] % 8
        nrm_ctr[0] += 1
        ssk, rsk = ("ss", i), ("rs", i)
        P.op("scalar", lambda e: e.activation(out=junk[:], in_=src, func=AF.Square, accum_out=ss[:, i:i + 1]),
             reads=[src_key], writes=["junk", ssk])
        P.op("vector", lambda e: e.tensor_scalar(out=rs[:, i:i + 1], in0=ss[:, i:i + 1], scalar1=1.0 / D, scalar2=EPS,
                                                  op0=ALU.mult, op1=ALU.add), reads=[ssk], writes=[rsk])
        P.op("gpsimd", lambda e: e.tensor_tensor(out=rs[:, i:i + 1], in0=rs[:, i:i + 1], in1=mhalf[:, 0:1], op=ALU.pow),
             reads=[rsk, "mhalf"], writes=[rsk])
        P.op("vector", lambda e: e.scalar_tensor_tensor(out=dst_bf, in0=src, scalar=rs[:, i:i + 1], in1=g_bc,
                                                         op0=ALU.mult, op1=ALU.mult),
             reads=[src_key, rsk, g_key], writes=[dst_key])

    def transpose_tile(src_bf, nchunk, dst, src_key, dst_key, evac="scalar", interleave=0):
        for c in range(nchunk):
            if interleave:
                src_c = src_bf.rearrange("t (p c) -> t c p", c=interleave)[:, c, :]
            else:
                src_c = src_bf[:, c * 128:(c + 1) * 128]
            P.op("tensor", lambda e, c=c, src_c=src_c: e.transpose(out=pT[:, c, :], in_=src_c, identity=ident[:]),
                 reads=(list(src_key) if isinstance(src_key, list) else [src_key]) + ["ident"], writes=[("pT", c)])
        if evac == "scalar":
            P.op("scalar", lambda e: e.activation(out=dst, in_=pT[:, 0:nchunk, :], func=AF.Copy),
                 reads=[("pT", c) for c in range(nchunk)], writes=[dst_key])
        else:
            P.op("vector", lambda e: e.tensor_copy(out=dst, in_=pT[:, 0:nchunk, :]),
                 reads=[("pT", c) for c in range(nchunk)], writes=[dst_key])

    _breg = {}

    def breg(e):
        if "r" not in _breg:
            _breg["r"] = e.to_reg(NSLOT - 1)
        return _breg["r"]

    base_mark = sb.mark()

    def phase1():
        Wa = sb.alloc("Wa", [128, 8, 3072], BF16)
        gmix = sb.alloc("gmix", [128, D], F32)
        gqk = sb.alloc("gqk", [128, 2], F32)
        bvb = sb.alloc("bvb", [128, 512], F32)
        cw = sb.alloc("cw", [128, 12], F32)
        cb = sb.alloc("cb", [128, 4], F32)
        expB = sb.alloc("expB", [128, NH, 5, 128], BF16)
        maskT = sb.alloc("maskT", [128, 5, 128], F32)
        kring = sb.alloc("kring", [128, 4, KR * 128], BF16)
        vring = sb.alloc("vring", [128, KR, NH, 65], BF16)
        xt = [sb.alloc("xt%d" % i, [128, D], F32) for i in range(2)]
        nb = [sb.alloc("nb%d" % i, [128, D], BF16) for i in range(2)]
        nTb = [sb.alloc("nTb%d" % i, [128, 8, BT], BF16) for i in range(2)]
        qT = [sb.alloc("qT%d" % i, [128, 4, BT], BF16) for i in range(2)]
        zq = [sb.alloc("zq%d" % i, [128, BT], F32) for i in range(8)]
        sq = [sb.alloc("sq%d" % i, [128, BT], BF16) for i in range(3)]
        rs = sb.alloc("rs_all", [128, BT], F32)
        r1b = sb.alloc("r1b", [128, BT], BF16)
        selA = sb.alloc("selA", [128, 8, 128], BF16)
        selB = sb.alloc("selB", [128, 8, 128], BF16)
        gw = sb.alloc("gw", [128, 1], F32)
        us = [sb.alloc("us%d" % i, [128, BT], F32) for i in range(2)]
        t1 = [sb.alloc("t1%d" % i, [128, BT], F32) for i in range(2)]
        cu = sb.alloc("cu", [128, 4, BT + 2], F32)
        ycT = [sb.alloc("ycT%d" % i, [128, 4, BT], BF16) for i in range(2)]
        yaT = [sb.alloc("yaT%d" % i, [128, 4, BT], BF16) for i in range(2)]
        pt = [sb.alloc("pt%d" % i, [128, 4, 128], BF16) for i in range(4)]
        rden = [sb.alloc("rden%d" % i, [128, 4], F32) for i in range(2)]
        ya = sb.alloc("ya", [128, 4, 512], BF16)

        win_v = win_d.ap().rearrange("(c p) n -> p c n", p=128)
        for (c0_, c1_) in ((0, 1024), (1024, 1536), (1536, 3072)):
            for c in range(0, 8, 2):
                P.op("gpsimd", lambda e, c=c, c0_=c0_, c1_=c1_: e.dma_start(out=Wa[:, c:c + 2, c0_:c1_], in_=win_v[:, c:c + 2, c0_:c1_]),
                     writes=[("Wa", c, c0_), ("Wa", c + 1, c0_)], dma=True)
        zt = sb.alloc("zt", [128, 2 * D], BF16)
        P.op("gpsimd", lambda e: e.memset(zt[:], 0.0), writes=["zt"])
        NR = (NSLOT + 128) // 128
        xs_z = xs_d.ap().rearrange("(p r) d -> p (r d)", p=128)
        zchunks = [(r0, min(r0 + 2, NR)) for r0 in range(0, NR, 2)]

        def zero_fill(k):
            for (r0, r1_) in zchunks[k::NB]:
                P.op("sync", lambda e, r0=r0, r1_=r1_: e.dma_start(out=xs_z[:, r0 * D:r1_ * D], in_=zt[:, 0:(r1_ - r0) * D]),
                     reads=["zt"], writes=[("xs_zero", r0)], dma=True)

        ld("sync", gmix[:], gmix_d.ap().partition_broadcast(128), "gmix")
        ld("sync", gqk[:], gqk_d.ap(), "gqk")
        ld("sync", selA[:], selA_d.ap().rearrange("p (j m) -> p j m", j=8), "selA")
        ld("sync", selB[:], selB_d.ap().rearrange("p (j m) -> p j m", j=8), "selB")
        ld("sync", bvb[:], bv_d.ap().partition_broadcast(128), "bvb")
        P.op("vector", lambda e: e.tensor_tensor(out=gw[:], in0=gqk[:, 0:1], in1=gqk[:, 1:2], op=ALU.mult), reads=["gqk"], writes=["gw"])
        ld("sync", cw[:], cw_d.ap(), "cw")
        ld("sync", cb[:], cb_d.ap(), "cb")
        ld("sync", maskT[:], maskT_d.ap().rearrange("p (j q) -> p j q", j=5), "maskT")
        rb_v = rbT_d.ap().rearrange("p (h n) -> p h n", h=NH)
        stg = [sb.alloc("stg%d" % i, [128, 640], F32) for i in range(2)]
        for h in range(NH):
            st = stg[h % 2]
            P.op("sync", lambda e, h=h, st=st: e.dma_start(out=st[:], in_=rb_v[:, h, :]),
                 writes=[("stg", h % 2)], dma=True)
            P.op("scalar", lambda e, st=st: e.activation(out=st[:], in_=st[:], func=AF.Exp),
                 reads=[("stg", h % 2)], writes=[("stg", h % 2)])
            P.op("vector", lambda e, h=h, st=st: e.tensor_tensor(
                out=expB[:, h, :, :], in0=st[:].rearrange("p (j q) -> p j q", j=5), in1=maskT[:], op=ALU.mult),
                reads=[("stg", h % 2), "maskT"], writes=[("expB", h)])
        P.op("gpsimd", lambda e: e.memset(vring[:], 1.0), writes=[("v", s_) for s_ in range(KR)])
        P.op("gpsimd", lambda e: e.memset(cu[:], 0.0), writes=[("cu", ct) for ct in range(4)] + [("cuh", ct) for ct in range(4)])

        nT_v = nT_d.ap().rearrange("(c p) t -> p c t", p=128)
        yaT_v = yaT_d.ap().rearrange("(c p) t -> p c t", p=128)
        ycT_v = ycT_d.ap().rearrange("(c p) t -> p c t", p=128)
        acc_rot = [0]
        ACC = [(A0, "A0"), (A1, "A1")]

        def next_acc():
            a = ACC[acc_rot[0] % 2]
            acc_rot[0] += 1
            return a

        def A_norm(b, tl):
            t = 4 * b + tl
            s2 = t % 2
            P.op("sync", lambda e, t=t, s2=s2: e.dma_start(out=xt[s2][:], in_=x_d.ap()[t * 128:(t + 1) * 128, :]),
                 writes=[("xt", s2)], dma=True)
            rmsnorm_tile(xt[s2][:], gmix[:], nb[s2][:], ("xt", s2), "gmix", ("nb", s2))

        def A_tr(b, tl):
            t = 4 * b + tl
            s2 = t % 2
            bb = b % 2
            transpose_tile(nb[s2], 8, nTb[bb][:, :, tl * 128:(tl + 1) * 128], ("nb", s2), ("nTb", bb, tl))
            if tl == 3:
                P.op("sync", lambda e, bb=bb, b=b: e.dma_start(out=nT_v[:, :, b * BT:(b + 1) * BT], in_=nTb[bb][:]),
                     reads=[("nTb", bb, q_) for q_ in range(4)], writes=[("nT_d", b)], dma=True)

        def secC(b):
            bb = b % 2
            tok0 = b * BT
            nkeys = [("nTb", bb, tl) for tl in range(4)]
            PACC = [(A0[:], "A0"), (A1[:], "A1"), (B0[:, 0:512], ("B0", 0))]
            prot = [0]

            def nacc():
                a = PACC[prot[0] % 3]
                prot[0] += 1
                return a

            def proj(j):
                acc, akey = nacc()
                for c in range(8):
                    P.op("tensor", lambda e, j=j, c=c, acc=acc: e.matmul(
                        acc, lhsT=Wa[:, c, j * 128:(j + 1) * 128], rhs=nTb[bb][:, c, :], start=(c == 0), stop=(c == 7)),
                        reads=[("Wa", c, 0)] + nkeys, writes=[akey])
                z = j % 3
                P.op("scalar", lambda e, j=j, acc=acc: e.activation(out=zq[j][:], in_=acc, func=AF.Identity, bias=bcol[:, j:j + 1]),
                     reads=[akey, "bcol"], writes=[("zq", j)])
                P.op("gpsimd", lambda e, z=z, j=j: e.tensor_tensor(out=sq[z][:], in0=zq[j][:], in1=zq[j][:], op=ALU.mult),
                     reads=[("zq", j)], writes=[("sq", z)])

            def msacc(j):
                z = j % 3
                P.op("tensor", lambda e, z=z, j=j: e.matmul(A2[:], lhsT=selA[:, j, :], rhs=sq[z][:], start=(j == 0), stop=(j == 7)),
                     reads=[("sq", z), "selA"], writes=["A2"])

            def vproj():
                for tl in range(4):
                    t = 4 * b + tl
                    sl = t % KR
                    acc, akey = nacc()
                    for c in range(8):
                        P.op("tensor", lambda e, c=c, tl=tl, acc=acc: e.matmul(
                            acc, lhsT=nTb[bb][:, c, tl * 128:(tl + 1) * 128], rhs=Wa[:, c, 1024:1536], start=(c == 0), stop=(c == 7)),
                            reads=[("Wa", c, 1024), ("nTb", bb, tl)], writes=[akey])
                    P.op("vector", lambda e, sl=sl, acc=acc: e.tensor_tensor(
                        out=vring[:, sl, :, 0:64], in0=acc.rearrange("p (h d) -> p h d", h=NH),
                        in1=bvb[:].rearrange("p (h d) -> p h d", h=NH), op=ALU.add),
                        reads=[akey, "bvb"], writes=[("v", sl)])

            def fin(j):
                acc, akey = nacc()
                P.op("tensor", lambda e, j=j, acc=acc: e.matmul(acc, lhsT=selB[:, j, :], rhs=r1b[:], start=True, stop=True),
                     reads=["selB", "r1b"], writes=[akey])
                if j < 4:
                    P.op("vector", lambda e, j=j, acc=acc: e.tensor_tensor(out=qT[bb][:, j, :], in0=zq[j][:], in1=acc, op=ALU.mult),
                         reads=[("zq", j), akey], writes=[("qT", bb, j)])
                else:
                    hp = j - 4
                    sl0 = (4 * b) % KR
                    P.op("vector", lambda e, hp=hp, j=j, sl0=sl0, acc=acc: e.scalar_tensor_tensor(
                        out=kring[:, hp, sl0 * 128:(sl0 + 4) * 128], in0=zq[j][:], scalar=gw[:, 0:1], in1=acc, op0=ALU.mult, op1=ALU.mult),
                        reads=[("zq", j), akey, "gw"], writes=[("k", hp, sl0 + q_) for q_ in range(4)])

            proj(0)
            for j in range(8):
                if j + 1 < 8:
                    proj(j + 1)
                msacc(j)
            P.op("scalar", lambda e: e.activation(out=rs[:], in_=A2[:], func=AF.Sqrt, bias=epsc[:, 0:1]), reads=["A2", "epsc"], writes=["rs"])
            P.op("vector", lambda e: e.reciprocal(out=rs[:], in_=rs[:]), reads=["rs"], writes=["rs"])
            P.op("scalar", lambda e: e.activation(out=r1b[:], in_=rs[:], func=AF.Copy), reads=["rs"], writes=["r1b"])
            vproj()
            for j in range(8):
                fin(j)
            SETS = [((A0[:], ["A0"]), (A1[:], ["A1"]), (A2[:], ["A2"])),
                    ((B0[:, 0:512], [("B0", 0)]), (B0[:, 512:1024], [("B0", 4)]), (B1[:, 0:512], [("B1h", 0)]))]
            for ct in range(4):
                z = ct % 2
                (pu, ku), (pb, kb), (pc, kc) = SETS[ct % 2]
                for (dst, dkey, col0) in ((pu, ku, 1536), (pb, kb, 2048), (pc, kc, 2560)):
                    for c in range(8):
                        P.op("tensor", lambda e, c=c, dst=dst, col0=col0, ct=ct: e.matmul(
                            dst, lhsT=Wa[:, c, col0 + ct * 128:col0 + (ct + 1) * 128], rhs=nTb[bb][:, c, :],
                            start=(c == 0), stop=(c == 7)),
                            reads=[("Wa", c, 1536)] + nkeys, writes=dkey)
                ju, jb, jc = 12 + ct, 16 + ct, 20 + ct
                P.op("scalar", lambda e, z=z, ju=ju, pu=pu: e.activation(out=us[z][:], in_=pu, func=AF.Identity, bias=bcol[:, ju:ju + 1]),
                     reads=ku + ["bcol"], writes=[("us", z)])
                P.op("vector", lambda e, z=z, jc=jc, ct=ct, pc=pc: e.scalar_tensor_tensor(
                    out=cu[:, ct, 2:BT + 2], in0=pc, scalar=bcol[:, jc:jc + 1], in1=us[z][:], op0=ALU.add, op1=ALU.mult),
                    reads=kc + ["bcol", ("us", z)], writes=[("cu", ct)])
                P.op("scalar", lambda e, z=z, ct=ct: e.activation(out=t1[z][:], in_=cu[:, ct, 2:BT + 2], func=AF.Identity,
                                                               scale=cw[:, ct * 3 + 2:ct * 3 + 3], bias=cb[:, ct:ct + 1]),
                     reads=[("cu", ct), "cw", "cb"], writes=[("t1", z)])
                P.op("vector", lambda e, z=z, ct=ct: e.scalar_tensor_tensor(
                    out=t1[z][:], in0=cu[:, ct, 1:BT + 1], scalar=cw[:, ct * 3 + 1:ct * 3 + 2], in1=t1[z][:], op0=ALU.mult, op1=ALU.add),
                    reads=[("cu", ct), ("cuh", ct), "cw", ("t1", z)], writes=[("t1", z)])
                P.op("vector", lambda e, z=z, ct=ct: e.scalar_tensor_tensor(
                    out=t1[z][:], in0=cu[:, ct, 0:BT], scalar=cw[:, ct * 3:ct * 3 + 1], in1=t1[z][:], op0=ALU.mult, op1=ALU.add),
                    reads=[("cu", ct), ("cuh", ct), "cw", ("t1", z)], writes=[("t1", z)])
                P.op("vector", lambda e, z=z, jb=jb, ct=ct, pb=pb: e.scalar_tensor_tensor(
                    out=ycT[bb][:, ct, :], in0=pb, scalar=bcol[:, jb:jb + 1], in1=t1[z][:], op0=ALU.add, op1=ALU.mult),
                    reads=kb + ["bcol", ("t1", z)], writes=[("ycT", bb, ct)])
                P.op("vector", lambda e, ct=ct: e.tensor_copy(out=cu[:, ct, 0:2], in_=cu[:, ct, BT:BT + 2]),
                     reads=[("cu", ct)], writes=[("cuh", ct)])
            P.op("sync", lambda e, tok0=tok0: e.dma_start(out=ycT_v[:, :, tok0:tok0 + BT], in_=ycT[bb][:]),
                 reads=[("ycT", bb, ct) for ct in range(4)], writes=[("ycT_d", b)], dma=True)

        def secD(b):
            bb = b % 2
            tok0 = b * BT
            nxt = b + 1 < NB
            units = []
            for h in range(NH):
                for m in range(8):
                    kt = 4 * b - 4 + m
                    if kt < 0:
                        continue
                    units.append((h, m, kt, max(m - 4, 0), min(m, 3)))
            SPS = [(A0[:], "A0"), (A1[:], "A1"), (B0[:, 0:512], ("B0", 0))]

            def QKEXP(u):
                h, m, kt, tlo, thi = units[u]
                hp, r0 = h // 2, (h % 2) * 64
                nq = thi - tlo + 1
                sp, sk = SPS[u % 3]
                pz = u % 4
                sl = kt % KR
                P.op("tensor", lambda e, hp=hp, r0=r0, sl=sl, sp=sp, tlo=tlo, thi=thi: e.matmul(
                    sp[:, 0:(thi - tlo + 1) * 128], lhsT=kring[r0:r0 + 64, hp, sl * 128:(sl + 1) * 128],
                    rhs=qT[bb][r0:r0 + 64, hp, tlo * 128:(thi + 1) * 128], start=True, stop=True),
                    reads=[("k", hp, sl), ("qT", bb, hp)], writes=[sk])
                P.op("scalar", lambda e, pz=pz, sp=sp, nq=nq: e.activation(
                    out=pt[pz][:, 0:nq, :], in_=sp[:, 0:nq * 128].rearrange("p (j q) -> p j q", q=128), func=AF.Exp, scale=DH ** -0.5),
                    reads=[sk], writes=[("pt", pz)])
                rlo = 4 - m + tlo
                P.op("vector", lambda e, pz=pz, nq=nq, h=h, rlo=rlo: e.tensor_tensor(
                    out=pt[pz][:, 0:nq, :], in0=pt[pz][:, 0:nq, :], in1=expB[:, h, rlo:rlo + nq, :], op=ALU.mult),
                    reads=[("pt", pz), ("expB", h)], writes=[("pt", pz)])

            def PV(u):
                h, m, kt, tlo, thi = units[u]
                pz = u % 4
                sl = kt % KR
                hb2 = h % 2
                first = (u == 0) or units[u - 1][0] != h
                last_u = (u + 1 == len(units)) or units[u + 1][0] != h
                if first:
                    P.op("tensor", lambda e, hb2=hb2: e.matmul(
                        B1[:, hb2 * 512:hb2 * 512 + 260], lhsT=zt[:, 0:128], rhs=zt[:, 0:260], start=True, stop=False),
                        reads=["zt"], writes=[("B1h", hb2)])
                for tl in range(tlo, thi + 1):
                    c0 = hb2 * 512 + tl * 65
                    P.op("tensor", lambda e, h=h, tl=tl, tlo=tlo, sl=sl, pz=pz, c0=c0, fin=(last_u and tl == thi): e.matmul(
                        B1[:, c0:c0 + 65], lhsT=pt[pz][:, tl - tlo, :], rhs=vring[:, sl, h, :],
                        start=False, stop=fin),
                        reads=[("pt", pz), ("v", sl)], writes=[("B1h", hb2)])

            def FINH(h):
                hb2 = h % 2
                Bv = B1[:, hb2 * 512:hb2 * 512 + 260].rearrange("p (t d) -> p t d", d=65)
                P.op("vector", lambda e, hb2=hb2, Bv=Bv: e.reciprocal(out=rden[hb2][:], in_=Bv[:, :, 64]),
                     reads=[("B1h", hb2)], writes=[("rden", hb2)])
                P.op("vector", lambda e, hb2=hb2, Bv=Bv, h=h: e.tensor_tensor(
                    out=ya[:, :, h * 64:(h + 1) * 64], in0=Bv[:, :, 0:64], in1=bc_last(rden[hb2][:], 64), op=ALU.mult),
                    reads=[("B1h", hb2), ("rden", hb2)], writes=[("ya", h)])

            if nxt:
                A_norm(b + 1, 0)
            QKEXP(0)
            if len(units) > 1:
                QKEXP(1)
            for u in range(len(units)):
                if u + 2 < len(units):
                    QKEXP(u + 2)
                PV(u)
                h = units[u][0]
                if u + 1 == len(units) or units[u + 1][0] != h:
                    FINH(h)
                    if nxt and h % 2 == 1:
                        tl = h // 2
                        A_tr(b + 1, tl)
                        if tl + 1 < 4:
                            A_norm(b + 1, tl + 1)
            for tl in range(4):
                transpose_tile(ya[:, tl, :], 4, yaT[bb][:, :, tl * 128:(tl + 1) * 128], [("ya", h) for h in range(NH)], ("yaT", bb, tl))
            P.op("sync", lambda e, tok0=tok0: e.dma_start(out=yaT_v[:, :, tok0:tok0 + BT], in_=yaT[bb][:]),
                 reads=[("yaT", bb, tl) for tl in range(4)], writes=[("yaT_d", b)], dma=True)

        for tl in range(4):
            A_norm(0, tl)
            A_tr(0, tl)
        for b in range(NB):
            secC(b)
            zero_fill(b)
            secD(b)
        P.barrier()
    if 1 in phases:
        phase1()
    sb.reset(base_mark)

    def phase2():
        Wg = sb.alloc("Wg", [128, 8, 2048], BF16)
        wpa = sb.alloc("wpa", [128, 4, D], BF16)
        wpc = sb.alloc("wpc", [128, 4, D], BF16)
        wo = sb.alloc("wo", [128, 8, D], BF16)
        wrg = sb.alloc("wrg", [128, 8, 36], BF16)
        brg = sb.alloc("brg", [128, 36], F32)
        gffn = sb.alloc("gffn", [128, D], F32)
        nTb = [sb.alloc("nTb%d" % i, [128, 8, BT], BF16) for i in range(2)]
        yaT = [sb.alloc("yaT%d" % i, [128, 4, BT], BF16) for i in range(2)]
        ycT = [sb.alloc("ycT%d" % i, [128, 4, BT], BF16) for i in range(2)]
        xt = [sb.alloc("xt%d" % i, [128, D], F32) for i in range(4)]
        tA = [sb.alloc("tA%d" % i, [128, BT], F32) for i in range(2)]
        tC = [sb.alloc("tC%d" % i, [128, BT], F32) for i in range(2)]
        mA = [sb.alloc("mA%d" % i, [128, BT], F32) for i in range(2)]
        mC = [sb.alloc("mC%d" % i, [128, BT], F32) for i in range(2)]
        mT = [sb.alloc("mT%d" % i, [128, 8, BT], BF16) for i in range(2)]
        ht = [sb.alloc("ht%d" % i, [128, D], F32) for i in range(3)]
        n2 = [sb.alloc("n2%d" % i, [128, 4, D], BF16) for i in range(2)]
        n2T = [sb.alloc("n2T%d" % i, [128, 8, 128], BF16) for i in range(2)]
        lg = sb.alloc("lg", [128, 4, 36], F32)
        gmax = sb.alloc("gmax", [128, 4], F32)
        gmask = sb.alloc("gmask", [128, 4, 4], F32)
        gex = sb.alloc("gex", [128, 4, 4], F32)
        gse = sb.alloc("gse", [128, 4], F32)
        pen = sb.alloc("pen", [128, 4, 4], F32)
        elm = sb.alloc("elm", [128, 4, 32], F32)
        elm2 = sb.alloc("elm2", [128, 4, 32], F32)
        m1 = sb.alloc("m1", [128, 4], F32)
        m2 = sb.alloc("m2", [128, 4], F32)
        mk1 = sb.alloc("mk1", [128, 4, 32], F32)
        mk2 = sb.alloc("mk2", [128, 4, 32], F32)
        Mb = sb.alloc("Mb", [128, 4, 32], BF16)
        dd = sb.alloc("dd", [128, 4], F32)
        ee = sb.alloc("ee", [128, 4], F32)
        rr = sb.alloc("rr", [128, 4], F32)
        wA = sb.alloc("wA", [128, 4], F32)
        wB = sb.alloc("wB", [128, 4], F32)
        pos = sb.alloc("pos", [128, 4, 32], F32)
        okm = sb.alloc("okm", [128, 4, 32], F32)
        slot = sb.alloc("slot", [128, 4, 32], F32)
        tmp = sb.alloc("tmp", [128, 4, 32], F32)
        dsel = sb.alloc("dsel", [128, 4, 2], F32)
        oksel = sb.alloc("oksel", [128, 4, 2], F32)

        win_v = win_d.ap().rearrange("(c p) n -> p c n", p=128)
        def wg_load(q4):
            for g0 in (0, 1024):
                c0_ = g0 + q4 * 256
                P.op("gpsimd", lambda e, c0_=c0_: e.dma_start(out=Wg[:, :, c0_:c0_ + 256], in_=win_v[:, :, 3072 + c0_:3072 + c0_ + 256]),
                     writes=[("Wg", c0_)], dma=True)

        wg_load(0)
        P.op("gpsimd", lambda e: e.dma_start(out=wpa[:], in_=wpa_d.ap().rearrange("(c p) n -> p c n", p=128)), writes=["wpa"], dma=True)
        P.op("gpsimd", lambda e: e.dma_start(out=wpc[:], in_=wpc_d.ap().rearrange("(c p) n -> p c n", p=128)), writes=["wpc"], dma=True)
        for q4 in range(1, 4):
            wg_load(q4)
        wo_v = wo_d.ap().rearrange("(c p) n -> p c n", p=128)
        for c in range(0, 8, 4):
            P.op("gpsimd", lambda e, c=c: e.dma_start(out=wo[:, c:c + 4, :], in_=wo_v[:, c:c + 4, :]), writes=[("wo", c)], dma=True)
        P.op("gpsimd", lambda e: e.dma_start(out=wrg[:], in_=wrg_d.ap().rearrange("(c p) n -> p c n", p=128)), writes=["wrg"], dma=True)
        ld("sync", brg[:], brg_d.ap().partition_broadcast(128), "brg")
        ld("sync", gffn[:], gffn_d.ap().partition_broadcast(128), "gffn")

        nT_v = nT_d.ap().rearrange("(c p) t -> p c t", p=128)
        yaT_v = yaT_d.ap().rearrange("(c p) t -> p c t", p=128)
        ycT_v = ycT_d.ap().rearrange("(c p) t -> p c t", p=128)
        wokeys = [("wo", 0), ("wo", 4)]
        xctr = [0]
        hctr = [0]

        def loads2(b):
            bb = b % 2
            tok0 = b * BT
            P.op("sync", lambda e, bb=bb, tok0=tok0: e.dma_start(out=nTb[bb][:], in_=nT_v[:, :, tok0:tok0 + BT]), writes=[("nTb", bb)], dma=True)
            P.op("sync", lambda e, bb=bb, tok0=tok0: e.dma_start(out=yaT[bb][:], in_=yaT_v[:, :, tok0:tok0 + BT]), writes=[("yaT", bb)], dma=True)
            P.op("sync", lambda e, bb=bb, tok0=tok0: e.dma_start(out=ycT[bb][:], in_=ycT_v[:, :, tok0:tok0 + BT]), writes=[("ycT", bb)], dma=True)

        def xload(t):
            P.op("sync", lambda e, t=t: e.dma_start(out=xt[t % 4][:], in_=x_d.ap()[t * 128:(t + 1) * 128, :]), writes=[("xt", t % 4)], dma=True)

        loads2(0)
        for t_ in range(3):
            xload(t_)
        def gates2(b):
            bb = b % 2
            tok0 = b * BT
            if b + 1 < NB:
                loads2(b + 1)
            for j in range(8):
                z = j % 2
                for (dst, dkey, col0) in ((A0, "A0", 0), (A1, "A1", 1024)):
                    for c in range(8):
                        P.op("tensor", lambda e, c=c, dst=dst, col0=col0, j=j, bb=bb: e.matmul(
                            dst[:], lhsT=Wg[:, c, col0 + j * 128:col0 + (j + 1) * 128], rhs=nTb[bb][:, c, :], start=(c == 0), stop=(c == 7)),
                            reads=[("Wg", col0 + (j // 2) * 256), ("nTb", bb)], writes=[dkey])
                for (dst, dkey, wsrc, wkey, asrc, akey) in ((A2, "A2", wpa, "wpa", yaT, "yaT"), (B0, ("B0", 0), wpc, "wpc", ycT, "ycT")):
                    for c in range(4):
                        P.op("tensor", lambda e, c=c, dst=dst, wsrc=wsrc, asrc=asrc, j=j, bb=bb: e.matmul(
                            dst[:, 0:512], lhsT=wsrc[:, c, j * 128:(j + 1) * 128], rhs=asrc[bb][:, c, :], start=(c == 0), stop=(c == 3)),
                            reads=[wkey, (akey, bb)], writes=[dkey])
                P.op("scalar", lambda e, z=z, j=j: e.activation(out=tA[z][:], in_=A0[:], func=AF.Tanh, scale=0.5, bias=hbcol[:, 24 + j:25 + j]),
                     reads=["A0", "hbcol"], writes=[("tA", z)])
                P.op("scalar", lambda e, z=z, j=j: e.activation(out=tC[z][:], in_=A1[:], func=AF.Tanh, scale=0.5, bias=hbcol[:, 32 + j:33 + j]),
                     reads=["A1", "hbcol"], writes=[("tC", z)])
                P.op("vector", lambda e, z=z: e.scalar_tensor_tensor(out=mA[z][:], in0=tA[z][:], scalar=1.0, in1=A2[:], op0=ALU.add, op1=ALU.mult),
                     reads=[("tA", z), "A2"], writes=[("mA", z)])
                P.op("vector", lambda e, z=z: e.scalar_tensor_tensor(out=mC[z][:], in0=tC[z][:], scalar=1.0, in1=B0[:, 0:512], op0=ALU.add, op1=ALU.mult),
                     reads=[("tC", z), ("B0", 0)], writes=[("mC", z)])
                P.op("gpsimd", lambda e, z=z, j=j, bb=bb: e.tensor_tensor(out=mT[bb][:, j, :], in0=mA[z][:], in1=mC[z][:], op=ALU.add),
                     reads=[("mA", z), ("mC", z)], writes=[("mT", bb, j)])

        def hsec2(b):
            bb = b % 2
            tok0 = b * BT
            mkeys = [("mT", bb, j) for j in range(8)]
            def hmm(tl):
                t = 4 * b + tl
                xs_ = t % 4
                hs_ = t % 3
                if t + 3 < NT:
                    xload(t + 3)
                for half in range(2):
                    for j in range(8):
                        P.op("tensor", lambda e, j=j, half=half, tl=tl, bb=bb: e.matmul(
                            B1[:, half * 512:(half + 1) * 512], lhsT=mT[bb][:, j, tl * 128:(tl + 1) * 128], rhs=wo[:, j, half * 512:(half + 1) * 512],
                            start=(j == 0), stop=(j == 7)),
                            reads=mkeys + wokeys, writes=[("B1", half)])
                    P.op("vector", lambda e, half=half, xs_=xs_, hs_=hs_: e.scalar_tensor_tensor(
                        out=ht[hs_][:, half * 512:(half + 1) * 512], in0=B1[:, half * 512:(half + 1) * 512], scalar=0.5,
                        in1=xt[xs_][:, half * 512:(half + 1) * 512], op0=ALU.mult, op1=ALU.add),
                        reads=[("B1", half), ("xt", xs_)] + ([("ht", hs_)] if half == 1 else []), writes=[("ht", hs_)])
                P.op("sync", lambda e, t=t, hs_=hs_: e.dma_start(out=h_d.ap()[t * 128:(t + 1) * 128, :], in_=ht[hs_][:]),
                     reads=[("ht", hs_)], writes=[("h_d", t)], dma=True)

            def hnorm(tl):
                t = 4 * b + tl
                hs_ = t % 3
                rmsnorm_tile(ht[hs_][:], gffn[:], n2[bb][:, tl, :], ("ht", hs_), "gffn", ("n2", bb, tl))

            def htr(tl):
                t = 4 * b + tl
                z2 = t % 2
                transpose_tile(n2[bb][:, tl, :], 8, n2T[z2][:], ("n2", bb, tl), ("n2T", z2))
                for c in range(8):
                    P.op("tensor", lambda e, c=c, tl=tl, z2=z2: e.matmul(
                        B0[:, 512 + tl * 36:512 + (tl + 1) * 36], lhsT=n2T[z2][:, c, :], rhs=wrg[:, c, :], start=(c == 0), stop=(c == 7)),
                        reads=[("n2T", z2), "wrg"], writes=[("lgp", tl)])

            hmm(0)
            hnorm(0)
            for tl in range(4):
                if tl + 1 < 4:
                    hmm(tl + 1)
                htr(tl)
                if tl + 1 < 4:
                    hnorm(tl + 1)

        def rout2(b):
            bb = b % 2
            tok0 = b * BT
            lgp = B0[:, 512:512 + 144].rearrange("p (t n) -> p t n", t=4)
            R = []

            def V(fn, reads, writes):
                P.op("vector", fn, reads=reads, writes=writes)

            V(lambda e: e.tensor_tensor(out=lg[:], in0=lgp, in1=bc_mid(brg[:], 4), op=ALU.add),
              [("lgp", tl) for tl in range(4)] + ["brg"], ["lg"])
            V(lambda e: e.tensor_reduce(out=gmax[:], in_=lg[:, :, 0:4], axis=AX.X, op=ALU.max), ["lg"], ["gmax"])
            V(lambda e: e.tensor_tensor(out=gmask[:], in0=lg[:, :, 0:4], in1=bc_last(gmax[:], 4), op=ALU.is_equal), ["lg", "gmax"], ["gmask"])
            V(lambda e: e.tensor_tensor(out=gex[:], in0=lg[:, :, 0:4], in1=bc_last(gmax[:], 4), op=ALU.subtract), ["lg", "gmax"], ["gex"])
            P.op("scalar", lambda e: e.activation(out=gex[:], in_=gex[:], func=AF.Exp), reads=["gex"], writes=["gex"])
            V(lambda e: e.tensor_reduce(out=gse[:], in_=gex[:], axis=AX.X, op=ALU.add), ["gex"], ["gse"])
            V(lambda e: e.reciprocal(out=gse[:], in_=gse[:]), ["gse"], ["gse"])
            V(lambda e: e.tensor_scalar(out=pen[:], in0=gmask[:], scalar1=1.0, scalar2=1e30, op0=ALU.subtract, op1=ALU.mult), ["gmask"], ["pen"])
            V(lambda e: e.tensor_tensor(out=elm[:].rearrange("p t (g k) -> p t g k", g=4),
                                        in0=lg[:, :, 4:36].rearrange("p t (g k) -> p t g k", g=4),
                                        in1=bc_last(pen[:], 8), op=ALU.add), ["lg", "pen"], ["elm"])
            V(lambda e: e.tensor_reduce(out=m1[:], in_=elm[:], axis=AX.X, op=ALU.max), ["elm"], ["m1"])
            V(lambda e: e.tensor_tensor(out=mk1[:], in0=elm[:], in1=bc_last(m1[:], 32), op=ALU.is_equal), ["elm", "m1"], ["mk1"])
            V(lambda e: e.scalar_tensor_tensor(out=elm2[:], in0=mk1[:], scalar=-1e30, in1=elm[:], op0=ALU.mult, op1=ALU.add), ["mk1", "elm"], ["elm2"])
            V(lambda e: e.tensor_reduce(out=m2[:], in_=elm2[:], axis=AX.X, op=ALU.max), ["elm2"], ["m2"])
            V(lambda e: e.tensor_tensor(out=mk2[:], in0=elm2[:], in1=bc_last(m2[:], 32), op=ALU.is_equal), ["elm2", "m2"], ["mk2"])
            V(lambda e: e.tensor_tensor(out=dd[:], in0=m2[:], in1=m1[:], op=ALU.subtract), ["m1", "m2"], ["dd"])
            P.op("scalar", lambda e: e.activation(out=ee[:], in_=dd[:], func=AF.Exp), reads=["dd"], writes=["ee"])
            V(lambda e: e.tensor_scalar(out=rr[:], in0=ee[:], scalar1=1.0, scalar2=None, op0=ALU.add), ["ee"], ["rr"])
            V(lambda e: e.reciprocal(out=rr[:], in_=rr[:]), ["rr"], ["rr"])
            V(lambda e: e.tensor_tensor(out=wA[:], in0=gse[:], in1=rr[:], op=ALU.mult), ["gse", "rr"], ["wA"])
            V(lambda e: e.tensor_tensor(out=wB[:], in0=wA[:], in1=ee[:], op=ALU.mult), ["wA", "ee"], ["wB"])
            V(lambda e: e.tensor_tensor(out=Mb[:], in0=mk1[:], in1=mk2[:], op=ALU.add), ["mk1", "mk2"], ["Mb"])
            for tl in range(4):
                P.op("tensor", lambda e, tl=tl: e.matmul(A0[:, tl * 32:(tl + 1) * 32], lhsT=utri[:], rhs=Mb[:, tl, :], start=True, stop=(tl == 0)),
                     reads=["utri", "Mb"], writes=["A0"])
                for t2 in range(tl):
                    P.op("tensor", lambda e, tl=tl, t2=t2: e.matmul(A0[:, tl * 32:(tl + 1) * 32], lhsT=ones[:], rhs=Mb[:, t2, :], start=False, stop=(t2 == tl - 1)),
                         reads=["ones", "Mb"], writes=["A0"])
            for tl in range(4):
                P.op("tensor", lambda e, tl=tl: e.matmul(A1[:, 0:32], lhsT=ones[:], rhs=Mb[:, tl, :], start=(tl == 0), stop=(tl == 3)),
                     reads=["ones", "Mb"], writes=["A1"])
            V(lambda e: e.tensor_tensor(out=pos[:], in0=A0[:, 0:128].rearrange("p (t n) -> p t n", t=4), in1=bc_mid(cnt[:], 4), op=ALU.add),
              ["A0", "cnt"], ["pos"])
            V(lambda e: e.tensor_tensor(out=cnt[:], in0=cnt[:], in1=A1[:, 0:32], op=ALU.add), ["A1", "cnt", "pos"], ["cnt"])
            V(lambda e: e.tensor_scalar(out=okm[:], in0=pos[:], scalar1=float(CAP), scalar2=None, op0=ALU.is_lt), ["pos"], ["okm"])
            V(lambda e: e.tensor_tensor(out=slot[:], in0=pos[:], in1=bc_mid(ecap[:], 4), op=ALU.add), ["pos", "ecap"], ["slot"])
            V(lambda e: e.tensor_scalar(out=tmp[:], in0=okm[:], scalar1=-1.0e6, scalar2=1.0e6, op0=ALU.mult, op1=ALU.add), ["okm"], ["tmp"])
            V(lambda e: e.tensor_tensor(out=slot[:], in0=slot[:], in1=tmp[:], op=ALU.add), ["slot", "tmp"], ["slot"])
            V(lambda e: e.tensor_scalar(out=slot[:], in0=slot[:], scalar1=float(NSLOT), scalar2=None, op0=ALU.min), ["slot"], ["slot"])
            for k, mk in ((0, mk1), (1, mk2)):
                V(lambda e, mk=mk: e.tensor_tensor(out=tmp[:], in0=mk[:], in1=slot[:], op=ALU.mult), ["mk1", "mk2", "slot"], ["tmp"])
                V(lambda e, k=k: e.tensor_reduce(out=dsel[:, :, k], in_=tmp[:], axis=AX.X, op=ALU.add), ["tmp"], [("dsel", k)])
                V(lambda e, mk=mk: e.tensor_tensor(out=tmp[:], in0=mk[:], in1=okm[:], op=ALU.mult), ["mk1", "mk2", "okm", ("dsel", k)], ["tmp"])
                V(lambda e, k=k: e.tensor_reduce(out=oksel[:, :, k], in_=tmp[:], axis=AX.X, op=ALU.add), ["tmp"], [("oksel", k)])
            tb = 4 * b
            V(lambda e, tb=tb: e.tensor_copy(out=dtab[:, tb:tb + 4, :], in_=dsel[:]), [("dsel", 0), ("dsel", 1)], [("dtab", b)])
            V(lambda e, tb=tb: e.tensor_tensor(out=wtab[:, tb:tb + 4, 0], in0=wA[:], in1=oksel[:, :, 0], op=ALU.mult), ["wA", ("oksel", 0)], [("wtab", b, 0)])
            V(lambda e, tb=tb: e.tensor_tensor(out=wtab[:, tb:tb + 4, 1], in0=wB[:], in1=oksel[:, :, 1], op=ALU.mult), ["wB", ("oksel", 1)], [("wtab", b, 1)])
            for tl in range(4):
                t = 4 * b + tl
                for k in range(2):
                    P.op("gpsimd", lambda e, t=t, k=k, tl=tl, bb=bb: e.indirect_dma_start(
                        out=xs_d[:, :], out_offset=bass.IndirectOffsetOnAxis(ap=dtab[:, t, k:k + 1], axis=0),
                        in_=n2[bb][:, tl, :], in_offset=None),
                        reads=[("n2", bb, tl), ("dtab", b)], writes=[("xs_d", t, k)], dma=True)

        gates2(0)
        for b in range(NB):
            hsec2(b)
            if b + 1 < NB:
                gates2(b + 1)
            rout2(b)
        if debug:
            P.op("sync", lambda e: e.dma_start(out=dtab_o.ap(), in_=dtab[:].rearrange("p t k -> p (t k)")),
                 reads=[("dtab", b) for b in range(NB)], dma=True, is_out=True)
            P.op("sync", lambda e: e.dma_start(out=wtab_o.ap(), in_=wtab[:].rearrange("p t k -> p (t k)")),
                 reads=[("wtab", b, k) for b in range(NB) for k in range(2)], dma=True, is_out=True)
        P.barrier()
    if 2 in phases:
        phase2()
    sb.reset(base_mark)

    TOP = SBAlloc.HI - 20 * 1024
    p4w = {"wpg": nc.alloc_sbuf_tensor_at("wpg_top", [128, 8, D], BF16, offset=TOP),
           "wpp": nc.alloc_sbuf_tensor_at("wpp_top", [128, 2, D], BF16, offset=TOP + 16 * 1024)}

    def p4_weight_loads():
        wpg_v = wpg_d.ap().rearrange("(c p) n -> p c n", p=128)
        for c in range(0, 8, 4):
            P.op("gpsimd", lambda e, c=c: e.dma_start(out=p4w["wpg"][:, c:c + 4, :], in_=wpg_v[:, c:c + 4, :]), writes=[("wpg", c)], dma=True)
        P.op("gpsimd", lambda e: e.dma_start(out=p4w["wpp"][:], in_=wpp_d.ap().rearrange("(c p) n -> p c n", p=128)), writes=["wpp"], dma=True)

    def phase3():
        NWB = 3
        w1b = [sb.alloc("w1b%d" % i, [128, 8, 512], BF16) for i in range(NWB)]
        w3b = [sb.alloc("w3b%d" % i, [128, 8, 512], BF16) for i in range(NWB)]
        w2b = [sb.alloc("w2b%d" % i, [128, 4, D], BF16) for i in range(NWB)]
        xr = [sb.alloc("xr%d" % i, [128, 3, D], BF16) for i in range(3)]
        xsT = [sb.alloc("xsT%d" % i, [128, 8, CAP], BF16) for i in range(2)]
        s1 = [sb.alloc("s1%d" % i, [128, CAP], F32) for i in range(2)]
        hdn = [sb.alloc("hdn%d" % i, [128, 4, CAP], BF16) for i in range(2)]
        yb = [sb.alloc("yb%d" % i, [128, D], BF16) for i in range(3)]
        HACC = [(A0, "A0", A1, "A1"), (A2, "A2", B0, ("B0", 0))]
        yctr = [0]
        P.op("gpsimd", lambda e: e.memset(yb[0][:], 0.0), writes=[("yb", 0, 0), ("yb", 0, 1)])
        P.op("sync", lambda e: e.dma_start(out=ys_d.ap()[NSLOT:NSLOT + 128, :], in_=yb[0][:]),
             reads=[("yb", 0, 0), ("yb", 0, 1)], writes=["ys_trash"], dma=True)
        def wload(ex, after=()):
            wb_ = ex % NWB
            if after:
                P.op("gpsimd", lambda e: e.memset(junk[:, 0:8], 0.0), reads=list(after), writes=["junk_g"])
            P.op("gpsimd", lambda e, ex=ex, wb_=wb_: e.dma_start(out=w1b[wb_][:], in_=w1_d.ap()[ex].rearrange("(p c) f -> p c f", c=8)),
                 writes=[("w1b", wb_)], dma=True)
            P.op("gpsimd", lambda e, ex=ex, wb_=wb_: e.dma_start(out=w3b[wb_][:], in_=w3_d.ap()[ex].rearrange("(p c) f -> p c f", c=8)),
                 writes=[("w3b", wb_)], dma=True)
            P.op("gpsimd", lambda e, ex=ex, wb_=wb_: e.dma_start(out=w2b[wb_][:], in_=w2_d.ap()[ex].rearrange("(c p) f -> p c f", p=128)),
                 writes=[("w2b", wb_)], dma=True)

        def xsload(ex):
            e3 = ex % 3
            P.op("sync", lambda e, ex=ex, e3=e3: e.dma_start(
                out=xr[e3][:], in_=xs_d.ap()[ex * CAP:(ex + 1) * CAP, :].rearrange("(r p) d -> p r d", p=128)),
                writes=[("xr", e3)], dma=True)

        def xsT_group(ex, r):
            eb = ex % 2
            e3 = ex % 3
            transpose_tile(xr[e3][:, r, :], 8, xsT[eb][:, :, r * 128:(r + 1) * 128], ("xr", e3), ("xsT", eb, r),
                           evac=("scalar" if r % 2 == 0 else "vector"), interleave=8)

        xsload(0)
        wload(0)
        xsload(1)
        wload(1, after=[("w1b", 0), ("w3b", 0), ("w2b", 0)])
        for r in range(3):
            xsT_group(0, r)
        for ex in range(NE):
            eb = ex % 2
            wb_ = ex % NWB
            if ex + 2 < NE:
                wload(ex + 2)
                xsload(ex + 2)
            if ex == 2:
                p4_weight_loads()
            xk = [("xsT", eb, r) for r in range(3)]
            for f in range(4):
                a1, k1, a3, k3 = HACC[f % 2]
                z = f % 2
                for (dst, dkey, wsrc, wkey) in ((a1, k1, w1b, "w1b"), (a3, k3, w3b, "w3b")):
                    for c in range(8):
                        P.op("tensor", lambda e, c=c, dst=dst, wsrc=wsrc, f=f, eb=eb, wb_=wb_: e.matmul(
                            dst[:, 0:CAP], lhsT=wsrc[wb_][:, c, f * 128:(f + 1) * 128], rhs=xsT[eb][:, c, :], start=(c == 0), stop=(c == 7)),
                            reads=[(wkey, wb_)] + xk, writes=[dkey])
                P.op("scalar", lambda e, a1=a1, z=z: e.activation(out=s1[z][:], in_=a1[:, 0:CAP], func=AF.Silu), reads=[k1], writes=[("s1", z)])
                P.op("vector", lambda e, a3=a3, z=z, f=f, eb=eb: e.tensor_tensor(out=hdn[eb][:, f, :], in0=s1[z][:], in1=a3[:, 0:CAP], op=ALU.mult),
                     reads=[("s1", z), k3], writes=[("hdn", eb, f)])
            hk_ = [("hdn", eb, f) for f in range(4)]
            for r in range(3):
                if ex + 1 < NE:
                    xsT_group(ex + 1, r)
                ys_ = yctr[0] % 3
                yctr[0] += 1
                for half in range(2):
                    for f in range(4):
                        P.op("tensor", lambda e, f=f, half=half, r=r, eb=eb, wb_=wb_: e.matmul(
                            B1[:, half * 512:(half + 1) * 512], lhsT=hdn[eb][:, f, r * 128:(r + 1) * 128], rhs=w2b[wb_][:, f, half * 512:(half + 1) * 512],
                            start=(f == 0), stop=(f == 3)),
                            reads=hk_ + [("w2b", wb_)], writes=[("B1", half)])
                    if half == 0:
                        P.op("scalar", lambda e, ys_=ys_: e.activation(out=yb[ys_][:, 0:512], in_=B1[:, 0:512], func=AF.Copy),
                             reads=[("B1", 0)], writes=[("yb", ys_, 0)])
                    else:
                        P.op("vector", lambda e, ys_=ys_: e.tensor_copy(out=yb[ys_][:, 512:1024], in_=B1[:, 512:1024]),
                             reads=[("B1", 1)], writes=[("yb", ys_, 1)])
                row0 = ex * CAP + r * 128
                P.op("sync", lambda e, row0=row0, ys_=ys_: e.dma_start(out=ys_d.ap()[row0:row0 + 128, :], in_=yb[ys_][:]),
                     reads=[("yb", ys_, 0), ("yb", ys_, 1)], writes=[("ys_d", ex, r)], dma=True)
        P.barrier()
    if 3 in phases:
        phase3()
    sb.reset(base_mark)

    def phase4():
        wpg, wpp = p4w["wpg"], p4w["wpp"]
        gple = sb.alloc("gple", [128, D], F32)
        bpg = sb.alloc("bpg", [128, D], F32)
        hb = [sb.alloc("hb%d" % i, [128, D], F32) for i in range(3)]
        y1 = [sb.alloc("y1%d" % i, [128, D], BF16) for i in range(3)]
        y2 = [sb.alloc("y2%d" % i, [128, D], BF16) for i in range(3)]
        pin = [sb.alloc("pin%d" % i, [128, 256], F32) for i in range(3)]
        pbf = [sb.alloc("pbf%d" % i, [128, 256], BF16) for i in range(2)]
        ppT = [sb.alloc("ppT%d" % i, [128, 2, 128], BF16) for i in range(2)]
        n3 = [sb.alloc("n3%d" % i, [128, D], BF16) for i in range(2)]
        n3T = [sb.alloc("n3T%d" % i, [128, 8, 128], BF16) for i in range(2)]
        gz = [sb.alloc("gz%d" % i, [128, D], F32) for i in range(2)]
        ob = [sb.alloc("ob%d" % i, [128, D], F32) for i in range(2)]
        ld("sync", gple[:], gple_d.ap().partition_broadcast(128), "gple")
        ld("sync", bpg[:], bpg_d.ap().partition_broadcast(128), "bpg")
        def loads4(t):
            h3 = t % 3
            P.op("sync", lambda e, t=t, h3=h3: e.dma_start(out=hb[h3][:], in_=h_d.ap()[t * 128:(t + 1) * 128, :]), writes=[("hb", h3)], dma=True)
            P.op("sync", lambda e, t=t, h3=h3: e.dma_start(out=pin[h3][:], in_=p_d.ap()[t * 128:(t + 1) * 128, :]), writes=[("pin", h3)], dma=True)
            for (yy, ykey, k) in ((y1, "y1", 0), (y2, "y2", 1)):
                P.op("gpsimd", lambda e, yy=yy, k=k, t=t, h3=h3: e.indirect_dma_start(
                    out=yy[h3][:, :], out_offset=None, in_=ys_d[:, :], in_offset=bass.IndirectOffsetOnAxis(ap=dtab[:, t, k:k + 1], axis=0)), reads=["dtab_all"], writes=[(ykey, h3)], dma=True)

        def S1a(t):
            h3 = t % 3
            z = t % 2
            P.op("vector", lambda e, h3=h3, t=t: e.scalar_tensor_tensor(out=hb[h3][:], in0=y1[h3][:], scalar=wtab[:, t, 0:1], in1=hb[h3][:],
                                                                       op0=ALU.mult, op1=ALU.add), reads=[("y1", h3), ("hb", h3)], writes=[("hb", h3)])
            P.op("vector", lambda e, h3=h3, t=t: e.scalar_tensor_tensor(out=hb[h3][:], in0=y2[h3][:], scalar=wtab[:, t, 1:2], in1=hb[h3][:],
                                                                       op0=ALU.mult, op1=ALU.add), reads=[("y2", h3), ("hb", h3)], writes=[("hb", h3)])
            rmsnorm_tile(hb[h3][:], gple[:], n3[z][:], ("hb", h3), "gple", ("n3", z))
            P.op("scalar", lambda e, z=z, h3=h3: e.activation(out=pbf[z][:], in_=pin[h3][:], func=AF.Copy), reads=[("pin", h3)], writes=[("pbf", z)])

        def S1b(t):
            z = t % 2
            transpose_tile(n3[z], 8, n3T[z][:], ("n3", z), ("n3T", z))
            transpose_tile(pbf[z], 2, ppT[z][:], ("pbf", z), ("ppT", z))

        GACC = [((A0, "A0"), (A2, "A2")), ((A1, "A1"), (B0, ("B0", 0)))]

        def S2mm(t, half):
            z = t % 2
            (ga, gk), (pa_, pk) = GACC[half]
            for c in range(8):
                P.op("tensor", lambda e, c=c, half=half, ga=ga, z=z: e.matmul(
                    ga[:], lhsT=n3T[z][:, c, :], rhs=wpg[:, c, half * 512:(half + 1) * 512], start=(c == 0), stop=(c == 7)),
                    reads=[("n3T", z), ("wpg", 0), ("wpg", 4)], writes=[gk])
            for c in range(2):
                P.op("tensor", lambda e, c=c, half=half, pa_=pa_, z=z: e.matmul(
                    pa_[:, 0:512], lhsT=ppT[z][:, c, :], rhs=wpp[:, c, half * 512:(half + 1) * 512], start=(c == 0), stop=(c == 1)),
                    reads=[("ppT", z), "wpp"], writes=[pk])

        def S2tail(t):
            z = t % 2
            h3 = t % 3
            hsl = [slice(0, 512), slice(512, 1024)]
            for half in range(2):
                (ga, gk), (pa_, pk) = GACC[half]
                hs = hsl[half]
                P.op("vector", lambda e, ga=ga, z=z, hs=hs: e.tensor_tensor(out=gz[z][:, hs], in0=ga[:], in1=bpg[:, hs], op=ALU.add),
                     reads=[gk, "bpg"], writes=[("gz", z, half)])
                P.op("scalar", lambda e, z=z, hs=hs: e.activation(out=gz[z][:, hs], in_=gz[z][:, hs], func=AF.Tanh, scale=0.5),
                     reads=[("gz", z, half)], writes=[("gz", z, half)])
            for half in range(2):
                (ga, gk), (pa_, pk) = GACC[half]
                hs = hsl[half]
                P.op("vector", lambda e, pa_=pa_, z=z, hs=hs: e.scalar_tensor_tensor(out=gz[z][:, hs], in0=gz[z][:, hs], scalar=1.0, in1=pa_[:, 0:512],
                                                                                    op0=ALU.add, op1=ALU.mult), reads=[("gz", z, half), pk], writes=[("gz", z, half)])
                P.op("vector", lambda e, z=z, hs=hs, h3=h3: e.scalar_tensor_tensor(out=ob[z][:, hs], in0=gz[z][:, hs], scalar=0.5, in1=hb[h3][:, hs],
                                                                                  op0=ALU.mult, op1=ALU.add), reads=[("gz", z, half), ("hb", h3)], writes=[("ob", z, half)])

        loads4(0)
        loads4(1)
        S1a(0)
        S1b(0)
        for t in range(NT):
            z = t % 2
            if t + 2 < NT:
                loads4(t + 2)
            if t + 1 < NT:
                S1a(t + 1)
            S2mm(t, 0)
            S2mm(t, 1)
            S2tail(t)
            if t + 1 < NT:
                S1b(t + 1)
            P.op("sync", lambda e, t=t, z=z: e.dma_start(out=out_d.ap()[t * 128:(t + 1) * 128, :], in_=ob[z][:]),
                 reads=[("ob", z, 0), ("ob", z, 1)], dma=True, is_out=True)
    if 4 in phases:
        phase4()
    P.emit()
    return nc, P


def _sel_tables():
    f = np.arange(128)[:, None, None]
    j = np.arange(8)[None, :, None]
    m = np.arange(128)[None, None, :]
    hit = ((m // 8) == (2 * j + f // 64)).astype(np.float32)
    selA = (hit / 64.0).reshape(128, 8 * 128).astype(ml_dtypes.bfloat16)
    selB = (hit.transpose(2, 1, 0) / 8.0).reshape(128, 8 * 128).astype(ml_dtypes.bfloat16)
    return np.ascontiguousarray(selA), np.ascontiguousarray(selB)


def _host_layout(inp):
    f = lambda a: np.ascontiguousarray(np.asarray(a, dtype=np.float32))
    bf = ml_dtypes.bfloat16
    b_in = f(inp["b_in"])[0]
    rel = f(inp["rel_bias"])[0]
    jj = np.arange(5)[::-1][:, None, None]
    kk = np.arange(128)[None, :, None]
    qq = np.arange(128)[None, None, :]
    dist = qq - kk + 128 * (4 - jj)
    idx = np.clip(dist, -63, 256) + 63
    cdiff = (qq // 64) - (kk // 64) + 2 * (4 - jj)
    mask = ((cdiff >= 0) & (cdiff <= 8)).astype(np.float32)
    rbT = rel[:, idx]
    rbT = np.ascontiguousarray(rbT.transpose(2, 0, 1, 3)).reshape(128, NH * 5 * 128)
    maskT = np.ascontiguousarray(mask.transpose(1, 0, 2)).reshape(128, 5 * 128)
    cwv = f(inp["conv_w"])[0]
    cw = np.ascontiguousarray(cwv.reshape(3, 4, 128).transpose(2, 1, 0)).reshape(128, 12)
    cb = np.ascontiguousarray(f(inp["conv_b"])[0].reshape(4, 128).T)
    gq = f(inp["g_q"])[0]
    gk = f(inp["g_k"])[0]
    shared = {
        "g_mix": f(inp["g_mix"]),
        "w_in": f(inp["w_in"])[0],
        "bcol": np.ascontiguousarray(b_in.reshape(40, 128).T),
        "gqk": np.ascontiguousarray(np.stack([np.tile(gq, 2), np.tile(gk, 2)], axis=1)),
        "bv": np.ascontiguousarray(b_in[1024:1536].reshape(1, 512)),
        "rbT": rbT, "maskT": maskT, "cw": cw, "cb": cb,
        "w_pa": f(inp["w_pa"])[0], "w_pc": f(inp["w_pc"])[0], "w_o": f(inp["w_o"])[0],
        "g_ffn": f(inp["g_ffn"]),
        "w_rg": np.ascontiguousarray(np.concatenate([f(inp["w_group"])[0], f(inp["w_router"])[0]], axis=1)),
        "b_rg": np.ascontiguousarray(np.concatenate([f(inp["b_group"])[0], f(inp["b_router"])[0]])[None, :]),
        "w1": f(inp["w1"])[0], "w3": f(inp["w3"])[0], "w2": f(inp["w2"])[0],
        "g_ple": f(inp["g_ple"]), "w_pg": f(inp["w_ple_gate"])[0], "b_pg": f(inp["b_ple_gate"]),
        "w_pp": f(inp["w_ple_proj"])[0],
        "ident": np.eye(128, dtype=np.float32).astype(bf),
        "utri": np.triu(np.ones((128, 128), np.float32), 1).astype(bf),
        "ones": np.ones((128, 128), np.float32).astype(bf),
        "bdiag": (np.kron(np.eye(2, dtype=np.float32), np.ones((64, 64), np.float32)) / 64.0).astype(bf),
        "ecap": np.ascontiguousarray(np.broadcast_to((np.arange(NE, dtype=np.float32) * CAP)[None, :], (128, NE))),
        "selA": _sel_tables()[0], "selB": _sel_tables()[1],
    }
    x = f(inp["x"])
    p = f(inp["p"])[0]
    maps = []
    for c in range(NCORES):
        m = dict(shared)
        m["x"] = x[c]
        m["p"] = p[c]
        maps.append(m)
    return maps


_CACHE = {}


def kernel(**inputs):
    if "nc" not in _CACHE:
        _CACHE["nc"] = build(debug=False)[0]
    nc = _CACHE["nc"]
    maps = _host_layout(inputs)
    res = run_bass_kernel_spmd(nc, maps, core_ids=list(range(NCORES)))
    out = np.stack([np.asarray(res.results[c]["out"], dtype=np.float32) for c in range(NCORES)], axis=0)
    return out
```

```python
import numpy as np
import ml_dtypes
import concourse.bass as bass
import concourse.mybir as mybir
from concourse.bass_utils import run_bass_kernel_spmd

F32 = mybir.dt.float32
BF16 = mybir.dt.bfloat16
I32 = mybir.dt.int32
ALU = mybir.AluOpType
AF = mybir.ActivationFunctionType
AX = mybir.AxisListType

NCORES = 8
S = 4096
D = 1024
NT = S // 128
BT = 512
NB = S // BT
NH = 8
DH = 64
NE = 32
CAP = 384
NSLOT = NE * CAP
KR = 12
EPS = 1e-6

ENGS = ("sync", "scalar", "vector", "gpsimd", "tensor")
NDMASEM = 24
SAME_ENG_WINDOW = 10 ** 9


class Op:
    __slots__ = ("idx", "eng", "fn", "reads", "writes", "dma", "deps", "sig",
                 "sem", "val", "clock", "epos", "barrier")


class Prog:
    def __init__(self, nc):
        self.nc = nc
        self.ops = []
        self.last_w = {}
        self.readers = {}
        self.out_ops = []
        self.last_barrier = None
        self.since_barrier = []

    def op(self, eng, fn, reads=(), writes=(), dma=False, is_out=False):
        o = Op()
        o.idx = len(self.ops)
        o.eng = eng
        o.fn = fn
        o.dma = dma
        o.barrier = False
        o.reads = tuple(reads)
        o.writes = tuple(writes)
        deps = set()
        for k in o.reads:
            w = self.last_w.get(k)
            if w is not None:
                deps.add(w)
        for k in o.writes:
            w = self.last_w.get(k)
            if w is not None:
                deps.add(w)
            for r in self.readers.get(k, ()):
                deps.add(r)
        for k in o.writes:
            self.last_w[k] = o.idx
            self.readers[k] = []
        for k in o.reads:
            if k not in o.writes:
                self.readers.setdefault(k, []).append(o.idx)
        if self.last_barrier is not None:
            deps.add(self.last_barrier)
        deps.discard(o.idx)
        o.deps = sorted(deps)
        o.sig = False
        self.ops.append(o)
        self.since_barrier.append(o.idx)
        if is_out:
            self.out_ops.append(o.idx)
        return o.idx

    def barrier(self):
        o = Op()
        o.idx = len(self.ops)
        o.eng = "sync"
        o.fn = "BARRIER"
        o.dma = False
        o.barrier = True
        o.reads = ()
        o.writes = ()
        last = {}
        deps = []
        for i in self.since_barrier:
            p = self.ops[i]
            if p.dma:
                deps.append(i)
            else:
                last[p.eng] = i
        deps.extend(last.values())
        if self.last_barrier is not None:
            deps.append(self.last_barrier)
        o.deps = sorted(set(deps))
        o.sig = True
        self.ops.append(o)
        self.last_barrier = o.idx
        self.since_barrier = []
        self.last_w = {}
        self.readers = {}

    def emit(self):
        nc = self.nc
        ops = self.ops
        epos = {e: 0 for e in ENGS}
        for o in ops:
            o.epos = epos[o.eng]
            epos[o.eng] += 1
        fin = Op()
        fin.idx = len(ops)
        fin.eng = "sync"
        fin.fn = None
        fin.dma = False
        fin.barrier = False
        fin.reads = ()
        fin.writes = ()
        fin.deps = list(self.out_ops)
        fin.sig = False
        fin.epos = epos["sync"]
        ops = ops + [fin]
        for o in ops:
            nd = []
            for d in o.deps:
                do = ops[d]
                if do.eng == o.eng and not do.dma and not o.barrier:
                    if o.eng == "tensor" and not o.dma:
                        continue
                    if o.dma:
                        pass
                    elif o.epos - do.epos > SAME_ENG_WINDOW:
                        continue
                nd.append(d)
            o.deps = nd
            for d in nd:
                ops[d].sig = True
        sems = {}
        dma_engs = set(o.eng for o in ops if o.dma)
        for e in ENGS:
            sems[("c", e)] = nc.alloc_semaphore("c_" + e)
            if e in dma_engs:
                for i in range(NDMASEM):
                    sems[("d", e, i)] = nc.alloc_semaphore("d_%s_%d" % (e, i))
        ccount = {e: 0 for e in ENGS}
        dcount = {e: 0 for e in ENGS}
        dma_prev = {}
        for o in ops:
            if o.dma:
                k = dcount[o.eng]
                dcount[o.eng] += 1
                slot = k % NDMASEM
                o.sem = ("d", o.eng, slot)
                o.val = 16 * (k // NDMASEM + 1)
                prev = dma_prev.get((o.eng, slot))
                if prev is not None and prev not in o.deps:
                    o.deps.append(prev)
                dma_prev[(o.eng, slot)] = o.idx
            elif o.sig:
                ccount[o.eng] += 1
                o.sem = ("c", o.eng)
                o.val = ccount[o.eng]
            else:
                o.sem = None
                o.val = 0
        known = {e: {} for e in ENGS}
        streams = {e: [] for e in ENGS}
        for o in ops:
            kn = known[o.eng]
            wm = {}
            for d in sorted(o.deps, reverse=True):
                do = ops[d]
                if kn.get(do.sem, 0) >= do.val:
                    continue
                if wm.get(do.sem, 0) < do.val:
                    wm[do.sem] = do.val
                for s, v in do.clock.items():
                    if kn.get(s, 0) < v:
                        kn[s] = v
            o.clock = dict(kn)
            if o.sem is not None:
                o.clock[o.sem] = o.val
            streams[o.eng].append((o, list(wm.items())))
        self.n_waits = sum(len(w) for st in streams.values() for _, w in st)
        self.counts = (dict(ccount), dict(dcount))

        def run_stream(eng_name):
            def body(eng):
                for o, waits in streams[eng_name]:
                    for s, v in waits:
                        eng.wait_ge(sems[s], v)
                    if o.fn is None:
                        continue
                    if o.barrier:
                        eng.sem_inc(sems[o.sem], 1)
                        continue
                    ins = o.fn(eng)
                    if o.sem is not None:
                        ins.then_inc(sems[o.sem], 16 if o.dma else 1)
            return body

        with nc.Block() as block:
            for e in ENGS:
                if streams[e]:
                    getattr(block, e)(run_stream(e))


class SBAlloc:
    LO = 16512
    HI = 229344

    def __init__(self, nc):
        self.nc = nc
        self.cur = self.LO
        self.n = 0

    def alloc(self, name, shape, dt):
        esz = {F32: 4, BF16: 2, I32: 4}[dt]
        nbytes = esz
        for s in shape[1:]:
            nbytes *= s
        off = (self.cur + 31) // 32 * 32
        assert off + nbytes <= self.HI, "SBUF overflow at %s: need %d have %d" % (name, nbytes, self.HI - off)
        self.n += 1
        t = self.nc.alloc_sbuf_tensor_at("%s_%d" % (name, self.n), list(shape), dt, offset=off)
        self.cur = off + nbytes
        self.off = getattr(self, "off", {})
        self.off[name] = off
        return t

    def alloc_alias(self, name, shape, dt, of):
        self.n += 1
        return self.nc.alloc_sbuf_tensor_at("%s_%d" % (name, self.n), list(shape), dt, offset=self.off[of])

    def mark(self):
        return self.cur

    def reset(self, m):
        self.cur = m


def bc_last(ap, n):
    shp = list(ap.shape)
    return ap.unsqueeze(len(shp)).broadcast_to(shp + [n])


def bc_mid(ap, n):
    shp = list(ap.shape)
    return ap.unsqueeze(1).broadcast_to([shp[0], n] + shp[1:])


def build(debug=False, phases=(1, 2, 3, 4)):
    nc = bass.Bass("TRN2", target_bir_lowering=False)
    P = Prog(nc)
    sb = SBAlloc(nc)

    def din(name, shape, dt=F32):
        return nc.dram_tensor(name, list(shape), dt, kind="ExternalInput")

    def dscr(name, shape, dt):
        return nc.dram_tensor(name, list(shape), dt, kind="ExternalOutput" if debug else "Internal")

    x_d = din("x", [S, D])
    p_d = din("p", [S, 256])
    gmix_d = din("g_mix", [1, D])
    win_d = din("w_in", [D, 5120])
    bcol_d = din("bcol", [128, 40])
    gqk_d = din("gqk", [128, 2])
    bv_d = din("bv", [1, 512])
    rbT_d = din("rbT", [128, NH * 5 * 128])
    maskT_d = din("maskT", [128, 5 * 128])
    cw_d = din("cw", [128, 12])
    cb_d = din("cb", [128, 4])
    wpa_d = din("w_pa", [512, D])
    wpc_d = din("w_pc", [512, D])
    wo_d = din("w_o", [D, D])
    gffn_d = din("g_ffn", [1, D])
    wrg_d = din("w_rg", [D, 36])
    brg_d = din("b_rg", [1, 36])
    w1_d = din("w1", [NE, D, 512])
    w3_d = din("w3", [NE, D, 512])
    w2_d = din("w2", [NE, 512, D])
    gple_d = din("g_ple", [1, D])
    wpg_d = din("w_pg", [D, D])
    bpg_d = din("b_pg", [1, D])
    wpp_d = din("w_pp", [256, D])
    ident_d = din("ident", [128, 128], BF16)
    utri_d = din("utri", [128, 128], BF16)
    ones_d = din("ones", [128, 128], BF16)
    bdiag_d = din("bdiag", [128, 128], BF16)
    ecap_d = din("ecap", [128, NE])
    selA_d = din("selA", [128, 8 * 128], BF16)
    selB_d = din("selB", [128, 8 * 128], BF16)
    out_d = nc.dram_tensor("out", [S, D], F32, kind="ExternalOutput")

    nT_d = dscr("nT_s", [D, S], BF16)
    yaT_d = dscr("yaT_s", [512, S], BF16)
    ycT_d = dscr("ycT_s", [512, S], BF16)
    h_d = dscr("h_s", [S, D], F32)
    xs_d = dscr("xs_s", [NSLOT + 128, D], BF16)
    ys_d = dscr("ys_s", [NSLOT + 128, D], BF16)
    if debug:
        dtab_o = nc.dram_tensor("dtab_o", [128, NT * 2], I32, kind="ExternalOutput")
        wtab_o = nc.dram_tensor("wtab_o", [128, NT * 2], F32, kind="ExternalOutput")

    pT = nc.alloc_psum_tensor("pT", [128, 8, 128], BF16)
    A0 = nc.alloc_psum_tensor("A0", [128, 512], F32)
    A1 = nc.alloc_psum_tensor("A1", [128, 512], F32)
    A2 = nc.alloc_psum_tensor("A2", [128, 512], F32)
    B0 = nc.alloc_psum_tensor("B0", [128, 1024], F32)
    B1 = nc.alloc_psum_tensor("B1", [128, 1024], F32)

    ident = sb.alloc("ident", [128, 128], BF16)
    utri = sb.alloc("utri", [128, 128], BF16)
    ones = sb.alloc("ones", [128, 128], BF16)
    bdiag = sb.alloc("bdiag", [128, 128], BF16)
    ecap = sb.alloc("ecap", [128, NE], F32)
    bcol = sb.alloc("bcol", [128, 40], F32)
    hbcol = sb.alloc("hbcol", [128, 40], F32)
    mhalf = sb.alloc("mhalf", [128, 8], F32)
    epsc = sb.alloc("epsc", [128, 8], F32)
    dtab = sb.alloc("dtab", [128, NT, 2], I32)
    wtab = sb.alloc("wtab", [128, NT, 2], F32)
    cnt = sb.alloc("cnt", [128, NE], F32)
    ss = sb.alloc("ss", [128, 8], F32)
    rs = sb.alloc("rs", [128, 8], F32)
    junk = sb.alloc("junk", [128, D], BF16)

    def ld(eng, dst, src, key):
        P.op(eng, lambda e: e.dma_start(out=dst, in_=src), writes=[key], dma=True)

    ld("sync", ident[:], ident_d.ap(), "ident")
    ld("sync", utri[:], utri_d.ap(), "utri")
    ld("sync", ones[:], ones_d.ap(), "ones")
    ld("sync", bdiag[:], bdiag_d.ap(), "bdiag")
    ld("sync", ecap[:], ecap_d.ap(), "ecap")
    ld("sync", bcol[:], bcol_d.ap(), "bcol")
    P.op("vector", lambda e: e.tensor_scalar(out=hbcol[:], in0=bcol[:], scalar1=0.5, scalar2=None, op0=ALU.mult),
         reads=["bcol"], writes=["hbcol"])
    P.op("gpsimd", lambda e: e.memset(mhalf[:], -0.5), writes=["mhalf"])
    P.op("gpsimd", lambda e: e.memset(epsc[:], EPS), writes=["epsc"])
    P.op("gpsimd", lambda e: e.memset(cnt[:], 0.0), writes=["cnt"])
    P.op("gpsimd", lambda e: e.memset(wtab[:], 0.0), writes=["wtab"])

    nrm_ctr = [0]

    def rmsnorm_tile(src, g_bc, dst_bf, src_key, g_key, dst_key):
        i = nrm_ctr[0] % 8
        nrm_ctr[0] += 1
        ssk, rsk = ("ss", i), ("rs", i)
        P.op("scalar", lambda e: e.activation(out=junk[:], in_=src, func=AF.Square, accum_out=ss[:, i:i + 1]),
             reads=[src_key], writes=["junk", ssk])
        P.op("vector", lambda e: e.tensor_scalar(out=rs[:, i:i + 1], in0=ss[:, i:i + 1], scalar1=1.0 / D, scalar2=EPS,
                                                  op0=ALU.mult, op1=ALU.add), reads=[ssk], writes=[rsk])
        P.op("gpsimd", lambda e: e.tensor_tensor(out=rs[:, i:i + 1], in0=rs[:, i:i + 1], in1=mhalf[:, 0:1], op=ALU.pow),
             reads=[rsk, "mhalf"], writes=[rsk])
        P.op("vector", lambda e: e.scalar_tensor_tensor(out=dst_bf, in0=src, scalar=rs[:, i:i + 1], in1=g_bc,
                                                         op0=ALU.mult, op1=ALU.mult),
             reads=[src_key, rsk, g_key], writes=[dst_key])

    def transpose_tile(src_bf, nchunk, dst, src_key, dst_key, evac="scalar", interleave=0):
        for c in range(nchunk):
            if interleave:
                src_c = src_bf.rearrange("t (p c) -> t c p", c=interleave)[:, c, :]
            else:
                src_c = src_bf[:, c * 128:(c + 1) * 128]
            P.op("tensor", lambda e, c=c, src_c=src_c: e.transpose(out=pT[:, c, :], in_=src_c, identity=ident[:]),
                 reads=(list(src_key) if isinstance(src_key, list) else [src_key]) + ["ident"], writes=[("pT", c)])
        if evac == "scalar":
            P.op("scalar", lambda e: e.activation(out=dst, in_=pT[:, 0:nchunk, :], func=AF.Copy),
                 reads=[("pT", c) for c in range(nchunk)], writes=[dst_key])
        else:
            P.op("vector", lambda e: e.tensor_copy(out=dst, in_=pT[:, 0:nchunk, :]),
                 reads=[("pT", c) for c in range(nchunk)], writes=[dst_key])

    _breg = {}

    def breg(e):
        if "r" not in _breg:
            _breg["r"] = e.to_reg(NSLOT - 1)
        return _breg["r"]

    base_mark = sb.mark()

    def phase1():
        Wa = sb.alloc("Wa", [128, 8, 3072], BF16)
        gmix = sb.alloc("gmix", [128, D], F32)
        gqk = sb.alloc("gqk", [128, 2], F32)
        bvb = sb.alloc("bvb", [128, 512], F32)
        cw = sb.alloc("cw", [128, 12], F32)
        cb = sb.alloc("cb", [128, 4], F32)
        expB = sb.alloc("expB", [128, NH, 5, 128], BF16)
        maskT = sb.alloc("maskT", [128, 5, 128], F32)
        kring = sb.alloc("kring", [128, 4, KR * 128], BF16)
        vring = sb.alloc("vring", [128, KR, NH, 65], BF16)
        xt = [sb.alloc("xt%d" % i, [128, D], F32) for i in range(2)]
        nb = [sb.alloc("nb%d" % i, [128, D], BF16) for i in range(2)]
        nTb = [sb.alloc("nTb%d" % i, [128, 8, BT], BF16) for i in range(2)]
        qT = [sb.alloc("qT%d" % i, [128, 4, BT], BF16) for i in range(2)]
        zq = [sb.alloc("zq%d" % i, [128, BT], F32) for i in range(8)]
        sq = [sb.alloc("sq%d" % i, [128, BT], BF16) for i in range(3)]
        rs = sb.alloc("rs_all", [128, BT], F32)
        r1b = sb.alloc("r1b", [128, BT], BF16)
        selA = sb.alloc("selA", [128, 8, 128], BF16)
        selB = sb.alloc("selB", [128, 8, 128], BF16)
        gw = sb.alloc("gw", [128, 1], F32)
        us = [sb.alloc("us%d" % i, [128, BT], F32) for i in range(2)]
        t1 = [sb.alloc("t1%d" % i, [128, BT], F32) for i in range(2)]
        cu = sb.alloc("cu", [128, 4, BT + 2], F32)
        ycT = [sb.alloc("ycT%d" % i, [128, 4, BT], BF16) for i in range(2)]
        yaT = [sb.alloc("yaT%d" % i, [128, 4, BT], BF16) for i in range(2)]
        pt = [sb.alloc("pt%d" % i, [128, 4, 128], BF16) for i in range(6)]
        rden = [sb.alloc("rden%d" % i, [128, 4], F32) for i in range(2)]
        ya = sb.alloc("ya", [128, 4, 512], BF16)

        win_v = win_d.ap().rearrange("(c p) n -> p c n", p=128)
        for (c0_, c1_) in ((0, 1024), (1024, 1536), (1536, 3072)):
            for c in range(0, 8, 2):
                P.op("gpsimd", lambda e, c=c, c0_=c0_, c1_=c1_: e.dma_start(out=Wa[:, c:c + 2, c0_:c1_], in_=win_v[:, c:c + 2, c0_:c1_]),
                     writes=[("Wa", c, c0_), ("Wa", c + 1, c0_)], dma=True)
        zt = sb.alloc("zt", [128, 2 * D], BF16)
        P.op("gpsimd", lambda e: e.memset(zt[:], 0.0), writes=["zt"])
        NR = (NSLOT + 128) // 128
        xs_z = xs_d.ap().rearrange("(p r) d -> p (r d)", p=128)
        zchunks = [(r0, min(r0 + 2, NR)) for r0 in range(0, NR, 2)]

        def zero_fill(k):
            for (r0, r1_) in zchunks[k::NB]:
                P.op("sync", lambda e, r0=r0, r1_=r1_: e.dma_start(out=xs_z[:, r0 * D:r1_ * D], in_=zt[:, 0:(r1_ - r0) * D]),
                     reads=["zt"], writes=[("xs_zero", r0)], dma=True)

        ld("sync", gmix[:], gmix_d.ap().partition_broadcast(128), "gmix")
        ld("sync", gqk[:], gqk_d.ap(), "gqk")
        ld("sync", selA[:], selA_d.ap().rearrange("p (j m) -> p j m", j=8), "selA")
        ld("sync", selB[:], selB_d.ap().rearrange("p (j m) -> p j m", j=8), "selB")
        ld("sync", bvb[:], bv_d.ap().partition_broadcast(128), "bvb")
        P.op("vector", lambda e: e.tensor_tensor(out=gw[:], in0=gqk[:, 0:1], in1=gqk[:, 1:2], op=ALU.mult), reads=["gqk"], writes=["gw"])
        ld("sync", cw[:], cw_d.ap(), "cw")
        ld("sync", cb[:], cb_d.ap(), "cb")
        ld("sync", maskT[:], maskT_d.ap().rearrange("p (j q) -> p j q", j=5), "maskT")
        rb_v = rbT_d.ap().rearrange("p (h n) -> p h n", h=NH)
        stg = [sb.alloc_alias("stg0", [128, 640], F32, "zq0"), sb.alloc_alias("stg1", [128, 640], F32, "zq2")]
        for h in range(NH):
            st = stg[h % 2]
            sk = [("zq", 2 * (h % 2)), ("zq", 2 * (h % 2) + 1)]
            P.op("sync", lambda e, h=h, st=st: e.dma_start(out=st[:], in_=rb_v[:, h, :]), writes=sk, dma=True)
            P.op("scalar", lambda e, st=st: e.activation(out=st[:], in_=st[:], func=AF.Exp), reads=sk, writes=sk)
            P.op("vector", lambda e, h=h, st=st: e.tensor_tensor(
                out=expB[:, h, :, :], in0=st[:].rearrange("p (j q) -> p j q", j=5), in1=maskT[:], op=ALU.mult),
                reads=sk + ["maskT"], writes=[("expB", h)])
        P.op("gpsimd", lambda e: e.memset(vring[:], 1.0), writes=[("v", s_) for s_ in range(KR)])
        P.op("gpsimd", lambda e: e.memset(cu[:], 0.0), writes=[("cu", ct) for ct in range(4)] + [("cuh", ct) for ct in range(4)])

        nT_v = nT_d.ap().rearrange("(c p) t -> p c t", p=128)
        yaT_v = yaT_d.ap().rearrange("(c p) t -> p c t", p=128)
        ycT_v = ycT_d.ap().rearrange("(c p) t -> p c t", p=128)
        acc_rot = [0]
        ACC = [(A0, "A0"), (A1, "A1")]

        def next_acc():
            a = ACC[acc_rot[0] % 2]
            acc_rot[0] += 1
            return a

        def A_norm(b, tl):
            t = 4 * b + tl
            s2 = t % 2
            P.op("sync", lambda e, t=t, s2=s2: e.dma_start(out=xt[s2][:], in_=x_d.ap()[t * 128:(t + 1) * 128, :]),
                 writes=[("xt", s2)], dma=True)
            rmsnorm_tile(xt[s2][:], gmix[:], nb[s2][:], ("xt", s2), "gmix", ("nb", s2))

        def A_tr(b, tl):
            t = 4 * b + tl
            s2 = t % 2
            bb = b % 2
            transpose_tile(nb[s2], 8, nTb[bb][:, :, tl * 128:(tl + 1) * 128], ("nb", s2), ("nTb", bb, tl))
            if tl == 3:
                P.op("sync", lambda e, bb=bb, b=b: e.dma_start(out=nT_v[:, :, b * BT:(b + 1) * BT], in_=nTb[bb][:]),
                     reads=[("nTb", bb, q_) for q_ in range(4)], writes=[("nT_d", b)], dma=True)

        def secC(b):
            bb = b % 2
            tok0 = b * BT
            nkeys = [("nTb", bb, tl) for tl in range(4)]
            PACC = [(A0[:], "A0"), (A1[:], "A1"), (B0[:, 0:512], ("B0", 0))]
            prot = [0]

            def nacc():
                a = PACC[prot[0] % 3]
                prot[0] += 1
                return a

            def proj(j):
                acc, akey = nacc()
                for c in range(8):
                    P.op("tensor", lambda e, j=j, c=c, acc=acc: e.matmul(
                        acc, lhsT=Wa[:, c, j * 128:(j + 1) * 128], rhs=nTb[bb][:, c, :], start=(c == 0), stop=(c == 7)),
                        reads=[("Wa", c, 0)] + nkeys, writes=[akey])
                z = j % 3
                P.op("scalar", lambda e, j=j, acc=acc: e.activation(out=zq[j][:], in_=acc, func=AF.Identity, bias=bcol[:, j:j + 1]),
                     reads=[akey, "bcol"], writes=[("zq", j)])
                P.op("gpsimd", lambda e, z=z, j=j: e.tensor_tensor(out=sq[z][:], in0=zq[j][:], in1=zq[j][:], op=ALU.mult),
                     reads=[("zq", j)], writes=[("sq", z)])

            def msacc(j):
                z = j % 3
                P.op("tensor", lambda e, z=z, j=j: e.matmul(A2[:], lhsT=selA[:, j, :], rhs=sq[z][:], start=(j == 0), stop=(j == 7)),
                     reads=[("sq", z), "selA"], writes=["A2"])

            def vproj():
                for tl in range(4):
                    t = 4 * b + tl
                    sl = t % KR
                    acc, akey = nacc()
                    for c in range(8):
                        P.op("tensor", lambda e, c=c, tl=tl, acc=acc: e.matmul(
                            acc, lhsT=nTb[bb][:, c, tl * 128:(tl + 1) * 128], rhs=Wa[:, c, 1024:1536], start=(c == 0), stop=(c == 7)),
                            reads=[("Wa", c, 1024), ("nTb", bb, tl)], writes=[akey])
                    P.op("vector", lambda e, sl=sl, acc=acc: e.tensor_tensor(
                        out=vring[:, sl, :, 0:64], in0=acc.rearrange("p (h d) -> p h d", h=NH),
                        in1=bvb[:].rearrange("p (h d) -> p h d", h=NH), op=ALU.add),
                        reads=[akey, "bvb"], writes=[("v", sl)])

            def fin(j):
                acc, akey = nacc()
                P.op("tensor", lambda e, j=j, acc=acc: e.matmul(acc, lhsT=selB[:, j, :], rhs=r1b[:], start=True, stop=True),
                     reads=["selB", "r1b"], writes=[akey])
                if j < 4:
                    P.op("vector", lambda e, j=j, acc=acc: e.tensor_tensor(out=qT[bb][:, j, :], in0=zq[j][:], in1=acc, op=ALU.mult),
                         reads=[("zq", j), akey], writes=[("qT", bb, j)])
                else:
                    hp = j - 4
                    sl0 = (4 * b) % KR
                    P.op("vector", lambda e, hp=hp, j=j, sl0=sl0, acc=acc: e.scalar_tensor_tensor(
                        out=kring[:, hp, sl0 * 128:(sl0 + 4) * 128], in0=zq[j][:], scalar=gw[:, 0:1], in1=acc, op0=ALU.mult, op1=ALU.mult),
                        reads=[("zq", j), akey, "gw"], writes=[("k", hp, sl0 + q_) for q_ in range(4)])

            proj(0)
            for j in range(8):
                if j + 1 < 8:
                    proj(j + 1)
                msacc(j)
            P.op("scalar", lambda e: e.activation(out=rs[:], in_=A2[:], func=AF.Sqrt, bias=epsc[:, 0:1]), reads=["A2", "epsc"], writes=["rs"])
            P.op("vector", lambda e: e.reciprocal(out=rs[:], in_=rs[:]), reads=["rs"], writes=["rs"])
            P.op("scalar", lambda e: e.activation(out=r1b[:], in_=rs[:], func=AF.Copy), reads=["rs"], writes=["r1b"])
            vproj()
            for j in range(8):
                fin(j)
        def secC3(b):
            bb = b % 2
            tok0 = b * BT
            nkeys = [("nTb", bb, tl) for tl in range(4)]
            SETS = [((A0[:], ["A0"]), (A1[:], ["A1"]), (A2[:], ["A2"])),
                    ((B0[:, 0:512], [("B0", 0)]), (B0[:, 512:1024], [("B0", 4)]), (B1[:, 0:512], [("B1h", 0)]))]
            for ct in range(4):
                z = ct % 2
                (pu, ku), (pb, kb), (pc, kc) = SETS[ct % 2]
                for (dst, dkey, col0) in ((pu, ku, 1536), (pb, kb, 2048), (pc, kc, 2560)):
                    for c in range(8):
                        P.op("tensor", lambda e, c=c, dst=dst, col0=col0, ct=ct: e.matmul(
                            dst, lhsT=Wa[:, c, col0 + ct * 128:col0 + (ct + 1) * 128], rhs=nTb[bb][:, c, :],
                            start=(c == 0), stop=(c == 7)),
                            reads=[("Wa", c, 1536)] + nkeys, writes=dkey)
                ju, jb, jc = 12 + ct, 16 + ct, 20 + ct
                P.op("scalar", lambda e, z=z, ju=ju, pu=pu: e.activation(out=us[z][:], in_=pu, func=AF.Identity, bias=bcol[:, ju:ju + 1]),
                     reads=ku + ["bcol"], writes=[("us", z)])
                P.op("vector", lambda e, z=z, jc=jc, ct=ct, pc=pc: e.scalar_tensor_tensor(
                    out=cu[:, ct, 2:BT + 2], in0=pc, scalar=bcol[:, jc:jc + 1], in1=us[z][:], op0=ALU.add, op1=ALU.mult),
                    reads=kc + ["bcol", ("us", z)], writes=[("cu", ct)])
                P.op("scalar", lambda e, z=z, ct=ct: e.activation(out=t1[z][:], in_=cu[:, ct, 2:BT + 2], func=AF.Identity,
                                                               scale=cw[:, ct * 3 + 2:ct * 3 + 3], bias=cb[:, ct:ct + 1]),
                     reads=[("cu", ct), "cw", "cb"], writes=[("t1", z)])
                P.op("vector", lambda e, z=z, ct=ct: e.scalar_tensor_tensor(
                    out=t1[z][:], in0=cu[:, ct, 1:BT + 1], scalar=cw[:, ct * 3 + 1:ct * 3 + 2], in1=t1[z][:], op0=ALU.mult, op1=ALU.add),
                    reads=[("cu", ct), ("cuh", ct), "cw", ("t1", z)], writes=[("t1", z)])
                P.op("vector", lambda e, z=z, ct=ct: e.scalar_tensor_tensor(
                    out=t1[z][:], in0=cu[:, ct, 0:BT], scalar=cw[:, ct * 3:ct * 3 + 1], in1=t1[z][:], op0=ALU.mult, op1=ALU.add),
                    reads=[("cu", ct), ("cuh", ct), "cw", ("t1", z)], writes=[("t1", z)])
                P.op("vector", lambda e, z=z, jb=jb, ct=ct, pb=pb: e.scalar_tensor_tensor(
                    out=ycT[bb][:, ct, :], in0=pb, scalar=bcol[:, jb:jb + 1], in1=t1[z][:], op0=ALU.add, op1=ALU.mult),
                    reads=kb + ["bcol", ("t1", z)], writes=[("ycT", bb, ct)])
                P.op("vector", lambda e, ct=ct: e.tensor_copy(out=cu[:, ct, 0:2], in_=cu[:, ct, BT:BT + 2]),
                     reads=[("cu", ct)], writes=[("cuh", ct)])
            P.op("sync", lambda e, tok0=tok0: e.dma_start(out=ycT_v[:, :, tok0:tok0 + BT], in_=ycT[bb][:]),
                 reads=[("ycT", bb, ct) for ct in range(4)], writes=[("ycT_d", b)], dma=True)

        def secD(b):
            bb = b % 2
            tok0 = b * BT
            nxt = b + 1 < NB
            units = []
            for h in range(NH):
                for m in range(8):
                    kt = 4 * b - 4 + m
                    if kt < 0:
                        continue
                    units.append((h, m, kt, max(m - 4, 0), min(m, 3)))
            SPS = [(A0[:], "A0"), (A1[:], "A1"), (A2[:], "A2"), (B0[:, 0:512], ("B0", 0)), (B0[:, 512:1024], ("B0", 4))]
            LA = 4

            def QKEXP(u):
                h, m, kt, tlo, thi = units[u]
                hp, r0 = h // 2, (h % 2) * 64
                nq = thi - tlo + 1
                sp, sk = SPS[u % 5]
                pz = u % 6
                sl = kt % KR
                P.op("tensor", lambda e, hp=hp, r0=r0, sl=sl, sp=sp, tlo=tlo, thi=thi: e.matmul(
                    sp[:, 0:(thi - tlo + 1) * 128], lhsT=kring[r0:r0 + 64, hp, sl * 128:(sl + 1) * 128],
                    rhs=qT[bb][r0:r0 + 64, hp, tlo * 128:(thi + 1) * 128], start=True, stop=True),
                    reads=[("k", hp, sl), ("qT", bb, hp)], writes=[sk])
                P.op("scalar", lambda e, pz=pz, sp=sp, nq=nq: e.activation(
                    out=pt[pz][:, 0:nq, :], in_=sp[:, 0:nq * 128].rearrange("p (j q) -> p j q", q=128), func=AF.Exp, scale=DH ** -0.5),
                    reads=[sk], writes=[("pt", pz)])
                rlo = 4 - m + tlo
                P.op("vector", lambda e, pz=pz, nq=nq, h=h, rlo=rlo: e.tensor_tensor(
                    out=pt[pz][:, 0:nq, :], in0=pt[pz][:, 0:nq, :], in1=expB[:, h, rlo:rlo + nq, :], op=ALU.mult),
                    reads=[("pt", pz), ("expB", h)], writes=[("pt", pz)])

            def PV(u):
                h, m, kt, tlo, thi = units[u]
                pz = u % 6
                sl = kt % KR
                hb2 = h % 2
                first = (u == 0) or units[u - 1][0] != h
                last_u = (u + 1 == len(units)) or units[u + 1][0] != h
                if first:
                    P.op("tensor", lambda e, hb2=hb2: e.matmul(
                        B1[:, hb2 * 512:hb2 * 512 + 260], lhsT=zt[:, 0:128], rhs=zt[:, 0:260], start=True, stop=False),
                        reads=["zt"], writes=[("B1h", hb2)])
                for tl in range(tlo, thi + 1):
                    c0 = hb2 * 512 + tl * 65
                    P.op("tensor", lambda e, h=h, tl=tl, tlo=tlo, sl=sl, pz=pz, c0=c0, fin=(last_u and tl == thi): e.matmul(
                        B1[:, c0:c0 + 65], lhsT=pt[pz][:, tl - tlo, :], rhs=vring[:, sl, h, :],
                        start=False, stop=fin),
                        reads=[("pt", pz), ("v", sl)], writes=[("B1h", hb2)])

            def FINH(h):
                hb2 = h % 2
                Bv = B1[:, hb2 * 512:hb2 * 512 + 260].rearrange("p (t d) -> p t d", d=65)
                P.op("vector", lambda e, hb2=hb2, Bv=Bv: e.reciprocal(out=rden[hb2][:], in_=Bv[:, :, 64]),
                     reads=[("B1h", hb2)], writes=[("rden", hb2)])
                P.op("vector", lambda e, hb2=hb2, Bv=Bv, h=h: e.tensor_tensor(
                    out=ya[:, :, h * 64:(h + 1) * 64], in0=Bv[:, :, 0:64], in1=bc_last(rden[hb2][:], 64), op=ALU.mult),
                    reads=[("B1h", hb2), ("rden", hb2)], writes=[("ya", h)])

            if nxt:
                A_norm(b + 1, 0)
            for u in range(min(LA, len(units))):
                QKEXP(u)
            secC3(b)
            zero_fill(b)
            for u in range(len(units)):
                if u + LA < len(units):
                    QKEXP(u + LA)
                PV(u)
                h = units[u][0]
                if u + 1 == len(units) or units[u + 1][0] != h:
                    FINH(h)
                    if nxt and h % 2 == 1:
                        tl = h // 2
                        A_tr(b + 1, tl)
                        if tl + 1 < 4:
                            A_norm(b + 1, tl + 1)
            for tl in range(4):
                transpose_tile(ya[:, tl, :], 4, yaT[bb][:, :, tl * 128:(tl + 1) * 128], [("ya", h) for h in range(NH)], ("yaT", bb, tl))
            P.op("sync", lambda e, tok0=tok0: e.dma_start(out=yaT_v[:, :, tok0:tok0 + BT], in_=yaT[bb][:]),
                 reads=[("yaT", bb, tl) for tl in range(4)], writes=[("yaT_d", b)], dma=True)

        for tl in range(4):
            A_norm(0, tl)
            A_tr(0, tl)
        for b in range(NB):
            secC(b)
            secD(b)
        P.barrier()
    if 1 in phases:
        phase1()
    sb.reset(base_mark)

    def phase2():
        Wg = sb.alloc("Wg", [128, 8, 2048], BF16)
        wpa = sb.alloc("wpa", [128, 4, D], BF16)
        wpc = sb.alloc("wpc", [128, 4, D], BF16)
        wo = sb.alloc("wo", [128, 8, D], BF16)
        wrg = sb.alloc("wrg", [128, 8, 36], BF16)
        brg = sb.alloc("brg", [128, 36], F32)
        gffn = sb.alloc("gffn", [128, D], F32)
        nTb = [sb.alloc("nTb%d" % i, [128, 8, BT], BF16) for i in range(2)]
        yaT = [sb.alloc("yaT%d" % i, [128, 4, BT], BF16) for i in range(2)]
        ycT = [sb.alloc("ycT%d" % i, [128, 4, BT], BF16) for i in range(2)]
        xt = [sb.alloc("xt%d" % i, [128, D], F32) for i in range(4)]
        tA = [sb.alloc("tA%d" % i, [128, BT], F32) for i in range(2)]
        tC = [sb.alloc("tC%d" % i, [128, BT], F32) for i in range(2)]
        mA = [sb.alloc("mA%d" % i, [128, BT], F32) for i in range(2)]
        mC = [sb.alloc("mC%d" % i, [128, BT], F32) for i in range(2)]
        mT = [sb.alloc("mT%d" % i, [128, 8, BT], BF16) for i in range(2)]
        ht = [sb.alloc("ht%d" % i, [128, D], F32) for i in range(3)]
        n2 = [sb.alloc("n2%d" % i, [128, 4, D], BF16) for i in range(2)]
        n2T = [sb.alloc("n2T%d" % i, [128, 8, 128], BF16) for i in range(2)]
        lg = sb.alloc("lg", [128, 4, 36], F32)
        gmax = sb.alloc("gmax", [128, 4], F32)
        gmask = sb.alloc("gmask", [128, 4, 4], F32)
        gex = sb.alloc("gex", [128, 4, 4], F32)
        gse = sb.alloc("gse", [128, 4], F32)
        pen = sb.alloc("pen", [128, 4, 4], F32)
        elm = sb.alloc("elm", [128, 4, 32], F32)
        elm2 = sb.alloc("elm2", [128, 4, 32], F32)
        m1 = sb.alloc("m1", [128, 4], F32)
        m2 = sb.alloc("m2", [128, 4], F32)
        mk1 = sb.alloc("mk1", [128, 4, 32], F32)
        mk2 = sb.alloc("mk2", [128, 4, 32], F32)
        Mb = sb.alloc("Mb", [128, 4, 32], BF16)
        dd = sb.alloc("dd", [128, 4], F32)
        ee = sb.alloc("ee", [128, 4], F32)
        rr = sb.alloc("rr", [128, 4], F32)
        wA = sb.alloc("wA", [128, 4], F32)
        wB = sb.alloc("wB", [128, 4], F32)
        pos = sb.alloc("pos", [128, 4, 32], F32)
        okm = sb.alloc("okm", [128, 4, 32], F32)
        slot = sb.alloc("slot", [128, 4, 32], F32)
        tmp = sb.alloc("tmp", [128, 4, 32], F32)
        dsel = sb.alloc("dsel", [128, 4, 2], F32)
        oksel = sb.alloc("oksel", [128, 4, 2], F32)

        win_v = win_d.ap().rearrange("(c p) n -> p c n", p=128)
        def wg_load(q4):
            for g0 in (0, 1024):
                c0_ = g0 + q4 * 256
                P.op("gpsimd", lambda e, c0_=c0_: e.dma_start(out=Wg[:, :, c0_:c0_ + 256], in_=win_v[:, :, 3072 + c0_:3072 + c0_ + 256]),
                     writes=[("Wg", c0_)], dma=True)

        wg_load(0)
        P.op("gpsimd", lambda e: e.dma_start(out=wpa[:], in_=wpa_d.ap().rearrange("(c p) n -> p c n", p=128)), writes=["wpa"], dma=True)
        P.op("gpsimd", lambda e: e.dma_start(out=wpc[:], in_=wpc_d.ap().rearrange("(c p) n -> p c n", p=128)), writes=["wpc"], dma=True)
        for q4 in range(1, 4):
            wg_load(q4)
        wo_v = wo_d.ap().rearrange("(c p) n -> p c n", p=128)
        for c in range(0, 8, 4):
            P.op("gpsimd", lambda e, c=c: e.dma_start(out=wo[:, c:c + 4, :], in_=wo_v[:, c:c + 4, :]), writes=[("wo", c)], dma=True)
        P.op("gpsimd", lambda e: e.dma_start(out=wrg[:], in_=wrg_d.ap().rearrange("(c p) n -> p c n", p=128)), writes=["wrg"], dma=True)
        ld("sync", brg[:], brg_d.ap().partition_broadcast(128), "brg")
        ld("sync", gffn[:], gffn_d.ap().partition_broadcast(128), "gffn")

        nT_v = nT_d.ap().rearrange("(c p) t -> p c t", p=128)
        yaT_v = yaT_d.ap().rearrange("(c p) t -> p c t", p=128)
        ycT_v = ycT_d.ap().rearrange("(c p) t -> p c t", p=128)
        wokeys = [("wo", 0), ("wo", 4)]
        xctr = [0]
        hctr = [0]

        def loads2(b):
            bb = b % 2
            tok0 = b * BT
            P.op("sync", lambda e, bb=bb, tok0=tok0: e.dma_start(out=nTb[bb][:], in_=nT_v[:, :, tok0:tok0 + BT]), writes=[("nTb", bb)], dma=True)
            P.op("sync", lambda e, bb=bb, tok0=tok0: e.dma_start(out=yaT[bb][:], in_=yaT_v[:, :, tok0:tok0 + BT]), writes=[("yaT", bb)], dma=True)
            P.op("sync", lambda e, bb=bb, tok0=tok0: e.dma_start(out=ycT[bb][:], in_=ycT_v[:, :, tok0:tok0 + BT]), writes=[("ycT", bb)], dma=True)

        def xload(t):
            P.op("sync", lambda e, t=t: e.dma_start(out=xt[t % 4][:], in_=x_d.ap()[t * 128:(t + 1) * 128, :]), writes=[("xt", t % 4)], dma=True)

        loads2(0)
        for t_ in range(3):
            xload(t_)
        def gates2(b):
            bb = b % 2
            tok0 = b * BT
            if b + 1 < NB:
                loads2(b + 1)
            for j in range(8):
                z = j % 2
                for (dst, dkey, col0) in ((A0, "A0", 0), (A1, "A1", 1024)):
                    for c in range(8):
                        P.op("tensor", lambda e, c=c, dst=dst, col0=col0, j=j, bb=bb: e.matmul(
                            dst[:], lhsT=Wg[:, c, col0 + j * 128:col0 + (j + 1) * 128], rhs=nTb[bb][:, c, :], start=(c == 0), stop=(c == 7)),
                            reads=[("Wg", col0 + (j // 2) * 256), ("nTb", bb)], writes=[dkey])
                for (dst, dkey, wsrc, wkey, asrc, akey) in ((A2, "A2", wpa, "wpa", yaT, "yaT"), (B0, ("B0", 0), wpc, "wpc", ycT, "ycT")):
                    for c in range(4):
                        P.op("tensor", lambda e, c=c, dst=dst, wsrc=wsrc, asrc=asrc, j=j, bb=bb: e.matmul(
                            dst[:, 0:512], lhsT=wsrc[:, c, j * 128:(j + 1) * 128], rhs=asrc[bb][:, c, :], start=(c == 0), stop=(c == 3)),
                            reads=[wkey, (akey, bb)], writes=[dkey])
                P.op("scalar", lambda e, z=z, j=j: e.activation(out=tA[z][:], in_=A0[:], func=AF.Tanh, scale=0.5, bias=hbcol[:, 24 + j:25 + j]),
                     reads=["A0", "hbcol"], writes=[("tA", z)])
                P.op("scalar", lambda e, z=z, j=j: e.activation(out=tC[z][:], in_=A1[:], func=AF.Tanh, scale=0.5, bias=hbcol[:, 32 + j:33 + j]),
                     reads=["A1", "hbcol"], writes=[("tC", z)])
                P.op("vector", lambda e, z=z: e.scalar_tensor_tensor(out=mA[z][:], in0=tA[z][:], scalar=1.0, in1=A2[:], op0=ALU.add, op1=ALU.mult),
                     reads=[("tA", z), "A2"], writes=[("mA", z)])
                P.op("vector", lambda e, z=z: e.scalar_tensor_tensor(out=mC[z][:], in0=tC[z][:], scalar=1.0, in1=B0[:, 0:512], op0=ALU.add, op1=ALU.mult),
                     reads=[("tC", z), ("B0", 0)], writes=[("mC", z)])
                P.op("gpsimd", lambda e, z=z, j=j, bb=bb: e.tensor_tensor(out=mT[bb][:, j, :], in0=mA[z][:], in1=mC[z][:], op=ALU.add),
                     reads=[("mA", z), ("mC", z)], writes=[("mT", bb, j)])

        def hsec2(b):
            bb = b % 2
            tok0 = b * BT
            mkeys = [("mT", bb, j) for j in range(8)]
            def hmm(tl):
                t = 4 * b + tl
                xs_ = t % 4
                hs_ = t % 3
                if t + 3 < NT:
                    xload(t + 3)
                for half in range(2):
                    for j in range(8):
                        P.op("tensor", lambda e, j=j, half=half, tl=tl, bb=bb: e.matmul(
                            B1[:, half * 512:(half + 1) * 512], lhsT=mT[bb][:, j, tl * 128:(tl + 1) * 128], rhs=wo[:, j, half * 512:(half + 1) * 512],
                            start=(j == 0), stop=(j == 7)),
                            reads=mkeys + wokeys, writes=[("B1", half)])
                    P.op("vector", lambda e, half=half, xs_=xs_, hs_=hs_: e.scalar_tensor_tensor(
                        out=ht[hs_][:, half * 512:(half + 1) * 512], in0=B1[:, half * 512:(half + 1) * 512], scalar=0.5,
                        in1=xt[xs_][:, half * 512:(half + 1) * 512], op0=ALU.mult, op1=ALU.add),
                        reads=[("B1", half), ("xt", xs_)] + ([("ht", hs_)] if half == 1 else []), writes=[("ht", hs_)])
                P.op("sync", lambda e, t=t, hs_=hs_: e.dma_start(out=h_d.ap()[t * 128:(t + 1) * 128, :], in_=ht[hs_][:]),
                     reads=[("ht", hs_)], writes=[("h_d", t)], dma=True)

            def hnorm(tl):
                t = 4 * b + tl
                hs_ = t % 3
                rmsnorm_tile(ht[hs_][:], gffn[:], n2[bb][:, tl, :], ("ht", hs_), "gffn", ("n2", bb, tl))

            def htr(tl):
                t = 4 * b + tl
                z2 = t % 2
                transpose_tile(n2[bb][:, tl, :], 8, n2T[z2][:], ("n2", bb, tl), ("n2T", z2))
                for c in range(8):
                    P.op("tensor", lambda e, c=c, tl=tl, z2=z2: e.matmul(
                        B0[:, 512 + tl * 36:512 + (tl + 1) * 36], lhsT=n2T[z2][:, c, :], rhs=wrg[:, c, :], start=(c == 0), stop=(c == 7)),
                        reads=[("n2T", z2), "wrg"], writes=[("lgp", tl)])

            hmm(0)
            hnorm(0)
            for tl in range(4):
                if tl + 1 < 4:
                    hmm(tl + 1)
                htr(tl)
                if tl + 1 < 4:
                    hnorm(tl + 1)

        def rout2(b):
            bb = b % 2
            tok0 = b * BT
            lgp = B0[:, 512:512 + 144].rearrange("p (t n) -> p t n", t=4)
            R = []

            def V(fn, reads, writes):
                P.op("vector", fn, reads=reads, writes=writes)

            V(lambda e: e.tensor_tensor(out=lg[:], in0=lgp, in1=bc_mid(brg[:], 4), op=ALU.add),
              [("lgp", tl) for tl in range(4)] + ["brg"], ["lg"])
            V(lambda e: e.tensor_reduce(out=gmax[:], in_=lg[:, :, 0:4], axis=AX.X, op=ALU.max), ["lg"], ["gmax"])
            V(lambda e: e.tensor_tensor(out=gmask[:], in0=lg[:, :, 0:4], in1=bc_last(gmax[:], 4), op=ALU.is_equal), ["lg", "gmax"], ["gmask"])
            V(lambda e: e.tensor_tensor(out=gex[:], in0=lg[:, :, 0:4], in1=bc_last(gmax[:], 4), op=ALU.subtract), ["lg", "gmax"], ["gex"])
            P.op("scalar", lambda e: e.activation(out=gex[:], in_=gex[:], func=AF.Exp), reads=["gex"], writes=["gex"])
            V(lambda e: e.tensor_reduce(out=gse[:], in_=gex[:], axis=AX.X, op=ALU.add), ["gex"], ["gse"])
            V(lambda e: e.reciprocal(out=gse[:], in_=gse[:]), ["gse"], ["gse"])
            V(lambda e: e.tensor_scalar(out=pen[:], in0=gmask[:], scalar1=1.0, scalar2=1e30, op0=ALU.subtract, op1=ALU.mult), ["gmask"], ["pen"])
            V(lambda e: e.tensor_tensor(out=elm[:].rearrange("p t (g k) -> p t g k", g=4),
                                        in0=lg[:, :, 4:36].rearrange("p t (g k) -> p t g k", g=4),
                                        in1=bc_last(pen[:], 8), op=ALU.add), ["lg", "pen"], ["elm"])
            V(lambda e: e.tensor_reduce(out=m1[:], in_=elm[:], axis=AX.X, op=ALU.max), ["elm"], ["m1"])
            V(lambda e: e.tensor_tensor(out=mk1[:], in0=elm[:], in1=bc_last(m1[:], 32), op=ALU.is_equal), ["elm", "m1"], ["mk1"])
            V(lambda e: e.scalar_tensor_tensor(out=elm2[:], in0=mk1[:], scalar=-1e30, in1=elm[:], op0=ALU.mult, op1=ALU.add), ["mk1", "elm"], ["elm2"])
            V(lambda e: e.tensor_reduce(out=m2[:], in_=elm2[:], axis=AX.X, op=ALU.max), ["elm2"], ["m2"])
            V(lambda e: e.tensor_tensor(out=mk2[:], in0=elm2[:], in1=bc_last(m2[:], 32), op=ALU.is_equal), ["elm2", "m2"], ["mk2"])
            V(lambda e: e.tensor_tensor(out=dd[:], in0=m2[:], in1=m1[:], op=ALU.subtract), ["m1", "m2"], ["dd"])
            P.op("scalar", lambda e: e.activation(out=ee[:], in_=dd[:], func=AF.Exp), reads=["dd"], writes=["ee"])
            V(lambda e: e.tensor_scalar(out=rr[:], in0=ee[:], scalar1=1.0, scalar2=None, op0=ALU.add), ["ee"], ["rr"])
            V(lambda e: e.reciprocal(out=rr[:], in_=rr[:]), ["rr"], ["rr"])
            V(lambda e: e.tensor_tensor(out=wA[:], in0=gse[:], in1=rr[:], op=ALU.mult), ["gse", "rr"], ["wA"])
            V(lambda e: e.tensor_tensor(out=wB[:], in0=wA[:], in1=ee[:], op=ALU.mult), ["wA", "ee"], ["wB"])
            V(lambda e: e.tensor_tensor(out=Mb[:], in0=mk1[:], in1=mk2[:], op=ALU.add), ["mk1", "mk2"], ["Mb"])
            for tl in range(4):
                P.op("tensor", lambda e, tl=tl: e.matmul(A0[:, tl * 32:(tl + 1) * 32], lhsT=utri[:], rhs=Mb[:, tl, :], start=True, stop=(tl == 0)),
                     reads=["utri", "Mb"], writes=["A0"])
                for t2 in range(tl):
                    P.op("tensor", lambda e, tl=tl, t2=t2: e.matmul(A0[:, tl * 32:(tl + 1) * 32], lhsT=ones[:], rhs=Mb[:, t2, :], start=False, stop=(t2 == tl - 1)),
                         reads=["ones", "Mb"], writes=["A0"])
            for tl in range(4):
                P.op("tensor", lambda e, tl=tl: e.matmul(A1[:, 0:32], lhsT=ones[:], rhs=Mb[:, tl, :], start=(tl == 0), stop=(tl == 3)),
                     reads=["ones", "Mb"], writes=["A1"])
            V(lambda e: e.tensor_tensor(out=pos[:], in0=A0[:, 0:128].rearrange("p (t n) -> p t n", t=4), in1=bc_mid(cnt[:], 4), op=ALU.add),
              ["A0", "cnt"], ["pos"])
            V(lambda e: e.tensor_tensor(out=cnt[:], in0=cnt[:], in1=A1[:, 0:32], op=ALU.add), ["A1", "cnt", "pos"], ["cnt"])
            V(lambda e: e.tensor_scalar(out=okm[:], in0=pos[:], scalar1=float(CAP), scalar2=None, op0=ALU.is_lt), ["pos"], ["okm"])
            V(lambda e: e.tensor_tensor(out=slot[:], in0=pos[:], in1=bc_mid(ecap[:], 4), op=ALU.add), ["pos", "ecap"], ["slot"])
            V(lambda e: e.tensor_scalar(out=tmp[:], in0=okm[:], scalar1=-1.0e6, scalar2=1.0e6, op0=ALU.mult, op1=ALU.add), ["okm"], ["tmp"])
            V(lambda e: e.tensor_tensor(out=slot[:], in0=slot[:], in1=tmp[:], op=ALU.add), ["slot", "tmp"], ["slot"])
            V(lambda e: e.tensor_scalar(out=slot[:], in0=slot[:], scalar1=float(NSLOT), scalar2=None, op0=ALU.min), ["slot"], ["slot"])
            for k, mk in ((0, mk1), (1, mk2)):
                V(lambda e, mk=mk: e.tensor_tensor(out=tmp[:], in0=mk[:], in1=slot[:], op=ALU.mult), ["mk1", "mk2", "slot"], ["tmp"])
                V(lambda e, k=k: e.tensor_reduce(out=dsel[:, :, k], in_=tmp[:], axis=AX.X, op=ALU.add), ["tmp"], [("dsel", k)])
                V(lambda e, mk=mk: e.tensor_tensor(out=tmp[:], in0=mk[:], in1=okm[:], op=ALU.mult), ["mk1", "mk2", "okm", ("dsel", k)], ["tmp"])
                V(lambda e, k=k: e.tensor_reduce(out=oksel[:, :, k], in_=tmp[:], axis=AX.X, op=ALU.add), ["tmp"], [("oksel", k)])
            tb = 4 * b
            V(lambda e, tb=tb: e.tensor_copy(out=dtab[:, tb:tb + 4, :], in_=dsel[:]), [("dsel", 0), ("dsel", 1)], [("dtab", b)])
            V(lambda e, tb=tb: e.tensor_tensor(out=wtab[:, tb:tb + 4, 0], in0=wA[:], in1=oksel[:, :, 0], op=ALU.mult), ["wA", ("oksel", 0)], [("wtab", b, 0)])
            V(lambda e, tb=tb: e.tensor_tensor(out=wtab[:, tb:tb + 4, 1], in0=wB[:], in1=oksel[:, :, 1], op=ALU.mult), ["wB", ("oksel", 1)], [("wtab", b, 1)])
            for tl in range(4):
                t = 4 * b + tl
                for k in range(2):
                    P.op("gpsimd", lambda e, t=t, k=k, tl=tl, bb=bb: e.indirect_dma_start(
                        out=xs_d[:, :], out_offset=bass.IndirectOffsetOnAxis(ap=dtab[:, t, k:k + 1], axis=0),
                        in_=n2[bb][:, tl, :], in_offset=None),
                        reads=[("n2", bb, tl), ("dtab", b)], writes=[("xs_d", t, k)], dma=True)

        gates2(0)
        for b in range(NB):
            hsec2(b)
            if b + 1 < NB:
                gates2(b + 1)
            rout2(b)
        if debug:
            P.op("sync", lambda e: e.dma_start(out=dtab_o.ap(), in_=dtab[:].rearrange("p t k -> p (t k)")),
                 reads=[("dtab", b) for b in range(NB)], dma=True, is_out=True)
            P.op("sync", lambda e: e.dma_start(out=wtab_o.ap(), in_=wtab[:].rearrange("p t k -> p (t k)")),
                 reads=[("wtab", b, k) for b in range(NB) for k in range(2)], dma=True, is_out=True)
        P.barrier()
    if 2 in phases:
        phase2()
    sb.reset(base_mark)

    TOP = SBAlloc.HI - 20 * 1024
    p4w = {"wpg": nc.alloc_sbuf_tensor_at("wpg_top", [128, 8, D], BF16, offset=TOP),
           "wpp": nc.alloc_sbuf_tensor_at("wpp_top", [128, 2, D], BF16, offset=TOP + 16 * 1024)}

    def p4_weight_loads():
        wpg_v = wpg_d.ap().rearrange("(c p) n -> p c n", p=128)
        for c in range(0, 8, 4):
            P.op("gpsimd", lambda e, c=c: e.dma_start(out=p4w["wpg"][:, c:c + 4, :], in_=wpg_v[:, c:c + 4, :]), writes=[("wpg", c)], dma=True)
        P.op("gpsimd", lambda e: e.dma_start(out=p4w["wpp"][:], in_=wpp_d.ap().rearrange("(c p) n -> p c n", p=128)), writes=["wpp"], dma=True)

    def phase3():
        NWB = 3
        w1b = [sb.alloc("w1b%d" % i, [128, 8, 512], BF16) for i in range(NWB)]
        w3b = [sb.alloc("w3b%d" % i, [128, 8, 512], BF16) for i in range(NWB)]
        w2b = [sb.alloc("w2b%d" % i, [128, 4, D], BF16) for i in range(NWB)]
        xr = [sb.alloc("xr%d" % i, [128, 3, D], BF16) for i in range(3)]
        xsT = [sb.alloc("xsT%d" % i, [128, 8, CAP], BF16) for i in range(2)]
        s1 = [sb.alloc("s1%d" % i, [128, CAP], F32) for i in range(2)]
        hdn = [sb.alloc("hdn%d" % i, [128, 4, CAP], BF16) for i in range(2)]
        yb = [sb.alloc("yb%d" % i, [128, D], BF16) for i in range(3)]
        HACC = [(A0, "A0", A1, "A1"), (A2, "A2", B0, ("B0", 0))]
        yctr = [0]
        P.op("gpsimd", lambda e: e.memset(yb[0][:], 0.0), writes=[("yb", 0, 0), ("yb", 0, 1)])
        P.op("sync", lambda e: e.dma_start(out=ys_d.ap()[NSLOT:NSLOT + 128, :], in_=yb[0][:]),
             reads=[("yb", 0, 0), ("yb", 0, 1)], writes=["ys_trash"], dma=True)
        def wload(ex, after=()):
            wb_ = ex % NWB
            if after:
                P.op("gpsimd", lambda e: e.memset(junk[:, 0:8], 0.0), reads=list(after), writes=["junk_g"])
            P.op("gpsimd", lambda e, ex=ex, wb_=wb_: e.dma_start(out=w1b[wb_][:], in_=w1_d.ap()[ex].rearrange("(p c) f -> p c f", c=8)),
                 writes=[("w1b", wb_)], dma=True)
            P.op("gpsimd", lambda e, ex=ex, wb_=wb_: e.dma_start(out=w3b[wb_][:], in_=w3_d.ap()[ex].rearrange("(p c) f -> p c f", c=8)),
                 writes=[("w3b", wb_)], dma=True)
            P.op("gpsimd", lambda e, ex=ex, wb_=wb_: e.dma_start(out=w2b[wb_][:], in_=w2_d.ap()[ex].rearrange("(c p) f -> p c f", p=128)),
                 writes=[("w2b", wb_)], dma=True)

        def xsload(ex):
            e3 = ex % 3
            P.op("sync", lambda e, ex=ex, e3=e3: e.dma_start(
                out=xr[e3][:], in_=xs_d.ap()[ex * CAP:(ex + 1) * CAP, :].rearrange("(r p) d -> p r d", p=128)),
                writes=[("xr", e3)], dma=True)

        def xsT_group(ex, r):
            eb = ex % 2
            e3 = ex % 3
            transpose_tile(xr[e3][:, r, :], 8, xsT[eb][:, :, r * 128:(r + 1) * 128], ("xr", e3), ("xsT", eb, r),
                           evac=("scalar" if r % 2 == 0 else "vector"), interleave=8)

        xsload(0)
        wload(0)
        xsload(1)
        wload(1, after=[("w1b", 0), ("w3b", 0), ("w2b", 0)])
        for r in range(3):
            xsT_group(0, r)
        for ex in range(NE):
            eb = ex % 2
            wb_ = ex % NWB
            if ex + 2 < NE:
                wload(ex + 2)
                xsload(ex + 2)
            if ex == 2:
                p4_weight_loads()
            xk = [("xsT", eb, r) for r in range(3)]
            for f in range(4):
                a1, k1, a3, k3 = HACC[f % 2]
                z = f % 2
                for (dst, dkey, wsrc, wkey) in ((a1, k1, w1b, "w1b"), (a3, k3, w3b, "w3b")):
                    for c in range(8):
                        P.op("tensor", lambda e, c=c, dst=dst, wsrc=wsrc, f=f, eb=eb, wb_=wb_: e.matmul(
                            dst[:, 0:CAP], lhsT=wsrc[wb_][:, c, f * 128:(f + 1) * 128], rhs=xsT[eb][:, c, :], start=(c == 0), stop=(c == 7)),
                            reads=[(wkey, wb_)] + xk, writes=[dkey])
                P.op("scalar", lambda e, a1=a1, z=z: e.activation(out=s1[z][:], in_=a1[:, 0:CAP], func=AF.Silu), reads=[k1], writes=[("s1", z)])
                P.op("vector", lambda e, a3=a3, z=z, f=f, eb=eb: e.tensor_tensor(out=hdn[eb][:, f, :], in0=s1[z][:], in1=a3[:, 0:CAP], op=ALU.mult),
                     reads=[("s1", z), k3], writes=[("hdn", eb, f)])
            hk_ = [("hdn", eb, f) for f in range(4)]
            for r in range(3):
                if ex + 1 < NE:
                    xsT_group(ex + 1, r)
                ys_ = yctr[0] % 3
                yctr[0] += 1
                for half in range(2):
                    for f in range(4):
                        P.op("tensor", lambda e, f=f, half=half, r=r, eb=eb, wb_=wb_: e.matmul(
                            B1[:, half * 512:(half + 1) * 512], lhsT=hdn[eb][:, f, r * 128:(r + 1) * 128], rhs=w2b[wb_][:, f, half * 512:(half + 1) * 512],
                            start=(f == 0), stop=(f == 3)),
                            reads=hk_ + [("w2b", wb_)], writes=[("B1", half)])
                    if half == 0:
                        P.op("scalar", lambda e, ys_=ys_: e.activation(out=yb[ys_][:, 0:512], in_=B1[:, 0:512], func=AF.Copy),
                             reads=[("B1", 0)], writes=[("yb", ys_, 0)])
                    else:
                        P.op("vector", lambda e, ys_=ys_: e.tensor_copy(out=yb[ys_][:, 512:1024], in_=B1[:, 512:1024]),
                             reads=[("B1", 1)], writes=[("yb", ys_, 1)])
                row0 = ex * CAP + r * 128
                P.op("sync", lambda e, row0=row0, ys_=ys_: e.dma_start(out=ys_d.ap()[row0:row0 + 128, :], in_=yb[ys_][:]),
                     reads=[("yb", ys_, 0), ("yb", ys_, 1)], writes=[("ys_d", ex, r)], dma=True)
        P.barrier()
    if 3 in phases:
        phase3()
    sb.reset(base_mark)

    def phase4():
        wpg, wpp = p4w["wpg"], p4w["wpp"]
        gple = sb.alloc("gple", [128, D], F32)
        bpg = sb.alloc("bpg", [128, D], F32)
        hb = [sb.alloc("hb%d" % i, [128, D], F32) for i in range(3)]
        y1 = [sb.alloc("y1%d" % i, [128, D], BF16) for i in range(3)]
        y2 = [sb.alloc("y2%d" % i, [128, D], BF16) for i in range(3)]
        pin = [sb.alloc("pin%d" % i, [128, 256], F32) for i in range(3)]
        pbf = [sb.alloc("pbf%d" % i, [128, 256], BF16) for i in range(2)]
        ppT = [sb.alloc("ppT%d" % i, [128, 2, 128], BF16) for i in range(2)]
        n3 = [sb.alloc("n3%d" % i, [128, D], BF16) for i in range(2)]
        n3T = [sb.alloc("n3T%d" % i, [128, 8, 128], BF16) for i in range(2)]
        gz = [sb.alloc("gz%d" % i, [128, D], F32) for i in range(2)]
        ob = [sb.alloc("ob%d" % i, [128, D], F32) for i in range(2)]
        ld("sync", gple[:], gple_d.ap().partition_broadcast(128), "gple")
        ld("sync", bpg[:], bpg_d.ap().partition_broadcast(128), "bpg")
        def loads4(t):
            h3 = t % 3
            P.op("sync", lambda e, t=t, h3=h3: e.dma_start(out=hb[h3][:], in_=h_d.ap()[t * 128:(t + 1) * 128, :]), writes=[("hb", h3)], dma=True)
            P.op("sync", lambda e, t=t, h3=h3: e.dma_start(out=pin[h3][:], in_=p_d.ap()[t * 128:(t + 1) * 128, :]), writes=[("pin", h3)], dma=True)
            for (yy, ykey, k) in ((y1, "y1", 0), (y2, "y2", 1)):
                P.op("gpsimd", lambda e, yy=yy, k=k, t=t, h3=h3: e.indirect_dma_start(
                    out=yy[h3][:, :], out_offset=None, in_=ys_d[:, :], in_offset=bass.IndirectOffsetOnAxis(ap=dtab[:, t, k:k + 1], axis=0)), reads=["dtab_all"], writes=[(ykey, h3)], dma=True)

        def S1a(t):
            h3 = t % 3
            z = t % 2
            P.op("vector", lambda e, h3=h3, t=t: e.scalar_tensor_tensor(out=hb[h3][:], in0=y1[h3][:], scalar=wtab[:, t, 0:1], in1=hb[h3][:],
                                                                       op0=ALU.mult, op1=ALU.add), reads=[("y1", h3), ("hb", h3)], writes=[("hb", h3)])
            P.op("vector", lambda e, h3=h3, t=t: e.scalar_tensor_tensor(out=hb[h3][:], in0=y2[h3][:], scalar=wtab[:, t, 1:2], in1=hb[h3][:],
                                                                       op0=ALU.mult, op1=ALU.add), reads=[("y2", h3), ("hb", h3)], writes=[("hb", h3)])
            rmsnorm_tile(hb[h3][:], gple[:], n3[z][:], ("hb", h3), "gple", ("n3", z))
            P.op("scalar", lambda e, z=z, h3=h3: e.activation(out=pbf[z][:], in_=pin[h3][:], func=AF.Copy), reads=[("pin", h3)], writes=[("pbf", z)])

        def S1b(t):
            z = t % 2
            transpose_tile(n3[z], 8, n3T[z][:], ("n3", z), ("n3T", z))
            transpose_tile(pbf[z], 2, ppT[z][:], ("pbf", z), ("ppT", z))

        GACC = [((A0, "A0"), (A2, "A2")), ((A1, "A1"), (B0, ("B0", 0)))]

        def S2mm(t, half):
            z = t % 2
            (ga, gk), (pa_, pk) = GACC[half]
            for c in range(8):
                P.op("tensor", lambda e, c=c, half=half, ga=ga, z=z: e.matmul(
                    ga[:], lhsT=n3T[z][:, c, :], rhs=wpg[:, c, half * 512:(half + 1) * 512], start=(c == 0), stop=(c == 7)),
                    reads=[("n3T", z), ("wpg", 0), ("wpg", 4)], writes=[gk])
            for c in range(2):
                P.op("tensor", lambda e, c=c, half=half, pa_=pa_, z=z: e.matmul(
                    pa_[:, 0:512], lhsT=ppT[z][:, c, :], rhs=wpp[:, c, half * 512:(half + 1) * 512], start=(c == 0), stop=(c == 1)),
                    reads=[("ppT", z), "wpp"], writes=[pk])

        def S2tail(t):
            z = t % 2
            h3 = t % 3
            hsl = [slice(0, 512), slice(512, 1024)]
            for half in range(2):
                (ga, gk), (pa_, pk) = GACC[half]
                hs = hsl[half]
                P.op("vector", lambda e, ga=ga, z=z, hs=hs: e.tensor_tensor(out=gz[z][:, hs], in0=ga[:], in1=bpg[:, hs], op=ALU.add),
                     reads=[gk, "bpg"], writes=[("gz", z, half)])
                P.op("scalar", lambda e, z=z, hs=hs: e.activation(out=gz[z][:, hs], in_=gz[z][:, hs], func=AF.Tanh, scale=0.5),
                     reads=[("gz", z, half)], writes=[("gz", z, half)])
            for half in range(2):
                (ga, gk), (pa_, pk) = GACC[half]
                hs = hsl[half]
                P.op("vector", lambda e, pa_=pa_, z=z, hs=hs: e.scalar_tensor_tensor(out=gz[z][:, hs], in0=gz[z][:, hs], scalar=1.0, in1=pa_[:, 0:512],
                                                                                    op0=ALU.add, op1=ALU.mult), reads=[("gz", z, half), pk], writes=[("gz", z, half)])
                P.op("vector", lambda e, z=z, hs=hs, h3=h3: e.scalar_tensor_tensor(out=ob[z][:, hs], in0=gz[z][:, hs], scalar=0.5, in1=hb[h3][:, hs],
                                                                                  op0=ALU.mult, op1=ALU.add), reads=[("gz", z, half), ("hb", h3)], writes=[("ob", z, half)])

        loads4(0)
        loads4(1)
        S1a(0)
        S1b(0)
        for t in range(NT):
            z = t % 2
            if t + 2 < NT:
                loads4(t + 2)
            if t + 1 < NT:
                S1a(t + 1)
            S2mm(t, 0)
            S2mm(t, 1)
            S2tail(t)
            if t + 1 < NT:
                S1b(t + 1)
            P.op("sync", lambda e, t=t, z=z: e.dma_start(out=out_d.ap()[t * 128:(t + 1) * 128, :], in_=ob[z][:]),
                 reads=[("ob", z, 0), ("ob", z, 1)], dma=True, is_out=True)
    if 4 in phases:
        phase4()
    P.emit()
    return nc, P


def _sel_tables():
    f = np.arange(128)[:, None, None]
    j = np.arange(8)[None, :, None]
    m = np.arange(128)[None, None, :]
    hit = ((m // 8) == (2 * j + f // 64)).astype(np.float32)
    selA = (hit / 64.0).reshape(128, 8 * 128).astype(ml_dtypes.bfloat16)
    selB = (hit.transpose(2, 1, 0) / 8.0).reshape(128, 8 * 128).astype(ml_dtypes.bfloat16)
    return np.ascontiguousarray(selA), np.ascontiguousarray(selB)


def _host_layout(inp):
    f = lambda a: np.ascontiguousarray(np.asarray(a, dtype=np.float32))
    bf = ml_dtypes.bfloat16
    b_in = f(inp["b_in"])[0]
    rel = f(inp["rel_bias"])[0]
    jj = np.arange(5)[::-1][:, None, None]
    kk = np.arange(128)[None, :, None]
    qq = np.arange(128)[None, None, :]
    dist = qq - kk + 128 * (4 - jj)
    idx = np.clip(dist, -63, 256) + 63
    cdiff = (qq // 64) - (kk // 64) + 2 * (4 - jj)
    mask = ((cdiff >= 0) & (cdiff <= 8)).astype(np.float32)
    rbT = rel[:, idx]
    rbT = np.ascontiguousarray(rbT.transpose(2, 0, 1, 3)).reshape(128, NH * 5 * 128)
    maskT = np.ascontiguousarray(mask.transpose(1, 0, 2)).reshape(128, 5 * 128)
    cwv = f(inp["conv_w"])[0]
    cw = np.ascontiguousarray(cwv.reshape(3, 4, 128).transpose(2, 1, 0)).reshape(128, 12)
    cb = np.ascontiguousarray(f(inp["conv_b"])[0].reshape(4, 128).T)
    gq = f(inp["g_q"])[0]
    gk = f(inp["g_k"])[0]
    shared = {
        "g_mix": f(inp["g_mix"]),
        "w_in": f(inp["w_in"])[0],
        "bcol": np.ascontiguousarray(b_in.reshape(40, 128).T),
        "gqk": np.ascontiguousarray(np.stack([np.tile(gq, 2), np.tile(gk, 2)], axis=1)),
        "bv": np.ascontiguousarray(b_in[1024:1536].reshape(1, 512)),
        "rbT": rbT, "maskT": maskT, "cw": cw, "cb": cb,
        "w_pa": f(inp["w_pa"])[0], "w_pc": f(inp["w_pc"])[0], "w_o": f(inp["w_o"])[0],
        "g_ffn": f(inp["g_ffn"]),
        "w_rg": np.ascontiguousarray(np.concatenate([f(inp["w_group"])[0], f(inp["w_router"])[0]], axis=1)),
        "b_rg": np.ascontiguousarray(np.concatenate([f(inp["b_group"])[0], f(inp["b_router"])[0]])[None, :]),
        "w1": f(inp["w1"])[0], "w3": f(inp["w3"])[0], "w2": f(inp["w2"])[0],
        "g_ple": f(inp["g_ple"]), "w_pg": f(inp["w_ple_gate"])[0], "b_pg": f(inp["b_ple_gate"]),
        "w_pp": f(inp["w_ple_proj"])[0],
        "ident": np.eye(128, dtype=np.float32).astype(bf),
        "utri": np.triu(np.ones((128, 128), np.float32), 1).astype(bf),
        "ones": np.ones((128, 128), np.float32).astype(bf),
        "bdiag": (np.kron(np.eye(2, dtype=np.float32), np.ones((64, 64), np.float32)) / 64.0).astype(bf),
        "ecap": np.ascontiguousarray(np.broadcast_to((np.arange(NE, dtype=np.float32) * CAP)[None, :], (128, NE))),
        "selA": _sel_tables()[0], "selB": _sel_tables()[1],
    }
    x = f(inp["x"])
    p = f(inp["p"])[0]
    maps = []
    for c in range(NCORES):
        m = dict(shared)
        m["x"] = x[c]
        m["p"] = p[c]
        maps.append(m)
    return maps


_CACHE = {}


def kernel(**inputs):
    if "nc" not in _CACHE:
        _CACHE["nc"] = build(debug=False)[0]
    nc = _CACHE["nc"]
    maps = _host_layout(inputs)
    res = run_bass_kernel_spmd(nc, maps, core_ids=list(range(NCORES)))
    out = np.stack([np.asarray(res.results[c]["out"], dtype=np.float32) for c in range(NCORES)], axis=0)
    return out
```

```python
import numpy as np
import ml_dtypes
import concourse.bass as bass
import concourse.mybir as mybir
from concourse.bass_utils import run_bass_kernel_spmd

F32 = mybir.dt.float32
BF16 = mybir.dt.bfloat16
I32 = mybir.dt.int32
ALU = mybir.AluOpType
AF = mybir.ActivationFunctionType
AX = mybir.AxisListType

NCORES = 8
S = 4096
D = 1024
NT = S // 128
BT = 512
NB = S // BT
NH = 8
DH = 64
NE = 32
CAP = 384
NSLOT = NE * CAP
KR = 12
EPS = 1e-6

ENGS = ("sync", "scalar", "vector", "gpsimd", "tensor")
NDMASEM = 24
SAME_ENG_WINDOW = 10 ** 9


class Op:
    __slots__ = ("idx", "eng", "fn", "reads", "writes", "dma", "deps", "sig",
                 "sem", "val", "clock", "epos", "barrier")


class Prog:
    def __init__(self, nc):
        self.nc = nc
        self.ops = []
        self.last_w = {}
        self.readers = {}
        self.out_ops = []
        self.last_barrier = None
        self.since_barrier = []

    def op(self, eng, fn, reads=(), writes=(), dma=False, is_out=False):
        o = Op()
        o.idx = len(self.ops)
        o.eng = eng
        o.fn = fn
        o.dma = dma
        o.barrier = False
        o.reads = tuple(reads)
        o.writes = tuple(writes)
        deps = set()
        for k in o.reads:
            w = self.last_w.get(k)
            if w is not None:
                deps.add(w)
        for k in o.writes:
            w = self.last_w.get(k)
            if w is not None:
                deps.add(w)
            for r in self.readers.get(k, ()):
                deps.add(r)
        for k in o.writes:
            self.last_w[k] = o.idx
            self.readers[k] = []
        for k in o.reads:
            if k not in o.writes:
                self.readers.setdefault(k, []).append(o.idx)
        if self.last_barrier is not None:
            deps.add(self.last_barrier)
        deps.discard(o.idx)
        o.deps = sorted(deps)
        o.sig = False
        self.ops.append(o)
        self.since_barrier.append(o.idx)
        if is_out:
            self.out_ops.append(o.idx)
        return o.idx

    def barrier(self):
        o = Op()
        o.idx = len(self.ops)
        o.eng = "sync"
        o.fn = "BARRIER"
        o.dma = False
        o.barrier = True
        o.reads = ()
        o.writes = ()
        last = {}
        deps = []
        for i in self.since_barrier:
            p = self.ops[i]
            if p.dma:
                deps.append(i)
            else:
                last[p.eng] = i
        deps.extend(last.values())
        if self.last_barrier is not None:
            deps.append(self.last_barrier)
        o.deps = sorted(set(deps))
        o.sig = True
        self.ops.append(o)
        self.last_barrier = o.idx
        self.since_barrier = []
        self.last_w = {}
        self.readers = {}

    def emit(self):
        nc = self.nc
        ops = self.ops
        epos = {e: 0 for e in ENGS}
        for o in ops:
            o.epos = epos[o.eng]
            epos[o.eng] += 1
        fin = Op()
        fin.idx = len(ops)
        fin.eng = "sync"
        fin.fn = None
        fin.dma = False
        fin.barrier = False
        fin.reads = ()
        fin.writes = ()
        fin.deps = list(self.out_ops)
        fin.sig = False
        fin.epos = epos["sync"]
        ops = ops + [fin]
        for o in ops:
            nd = []
            for d in o.deps:
                do = ops[d]
                if do.eng == o.eng and not do.dma and not o.barrier:
                    if o.eng == "tensor" and not o.dma:
                        continue
                    if o.dma:
                        pass
                    elif o.epos - do.epos > SAME_ENG_WINDOW:
                        continue
                nd.append(d)
            o.deps = nd
            for d in nd:
                ops[d].sig = True
        sems = {}
        dma_engs = set(o.eng for o in ops if o.dma)
        for e in ENGS:
            sems[("c", e)] = nc.alloc_semaphore("c_" + e)
            if e in dma_engs:
                for i in range(NDMASEM):
                    sems[("d", e, i)] = nc.alloc_semaphore("d_%s_%d" % (e, i))
        ccount = {e: 0 for e in ENGS}
        dcount = {e: 0 for e in ENGS}
        dma_prev = {}
        for o in ops:
            if o.dma:
                k = dcount[o.eng]
                dcount[o.eng] += 1
                slot = k % NDMASEM
                o.sem = ("d", o.eng, slot)
                o.val = 16 * (k // NDMASEM + 1)
                prev = dma_prev.get((o.eng, slot))
                if prev is not None and prev not in o.deps:
                    o.deps.append(prev)
                dma_prev[(o.eng, slot)] = o.idx
            elif o.sig:
                ccount[o.eng] += 1
                o.sem = ("c", o.eng)
                o.val = ccount[o.eng]
            else:
                o.sem = None
                o.val = 0
        known = {e: {} for e in ENGS}
        streams = {e: [] for e in ENGS}
        for o in ops:
            kn = known[o.eng]
            wm = {}
            for d in sorted(o.deps, reverse=True):
                do = ops[d]
                if kn.get(do.sem, 0) >= do.val:
                    continue
                if wm.get(do.sem, 0) < do.val:
                    wm[do.sem] = do.val
                for s, v in do.clock.items():
                    if kn.get(s, 0) < v:
                        kn[s] = v
            o.clock = dict(kn)
            if o.sem is not None:
                o.clock[o.sem] = o.val
            streams[o.eng].append((o, list(wm.items())))
        self.n_waits = sum(len(w) for st in streams.values() for _, w in st)
        self.counts = (dict(ccount), dict(dcount))

        def run_stream(eng_name):
            def body(eng):
                for o, waits in streams[eng_name]:
                    for s, v in waits:
                        eng.wait_ge(sems[s], v)
                    if o.fn is None:
                        continue
                    if o.barrier:
                        eng.sem_inc(sems[o.sem], 1)
                        continue
                    ins = o.fn(eng)
                    if o.sem is not None:
                        ins.then_inc(sems[o.sem], 16 if o.dma else 1)
            return body

        with nc.Block() as block:
            for e in ENGS:
                if streams[e]:
                    getattr(block, e)(run_stream(e))


class SBAlloc:
    LO = 16512
    HI = 229344

    def __init__(self, nc):
        self.nc = nc
        self.cur = self.LO
        self.n = 0

    def alloc(self, name, shape, dt):
        esz = {F32: 4, BF16: 2, I32: 4}[dt]
        nbytes = esz
        for s in shape[1:]:
            nbytes *= s
        off = (self.cur + 31) // 32 * 32
        assert off + nbytes <= self.HI, "SBUF overflow at %s: need %d have %d" % (name, nbytes, self.HI - off)
        self.n += 1
        t = self.nc.alloc_sbuf_tensor_at("%s_%d" % (name, self.n), list(shape), dt, offset=off)
        self.cur = off + nbytes
        self.off = getattr(self, "off", {})
        self.off[name] = off
        return t

    def alloc_alias(self, name, shape, dt, of):
        self.n += 1
        return self.nc.alloc_sbuf_tensor_at("%s_%d" % (name, self.n), list(shape), dt, offset=self.off[of])

    def mark(self):
        return self.cur

    def reset(self, m):
        self.cur = m


def bc_last(ap, n):
    shp = list(ap.shape)
    return ap.unsqueeze(len(shp)).broadcast_to(shp + [n])


def bc_mid(ap, n):
    shp = list(ap.shape)
    return ap.unsqueeze(1).broadcast_to([shp[0], n] + shp[1:])


def build(debug=False, phases=(1, 2, 3, 4)):
    nc = bass.Bass("TRN2", target_bir_lowering=False)
    P = Prog(nc)
    sb = SBAlloc(nc)

    def din(name, shape, dt=F32):
        return nc.dram_tensor(name, list(shape), dt, kind="ExternalInput")

    def dscr(name, shape, dt):
        return nc.dram_tensor(name, list(shape), dt, kind="ExternalOutput" if debug else "Internal")

    x_d = din("x", [S, D])
    p_d = din("p", [S, 256])
    gmix_d = din("g_mix", [1, D])
    win_d = din("w_in", [D, 5120])
    bcol_d = din("bcol", [128, 40])
    gqk_d = din("gqk", [128, 2])
    bv_d = din("bv", [1, 512])
    rbT_d = din("rbT", [128, NH * 5 * 128])
    maskT_d = din("maskT", [128, 5 * 128])
    cw_d = din("cw", [128, 12])
    cb_d = din("cb", [128, 4])
    wpa_d = din("w_pa", [512, D])
    wpc_d = din("w_pc", [512, D])
    wo_d = din("w_o", [D, D])
    gffn_d = din("g_ffn", [1, D])
    wrg_d = din("w_rg", [D, 36])
    brg_d = din("b_rg", [1, 36])
    w1_d = din("w1", [NE, D, 512])
    w3_d = din("w3", [NE, D, 512])
    w2_d = din("w2", [NE, 512, D])
    gple_d = din("g_ple", [1, D])
    wpg_d = din("w_pg", [D, D])
    bpg_d = din("b_pg", [1, D])
    wpp_d = din("w_pp", [256, D])
    ident_d = din("ident", [128, 128], BF16)
    utri_d = din("utri", [128, 128], BF16)
    ones_d = din("ones", [128, 128], BF16)
    bdiag_d = din("bdiag", [128, 128], BF16)
    ecap_d = din("ecap", [128, NE])
    selA_d = din("selA", [128, 8 * 128], BF16)
    selB_d = din("selB", [128, 8 * 128], BF16)
    out_d = nc.dram_tensor("out", [S, D], F32, kind="ExternalOutput")

    nT_d = dscr("nT_s", [D, S], BF16)
    yaT_d = dscr("yaT_s", [512, S], BF16)
    ycT_d = dscr("ycT_s", [512, S], BF16)
    h_d = dscr("h_s", [S, D], F32)
    xs_d = dscr("xs_s", [NSLOT + 128, D], BF16)
    ys_d = dscr("ys_s", [NSLOT + 128, D], BF16)
    if debug:
        dtab_o = nc.dram_tensor("dtab_o", [128, NT * 2], I32, kind="ExternalOutput")
        wtab_o = nc.dram_tensor("wtab_o", [128, NT * 2], F32, kind="ExternalOutput")

    pT = nc.alloc_psum_tensor("pT", [128, 8, 128], BF16)
    A0 = nc.alloc_psum_tensor("A0", [128, 512], F32)
    A1 = nc.alloc_psum_tensor("A1", [128, 512], F32)
    A2 = nc.alloc_psum_tensor("A2", [128, 512], F32)
    B0 = nc.alloc_psum_tensor("B0", [128, 1024], F32)
    B1 = nc.alloc_psum_tensor("B1", [128, 1024], F32)

    ident = sb.alloc("ident", [128, 128], BF16)
    utri = sb.alloc("utri", [128, 128], BF16)
    ones = sb.alloc("ones", [128, 128], BF16)
    bdiag = sb.alloc("bdiag", [128, 128], BF16)
    ecap = sb.alloc("ecap", [128, NE], F32)
    bcol = sb.alloc("bcol", [128, 40], F32)
    hbcol = sb.alloc("hbcol", [128, 40], F32)
    mhalf = sb.alloc("mhalf", [128, 8], F32)
    epsc = sb.alloc("epsc", [128, 8], F32)
    dtab = sb.alloc("dtab", [128, NT, 2], I32)
    wtab = sb.alloc("wtab", [128, NT, 2], F32)
    cnt = sb.alloc("cnt", [128, NE], F32)
    ss = sb.alloc("ss", [128, 8], F32)
    rs = sb.alloc("rs", [128, 8], F32)
    junk = sb.alloc("junk", [128, D], BF16)

    def ld(eng, dst, src, key):
        P.op(eng, lambda e: e.dma_start(out=dst, in_=src), writes=[key], dma=True)

    ld("sync", ident[:], ident_d.ap(), "ident")
    ld("sync", utri[:], utri_d.ap(), "utri")
    ld("sync", ones[:], ones_d.ap(), "ones")
    ld("sync", bdiag[:], bdiag_d.ap(), "bdiag")
    ld("sync", ecap[:], ecap_d.ap(), "ecap")
    ld("sync", bcol[:], bcol_d.ap(), "bcol")
    P.op("vector", lambda e: e.tensor_scalar(out=hbcol[:], in0=bcol[:], scalar1=0.5, scalar2=None, op0=ALU.mult),
         reads=["bcol"], writes=["hbcol"])
    P.op("gpsimd", lambda e: e.memset(mhalf[:], -0.5), writes=["mhalf"])
    P.op("gpsimd", lambda e: e.memset(epsc[:], EPS), writes=["epsc"])
    P.op("gpsimd", lambda e: e.memset(cnt[:], 0.0), writes=["cnt"])
    P.op("gpsimd", lambda e: e.memset(wtab[:], 0.0), writes=["wtab"])

    nrm_ctr = [0]

    def rmsnorm_tile(src, g_bc, dst_bf, src_key, g_key, dst_key):
        i = nrm_ctr[0] % 8
        nrm_ctr[0] += 1
        ssk, rsk = ("ss", i), ("rs", i)
        P.op("scalar", lambda e: e.activation(out=junk[:], in_=src, func=AF.Square, accum_out=ss[:, i:i + 1]),
             reads=[src_key], writes=["junk", ssk])
        P.op("vector", lambda e: e.tensor_scalar(out=rs[:, i:i + 1], in0=ss[:, i:i + 1], scalar1=1.0 / D, scalar2=EPS,
                                                  op0=ALU.mult, op1=ALU.add), reads=[ssk], writes=[rsk])
        P.op("gpsimd", lambda e: e.tensor_tensor(out=rs[:, i:i + 1], in0=rs[:, i:i + 1], in1=mhalf[:, 0:1], op=ALU.pow),
             reads=[rsk, "mhalf"], writes=[rsk])
        P.op("vector", lambda e: e.scalar_tensor_tensor(out=dst_bf, in0=src, scalar=rs[:, i:i + 1], in1=g_bc,
                                                         op0=ALU.mult, op1=ALU.mult),
             reads=[src_key, rsk, g_key], writes=[dst_key])

    def transpose_tile(src_bf, nchunk, dst, src_key, dst_key, evac="scalar", interleave=0):
        for c in range(nchunk):
            if interleave:
                src_c = src_bf.rearrange("t (p c) -> t c p", c=interleave)[:, c, :]
            else:
                src_c = src_bf[:, c * 128:(c + 1) * 128]
            P.op("tensor", lambda e, c=c, src_c=src_c: e.transpose(out=pT[:, c, :], in_=src_c, identity=ident[:]),
                 reads=(list(src_key) if isinstance(src_key, list) else [src_key]) + ["ident"], writes=[("pT", c)])
        if evac == "scalar":
            P.op("scalar", lambda e: e.activation(out=dst, in_=pT[:, 0:nchunk, :], func=AF.Copy),
                 reads=[("pT", c) for c in range(nchunk)], writes=[dst_key])
        else:
            P.op("vector", lambda e: e.tensor_copy(out=dst, in_=pT[:, 0:nchunk, :]),
                 reads=[("pT", c) for c in range(nchunk)], writes=[dst_key])

    _breg = {}

    def breg(e):
        if "r" not in _breg:
            _breg["r"] = e.to_reg(NSLOT - 1)
        return _breg["r"]

    base_mark = sb.mark()

    def phase1():
        Wa = sb.alloc("Wa", [128, 8, 3072], BF16)
        gmix = sb.alloc("gmix", [128, D], F32)
        gqk = sb.alloc("gqk", [128, 2], F32)
        bvb = sb.alloc("bvb", [128, 512], F32)
        cw = sb.alloc("cw", [128, 12], F32)
        cb = sb.alloc("cb", [128, 4], F32)
        expB = sb.alloc("expB", [128, NH, 5, 128], BF16)
        maskT = sb.alloc("maskT", [128, 5, 128], F32)
        kring = sb.alloc("kring", [128, 4, KR * 128], BF16)
        vring = sb.alloc("vring", [128, KR, NH, 65], BF16)
        xt = [sb.alloc("xt%d" % i, [128, D], F32) for i in range(2)]
        nb = [sb.alloc("nb%d" % i, [128, D], BF16) for i in range(2)]
        nTb = [sb.alloc("nTb%d" % i, [128, 8, BT], BF16) for i in range(2)]
        qT = [sb.alloc("qT%d" % i, [128, 4, BT], BF16) for i in range(2)]
        zq = [sb.alloc("zq%d" % i, [128, BT], F32) for i in range(8)]
        sq = [sb.alloc("sq%d" % i, [128, BT], BF16) for i in range(3)]
        rs = sb.alloc("rs_all", [128, BT], F32)
        r1b = sb.alloc("r1b", [128, BT], BF16)
        selA = sb.alloc("selA", [128, 8, 128], BF16)
        selB = sb.alloc("selB", [128, 8, 128], BF16)
        gw = sb.alloc("gw", [128, 1], F32)
        us = [sb.alloc("us%d" % i, [128, BT], F32) for i in range(2)]
        t1 = [sb.alloc("t1%d" % i, [128, BT], F32) for i in range(2)]
        cu = sb.alloc("cu", [128, 4, BT + 2], F32)
        ycT = [sb.alloc("ycT%d" % i, [128, 4, BT], BF16) for i in range(2)]
        yaT = [sb.alloc("yaT%d" % i, [128, 4, BT], BF16) for i in range(2)]
        pt = [sb.alloc("pt%d" % i, [128, 4, 128], BF16) for i in range(6)]
        rden = [sb.alloc("rden%d" % i, [128, 4], F32) for i in range(2)]
        ya = sb.alloc("ya", [128, 4, 512], BF16)

        win_v = win_d.ap().rearrange("(c p) n -> p c n", p=128)
        for (c0_, c1_) in ((0, 1024), (1024, 1536), (1536, 3072)):
            for c in range(0, 8, 2):
                P.op("gpsimd", lambda e, c=c, c0_=c0_, c1_=c1_: e.dma_start(out=Wa[:, c:c + 2, c0_:c1_], in_=win_v[:, c:c + 2, c0_:c1_]),
                     writes=[("Wa", c, c0_), ("Wa", c + 1, c0_)], dma=True)
        zt = sb.alloc("zt", [128, 2 * D], BF16)
        P.op("gpsimd", lambda e: e.memset(zt[:], 0.0), writes=["zt"])
        NR = (NSLOT + 128) // 128
        xs_z = xs_d.ap().rearrange("(p r) d -> p (r d)", p=128)
        zchunks = [(r0, min(r0 + 2, NR)) for r0 in range(0, NR, 2)]

        def zero_fill(k):
            for (r0, r1_) in zchunks[k::NB]:
                P.op("sync", lambda e, r0=r0, r1_=r1_: e.dma_start(out=xs_z[:, r0 * D:r1_ * D], in_=zt[:, 0:(r1_ - r0) * D]),
                     reads=["zt"], writes=[("xs_zero", r0)], dma=True)

        ld("sync", gmix[:], gmix_d.ap().partition_broadcast(128), "gmix")
        ld("sync", gqk[:], gqk_d.ap(), "gqk")
        ld("sync", selA[:], selA_d.ap().rearrange("p (j m) -> p j m", j=8), "selA")
        ld("sync", selB[:], selB_d.ap().rearrange("p (j m) -> p j m", j=8), "selB")
        ld("sync", bvb[:], bv_d.ap().partition_broadcast(128), "bvb")
        P.op("vector", lambda e: e.tensor_tensor(out=gw[:], in0=gqk[:, 0:1], in1=gqk[:, 1:2], op=ALU.mult), reads=["gqk"], writes=["gw"])
        ld("sync", cw[:], cw_d.ap(), "cw")
        ld("sync", cb[:], cb_d.ap(), "cb")
        ld("sync", maskT[:], maskT_d.ap().rearrange("p (j q) -> p j q", j=5), "maskT")
        rb_v = rbT_d.ap().rearrange("p (h n) -> p h n", h=NH)
        stg = [sb.alloc_alias("stg0", [128, 640], F32, "zq0"), sb.alloc_alias("stg1", [128, 640], F32, "zq2")]
        for h in range(NH):
            st = stg[h % 2]
            sk = [("zq", 2 * (h % 2)), ("zq", 2 * (h % 2) + 1)]
            P.op("sync", lambda e, h=h, st=st: e.dma_start(out=st[:], in_=rb_v[:, h, :]), writes=sk, dma=True)
            P.op("scalar", lambda e, st=st: e.activation(out=st[:], in_=st[:], func=AF.Exp), reads=sk, writes=sk)
            P.op("vector", lambda e, h=h, st=st: e.tensor_tensor(
                out=expB[:, h, :, :], in0=st[:].rearrange("p (j q) -> p j q", j=5), in1=maskT[:], op=ALU.mult),
                reads=sk + ["maskT"], writes=[("expB", h)])
        P.op("gpsimd", lambda e: e.memset(vring[:], 1.0), writes=[("v", s_) for s_ in range(KR)])
        P.op("gpsimd", lambda e: e.memset(cu[:], 0.0), writes=[("cu", ct) for ct in range(4)] + [("cuh", ct) for ct in range(4)])

        nT_v = nT_d.ap().rearrange("(c p) t -> p c t", p=128)
        yaT_v = yaT_d.ap().rearrange("(c p) t -> p c t", p=128)
        ycT_v = ycT_d.ap().rearrange("(c p) t -> p c t", p=128)
        acc_rot = [0]
        ACC = [(A0, "A0"), (A1, "A1")]

        def next_acc():
            a = ACC[acc_rot[0] % 2]
            acc_rot[0] += 1
            return a

        def A_norm(b, tl):
            t = 4 * b + tl
            s2 = t % 2
            P.op("sync", lambda e, t=t, s2=s2: e.dma_start(out=xt[s2][:], in_=x_d.ap()[t * 128:(t + 1) * 128, :]),
                 writes=[("xt", s2)], dma=True)
            rmsnorm_tile(xt[s2][:], gmix[:], nb[s2][:], ("xt", s2), "gmix", ("nb", s2))

        def A_tr(b, tl):
            t = 4 * b + tl
            s2 = t % 2
            bb = b % 2
            transpose_tile(nb[s2], 8, nTb[bb][:, :, tl * 128:(tl + 1) * 128], ("nb", s2), ("nTb", bb, tl))
            if tl == 3:
                P.op("sync", lambda e, bb=bb, b=b: e.dma_start(out=nT_v[:, :, b * BT:(b + 1) * BT], in_=nTb[bb][:]),
                     reads=[("nTb", bb, q_) for q_ in range(4)], writes=[("nT_d", b)], dma=True)

        def secC(b):
            bb = b % 2
            tok0 = b * BT
            nkeys = [("nTb", bb, tl) for tl in range(4)]
            PACC = [(A0[:], "A0"), (A1[:], "A1"), (B0[:, 0:512], ("B0", 0))]
            prot = [0]

            def nacc():
                a = PACC[prot[0] % 3]
                prot[0] += 1
                return a

            def proj(j):
                acc, akey = nacc()
                for c in range(8):
                    P.op("tensor", lambda e, j=j, c=c, acc=acc: e.matmul(
                        acc, lhsT=Wa[:, c, j * 128:(j + 1) * 128], rhs=nTb[bb][:, c, :], start=(c == 0), stop=(c == 7)),
                        reads=[("Wa", c, 0)] + nkeys, writes=[akey])
                z = j % 3
                P.op("scalar", lambda e, j=j, acc=acc: e.activation(out=zq[j][:], in_=acc, func=AF.Identity, bias=bcol[:, j:j + 1]),
                     reads=[akey, "bcol"], writes=[("zq", j)])
                P.op("gpsimd", lambda e, z=z, j=j: e.tensor_tensor(out=sq[z][:], in0=zq[j][:], in1=zq[j][:], op=ALU.mult),
                     reads=[("zq", j)], writes=[("sq", z)])

            def msacc(j):
                z = j % 3
                P.op("tensor", lambda e, z=z, j=j: e.matmul(A2[:], lhsT=selA[:, j, :], rhs=sq[z][:], start=(j == 0), stop=(j == 7)),
                     reads=[("sq", z), "selA"], writes=["A2"])

            def vproj():
                for tl in range(4):
                    t = 4 * b + tl
                    sl = t % KR
                    acc, akey = nacc()
                    for c in range(8):
                        P.op("tensor", lambda e, c=c, tl=tl, acc=acc: e.matmul(
                            acc, lhsT=nTb[bb][:, c, tl * 128:(tl + 1) * 128], rhs=Wa[:, c, 1024:1536], start=(c == 0), stop=(c == 7)),
                            reads=[("Wa", c, 1024), ("nTb", bb, tl)], writes=[akey])
                    P.op("vector", lambda e, sl=sl, acc=acc: e.tensor_tensor(
                        out=vring[:, sl, :, 0:64], in0=acc.rearrange("p (h d) -> p h d", h=NH),
                        in1=bvb[:].rearrange("p (h d) -> p h d", h=NH), op=ALU.add),
                        reads=[akey, "bvb"], writes=[("v", sl)])

            def fin(j):
                acc, akey = nacc()
                P.op("tensor", lambda e, j=j, acc=acc: e.matmul(acc, lhsT=selB[:, j, :], rhs=r1b[:], start=True, stop=True),
                     reads=["selB", "r1b"], writes=[akey])
                if j < 4:
                    P.op("vector", lambda e, j=j, acc=acc: e.tensor_tensor(out=qT[bb][:, j, :], in0=zq[j][:], in1=acc, op=ALU.mult),
                         reads=[("zq", j), akey], writes=[("qT", bb, j)])
                else:
                    hp = j - 4
                    sl0 = (4 * b) % KR
                    P.op("vector", lambda e, hp=hp, j=j, sl0=sl0, acc=acc: e.scalar_tensor_tensor(
                        out=kring[:, hp, sl0 * 128:(sl0 + 4) * 128], in0=zq[j][:], scalar=gw[:, 0:1], in1=acc, op0=ALU.mult, op1=ALU.mult),
                        reads=[("zq", j), akey, "gw"], writes=[("k", hp, sl0 + q_) for q_ in range(4)])

            proj(0)
            for j in range(8):
                if j + 1 < 8:
                    proj(j + 1)
                msacc(j)
            P.op("scalar", lambda e: e.activation(out=rs[:], in_=A2[:], func=AF.Sqrt, bias=epsc[:, 0:1]), reads=["A2", "epsc"], writes=["rs"])
            P.op("vector", lambda e: e.reciprocal(out=rs[:], in_=rs[:]), reads=["rs"], writes=["rs"])
            P.op("scalar", lambda e: e.activation(out=r1b[:], in_=rs[:], func=AF.Copy), reads=["rs"], writes=["r1b"])
            vproj()
            for j in range(8):
                fin(j)
        def secC3(b):
            bb = b % 2
            tok0 = b * BT
            nkeys = [("nTb", bb, tl) for tl in range(4)]
            SETS = [((A0[:], ["A0"]), (A1[:], ["A1"]), (A2[:], ["A2"])),
                    ((B0[:, 0:512], [("B0", 0)]), (B0[:, 512:1024], [("B0", 4)]), (B1[:, 0:512], [("B1h", 0)]))]
            for ct in range(4):
                z = ct % 2
                (pu, ku), (pb, kb), (pc, kc) = SETS[ct % 2]
                for (dst, dkey, col0) in ((pu, ku, 1536), (pb, kb, 2048), (pc, kc, 2560)):
                    for c in range(8):
                        P.op("tensor", lambda e, c=c, dst=dst, col0=col0, ct=ct: e.matmul(
                            dst, lhsT=Wa[:, c, col0 + ct * 128:col0 + (ct + 1) * 128], rhs=nTb[bb][:, c, :],
                            start=(c == 0), stop=(c == 7)),
                            reads=[("Wa", c, 1536)] + nkeys, writes=dkey)
                ju, jb, jc = 12 + ct, 16 + ct, 20 + ct
                P.op("scalar", lambda e, z=z, ju=ju, pu=pu: e.activation(out=us[z][:], in_=pu, func=AF.Identity, bias=bcol[:, ju:ju + 1]),
                     reads=ku + ["bcol"], writes=[("us", z)])
                P.op("vector", lambda e, z=z, jc=jc, ct=ct, pc=pc: e.scalar_tensor_tensor(
                    out=cu[:, ct, 2:BT + 2], in0=pc, scalar=bcol[:, jc:jc + 1], in1=us[z][:], op0=ALU.add, op1=ALU.mult),
                    reads=kc + ["bcol", ("us", z)], writes=[("cu", ct)])
                P.op("scalar", lambda e, z=z, ct=ct: e.activation(out=t1[z][:], in_=cu[:, ct, 2:BT + 2], func=AF.Identity,
                                                               scale=cw[:, ct * 3 + 2:ct * 3 + 3], bias=cb[:, ct:ct + 1]),
                     reads=[("cu", ct), "cw", "cb"], writes=[("t1", z)])
                P.op("vector", lambda e, z=z, ct=ct: e.scalar_tensor_tensor(
                    out=t1[z][:], in0=cu[:, ct, 1:BT + 1], scalar=cw[:, ct * 3 + 1:ct * 3 + 2], in1=t1[z][:], op0=ALU.mult, op1=ALU.add),
                    reads=[("cu", ct), ("cuh", ct), "cw", ("t1", z)], writes=[("t1", z)])
                P.op("vector", lambda e, z=z, ct=ct: e.scalar_tensor_tensor(
                    out=t1[z][:], in0=cu[:, ct, 0:BT], scalar=cw[:, ct * 3:ct * 3 + 1], in1=t1[z][:], op0=ALU.mult, op1=ALU.add),
                    reads=[("cu", ct), ("cuh", ct), "cw", ("t1", z)], writes=[("t1", z)])
                P.op("vector", lambda e, z=z, jb=jb, ct=ct, pb=pb: e.scalar_tensor_tensor(
                    out=ycT[bb][:, ct, :], in0=pb, scalar=bcol[:, jb:jb + 1], in1=t1[z][:], op0=ALU.add, op1=ALU.mult),
                    reads=kb + ["bcol", ("t1", z)], writes=[("ycT", bb, ct)])
                P.op("vector", lambda e, ct=ct: e.tensor_copy(out=cu[:, ct, 0:2], in_=cu[:, ct, BT:BT + 2]),
                     reads=[("cu", ct)], writes=[("cuh", ct)])
            P.op("sync", lambda e, tok0=tok0: e.dma_start(out=ycT_v[:, :, tok0:tok0 + BT], in_=ycT[bb][:]),
                 reads=[("ycT", bb, ct) for ct in range(4)], writes=[("ycT_d", b)], dma=True)

        def secD(b):
            bb = b % 2
            tok0 = b * BT
            nxt = b + 1 < NB
            units = []
            for h in range(NH):
                for m in range(8):
                    kt = 4 * b - 4 + m
                    if kt < 0:
                        continue
                    units.append((h, m, kt, max(m - 4, 0), min(m, 3)))
            SPS = [(A0[:], "A0"), (A1[:], "A1"), (A2[:], "A2"), (B0[:, 0:512], ("B0", 0)), (B0[:, 512:1024], ("B0", 4))]
            LA = 4

            def QKEXP(u):
                h, m, kt, tlo, thi = units[u]
                hp, r0 = h // 2, (h % 2) * 64
                nq = thi - tlo + 1
                sp, sk = SPS[u % 5]
                pz = u % 6
                sl = kt % KR
                P.op("tensor", lambda e, hp=hp, r0=r0, sl=sl, sp=sp, tlo=tlo, thi=thi: e.matmul(
                    sp[:, 0:(thi - tlo + 1) * 128], lhsT=kring[r0:r0 + 64, hp, sl * 128:(sl + 1) * 128],
                    rhs=qT[bb][r0:r0 + 64, hp, tlo * 128:(thi + 1) * 128], start=True, stop=True),
                    reads=[("k", hp, sl), ("qT", bb, hp)], writes=[sk])
                P.op("scalar", lambda e, pz=pz, sp=sp, nq=nq: e.activation(
                    out=pt[pz][:, 0:nq, :], in_=sp[:, 0:nq * 128].rearrange("p (j q) -> p j q", q=128), func=AF.Exp, scale=DH ** -0.5),
                    reads=[sk], writes=[("pt", pz)])
                rlo = 4 - m + tlo
                P.op("vector", lambda e, pz=pz, nq=nq, h=h, rlo=rlo: e.tensor_tensor(
                    out=pt[pz][:, 0:nq, :], in0=pt[pz][:, 0:nq, :], in1=expB[:, h, rlo:rlo + nq, :], op=ALU.mult),
                    reads=[("pt", pz), ("expB", h)], writes=[("pt", pz)])

            def PV(u):
                h, m, kt, tlo, thi = units[u]
                pz = u % 6
                sl = kt % KR
                hb2 = h % 2
                first = (u == 0) or units[u - 1][0] != h
                last_u = (u + 1 == len(units)) or units[u + 1][0] != h
                if first:
                    P.op("tensor", lambda e, hb2=hb2: e.matmul(
                        B1[:, hb2 * 512:hb2 * 512 + 260], lhsT=zt[:, 0:128], rhs=zt[:, 0:260], start=True, stop=False),
                        reads=["zt"], writes=[("B1h", hb2)])
                for tl in range(tlo, thi + 1):
                    c0 = hb2 * 512 + tl * 65
                    P.op("tensor", lambda e, h=h, tl=tl, tlo=tlo, sl=sl, pz=pz, c0=c0, fin=(last_u and tl == thi): e.matmul(
                        B1[:, c0:c0 + 65], lhsT=pt[pz][:, tl - tlo, :], rhs=vring[:, sl, h, :],
                        start=False, stop=fin),
                        reads=[("pt", pz), ("v", sl)], writes=[("B1h", hb2)])

            def FINH(h):
                hb2 = h % 2
                Bv = B1[:, hb2 * 512:hb2 * 512 + 260].rearrange("p (t d) -> p t d", d=65)
                P.op("vector", lambda e, hb2=hb2, Bv=Bv: e.reciprocal(out=rden[hb2][:], in_=Bv[:, :, 64]),
                     reads=[("B1h", hb2)], writes=[("rden", hb2)])
                P.op("vector", lambda e, hb2=hb2, Bv=Bv, h=h: e.tensor_tensor(
                    out=ya[:, :, h * 64:(h + 1) * 64], in0=Bv[:, :, 0:64], in1=bc_last(rden[hb2][:], 64), op=ALU.mult),
                    reads=[("B1h", hb2), ("rden", hb2)], writes=[("ya", h)])

            if nxt:
                A_norm(b + 1, 0)
            for u in range(min(LA, len(units))):
                QKEXP(u)
            secC3(b)
            zero_fill(b)
            for u in range(len(units)):
                if u + LA < len(units):
                    QKEXP(u + LA)
                PV(u)
                h = units[u][0]
                if u + 1 == len(units) or units[u + 1][0] != h:
                    FINH(h)
                    if nxt and h % 2 == 1:
                        tl = h // 2
                        A_tr(b + 1, tl)
                        if tl + 1 < 4:
                            A_norm(b + 1, tl + 1)
            for tl in range(4):
                transpose_tile(ya[:, tl, :], 4, yaT[bb][:, :, tl * 128:(tl + 1) * 128], [("ya", h) for h in range(NH)], ("yaT", bb, tl))
            P.op("sync", lambda e, tok0=tok0: e.dma_start(out=yaT_v[:, :, tok0:tok0 + BT], in_=yaT[bb][:]),
                 reads=[("yaT", bb, tl) for tl in range(4)], writes=[("yaT_d", b)], dma=True)

        for tl in range(4):
            A_norm(0, tl)
            A_tr(0, tl)
        for b in range(NB):
            secC(b)
            secD(b)
        P.barrier()
    if 1 in phases:
        phase1()
    sb.reset(base_mark)

    def phase2():
        Wg = sb.alloc("Wg", [128, 8, 2048], BF16)
        wpa = sb.alloc("wpa", [128, 4, D], BF16)
        wpc = sb.alloc("wpc", [128, 4, D], BF16)
        wo = sb.alloc("wo", [128, 8, D], BF16)
        wrg = sb.alloc("wrg", [128, 8, 36], BF16)
        brg = sb.alloc("brg", [128, 36], F32)
        gffn = sb.alloc("gffn", [128, D], F32)
        nTb = [sb.alloc("nTb%d" % i, [128, 8, BT], BF16) for i in range(2)]
        yaT = [sb.alloc("yaT%d" % i, [128, 4, BT], BF16) for i in range(2)]
        ycT = [sb.alloc("ycT%d" % i, [128, 4, BT], BF16) for i in range(2)]
        xt = [sb.alloc("xt%d" % i, [128, D], F32) for i in range(4)]
        tA = [sb.alloc("tA%d" % i, [128, BT], F32) for i in range(2)]
        tC = [sb.alloc("tC%d" % i, [128, BT], F32) for i in range(2)]
        mA = [sb.alloc("mA%d" % i, [128, BT], F32) for i in range(2)]
        mC = [sb.alloc("mC%d" % i, [128, BT], F32) for i in range(2)]
        mT = [sb.alloc("mT%d" % i, [128, 8, BT], BF16) for i in range(2)]
        ht = [sb.alloc("ht%d" % i, [128, D], F32) for i in range(3)]
        n2 = [sb.alloc("n2%d" % i, [128, 4, D], BF16) for i in range(2)]
        n2T = [sb.alloc("n2T%d" % i, [128, 8, 128], BF16) for i in range(2)]
        lg = sb.alloc("lg", [128, 4, 36], F32)
        gmax = sb.alloc("gmax", [128, 4], F32)
        gmask = sb.alloc("gmask", [128, 4, 4], F32)
        gex = sb.alloc("gex", [128, 4, 4], F32)
        gse = sb.alloc("gse", [128, 4], F32)
        pen = sb.alloc("pen", [128, 4, 4], F32)
        elm = sb.alloc("elm", [128, 4, 32], F32)
        elm2 = sb.alloc("elm2", [128, 4, 32], F32)
        m1 = sb.alloc("m1", [128, 4], F32)
        m2 = sb.alloc("m2", [128, 4], F32)
        mk1 = sb.alloc("mk1", [128, 4, 32], F32)
        mk2 = sb.alloc("mk2", [128, 4, 32], F32)
        Mb = sb.alloc("Mb", [128, 4, 32], BF16)
        dd = sb.alloc("dd", [128, 4], F32)
        ee = sb.alloc("ee", [128, 4], F32)
        rr = sb.alloc("rr", [128, 4], F32)
        wA = sb.alloc("wA", [128, 4], F32)
        wB = sb.alloc("wB", [128, 4], F32)
        pos = sb.alloc("pos", [128, 4, 32], F32)
        okm = sb.alloc("okm", [128, 4, 32], F32)
        slot = sb.alloc("slot", [128, 4, 32], F32)
        tmp = sb.alloc("tmp", [128, 4, 32], F32)
        dsel = sb.alloc("dsel", [128, 4, 2], F32)
        oksel = sb.alloc("oksel", [128, 4, 2], F32)

        win_v = win_d.ap().rearrange("(c p) n -> p c n", p=128)
        def wg_load(q4):
            for g0 in (0, 1024):
                c0_ = g0 + q4 * 256
                P.op("gpsimd", lambda e, c0_=c0_: e.dma_start(out=Wg[:, :, c0_:c0_ + 256], in_=win_v[:, :, 3072 + c0_:3072 + c0_ + 256]),
                     writes=[("Wg", c0_)], dma=True)

        wg_load(0)
        P.op("gpsimd", lambda e: e.dma_start(out=wpa[:], in_=wpa_d.ap().rearrange("(c p) n -> p c n", p=128)), writes=["wpa"], dma=True)
        P.op("gpsimd", lambda e: e.dma_start(out=wpc[:], in_=wpc_d.ap().rearrange("(c p) n -> p c n", p=128)), writes=["wpc"], dma=True)
        for q4 in range(1, 4):
            wg_load(q4)
        wo_v = wo_d.ap().rearrange("(c p) n -> p c n", p=128)
        for c in range(0, 8, 4):
            P.op("gpsimd", lambda e, c=c: e.dma_start(out=wo[:, c:c + 4, :], in_=wo_v[:, c:c + 4, :]), writes=[("wo", c)], dma=True)
        P.op("gpsimd", lambda e: e.dma_start(out=wrg[:], in_=wrg_d.ap().rearrange("(c p) n -> p c n", p=128)), writes=["wrg"], dma=True)
        ld("sync", brg[:], brg_d.ap().partition_broadcast(128), "brg")
        ld("sync", gffn[:], gffn_d.ap().partition_broadcast(128), "gffn")

        nT_v = nT_d.ap().rearrange("(c p) t -> p c t", p=128)
        yaT_v = yaT_d.ap().rearrange("(c p) t -> p c t", p=128)
        ycT_v = ycT_d.ap().rearrange("(c p) t -> p c t", p=128)
        wokeys = [("wo", 0), ("wo", 4)]
        xctr = [0]
        hctr = [0]

        def loads2(b):
            bb = b % 2
            tok0 = b * BT
            P.op("sync", lambda e, bb=bb, tok0=tok0: e.dma_start(out=nTb[bb][:], in_=nT_v[:, :, tok0:tok0 + BT]), writes=[("nTb", bb)], dma=True)
            P.op("sync", lambda e, bb=bb, tok0=tok0: e.dma_start(out=yaT[bb][:], in_=yaT_v[:, :, tok0:tok0 + BT]), writes=[("yaT", bb)], dma=True)
            P.op("sync", lambda e, bb=bb, tok0=tok0: e.dma_start(out=ycT[bb][:], in_=ycT_v[:, :, tok0:tok0 + BT]), writes=[("ycT", bb)], dma=True)

        def xload(t):
            P.op("sync", lambda e, t=t: e.dma_start(out=xt[t % 4][:], in_=x_d.ap()[t * 128:(t + 1) * 128, :]), writes=[("xt", t % 4)], dma=True)

        loads2(0)
        for t_ in range(3):
            xload(t_)
        def gates2(b):
            bb = b % 2
            tok0 = b * BT
            if b + 1 < NB:
                loads2(b + 1)
            for j in range(8):
                z = j % 2
                for (dst, dkey, col0) in ((A0, "A0", 0), (A1, "A1", 1024)):
                    for c in range(8):
                        P.op("tensor", lambda e, c=c, dst=dst, col0=col0, j=j, bb=bb: e.matmul(
                            dst[:], lhsT=Wg[:, c, col0 + j * 128:col0 + (j + 1) * 128], rhs=nTb[bb][:, c, :], start=(c == 0), stop=(c == 7)),
                            reads=[("Wg", col0 + (j // 2) * 256), ("nTb", bb)], writes=[dkey])
                for (dst, dkey, wsrc, wkey, asrc, akey) in ((A2, "A2", wpa, "wpa", yaT, "yaT"), (B0, ("B0", 0), wpc, "wpc", ycT, "ycT")):
                    for c in range(4):
                        P.op("tensor", lambda e, c=c, dst=dst, wsrc=wsrc, asrc=asrc, j=j, bb=bb: e.matmul(
                            dst[:, 0:512], lhsT=wsrc[:, c, j * 128:(j + 1) * 128], rhs=asrc[bb][:, c, :], start=(c == 0), stop=(c == 3)),
                            reads=[wkey, (akey, bb)], writes=[dkey])
                P.op("scalar", lambda e, z=z, j=j: e.activation(out=tA[z][:], in_=A0[:], func=AF.Tanh, scale=0.5, bias=hbcol[:, 24 + j:25 + j]),
                     reads=["A0", "hbcol"], writes=[("tA", z)])
                P.op("scalar", lambda e, z=z, j=j: e.activation(out=tC[z][:], in_=A1[:], func=AF.Tanh, scale=0.5, bias=hbcol[:, 32 + j:33 + j]),
                     reads=["A1", "hbcol"], writes=[("tC", z)])
                P.op("vector", lambda e, z=z: e.scalar_tensor_tensor(out=mA[z][:], in0=tA[z][:], scalar=1.0, in1=A2[:], op0=ALU.add, op1=ALU.mult),
                     reads=[("tA", z), "A2"], writes=[("mA", z)])
                P.op("vector", lambda e, z=z: e.scalar_tensor_tensor(out=mC[z][:], in0=tC[z][:], scalar=1.0, in1=B0[:, 0:512], op0=ALU.add, op1=ALU.mult),
                     reads=[("tC", z), ("B0", 0)], writes=[("mC", z)])
                P.op("gpsimd", lambda e, z=z, j=j, bb=bb: e.tensor_tensor(out=mT[bb][:, j, :], in0=mA[z][:], in1=mC[z][:], op=ALU.add),
                     reads=[("mA", z), ("mC", z)], writes=[("mT", bb, j)])

        def hsec2(b):
            bb = b % 2
            tok0 = b * BT
            mkeys = [("mT", bb, j) for j in range(8)]
            def hmm(tl):
                t = 4 * b + tl
                xs_ = t % 4
                hs_ = t % 3
                if t + 3 < NT:
                    xload(t + 3)
                HB = [(B1[:, 0:512], ("B1", 0)), (B1[:, 512:1024], ("B1", 1))] if tl % 2 == 0 else [(A0[:], "A0"), (A1[:], "A1")]
                for half in range(2):
                    hacc, hkey = HB[half]
                    for j in range(8):
                        P.op("tensor", lambda e, j=j, half=half, tl=tl, bb=bb, hacc=hacc: e.matmul(
                            hacc, lhsT=mT[bb][:, j, tl * 128:(tl + 1) * 128], rhs=wo[:, j, half * 512:(half + 1) * 512],
                            start=(j == 0), stop=(j == 7)),
                            reads=mkeys + wokeys, writes=[hkey])
                    P.op("vector", lambda e, half=half, xs_=xs_, hs_=hs_, hacc=hacc: e.scalar_tensor_tensor(
                        out=ht[hs_][:, half * 512:(half + 1) * 512], in0=hacc, scalar=0.5,
                        in1=xt[xs_][:, half * 512:(half + 1) * 512], op0=ALU.mult, op1=ALU.add),
                        reads=[hkey, ("xt", xs_)] + ([("ht", hs_)] if half == 1 else []), writes=[("ht", hs_)])
                P.op("sync", lambda e, t=t, hs_=hs_: e.dma_start(out=h_d.ap()[t * 128:(t + 1) * 128, :], in_=ht[hs_][:]),
                     reads=[("ht", hs_)], writes=[("h_d", t)], dma=True)

            def hnorm(tl):
                t = 4 * b + tl
                hs_ = t % 3
                rmsnorm_tile(ht[hs_][:], gffn[:], n2[bb][:, tl, :], ("ht", hs_), "gffn", ("n2", bb, tl))

            def htr(tl):
                t = 4 * b + tl
                z2 = t % 2
                transpose_tile(n2[bb][:, tl, :], 8, n2T[z2][:], ("n2", bb, tl), ("n2T", z2))
                for c in range(8):
                    P.op("tensor", lambda e, c=c, tl=tl, z2=z2: e.matmul(
                        B0[:, 512 + tl * 36:512 + (tl + 1) * 36], lhsT=n2T[z2][:, c, :], rhs=wrg[:, c, :], start=(c == 0), stop=(c == 7)),
                        reads=[("n2T", z2), "wrg"], writes=[("lgp", tl)])

            hmm(0)
            hnorm(0)
            for tl in range(4):
                if tl + 1 < 4:
                    hmm(tl + 1)
                htr(tl)
                if tl + 1 < 4:
                    hnorm(tl + 1)

        def rout2a(b):
            bb = b % 2
            tok0 = b * BT
            lgp = B0[:, 512:512 + 144].rearrange("p (t n) -> p t n", t=4)
            R = []

            def V(fn, reads, writes):
                P.op("vector", fn, reads=reads, writes=writes)

            V(lambda e: e.tensor_tensor(out=lg[:], in0=lgp, in1=bc_mid(brg[:], 4), op=ALU.add),
              [("lgp", tl) for tl in range(4)] + ["brg"], ["lg"])
            V(lambda e: e.tensor_reduce(out=gmax[:], in_=lg[:, :, 0:4], axis=AX.X, op=ALU.max), ["lg"], ["gmax"])
            V(lambda e: e.tensor_tensor(out=gmask[:], in0=lg[:, :, 0:4], in1=bc_last(gmax[:], 4), op=ALU.is_equal), ["lg", "gmax"], ["gmask"])
            V(lambda e: e.tensor_tensor(out=gex[:], in0=lg[:, :, 0:4], in1=bc_last(gmax[:], 4), op=ALU.subtract), ["lg", "gmax"], ["gex"])
            P.op("scalar", lambda e: e.activation(out=gex[:], in_=gex[:], func=AF.Exp), reads=["gex"], writes=["gex"])
            V(lambda e: e.tensor_reduce(out=gse[:], in_=gex[:], axis=AX.X, op=ALU.add), ["gex"], ["gse"])
            V(lambda e: e.reciprocal(out=gse[:], in_=gse[:]), ["gse"], ["gse"])
            V(lambda e: e.tensor_scalar(out=pen[:], in0=gmask[:], scalar1=1.0, scalar2=1e30, op0=ALU.subtract, op1=ALU.mult), ["gmask"], ["pen"])
            V(lambda e: e.tensor_tensor(out=elm[:].rearrange("p t (g k) -> p t g k", g=4),
                                        in0=lg[:, :, 4:36].rearrange("p t (g k) -> p t g k", g=4),
                                        in1=bc_last(pen[:], 8), op=ALU.add), ["lg", "pen"], ["elm"])
            V(lambda e: e.tensor_reduce(out=m1[:], in_=elm[:], axis=AX.X, op=ALU.max), ["elm"], ["m1"])
            V(lambda e: e.tensor_tensor(out=mk1[:], in0=elm[:], in1=bc_last(m1[:], 32), op=ALU.is_equal), ["elm", "m1"], ["mk1"])
            V(lambda e: e.scalar_tensor_tensor(out=elm2[:], in0=mk1[:], scalar=-1e30, in1=elm[:], op0=ALU.mult, op1=ALU.add), ["mk1", "elm"], ["elm2"])
            V(lambda e: e.tensor_reduce(out=m2[:], in_=elm2[:], axis=AX.X, op=ALU.max), ["elm2"], ["m2"])
            V(lambda e: e.tensor_tensor(out=mk2[:], in0=elm2[:], in1=bc_last(m2[:], 32), op=ALU.is_equal), ["elm2", "m2"], ["mk2"])
            V(lambda e: e.tensor_tensor(out=dd[:], in0=m2[:], in1=m1[:], op=ALU.subtract), ["m1", "m2"], ["dd"])
            P.op("scalar", lambda e: e.activation(out=ee[:], in_=dd[:], func=AF.Exp), reads=["dd"], writes=["ee"])
            V(lambda e: e.tensor_scalar(out=rr[:], in0=ee[:], scalar1=1.0, scalar2=None, op0=ALU.add), ["ee"], ["rr"])
            V(lambda e: e.reciprocal(out=rr[:], in_=rr[:]), ["rr"], ["rr"])
            V(lambda e: e.tensor_tensor(out=wA[:], in0=gse[:], in1=rr[:], op=ALU.mult), ["gse", "rr"], ["wA"])
            V(lambda e: e.tensor_tensor(out=wB[:], in0=wA[:], in1=ee[:], op=ALU.mult), ["wA", "ee"], ["wB"])
            V(lambda e: e.tensor_tensor(out=Mb[:], in0=mk1[:], in1=mk2[:], op=ALU.add), ["mk1", "mk2"], ["Mb"])

        def rout2b(b):
            bb = b % 2
            tok0 = b * BT

            def V(fn, reads, writes):
                P.op("vector", fn, reads=reads, writes=writes)

            for tl in range(4):
                P.op("tensor", lambda e, tl=tl: e.matmul(A0[:, tl * 32:(tl + 1) * 32], lhsT=utri[:], rhs=Mb[:, tl, :], start=True, stop=(tl == 0)),
                     reads=["utri", "Mb"], writes=["A0"])
                for t2 in range(tl):
                    P.op("tensor", lambda e, tl=tl, t2=t2: e.matmul(A0[:, tl * 32:(tl + 1) * 32], lhsT=ones[:], rhs=Mb[:, t2, :], start=False, stop=(t2 == tl - 1)),
                         reads=["ones", "Mb"], writes=["A0"])
            for tl in range(4):
                P.op("tensor", lambda e, tl=tl: e.matmul(A1[:, 0:32], lhsT=ones[:], rhs=Mb[:, tl, :], start=(tl == 0), stop=(tl == 3)),
                     reads=["ones", "Mb"], writes=["A1"])
            V(lambda e: e.tensor_tensor(out=pos[:], in0=A0[:, 0:128].rearrange("p (t n) -> p t n", t=4), in1=bc_mid(cnt[:], 4), op=ALU.add),
              ["A0", "cnt"], ["pos"])
            V(lambda e: e.tensor_tensor(out=cnt[:], in0=cnt[:], in1=A1[:, 0:32], op=ALU.add), ["A1", "cnt", "pos"], ["cnt"])
            V(lambda e: e.tensor_scalar(out=okm[:], in0=pos[:], scalar1=float(CAP), scalar2=None, op0=ALU.is_lt), ["pos"], ["okm"])
            V(lambda e: e.tensor_tensor(out=slot[:], in0=pos[:], in1=bc_mid(ecap[:], 4), op=ALU.add), ["pos", "ecap"], ["slot"])
            V(lambda e: e.tensor_scalar(out=tmp[:], in0=okm[:], scalar1=-1.0e6, scalar2=1.0e6, op0=ALU.mult, op1=ALU.add), ["okm"], ["tmp"])
            V(lambda e: e.tensor_tensor(out=slot[:], in0=slot[:], in1=tmp[:], op=ALU.add), ["slot", "tmp"], ["slot"])
            V(lambda e: e.tensor_scalar(out=slot[:], in0=slot[:], scalar1=float(NSLOT), scalar2=None, op0=ALU.min), ["slot"], ["slot"])
            for k, mk in ((0, mk1), (1, mk2)):
                V(lambda e, mk=mk: e.tensor_tensor(out=tmp[:], in0=mk[:], in1=slot[:], op=ALU.mult), ["mk1", "mk2", "slot"], ["tmp"])
                V(lambda e, k=k: e.tensor_reduce(out=dsel[:, :, k], in_=tmp[:], axis=AX.X, op=ALU.add), ["tmp"], [("dsel", k)])
                V(lambda e, mk=mk: e.tensor_tensor(out=tmp[:], in0=mk[:], in1=okm[:], op=ALU.mult), ["mk1", "mk2", "okm", ("dsel", k)], ["tmp"])
                V(lambda e, k=k: e.tensor_reduce(out=oksel[:, :, k], in_=tmp[:], axis=AX.X, op=ALU.add), ["tmp"], [("oksel", k)])
            tb = 4 * b
            V(lambda e, tb=tb: e.tensor_copy(out=dtab[:, tb:tb + 4, :], in_=dsel[:]), [("dsel", 0), ("dsel", 1)], [("dtab", b)])
            V(lambda e, tb=tb: e.tensor_tensor(out=wtab[:, tb:tb + 4, 0], in0=wA[:], in1=oksel[:, :, 0], op=ALU.mult), ["wA", ("oksel", 0)], [("wtab", b, 0)])
            V(lambda e, tb=tb: e.tensor_tensor(out=wtab[:, tb:tb + 4, 1], in0=wB[:], in1=oksel[:, :, 1], op=ALU.mult), ["wB", ("oksel", 1)], [("wtab", b, 1)])
            for tl in range(4):
                t = 4 * b + tl
                for k in range(2):
                    P.op("gpsimd", lambda e, t=t, k=k, tl=tl, bb=bb: e.indirect_dma_start(
                        out=xs_d[:, :], out_offset=bass.IndirectOffsetOnAxis(ap=dtab[:, t, k:k + 1], axis=0),
                        in_=n2[bb][:, tl, :], in_offset=None),
                        reads=[("n2", bb, tl), ("dtab", b)], writes=[("xs_d", t, k)], dma=True)

        gates2(0)
        for b in range(NB):
            hsec2(b)
            rout2a(b)
            if b + 1 < NB:
                gates2(b + 1)
            rout2b(b)
        if debug:
            P.op("sync", lambda e: e.dma_start(out=dtab_o.ap(), in_=dtab[:].rearrange("p t k -> p (t k)")),
                 reads=[("dtab", b) for b in range(NB)], dma=True, is_out=True)
            P.op("sync", lambda e: e.dma_start(out=wtab_o.ap(), in_=wtab[:].rearrange("p t k -> p (t k)")),
                 reads=[("wtab", b, k) for b in range(NB) for k in range(2)], dma=True, is_out=True)
        P.barrier()
    if 2 in phases:
        phase2()
    sb.reset(base_mark)

    TOP = SBAlloc.HI - 20 * 1024
    p4w = {"wpg": nc.alloc_sbuf_tensor_at("wpg_top", [128, 8, D], BF16, offset=TOP),
           "wpp": nc.alloc_sbuf_tensor_at("wpp_top", [128, 2, D], BF16, offset=TOP + 16 * 1024)}

    def p4_weight_loads():
        wpg_v = wpg_d.ap().rearrange("(c p) n -> p c n", p=128)
        for c in range(0, 8, 4):
            P.op("gpsimd", lambda e, c=c: e.dma_start(out=p4w["wpg"][:, c:c + 4, :], in_=wpg_v[:, c:c + 4, :]), writes=[("wpg", c)], dma=True)
        P.op("gpsimd", lambda e: e.dma_start(out=p4w["wpp"][:], in_=wpp_d.ap().rearrange("(c p) n -> p c n", p=128)), writes=["wpp"], dma=True)

    def phase3():
        NWB = 3
        w1b = [sb.alloc("w1b%d" % i, [128, 8, 512], BF16) for i in range(NWB)]
        w3b = [sb.alloc("w3b%d" % i, [128, 8, 512], BF16) for i in range(NWB)]
        w2b = [sb.alloc("w2b%d" % i, [128, 4, D], BF16) for i in range(NWB)]
        xr = [sb.alloc("xr%d" % i, [128, 3, D], BF16) for i in range(3)]
        xsT = [sb.alloc("xsT%d" % i, [128, 8, CAP], BF16) for i in range(2)]
        s1 = [sb.alloc("s1%d" % i, [128, CAP], F32) for i in range(2)]
        hdn = [sb.alloc("hdn%d" % i, [128, 4, CAP], BF16) for i in range(2)]
        yb = [sb.alloc("yb%d" % i, [128, D], BF16) for i in range(3)]
        HACC = [(A0, "A0", A1, "A1"), (A2, "A2", B0, ("B0", 0))]
        yctr = [0]
        P.op("gpsimd", lambda e: e.memset(yb[0][:], 0.0), writes=[("yb", 0, 0), ("yb", 0, 1)])
        P.op("sync", lambda e: e.dma_start(out=ys_d.ap()[NSLOT:NSLOT + 128, :], in_=yb[0][:]),
             reads=[("yb", 0, 0), ("yb", 0, 1)], writes=["ys_trash"], dma=True)
        def wload(ex, after=()):
            wb_ = ex % NWB
            if after:
                P.op("gpsimd", lambda e: e.memset(junk[:, 0:8], 0.0), reads=list(after), writes=["junk_g"])
            P.op("gpsimd", lambda e, ex=ex, wb_=wb_: e.dma_start(out=w1b[wb_][:], in_=w1_d.ap()[ex].rearrange("(p c) f -> p c f", c=8)),
                 writes=[("w1b", wb_)], dma=True)
            P.op("gpsimd", lambda e, ex=ex, wb_=wb_: e.dma_start(out=w3b[wb_][:], in_=w3_d.ap()[ex].rearrange("(p c) f -> p c f", c=8)),
                 writes=[("w3b", wb_)], dma=True)
            P.op("gpsimd", lambda e, ex=ex, wb_=wb_: e.dma_start(out=w2b[wb_][:], in_=w2_d.ap()[ex].rearrange("(c p) f -> p c f", p=128)),
                 writes=[("w2b", wb_)], dma=True)

        def xsload(ex):
            e3 = ex % 3
            P.op("sync", lambda e, ex=ex, e3=e3: e.dma_start(
                out=xr[e3][:], in_=xs_d.ap()[ex * CAP:(ex + 1) * CAP, :].rearrange("(r p) d -> p r d", p=128)),
                writes=[("xr", e3)], dma=True)

        def xsT_group(ex, r):
            eb = ex % 2
            e3 = ex % 3
            transpose_tile(xr[e3][:, r, :], 8, xsT[eb][:, :, r * 128:(r + 1) * 128], ("xr", e3), ("xsT", eb, r),
                           evac=("scalar" if r % 2 == 0 else "vector"), interleave=8)

        xsload(0)
        wload(0)
        xsload(1)
        wload(1, after=[("w1b", 0), ("w3b", 0), ("w2b", 0)])
        for r in range(3):
            xsT_group(0, r)
        for ex in range(NE):
            eb = ex % 2
            wb_ = ex % NWB
            if ex + 2 < NE:
                wload(ex + 2)
                xsload(ex + 2)
            if ex == 2:
                p4_weight_loads()
            xk = [("xsT", eb, r) for r in range(3)]
            for f in range(4):
                a1, k1, a3, k3 = HACC[f % 2]
                z = f % 2
                for (dst, dkey, wsrc, wkey) in ((a1, k1, w1b, "w1b"), (a3, k3, w3b, "w3b")):
                    for c in range(8):
                        P.op("tensor", lambda e, c=c, dst=dst, wsrc=wsrc, f=f, eb=eb, wb_=wb_: e.matmul(
                            dst[:, 0:CAP], lhsT=wsrc[wb_][:, c, f * 128:(f + 1) * 128], rhs=xsT[eb][:, c, :], start=(c == 0), stop=(c == 7)),
                            reads=[(wkey, wb_)] + xk, writes=[dkey])
                P.op("scalar", lambda e, a1=a1, z=z: e.activation(out=s1[z][:], in_=a1[:, 0:CAP], func=AF.Silu), reads=[k1], writes=[("s1", z)])
                P.op("vector", lambda e, a3=a3, z=z, f=f, eb=eb: e.tensor_tensor(out=hdn[eb][:, f, :], in0=s1[z][:], in1=a3[:, 0:CAP], op=ALU.mult),
                     reads=[("s1", z), k3], writes=[("hdn", eb, f)])
            hk_ = [("hdn", eb, f) for f in range(4)]
            for r in range(3):
                if ex + 1 < NE:
                    xsT_group(ex + 1, r)
                ys_ = yctr[0] % 3
                yctr[0] += 1
                for half in range(2):
                    for f in range(4):
                        P.op("tensor", lambda e, f=f, half=half, r=r, eb=eb, wb_=wb_: e.matmul(
                            B1[:, half * 512:(half + 1) * 512], lhsT=hdn[eb][:, f, r * 128:(r + 1) * 128], rhs=w2b[wb_][:, f, half * 512:(half + 1) * 512],
                            start=(f == 0), stop=(f == 3)),
                            reads=hk_ + [("w2b", wb_)], writes=[("B1", half)])
                    if half == 0:
                        P.op("scalar", lambda e, ys_=ys_: e.activation(out=yb[ys_][:, 0:512], in_=B1[:, 0:512], func=AF.Copy),
                             reads=[("B1", 0)], writes=[("yb", ys_, 0)])
                    else:
                        P.op("vector", lambda e, ys_=ys_: e.tensor_copy(out=yb[ys_][:, 512:1024], in_=B1[:, 512:1024]),
                             reads=[("B1", 1)], writes=[("yb", ys_, 1)])
                row0 = ex * CAP + r * 128
                P.op("sync", lambda e, row0=row0, ys_=ys_: e.dma_start(out=ys_d.ap()[row0:row0 + 128, :], in_=yb[ys_][:]),
                     reads=[("yb", ys_, 0), ("yb", ys_, 1)], writes=[("ys_d", ex, r)], dma=True)
        P.barrier()
    if 3 in phases:
        phase3()
    sb.reset(base_mark)

    def phase4():
        wpg, wpp = p4w["wpg"], p4w["wpp"]
        gple = sb.alloc("gple", [128, D], F32)
        bpg = sb.alloc("bpg", [128, D], F32)
        hb = [sb.alloc("hb%d" % i, [128, D], F32) for i in range(3)]
        y1 = [sb.alloc("y1%d" % i, [128, D], BF16) for i in range(3)]
        y2 = [sb.alloc("y2%d" % i, [128, D], BF16) for i in range(3)]
        pin = [sb.alloc("pin%d" % i, [128, 256], F32) for i in range(3)]
        pbf = [sb.alloc("pbf%d" % i, [128, 256], BF16) for i in range(2)]
        ppT = [sb.alloc("ppT%d" % i, [128, 2, 128], BF16) for i in range(2)]
        n3 = [sb.alloc("n3%d" % i, [128, D], BF16) for i in range(2)]
        n3T = [sb.alloc("n3T%d" % i, [128, 8, 128], BF16) for i in range(2)]
        gz = [sb.alloc("gz%d" % i, [128, D], F32) for i in range(2)]
        ob = [sb.alloc("ob%d" % i, [128, D], F32) for i in range(2)]
        ld("sync", gple[:], gple_d.ap().partition_broadcast(128), "gple")
        ld("sync", bpg[:], bpg_d.ap().partition_broadcast(128), "bpg")
        def loads4(t):
            h3 = t % 3
            P.op("sync", lambda e, t=t, h3=h3: e.dma_start(out=hb[h3][:], in_=h_d.ap()[t * 128:(t + 1) * 128, :]), writes=[("hb", h3)], dma=True)
            P.op("sync", lambda e, t=t, h3=h3: e.dma_start(out=pin[h3][:], in_=p_d.ap()[t * 128:(t + 1) * 128, :]), writes=[("pin", h3)], dma=True)
            for (yy, ykey, k) in ((y1, "y1", 0), (y2, "y2", 1)):
                P.op("gpsimd", lambda e, yy=yy, k=k, t=t, h3=h3: e.indirect_dma_start(
                    out=yy[h3][:, :], out_offset=None, in_=ys_d[:, :], in_offset=bass.IndirectOffsetOnAxis(ap=dtab[:, t, k:k + 1], axis=0)), reads=["dtab_all"], writes=[(ykey, h3)], dma=True)

        def S1a(t):
            h3 = t % 3
            z = t % 2
            P.op("vector", lambda e, h3=h3, t=t: e.scalar_tensor_tensor(out=hb[h3][:], in0=y1[h3][:], scalar=wtab[:, t, 0:1], in1=hb[h3][:],
                                                                       op0=ALU.mult, op1=ALU.add), reads=[("y1", h3), ("hb", h3)], writes=[("hb", h3)])
            P.op("vector", lambda e, h3=h3, t=t: e.scalar_tensor_tensor(out=hb[h3][:], in0=y2[h3][:], scalar=wtab[:, t, 1:2], in1=hb[h3][:],
                                                                       op0=ALU.mult, op1=ALU.add), reads=[("y2", h3), ("hb", h3)], writes=[("hb", h3)])
            rmsnorm_tile(hb[h3][:], gple[:], n3[z][:], ("hb", h3), "gple", ("n3", z))
            P.op("scalar", lambda e, z=z, h3=h3: e.activation(out=pbf[z][:], in_=pin[h3][:], func=AF.Copy), reads=[("pin", h3)], writes=[("pbf", z)])

        def S1b(t):
            z = t % 2
            transpose_tile(n3[z], 8, n3T[z][:], ("n3", z), ("n3T", z))
            transpose_tile(pbf[z], 2, ppT[z][:], ("pbf", z), ("ppT", z))

        GACC = [((A0, "A0"), (A2, "A2")), ((A1, "A1"), (B0, ("B0", 0)))]

        def S2mm(t, half):
            z = t % 2
            (ga, gk), (pa_, pk) = GACC[half]
            for c in range(8):
                P.op("tensor", lambda e, c=c, half=half, ga=ga, z=z: e.matmul(
                    ga[:], lhsT=n3T[z][:, c, :], rhs=wpg[:, c, half * 512:(half + 1) * 512], start=(c == 0), stop=(c == 7)),
                    reads=[("n3T", z), ("wpg", 0), ("wpg", 4)], writes=[gk])
            for c in range(2):
                P.op("tensor", lambda e, c=c, half=half, pa_=pa_, z=z: e.matmul(
                    pa_[:, 0:512], lhsT=ppT[z][:, c, :], rhs=wpp[:, c, half * 512:(half + 1) * 512], start=(c == 0), stop=(c == 1)),
                    reads=[("ppT", z), "wpp"], writes=[pk])

        def S2tail(t):
            z = t % 2
            h3 = t % 3
            hsl = [slice(0, 512), slice(512, 1024)]
            for half in range(2):
                (ga, gk), (pa_, pk) = GACC[half]
                hs = hsl[half]
                P.op("vector", lambda e, ga=ga, z=z, hs=hs: e.tensor_tensor(out=gz[z][:, hs], in0=ga[:], in1=bpg[:, hs], op=ALU.add),
                     reads=[gk, "bpg"], writes=[("gz", z, half)])
                P.op("scalar", lambda e, z=z, hs=hs: e.activation(out=gz[z][:, hs], in_=gz[z][:, hs], func=AF.Tanh, scale=0.5),
                     reads=[("gz", z, half)], writes=[("gz", z, half)])
            for half in range(2):
                (ga, gk), (pa_, pk) = GACC[half]
                hs = hsl[half]
                P.op("vector", lambda e, pa_=pa_, z=z, hs=hs: e.scalar_tensor_tensor(out=gz[z][:, hs], in0=gz[z][:, hs], scalar=1.0, in1=pa_[:, 0:512],
                                                                                    op0=ALU.add, op1=ALU.mult), reads=[("gz", z, half), pk], writes=[("gz", z, half)])
                P.op("vector", lambda e, z=z, hs=hs, h3=h3: e.scalar_tensor_tensor(out=ob[z][:, hs], in0=gz[z][:, hs], scalar=0.5, in1=hb[h3][:, hs],
                                                                                  op0=ALU.mult, op1=ALU.add), reads=[("gz", z, half), ("hb", h3)], writes=[("ob", z, half)])

        loads4(0)
        loads4(1)
        S1a(0)
        S1b(0)
        for t in range(NT):
            z = t % 2
            if t + 2 < NT:
                loads4(t + 2)
            if t + 1 < NT:
                S1a(t + 1)
            S2mm(t, 0)
            S2mm(t, 1)
            S2tail(t)
            if t + 1 < NT:
                S1b(t + 1)
            P.op("sync", lambda e, t=t, z=z: e.dma_start(out=out_d.ap()[t * 128:(t + 1) * 128, :], in_=ob[z][:]),
                 reads=[("ob", z, 0), ("ob", z, 1)], dma=True, is_out=True)
    if 4 in phases:
        phase4()
    P.emit()
    return nc, P


def _sel_tables():
    f = np.arange(128)[:, None, None]
    j = np.arange(8)[None, :, None]
    m = np.arange(128)[None, None, :]
    hit = ((m // 8) == (2 * j + f // 64)).astype(np.float32)
    selA = (hit / 64.0).reshape(128, 8 * 128).astype(ml_dtypes.bfloat16)
    selB = (hit.transpose(2, 1, 0) / 8.0).reshape(128, 8 * 128).astype(ml_dtypes.bfloat16)
    return np.ascontiguousarray(selA), np.ascontiguousarray(selB)


def _host_layout(inp):
    f = lambda a: np.ascontiguousarray(np.asarray(a, dtype=np.float32))
    bf = ml_dtypes.bfloat16
    b_in = f(inp["b_in"])[0]
    rel = f(inp["rel_bias"])[0]
    jj = np.arange(5)[::-1][:, None, None]
    kk = np.arange(128)[None, :, None]
    qq = np.arange(128)[None, None, :]
    dist = qq - kk + 128 * (4 - jj)
    idx = np.clip(dist, -63, 256) + 63
    cdiff = (qq // 64) - (kk // 64) + 2 * (4 - jj)
    mask = ((cdiff >= 0) & (cdiff <= 8)).astype(np.float32)
    rbT = rel[:, idx]
    rbT = np.ascontiguousarray(rbT.transpose(2, 0, 1, 3)).reshape(128, NH * 5 * 128)
    maskT = np.ascontiguousarray(mask.transpose(1, 0, 2)).reshape(128, 5 * 128)
    cwv = f(inp["conv_w"])[0]
    cw = np.ascontiguousarray(cwv.reshape(3, 4, 128).transpose(2, 1, 0)).reshape(128, 12)
    cb = np.ascontiguousarray(f(inp["conv_b"])[0].reshape(4, 128).T)
    gq = f(inp["g_q"])[0]
    gk = f(inp["g_k"])[0]
    shared = {
        "g_mix": f(inp["g_mix"]),
        "w_in": f(inp["w_in"])[0],
        "bcol": np.ascontiguousarray(b_in.reshape(40, 128).T),
        "gqk": np.ascontiguousarray(np.stack([np.tile(gq, 2), np.tile(gk, 2)], axis=1)),
        "bv": np.ascontiguousarray(b_in[1024:1536].reshape(1, 512)),
        "rbT": rbT, "maskT": maskT, "cw": cw, "cb": cb,
        "w_pa": f(inp["w_pa"])[0], "w_pc": f(inp["w_pc"])[0], "w_o": f(inp["w_o"])[0],
        "g_ffn": f(inp["g_ffn"]),
        "w_rg": np.ascontiguousarray(np.concatenate([f(inp["w_group"])[0], f(inp["w_router"])[0]], axis=1)),
        "b_rg": np.ascontiguousarray(np.concatenate([f(inp["b_group"])[0], f(inp["b_router"])[0]])[None, :]),
        "w1": f(inp["w1"])[0], "w3": f(inp["w3"])[0], "w2": f(inp["w2"])[0],
        "g_ple": f(inp["g_ple"]), "w_pg": f(inp["w_ple_gate"])[0], "b_pg": f(inp["b_ple_gate"]),
        "w_pp": f(inp["w_ple_proj"])[0],
        "ident": np.eye(128, dtype=np.float32).astype(bf),
        "utri": np.triu(np.ones((128, 128), np.float32), 1).astype(bf),
        "ones": np.ones((128, 128), np.float32).astype(bf),
        "bdiag": (np.kron(np.eye(2, dtype=np.float32), np.ones((64, 64), np.float32)) / 64.0).astype(bf),
        "ecap": np.ascontiguousarray(np.broadcast_to((np.arange(NE, dtype=np.float32) * CAP)[None, :], (128, NE))),
        "selA": _sel_tables()[0], "selB": _sel_tables()[1],
    }
    x = f(inp["x"])
    p = f(inp["p"])[0]
    maps = []
    for c in range(NCORES):
        m = dict(shared)
        m["x"] = x[c]
        m["p"] = p[c]
        maps.append(m)
    return maps


_CACHE = {}


def kernel(**inputs):
    if "nc" not in _CACHE:
        _CACHE["nc"] = build(debug=False)[0]
    nc = _CACHE["nc"]
    maps = _host_layout(inputs)
    res = run_bass_kernel_spmd(nc, maps, core_ids=list(range(NCORES)))
    out = np.stack([np.asarray(res.results[c]["out"], dtype=np.float32) for c in range(NCORES)], axis=0)
    return out
```

```python
import numpy as np
import ml_dtypes
import concourse.bass as bass
import concourse.mybir as mybir
from concourse.bass_utils import run_bass_kernel_spmd

F32 = mybir.dt.float32
BF16 = mybir.dt.bfloat16
I32 = mybir.dt.int32
ALU = mybir.AluOpType
AF = mybir.ActivationFunctionType
AX = mybir.AxisListType

NCORES = 8
S = 4096
D = 1024
NT = S // 128
BT = 512
NB = S // BT
NH = 8
DH = 64
NE = 32
CAP = 384
NSLOT = NE * CAP
KR = 12
EPS = 1e-6

ENGS = ("sync", "scalar", "vector", "gpsimd", "tensor")
NDMASEM = 24
SAME_ENG_WINDOW = 10 ** 9


class Op:
    __slots__ = ("idx", "eng", "fn", "reads", "writes", "dma", "deps", "sig",
                 "sem", "val", "clock", "epos", "barrier")


class Prog:
    def __init__(self, nc):
        self.nc = nc
        self.ops = []
        self.last_w = {}
        self.readers = {}
        self.out_ops = []
        self.last_barrier = None
        self.since_barrier = []

    def op(self, eng, fn, reads=(), writes=(), dma=False, is_out=False):
        o = Op()
        o.idx = len(self.ops)
        o.eng = eng
        o.fn = fn
        o.dma = dma
        o.barrier = False
        o.reads = tuple(reads)
        o.writes = tuple(writes)
        deps = set()
        for k in o.reads:
            w = self.last_w.get(k)
            if w is not None:
                deps.add(w)
        for k in o.writes:
            w = self.last_w.get(k)
            if w is not None:
                deps.add(w)
            for r in self.readers.get(k, ()):
                deps.add(r)
        for k in o.writes:
            self.last_w[k] = o.idx
            self.readers[k] = []
        for k in o.reads:
            if k not in o.writes:
                self.readers.setdefault(k, []).append(o.idx)
        if self.last_barrier is not None:
            deps.add(self.last_barrier)
        deps.discard(o.idx)
        o.deps = sorted(deps)
        o.sig = False
        self.ops.append(o)
        self.since_barrier.append(o.idx)
        if is_out:
            self.out_ops.append(o.idx)
        return o.idx

    def barrier(self):
        o = Op()
        o.idx = len(self.ops)
        o.eng = "sync"
        o.fn = "BARRIER"
        o.dma = False
        o.barrier = True
        o.reads = ()
        o.writes = ()
        last = {}
        deps = []
        for i in self.since_barrier:
            p = self.ops[i]
            if p.dma:
                deps.append(i)
            else:
                last[p.eng] = i
        deps.extend(last.values())
        if self.last_barrier is not None:
            deps.append(self.last_barrier)
        o.deps = sorted(set(deps))
        o.sig = True
        self.ops.append(o)
        self.last_barrier = o.idx
        self.since_barrier = []
        self.last_w = {}
        self.readers = {}

    def emit(self):
        nc = self.nc
        ops = self.ops
        epos = {e: 0 for e in ENGS}
        for o in ops:
            o.epos = epos[o.eng]
            epos[o.eng] += 1
        fin = Op()
        fin.idx = len(ops)
        fin.eng = "sync"
        fin.fn = None
        fin.dma = False
        fin.barrier = False
        fin.reads = ()
        fin.writes = ()
        fin.deps = list(self.out_ops)
        fin.sig = False
        fin.epos = epos["sync"]
        ops = ops + [fin]
        for o in ops:
            nd = []
            for d in o.deps:
                do = ops[d]
                if do.eng == o.eng and not do.dma and not o.barrier:
                    if o.eng == "tensor" and not o.dma:
                        continue
                    if o.dma:
                        pass
                    elif o.epos - do.epos > SAME_ENG_WINDOW:
                        continue
                nd.append(d)
            o.deps = nd
            for d in nd:
                ops[d].sig = True
        sems = {}
        dma_engs = set(o.eng for o in ops if o.dma)
        for e in ENGS:
            sems[("c", e)] = nc.alloc_semaphore("c_" + e)
            if e in dma_engs:
                for i in range(NDMASEM):
                    sems[("d", e, i)] = nc.alloc_semaphore("d_%s_%d" % (e, i))
        ccount = {e: 0 for e in ENGS}
        dcount = {e: 0 for e in ENGS}
        dma_prev = {}
        for o in ops:
            if o.dma:
                k = dcount[o.eng]
                dcount[o.eng] += 1
                slot = k % NDMASEM
                o.sem = ("d", o.eng, slot)
                o.val = 16 * (k // NDMASEM + 1)
                prev = dma_prev.get((o.eng, slot))
                if prev is not None and prev not in o.deps:
                    o.deps.append(prev)
                dma_prev[(o.eng, slot)] = o.idx
            elif o.sig:
                ccount[o.eng] += 1
                o.sem = ("c", o.eng)
                o.val = ccount[o.eng]
            else:
                o.sem = None
                o.val = 0
        known = {e: {} for e in ENGS}
        streams = {e: [] for e in ENGS}
        for o in ops:
            kn = known[o.eng]
            wm = {}
            for d in sorted(o.deps, reverse=True):
                do = ops[d]
                if kn.get(do.sem, 0) >= do.val:
                    continue
                if wm.get(do.sem, 0) < do.val:
                    wm[do.sem] = do.val
                for s, v in do.clock.items():
                    if kn.get(s, 0) < v:
                        kn[s] = v
            o.clock = dict(kn)
            if o.sem is not None:
                o.clock[o.sem] = o.val
            streams[o.eng].append((o, list(wm.items())))
        self.n_waits = sum(len(w) for st in streams.values() for _, w in st)
        self.counts = (dict(ccount), dict(dcount))

        def run_stream(eng_name):
            def body(eng):
                for o, waits in streams[eng_name]:
                    for s, v in waits:
                        eng.wait_ge(sems[s], v)
                    if o.fn is None:
                        continue
                    if o.barrier:
                        eng.sem_inc(sems[o.sem], 1)
                        continue
                    ins = o.fn(eng)
                    if o.sem is not None:
                        ins.then_inc(sems[o.sem], 16 if o.dma else 1)
            return body

        with nc.Block() as block:
            for e in ENGS:
                if streams[e]:
                    getattr(block, e)(run_stream(e))


class SBAlloc:
    LO = 16512
    HI = 229344

    def __init__(self, nc):
        self.nc = nc
        self.cur = self.LO
        self.n = 0

    def alloc(self, name, shape, dt):
        esz = {F32: 4, BF16: 2, I32: 4}[dt]
        nbytes = esz
        for s in shape[1:]:
            nbytes *= s
        off = (self.cur + 31) // 32 * 32
        assert off + nbytes <= self.HI, "SBUF overflow at %s: need %d have %d" % (name, nbytes, self.HI - off)
        self.n += 1
        t = self.nc.alloc_sbuf_tensor_at("%s_%d" % (name, self.n), list(shape), dt, offset=off)
        self.cur = off + nbytes
        self.off = getattr(self, "off", {})
        self.off[name] = off
        return t

    def alloc_alias(self, name, shape, dt, of):
        self.n += 1
        return self.nc.alloc_sbuf_tensor_at("%s_%d" % (name, self.n), list(shape), dt, offset=self.off[of])

    def mark(self):
        return self.cur

    def reset(self, m):
        self.cur = m


def bc_last(ap, n):
    shp = list(ap.shape)
    return ap.unsqueeze(len(shp)).broadcast_to(shp + [n])


def bc_mid(ap, n):
    shp = list(ap.shape)
    return ap.unsqueeze(1).broadcast_to([shp[0], n] + shp[1:])


def build(debug=False, phases=(1, 2, 3, 4)):
    nc = bass.Bass("TRN2", target_bir_lowering=False)
    P = Prog(nc)
    sb = SBAlloc(nc)

    def din(name, shape, dt=F32):
        return nc.dram_tensor(name, list(shape), dt, kind="ExternalInput")

    def dscr(name, shape, dt):
        return nc.dram_tensor(name, list(shape), dt, kind="ExternalOutput" if debug else "Internal")

    x_d = din("x", [S, D])
    p_d = din("p", [S, 256])
    gmix_d = din("g_mix", [1, D])
    win_d = din("w_in", [D, 5120])
    bcol_d = din("bcol", [128, 40])
    gqk_d = din("gqk", [128, 2])
    bv_d = din("bv", [1, 512])
    rbT_d = din("rbT", [128, NH * 5 * 128])
    maskT_d = din("maskT", [128, 5 * 128])
    cw_d = din("cw", [128, 12])
    cb_d = din("cb", [128, 4])
    wpa_d = din("w_pa", [512, D])
    wpc_d = din("w_pc", [512, D])
    wo_d = din("w_o", [D, D])
    gffn_d = din("g_ffn", [1, D])
    wrg_d = din("w_rg", [D, 36])
    brg_d = din("b_rg", [1, 36])
    w1_d = din("w1", [NE, D, 512])
    w3_d = din("w3", [NE, D, 512])
    w2_d = din("w2", [NE, 512, D])
    gple_d = din("g_ple", [1, D])
    wpg_d = din("w_pg", [D, D])
    bpg_d = din("b_pg", [1, D])
    wpp_d = din("w_pp", [256, D])
    ident_d = din("ident", [128, 128], BF16)
    utri_d = din("utri", [128, 128], BF16)
    ones_d = din("ones", [128, 128], BF16)
    bdiag_d = din("bdiag", [128, 128], BF16)
    ecap_d = din("ecap", [128, NE])
    selA_d = din("selA", [128, 8 * 128], BF16)
    selB_d = din("selB", [128, 8 * 128], BF16)
    out_d = nc.dram_tensor("out", [S, D], F32, kind="ExternalOutput")

    nT_d = dscr("nT_s", [D, S], BF16)
    yaT_d = dscr("yaT_s", [512, S], BF16)
    ycT_d = dscr("ycT_s", [512, S], BF16)
    h_d = dscr("h_s", [S, D], F32)
    xs_d = dscr("xs_s", [NSLOT + 128, D], BF16)
    ys_d = dscr("ys_s", [NSLOT + 128, D], BF16)
    if debug:
        dtab_o = nc.dram_tensor("dtab_o", [128, NT * 2], I32, kind="ExternalOutput")
        wtab_o = nc.dram_tensor("wtab_o", [128, NT * 2], F32, kind="ExternalOutput")

    pT = nc.alloc_psum_tensor("pT", [128, 8, 128], BF16)
    A0 = nc.alloc_psum_tensor("A0", [128, 512], F32)
    A1 = nc.alloc_psum_tensor("A1", [128, 512], F32)
    A2 = nc.alloc_psum_tensor("A2", [128, 512], F32)
    B0 = nc.alloc_psum_tensor("B0", [128, 1024], F32)
    B1 = nc.alloc_psum_tensor("B1", [128, 1024], F32)

    ident = sb.alloc("ident", [128, 128], BF16)
    utri = sb.alloc("utri", [128, 128], BF16)
    ones = sb.alloc("ones", [128, 128], BF16)
    bdiag = sb.alloc("bdiag", [128, 128], BF16)
    ecap = sb.alloc("ecap", [128, NE], F32)
    bcol = sb.alloc("bcol", [128, 40], F32)
    hbcol = sb.alloc("hbcol", [128, 40], F32)
    mhalf = sb.alloc("mhalf", [128, 8], F32)
    epsc = sb.alloc("epsc", [128, 8], F32)
    dtab = sb.alloc("dtab", [128, NT, 2], I32)
    wtab = sb.alloc("wtab", [128, NT, 2], F32)
    cnt = sb.alloc("cnt", [128, NE], F32)
    ss = sb.alloc("ss", [128, 8], F32)
    rs = sb.alloc("rs", [128, 8], F32)
    junk = sb.alloc("junk", [128, D], BF16)

    def ld(eng, dst, src, key):
        P.op(eng, lambda e: e.dma_start(out=dst, in_=src), writes=[key], dma=True)

    ld("sync", ident[:], ident_d.ap(), "ident")
    ld("sync", utri[:], utri_d.ap(), "utri")
    ld("sync", ones[:], ones_d.ap(), "ones")
    ld("sync", bdiag[:], bdiag_d.ap(), "bdiag")
    ld("sync", ecap[:], ecap_d.ap(), "ecap")
    ld("sync", bcol[:], bcol_d.ap(), "bcol")
    P.op("vector", lambda e: e.tensor_scalar(out=hbcol[:], in0=bcol[:], scalar1=0.5, scalar2=None, op0=ALU.mult),
         reads=["bcol"], writes=["hbcol"])
    P.op("gpsimd", lambda e: e.memset(mhalf[:], -0.5), writes=["mhalf"])
    P.op("gpsimd", lambda e: e.memset(epsc[:], EPS), writes=["epsc"])
    P.op("gpsimd", lambda e: e.memset(cnt[:], 0.0), writes=["cnt"])
    P.op("gpsimd", lambda e: e.memset(wtab[:], 0.0), writes=["wtab"])

    nrm_ctr = [0]

    def rmsnorm_tile(src, g_bc, dst_bf, src_key, g_key, dst_key):
        i = nrm_ctr[0] % 8
        nrm_ctr[0] += 1
        ssk, rsk = ("ss", i), ("rs", i)
        P.op("scalar", lambda e: e.activation(out=junk[:], in_=src, func=AF.Square, accum_out=ss[:, i:i + 1]),
             reads=[src_key], writes=["junk", ssk])
        P.op("vector", lambda e: e.tensor_scalar(out=rs[:, i:i + 1], in0=ss[:, i:i + 1], scalar1=1.0 / D, scalar2=EPS,
                                                  op0=ALU.mult, op1=ALU.add), reads=[ssk], writes=[rsk])
        P.op("gpsimd", lambda e: e.tensor_tensor(out=rs[:, i:i + 1], in0=rs[:, i:i + 1], in1=mhalf[:, 0:1], op=ALU.pow),
             reads=[rsk, "mhalf"], writes=[rsk])
        P.op("vector", lambda e: e.scalar_tensor_tensor(out=dst_bf, in0=src, scalar=rs[:, i:i + 1], in1=g_bc,
                                                         op0=ALU.mult, op1=ALU.mult),
             reads=[src_key, rsk, g_key], writes=[dst_key])

    def transpose_tile(src_bf, nchunk, dst, src_key, dst_key, evac="scalar", interleave=0):
        for c in range(nchunk):
            if interleave:
                src_c = src_bf.rearrange("t (p c) -> t c p", c=interleave)[:, c, :]
            else:
                src_c = src_bf[:, c * 128:(c + 1) * 128]
            P.op("tensor", lambda e, c=c, src_c=src_c: e.transpose(out=pT[:, c, :], in_=src_c, identity=ident[:]),
                 reads=(list(src_key) if isinstance(src_key, list) else [src_key]) + ["ident"], writes=[("pT", c)])
        if evac == "scalar":
            P.op("scalar", lambda e: e.activation(out=dst, in_=pT[:, 0:nchunk, :], func=AF.Copy),
                 reads=[("pT", c) for c in range(nchunk)], writes=[dst_key])
        else:
            P.op("vector", lambda e: e.tensor_copy(out=dst, in_=pT[:, 0:nchunk, :]),
                 reads=[("pT", c) for c in range(nchunk)], writes=[dst_key])

    _breg = {}

    def breg(e):
        if "r" not in _breg:
            _breg["r"] = e.to_reg(NSLOT - 1)
        return _breg["r"]

    base_mark = sb.mark()
    BASE = (base_mark + 31) // 32 * 32
    pre2 = {"Wg": nc.alloc_sbuf_tensor_at("Wg_pre", [128, 8, 2048], BF16, offset=BASE),
            "wpa": nc.alloc_sbuf_tensor_at("wpa_pre", [128, 4, D], BF16, offset=BASE + 32768),
            "wpc": nc.alloc_sbuf_tensor_at("wpc_pre", [128, 4, D], BF16, offset=BASE + 40960)}
    pre3 = [{"w1": nc.alloc_sbuf_tensor_at("w1_pre%d" % i, [128, 8, 512], BF16, offset=BASE + i * 24576),
             "w3": nc.alloc_sbuf_tensor_at("w3_pre%d" % i, [128, 8, 512], BF16, offset=BASE + i * 24576 + 8192),
             "w2": nc.alloc_sbuf_tensor_at("w2_pre%d" % i, [128, 4, D], BF16, offset=BASE + i * 24576 + 16384)} for i in range(2)]
    WGKEYS = [("Wg", g0 + q4 * 256) for q4 in range(4) for g0 in (0, 1024)]

    def p2_weight_loads(Wg, wpa, wpc, extra_writes=()):
        win_v2 = win_d.ap().rearrange("(c p) n -> p c n", p=128)
        ew = list(extra_writes)

        def wg_load(q4):
            for g0 in (0, 1024):
                c0_ = g0 + q4 * 256
                P.op("gpsimd", lambda e, c0_=c0_: e.dma_start(out=Wg[:, :, c0_:c0_ + 256], in_=win_v2[:, :, 3072 + c0_:3072 + c0_ + 256]),
                     writes=[("Wg", c0_)] + ew, dma=True)

        wg_load(0)
        P.op("gpsimd", lambda e: e.dma_start(out=wpa[:], in_=wpa_d.ap().rearrange("(c p) n -> p c n", p=128)), writes=["wpa"] + ew, dma=True)
        P.op("gpsimd", lambda e: e.dma_start(out=wpc[:], in_=wpc_d.ap().rearrange("(c p) n -> p c n", p=128)), writes=["wpc"] + ew, dma=True)
        for q4 in range(1, 4):
            wg_load(q4)

    def expert_weight_loads(ex, w1t, w3t, w2t, keys, extra_writes=()):
        ew = list(extra_writes)
        P.op("gpsimd", lambda e: e.dma_start(out=w1t[:], in_=w1_d.ap()[ex].rearrange("(p c) f -> p c f", c=8)), writes=[keys[0]] + ew, dma=True)
        P.op("gpsimd", lambda e: e.dma_start(out=w3t[:], in_=w3_d.ap()[ex].rearrange("(p c) f -> p c f", c=8)), writes=[keys[1]] + ew, dma=True)
        P.op("gpsimd", lambda e: e.dma_start(out=w2t[:], in_=w2_d.ap()[ex].rearrange("(c p) f -> p c f", p=128)), writes=[keys[2]] + ew, dma=True)

    def phase1():
        Wa = sb.alloc("Wa", [128, 8, 3072], BF16)
        gmix = sb.alloc("gmix", [128, D], F32)
        gqk = sb.alloc("gqk", [128, 2], F32)
        bvb = sb.alloc("bvb", [128, 512], F32)
        cw = sb.alloc("cw", [128, 12], F32)
        cb = sb.alloc("cb", [128, 4], F32)
        expB = sb.alloc("expB", [128, NH, 5, 128], BF16)
        maskT = sb.alloc("maskT", [128, 5, 128], F32)
        kring = sb.alloc("kring", [128, 4, KR * 128], BF16)
        vring = sb.alloc("vring", [128, KR, NH, 65], BF16)
        xt = [sb.alloc("xt%d" % i, [128, D], F32) for i in range(2)]
        nb = [sb.alloc("nb%d" % i, [128, D], BF16) for i in range(2)]
        nTb = [sb.alloc("nTb%d" % i, [128, 8, BT], BF16) for i in range(2)]
        qT = [sb.alloc("qT%d" % i, [128, 4, BT], BF16) for i in range(2)]
        zq = [sb.alloc("zq%d" % i, [128, BT], F32) for i in range(8)]
        sq = [sb.alloc("sq%d" % i, [128, BT], BF16) for i in range(3)]
        rs = sb.alloc("rs_all", [128, BT], F32)
        r1b = sb.alloc("r1b", [128, BT], BF16)
        selA = sb.alloc("selA", [128, 8, 128], BF16)
        selB = sb.alloc("selB", [128, 8, 128], BF16)
        gw = sb.alloc("gw", [128, 1], F32)
        us = [sb.alloc("us%d" % i, [128, BT], F32) for i in range(2)]
        t1 = [sb.alloc("t1%d" % i, [128, BT], F32) for i in range(2)]
        cu = sb.alloc("cu", [128, 4, BT + 2], F32)
        ycT = [sb.alloc("ycT%d" % i, [128, 4, BT], BF16) for i in range(2)]
        yaT = [sb.alloc("yaT%d" % i, [128, 4, BT], BF16) for i in range(2)]
        pt = [sb.alloc("pt%d" % i, [128, 4, 128], BF16) for i in range(6)]
        rden = [sb.alloc("rden%d" % i, [128, 4], F32) for i in range(2)]
        ya = sb.alloc("ya", [128, 4, 512], BF16)

        win_v = win_d.ap().rearrange("(c p) n -> p c n", p=128)
        for (c0_, c1_) in ((0, 1024), (1024, 1536), (1536, 3072)):
            for c in range(0, 8, 2):
                P.op("gpsimd", lambda e, c=c, c0_=c0_, c1_=c1_: e.dma_start(out=Wa[:, c:c + 2, c0_:c1_], in_=win_v[:, c:c + 2, c0_:c1_]),
                     writes=[("Wa", c, c0_), ("Wa", c + 1, c0_)], dma=True)
        zt = sb.alloc("zt", [128, 2 * D], BF16)
        P.op("gpsimd", lambda e: e.memset(zt[:], 0.0), writes=["zt"])
        NR = (NSLOT + 128) // 128
        xs_z = xs_d.ap().rearrange("(p r) d -> p (r d)", p=128)
        zchunks = [(r0, min(r0 + 2, NR)) for r0 in range(0, NR, 2)]

        def zero_fill(k):
            for (r0, r1_) in zchunks[k::NB]:
                P.op("sync", lambda e, r0=r0, r1_=r1_: e.dma_start(out=xs_z[:, r0 * D:r1_ * D], in_=zt[:, 0:(r1_ - r0) * D]),
                     reads=["zt"], writes=[("xs_zero", r0)], dma=True)

        ld("sync", gmix[:], gmix_d.ap().partition_broadcast(128), "gmix")
        ld("sync", gqk[:], gqk_d.ap(), "gqk")
        ld("sync", selA[:], selA_d.ap().rearrange("p (j m) -> p j m", j=8), "selA")
        ld("sync", selB[:], selB_d.ap().rearrange("p (j m) -> p j m", j=8), "selB")
        ld("sync", bvb[:], bv_d.ap().partition_broadcast(128), "bvb")
        P.op("vector", lambda e: e.tensor_tensor(out=gw[:], in0=gqk[:, 0:1], in1=gqk[:, 1:2], op=ALU.mult), reads=["gqk"], writes=["gw"])
        ld("sync", cw[:], cw_d.ap(), "cw")
        ld("sync", cb[:], cb_d.ap(), "cb")
        ld("sync", maskT[:], maskT_d.ap().rearrange("p (j q) -> p j q", j=5), "maskT")
        rb_v = rbT_d.ap().rearrange("p (h n) -> p h n", h=NH)
        stg = [sb.alloc_alias("stg0", [128, 640], F32, "zq0"), sb.alloc_alias("stg1", [128, 640], F32, "zq2")]
        for h in range(NH):
            st = stg[h % 2]
            sk = [("zq", 2 * (h % 2)), ("zq", 2 * (h % 2) + 1)]
            P.op("sync", lambda e, h=h, st=st: e.dma_start(out=st[:], in_=rb_v[:, h, :]), writes=sk, dma=True)
            P.op("scalar", lambda e, st=st: e.activation(out=st[:], in_=st[:], func=AF.Exp), reads=sk, writes=sk)
            P.op("vector", lambda e, h=h, st=st: e.tensor_tensor(
                out=expB[:, h, :, :], in0=st[:].rearrange("p (j q) -> p j q", j=5), in1=maskT[:], op=ALU.mult),
                reads=sk + ["maskT"], writes=[("expB", h)])
        P.op("gpsimd", lambda e: e.memset(vring[:], 1.0), writes=[("v", s_) for s_ in range(KR)])
        P.op("gpsimd", lambda e: e.memset(cu[:], 0.0), writes=[("cu", ct) for ct in range(4)] + [("cuh", ct) for ct in range(4)])

        nT_v = nT_d.ap().rearrange("(c p) t -> p c t", p=128)
        yaT_v = yaT_d.ap().rearrange("(c p) t -> p c t", p=128)
        ycT_v = ycT_d.ap().rearrange("(c p) t -> p c t", p=128)
        acc_rot = [0]
        ACC = [(A0, "A0"), (A1, "A1")]

        def next_acc():
            a = ACC[acc_rot[0] % 2]
            acc_rot[0] += 1
            return a

        def A_norm(b, tl):
            t = 4 * b + tl
            s2 = t % 2
            P.op("sync", lambda e, t=t, s2=s2: e.dma_start(out=xt[s2][:], in_=x_d.ap()[t * 128:(t + 1) * 128, :]),
                 writes=[("xt", s2)], dma=True)
            rmsnorm_tile(xt[s2][:], gmix[:], nb[s2][:], ("xt", s2), "gmix", ("nb", s2))

        def A_tr(b, tl):
            t = 4 * b + tl
            s2 = t % 2
            bb = b % 2
            transpose_tile(nb[s2], 8, nTb[bb][:, :, tl * 128:(tl + 1) * 128], ("nb", s2), ("nTb", bb, tl))
            if tl == 3:
                P.op("sync", lambda e, bb=bb, b=b: e.dma_start(out=nT_v[:, :, b * BT:(b + 1) * BT], in_=nTb[bb][:]),
                     reads=[("nTb", bb, q_) for q_ in range(4)], writes=[("nT_d", b)], dma=True)

        def secC(b):
            bb = b % 2
            tok0 = b * BT
            nkeys = [("nTb", bb, tl) for tl in range(4)]
            PACC = [(A0[:], "A0"), (A1[:], "A1"), (B0[:, 0:512], ("B0", 0))]
            prot = [0]

            def nacc():
                a = PACC[prot[0] % 3]
                prot[0] += 1
                return a

            def proj(j):
                acc, akey = nacc()
                for c in range(8):
                    P.op("tensor", lambda e, j=j, c=c, acc=acc: e.matmul(
                        acc, lhsT=Wa[:, c, j * 128:(j + 1) * 128], rhs=nTb[bb][:, c, :], start=(c == 0), stop=(c == 7)),
                        reads=[("Wa", c, 0)] + nkeys, writes=[akey])
                z = j % 3
                P.op("scalar", lambda e, j=j, acc=acc: e.activation(out=zq[j][:], in_=acc, func=AF.Identity, bias=bcol[:, j:j + 1]),
                     reads=[akey, "bcol"], writes=[("zq", j)])
                P.op("gpsimd", lambda e, z=z, j=j: e.tensor_tensor(out=sq[z][:], in0=zq[j][:], in1=zq[j][:], op=ALU.mult),
                     reads=[("zq", j)], writes=[("sq", z)])

            def msacc(j):
                z = j % 3
                P.op("tensor", lambda e, z=z, j=j: e.matmul(A2[:], lhsT=selA[:, j, :], rhs=sq[z][:], start=(j == 0), stop=(j == 7)),
                     reads=[("sq", z), "selA"], writes=["A2"])

            def vproj():
                for tl in range(4):
                    t = 4 * b + tl
                    sl = t % KR
                    acc, akey = nacc()
                    for c in range(8):
                        P.op("tensor", lambda e, c=c, tl=tl, acc=acc: e.matmul(
                            acc, lhsT=nTb[bb][:, c, tl * 128:(tl + 1) * 128], rhs=Wa[:, c, 1024:1536], start=(c == 0), stop=(c == 7)),
                            reads=[("Wa", c, 1024), ("nTb", bb, tl)], writes=[akey])
                    P.op("vector", lambda e, sl=sl, acc=acc: e.tensor_tensor(
                        out=vring[:, sl, :, 0:64], in0=acc.rearrange("p (h d) -> p h d", h=NH),
                        in1=bvb[:].rearrange("p (h d) -> p h d", h=NH), op=ALU.add),
                        reads=[akey, "bvb"], writes=[("v", sl)])

            def fin(j):
                acc, akey = nacc()
                P.op("tensor", lambda e, j=j, acc=acc: e.matmul(acc, lhsT=selB[:, j, :], rhs=r1b[:], start=True, stop=True),
                     reads=["selB", "r1b"], writes=[akey])
                if j < 4:
                    P.op("vector", lambda e, j=j, acc=acc: e.tensor_tensor(out=qT[bb][:, j, :], in0=zq[j][:], in1=acc, op=ALU.mult),
                         reads=[("zq", j), akey], writes=[("qT", bb, j)])
                else:
                    hp = j - 4
                    sl0 = (4 * b) % KR
                    P.op("vector", lambda e, hp=hp, j=j, sl0=sl0, acc=acc: e.scalar_tensor_tensor(
                        out=kring[:, hp, sl0 * 128:(sl0 + 4) * 128], in0=zq[j][:], scalar=gw[:, 0:1], in1=acc, op0=ALU.mult, op1=ALU.mult),
                        reads=[("zq", j), akey, "gw"], writes=[("k", hp, sl0 + q_) for q_ in range(4)])

            proj(0)
            for j in range(8):
                if j + 1 < 8:
                    proj(j + 1)
                msacc(j)
            P.op("scalar", lambda e: e.activation(out=rs[:], in_=A2[:], func=AF.Sqrt, bias=epsc[:, 0:1]), reads=["A2", "epsc"], writes=["rs"])
            P.op("vector", lambda e: e.reciprocal(out=rs[:], in_=rs[:]), reads=["rs"], writes=["rs"])
            P.op("scalar", lambda e: e.activation(out=r1b[:], in_=rs[:], func=AF.Copy), reads=["rs"], writes=["r1b"])
            vproj()
            for j in range(8):
                fin(j)
        def secC3(b):
            bb = b % 2
            tok0 = b * BT
            nkeys = [("nTb", bb, tl) for tl in range(4)]
            SETS = [((A0[:], ["A0"]), (A1[:], ["A1"]), (A2[:], ["A2"])),
                    ((B0[:, 0:512], [("B0", 0)]), (B0[:, 512:1024], [("B0", 4)]), (B1[:, 0:512], [("B1h", 0)]))]
            for ct in range(4):
                z = ct % 2
                (pu, ku), (pb, kb), (pc, kc) = SETS[ct % 2]
                for (dst, dkey, col0) in ((pu, ku, 1536), (pb, kb, 2048), (pc, kc, 2560)):
                    for c in range(8):
                        P.op("tensor", lambda e, c=c, dst=dst, col0=col0, ct=ct: e.matmul(
                            dst, lhsT=Wa[:, c, col0 + ct * 128:col0 + (ct + 1) * 128], rhs=nTb[bb][:, c, :],
                            start=(c == 0), stop=(c == 7)),
                            reads=[("Wa", c, 1536)] + nkeys, writes=dkey)
                ju, jb, jc = 12 + ct, 16 + ct, 20 + ct
                P.op("scalar", lambda e, z=z, ju=ju, pu=pu: e.activation(out=us[z][:], in_=pu, func=AF.Identity, bias=bcol[:, ju:ju + 1]),
                     reads=ku + ["bcol"], writes=[("us", z)])
                P.op("vector", lambda e, z=z, jc=jc, ct=ct, pc=pc: e.scalar_tensor_tensor(
                    out=cu[:, ct, 2:BT + 2], in0=pc, scalar=bcol[:, jc:jc + 1], in1=us[z][:], op0=ALU.add, op1=ALU.mult),
                    reads=kc + ["bcol", ("us", z)], writes=[("cu", ct)])
                P.op("scalar", lambda e, z=z, ct=ct: e.activation(out=t1[z][:], in_=cu[:, ct, 2:BT + 2], func=AF.Identity,
                                                               scale=cw[:, ct * 3 + 2:ct * 3 + 3], bias=cb[:, ct:ct + 1]),
                     reads=[("cu", ct), "cw", "cb"], writes=[("t1", z)])
                P.op("vector", lambda e, z=z, ct=ct: e.scalar_tensor_tensor(
                    out=t1[z][:], in0=cu[:, ct, 1:BT + 1], scalar=cw[:, ct * 3 + 1:ct * 3 + 2], in1=t1[z][:], op0=ALU.mult, op1=ALU.add),
                    reads=[("cu", ct), ("cuh", ct), "cw", ("t1", z)], writes=[("t1", z)])
                P.op("vector", lambda e, z=z, ct=ct: e.scalar_tensor_tensor(
                    out=t1[z][:], in0=cu[:, ct, 0:BT], scalar=cw[:, ct * 3:ct * 3 + 1], in1=t1[z][:], op0=ALU.mult, op1=ALU.add),
                    reads=[("cu", ct), ("cuh", ct), "cw", ("t1", z)], writes=[("t1", z)])
                P.op("vector", lambda e, z=z, jb=jb, ct=ct, pb=pb: e.scalar_tensor_tensor(
                    out=ycT[bb][:, ct, :], in0=pb, scalar=bcol[:, jb:jb + 1], in1=t1[z][:], op0=ALU.add, op1=ALU.mult),
                    reads=kb + ["bcol", ("t1", z)], writes=[("ycT", bb, ct)])
                P.op("vector", lambda e, ct=ct: e.tensor_copy(out=cu[:, ct, 0:2], in_=cu[:, ct, BT:BT + 2]),
                     reads=[("cu", ct)], writes=[("cuh", ct)])
            P.op("sync", lambda e, tok0=tok0: e.dma_start(out=ycT_v[:, :, tok0:tok0 + BT], in_=ycT[bb][:]),
                 reads=[("ycT", bb, ct) for ct in range(4)], writes=[("ycT_d", b)], dma=True)

        def secD(b):
            bb = b % 2
            tok0 = b * BT
            nxt = b + 1 < NB
            units = []
            for h in range(NH):
                for m in range(8):
                    kt = 4 * b - 4 + m
                    if kt < 0:
                        continue
                    units.append((h, m, kt, max(m - 4, 0), min(m, 3)))
            SPS = [(A0[:], "A0"), (A1[:], "A1"), (A2[:], "A2"), (B0[:, 0:512], ("B0", 0)), (B0[:, 512:1024], ("B0", 4))]
            LA = 4

            def QKEXP(u):
                h, m, kt, tlo, thi = units[u]
                hp, r0 = h // 2, (h % 2) * 64
                nq = thi - tlo + 1
                sp, sk = SPS[u % 5]
                pz = u % 6
                sl = kt % KR
                P.op("tensor", lambda e, hp=hp, r0=r0, sl=sl, sp=sp, tlo=tlo, thi=thi: e.matmul(
                    sp[:, 0:(thi - tlo + 1) * 128], lhsT=kring[r0:r0 + 64, hp, sl * 128:(sl + 1) * 128],
                    rhs=qT[bb][r0:r0 + 64, hp, tlo * 128:(thi + 1) * 128], start=True, stop=True),
                    reads=[("k", hp, sl), ("qT", bb, hp)], writes=[sk])
                P.op("scalar", lambda e, pz=pz, sp=sp, nq=nq: e.activation(
                    out=pt[pz][:, 0:nq, :], in_=sp[:, 0:nq * 128].rearrange("p (j q) -> p j q", q=128), func=AF.Exp, scale=DH ** -0.5),
                    reads=[sk], writes=[("pt", pz)])
                rlo = 4 - m + tlo
                P.op("vector", lambda e, pz=pz, nq=nq, h=h, rlo=rlo: e.tensor_tensor(
                    out=pt[pz][:, 0:nq, :], in0=pt[pz][:, 0:nq, :], in1=expB[:, h, rlo:rlo + nq, :], op=ALU.mult),
                    reads=[("pt", pz), ("expB", h)], writes=[("pt", pz)])

            def PV(u):
                h, m, kt, tlo, thi = units[u]
                pz = u % 6
                sl = kt % KR
                hb2 = h % 2
                first = (u == 0) or units[u - 1][0] != h
                last_u = (u + 1 == len(units)) or units[u + 1][0] != h
                if first:
                    P.op("tensor", lambda e, hb2=hb2: e.matmul(
                        B1[:, hb2 * 512:hb2 * 512 + 260], lhsT=zt[:, 0:128], rhs=zt[:, 0:260], start=True, stop=False),
                        reads=["zt"], writes=[("B1h", hb2)])
                for tl in range(tlo, thi + 1):
                    c0 = hb2 * 512 + tl * 65
                    P.op("tensor", lambda e, h=h, tl=tl, tlo=tlo, sl=sl, pz=pz, c0=c0, fin=(last_u and tl == thi): e.matmul(
                        B1[:, c0:c0 + 65], lhsT=pt[pz][:, tl - tlo, :], rhs=vring[:, sl, h, :],
                        start=False, stop=fin),
                        reads=[("pt", pz), ("v", sl)], writes=[("B1h", hb2)])

            def FINH(h):
                hb2 = h % 2
                Bv = B1[:, hb2 * 512:hb2 * 512 + 260].rearrange("p (t d) -> p t d", d=65)
                P.op("vector", lambda e, hb2=hb2, Bv=Bv: e.reciprocal(out=rden[hb2][:], in_=Bv[:, :, 64]),
                     reads=[("B1h", hb2)], writes=[("rden", hb2)])
                P.op("vector", lambda e, hb2=hb2, Bv=Bv, h=h: e.tensor_tensor(
                    out=ya[:, :, h * 64:(h + 1) * 64], in0=Bv[:, :, 0:64], in1=bc_last(rden[hb2][:], 64), op=ALU.mult),
                    reads=[("B1h", hb2), ("rden", hb2)], writes=[("ya", h)])

            if nxt:
                A_norm(b + 1, 0)
            for u in range(min(LA, len(units))):
                QKEXP(u)
            secC3(b)
            zero_fill(b)
            if b == NB - 1 and 2 in phases:
                p2_weight_loads(pre2["Wg"], pre2["wpa"], pre2["wpc"],
                                extra_writes=[("Wa", c, c0_) for c in range(8) for c0_ in (0, 1024, 1536)])
            for u in range(len(units)):
                if u + LA < len(units):
                    QKEXP(u + LA)
                PV(u)
                h = units[u][0]
                if u + 1 == len(units) or units[u + 1][0] != h:
                    FINH(h)
                    if nxt and h % 2 == 1:
                        tl = h // 2
                        A_tr(b + 1, tl)
                        if tl + 1 < 4:
                            A_norm(b + 1, tl + 1)
            for tl in range(4):
                transpose_tile(ya[:, tl, :], 4, yaT[bb][:, :, tl * 128:(tl + 1) * 128], [("ya", h) for h in range(NH)], ("yaT", bb, tl))
            P.op("sync", lambda e, tok0=tok0: e.dma_start(out=yaT_v[:, :, tok0:tok0 + BT], in_=yaT[bb][:]),
                 reads=[("yaT", bb, tl) for tl in range(4)], writes=[("yaT_d", b)], dma=True)

        for tl in range(4):
            A_norm(0, tl)
            A_tr(0, tl)
        for b in range(NB):
            secC(b)
            secD(b)
        P.barrier()
    if 1 in phases:
        phase1()
    sb.reset(base_mark)

    def phase2():
        Wg = sb.alloc("Wg", [128, 8, 2048], BF16)
        wpa = sb.alloc("wpa", [128, 4, D], BF16)
        wpc = sb.alloc("wpc", [128, 4, D], BF16)
        wo = sb.alloc("wo", [128, 8, D], BF16)
        wrg = sb.alloc("wrg", [128, 8, 36], BF16)
        brg = sb.alloc("brg", [128, 36], F32)
        gffn = sb.alloc("gffn", [128, D], F32)
        nTb = [sb.alloc("nTb%d" % i, [128, 8, BT], BF16) for i in range(2)]
        yaT = [sb.alloc("yaT%d" % i, [128, 4, BT], BF16) for i in range(2)]
        ycT = [sb.alloc("ycT%d" % i, [128, 4, BT], BF16) for i in range(2)]
        xt = [sb.alloc("xt%d" % i, [128, D], F32) for i in range(4)]
        tA = [sb.alloc("tA%d" % i, [128, BT], F32) for i in range(2)]
        tC = [sb.alloc("tC%d" % i, [128, BT], F32) for i in range(2)]
        mA = [sb.alloc("mA%d" % i, [128, BT], F32) for i in range(2)]
        mC = [sb.alloc("mC%d" % i, [128, BT], F32) for i in range(2)]
        mT = [sb.alloc("mT%d" % i, [128, 8, BT], BF16) for i in range(2)]
        ht = [sb.alloc("ht%d" % i, [128, D], F32) for i in range(3)]
        n2 = [sb.alloc("n2%d" % i, [128, 4, D], BF16) for i in range(2)]
        n2T = [sb.alloc("n2T%d" % i, [128, 8, 128], BF16) for i in range(2)]
        lg = sb.alloc("lg", [128, 4, 36], F32)
        gmax = sb.alloc("gmax", [128, 4], F32)
        gmask = sb.alloc("gmask", [128, 4, 4], F32)
        gex = sb.alloc("gex", [128, 4, 4], F32)
        gse = sb.alloc("gse", [128, 4], F32)
        pen = sb.alloc("pen", [128, 4, 4], F32)
        elm = sb.alloc("elm", [128, 4, 32], F32)
        elm2 = sb.alloc("elm2", [128, 4, 32], F32)
        m1 = sb.alloc("m1", [128, 4], F32)
        m2 = sb.alloc("m2", [128, 4], F32)
        mk1 = sb.alloc("mk1", [128, 4, 32], F32)
        mk2 = sb.alloc("mk2", [128, 4, 32], F32)
        Mb = sb.alloc("Mb", [128, 4, 32], BF16)
        dd = sb.alloc("dd", [128, 4], F32)
        ee = sb.alloc("ee", [128, 4], F32)
        rr = sb.alloc("rr", [128, 4], F32)
        wA = sb.alloc("wA", [128, 4], F32)
        wB = sb.alloc("wB", [128, 4], F32)
        pos = sb.alloc("pos", [128, 4, 32], F32)
        okm = sb.alloc("okm", [128, 4, 32], F32)
        slot = sb.alloc("slot", [128, 4, 32], F32)
        tmp = sb.alloc("tmp", [128, 4, 32], F32)
        dsel = sb.alloc("dsel", [128, 4, 2], F32)
        oksel = sb.alloc("oksel", [128, 4, 2], F32)

        assert sb.off["Wg"] == BASE and sb.off["wpa"] == BASE + 32768 and sb.off["wpc"] == BASE + 40960
        if 1 not in phases:
            p2_weight_loads(Wg, wpa, wpc)
        wo_v = wo_d.ap().rearrange("(c p) n -> p c n", p=128)
        for c in range(0, 8, 4):
            P.op("gpsimd", lambda e, c=c: e.dma_start(out=wo[:, c:c + 4, :], in_=wo_v[:, c:c + 4, :]), writes=[("wo", c)], dma=True)
        P.op("gpsimd", lambda e: e.dma_start(out=wrg[:], in_=wrg_d.ap().rearrange("(c p) n -> p c n", p=128)), writes=["wrg"], dma=True)
        ld("sync", brg[:], brg_d.ap().partition_broadcast(128), "brg")
        ld("sync", gffn[:], gffn_d.ap().partition_broadcast(128), "gffn")

        nT_v = nT_d.ap().rearrange("(c p) t -> p c t", p=128)
        yaT_v = yaT_d.ap().rearrange("(c p) t -> p c t", p=128)
        ycT_v = ycT_d.ap().rearrange("(c p) t -> p c t", p=128)
        wokeys = [("wo", 0), ("wo", 4)]
        xctr = [0]
        hctr = [0]

        def loads2(b):
            bb = b % 2
            tok0 = b * BT
            P.op("sync", lambda e, bb=bb, tok0=tok0: e.dma_start(out=nTb[bb][:], in_=nT_v[:, :, tok0:tok0 + BT]), writes=[("nTb", bb)], dma=True)
            P.op("sync", lambda e, bb=bb, tok0=tok0: e.dma_start(out=yaT[bb][:], in_=yaT_v[:, :, tok0:tok0 + BT]), writes=[("yaT", bb)], dma=True)
            P.op("sync", lambda e, bb=bb, tok0=tok0: e.dma_start(out=ycT[bb][:], in_=ycT_v[:, :, tok0:tok0 + BT]), writes=[("ycT", bb)], dma=True)

        def xload(t):
            P.op("sync", lambda e, t=t: e.dma_start(out=xt[t % 4][:], in_=x_d.ap()[t * 128:(t + 1) * 128, :]), writes=[("xt", t % 4)], dma=True)

        loads2(0)
        for t_ in range(3):
            xload(t_)
        def gates2(b):
            bb = b % 2
            tok0 = b * BT
            if b + 1 < NB:
                loads2(b + 1)
            for j in range(8):
                z = j % 2
                for (dst, dkey, col0) in ((A0, "A0", 0), (A1, "A1", 1024)):
                    for c in range(8):
                        P.op("tensor", lambda e, c=c, dst=dst, col0=col0, j=j, bb=bb: e.matmul(
                            dst[:], lhsT=Wg[:, c, col0 + j * 128:col0 + (j + 1) * 128], rhs=nTb[bb][:, c, :], start=(c == 0), stop=(c == 7)),
                            reads=[("Wg", col0 + (j // 2) * 256), ("nTb", bb)], writes=[dkey])
                for (dst, dkey, wsrc, wkey, asrc, akey) in ((A2, "A2", wpa, "wpa", yaT, "yaT"), (B0, ("B0", 0), wpc, "wpc", ycT, "ycT")):
                    for c in range(4):
                        P.op("tensor", lambda e, c=c, dst=dst, wsrc=wsrc, asrc=asrc, j=j, bb=bb: e.matmul(
                            dst[:, 0:512], lhsT=wsrc[:, c, j * 128:(j + 1) * 128], rhs=asrc[bb][:, c, :], start=(c == 0), stop=(c == 3)),
                            reads=[wkey, (akey, bb)], writes=[dkey])
                P.op("scalar", lambda e, z=z, j=j: e.activation(out=tA[z][:], in_=A0[:], func=AF.Tanh, scale=0.5, bias=hbcol[:, 24 + j:25 + j]),
                     reads=["A0", "hbcol"], writes=[("tA", z)])
                P.op("scalar", lambda e, z=z, j=j: e.activation(out=tC[z][:], in_=A1[:], func=AF.Tanh, scale=0.5, bias=hbcol[:, 32 + j:33 + j]),
                     reads=["A1", "hbcol"], writes=[("tC", z)])
                P.op("vector", lambda e, z=z: e.scalar_tensor_tensor(out=mA[z][:], in0=tA[z][:], scalar=1.0, in1=A2[:], op0=ALU.add, op1=ALU.mult),
                     reads=[("tA", z), "A2"], writes=[("mA", z)])
                P.op("vector", lambda e, z=z: e.scalar_tensor_tensor(out=mC[z][:], in0=tC[z][:], scalar=1.0, in1=B0[:, 0:512], op0=ALU.add, op1=ALU.mult),
                     reads=[("tC", z), ("B0", 0)], writes=[("mC", z)])
                P.op("gpsimd", lambda e, z=z, j=j, bb=bb: e.tensor_tensor(out=mT[bb][:, j, :], in0=mA[z][:], in1=mC[z][:], op=ALU.add),
                     reads=[("mA", z), ("mC", z)], writes=[("mT", bb, j)])

        def hsec2(b):
            bb = b % 2
            tok0 = b * BT
            mkeys = [("mT", bb, j) for j in range(8)]
            def hmm(tl):
                t = 4 * b + tl
                xs_ = t % 4
                hs_ = t % 3
                if t + 3 < NT:
                    xload(t + 3)
                HB = [(B1[:, 0:512], ("B1", 0)), (B1[:, 512:1024], ("B1", 1))] if tl % 2 == 0 else [(A0[:], "A0"), (A1[:], "A1")]
                for half in range(2):
                    hacc, hkey = HB[half]
                    for j in range(8):
                        P.op("tensor", lambda e, j=j, half=half, tl=tl, bb=bb, hacc=hacc: e.matmul(
                            hacc, lhsT=mT[bb][:, j, tl * 128:(tl + 1) * 128], rhs=wo[:, j, half * 512:(half + 1) * 512],
                            start=(j == 0), stop=(j == 7)),
                            reads=mkeys + wokeys, writes=[hkey])
                    P.op("vector", lambda e, half=half, xs_=xs_, hs_=hs_, hacc=hacc: e.scalar_tensor_tensor(
                        out=ht[hs_][:, half * 512:(half + 1) * 512], in0=hacc, scalar=0.5,
                        in1=xt[xs_][:, half * 512:(half + 1) * 512], op0=ALU.mult, op1=ALU.add),
                        reads=[hkey, ("xt", xs_)] + ([("ht", hs_)] if half == 1 else []), writes=[("ht", hs_)])
                P.op("sync", lambda e, t=t, hs_=hs_: e.dma_start(out=h_d.ap()[t * 128:(t + 1) * 128, :], in_=ht[hs_][:]),
                     reads=[("ht", hs_)], writes=[("h_d", t)], dma=True)

            def hnorm(tl):
                t = 4 * b + tl
                hs_ = t % 3
                rmsnorm_tile(ht[hs_][:], gffn[:], n2[bb][:, tl, :], ("ht", hs_), "gffn", ("n2", bb, tl))

            def htr(tl):
                t = 4 * b + tl
                z2 = t % 2
                transpose_tile(n2[bb][:, tl, :], 8, n2T[z2][:], ("n2", bb, tl), ("n2T", z2))
                for c in range(8):
                    P.op("tensor", lambda e, c=c, tl=tl, z2=z2: e.matmul(
                        B0[:, 512 + tl * 36:512 + (tl + 1) * 36], lhsT=n2T[z2][:, c, :], rhs=wrg[:, c, :], start=(c == 0), stop=(c == 7)),
                        reads=[("n2T", z2), "wrg"], writes=[("lgp", tl)])

            hmm(0)
            hnorm(0)
            for tl in range(4):
                if tl + 1 < 4:
                    hmm(tl + 1)
                htr(tl)
                if tl + 1 < 4:
                    hnorm(tl + 1)

        def rout2a(b):
            bb = b % 2
            tok0 = b * BT
            lgp = B0[:, 512:512 + 144].rearrange("p (t n) -> p t n", t=4)
            R = []

            def V(fn, reads, writes):
                P.op("vector", fn, reads=reads, writes=writes)

            V(lambda e: e.tensor_tensor(out=lg[:], in0=lgp, in1=bc_mid(brg[:], 4), op=ALU.add),
              [("lgp", tl) for tl in range(4)] + ["brg"], ["lg"])
            V(lambda e: e.tensor_reduce(out=gmax[:], in_=lg[:, :, 0:4], axis=AX.X, op=ALU.max), ["lg"], ["gmax"])
            V(lambda e: e.tensor_tensor(out=gmask[:], in0=lg[:, :, 0:4], in1=bc_last(gmax[:], 4), op=ALU.is_equal), ["lg", "gmax"], ["gmask"])
            V(lambda e: e.tensor_tensor(out=gex[:], in0=lg[:, :, 0:4], in1=bc_last(gmax[:], 4), op=ALU.subtract), ["lg", "gmax"], ["gex"])
            P.op("scalar", lambda e: e.activation(out=gex[:], in_=gex[:], func=AF.Exp), reads=["gex"], writes=["gex"])
            V(lambda e: e.tensor_reduce(out=gse[:], in_=gex[:], axis=AX.X, op=ALU.add), ["gex"], ["gse"])
            V(lambda e: e.reciprocal(out=gse[:], in_=gse[:]), ["gse"], ["gse"])
            V(lambda e: e.tensor_scalar(out=pen[:], in0=gmask[:], scalar1=1.0, scalar2=1e30, op0=ALU.subtract, op1=ALU.mult), ["gmask"], ["pen"])
            V(lambda e: e.tensor_tensor(out=elm[:].rearrange("p t (g k) -> p t g k", g=4),
                                        in0=lg[:, :, 4:36].rearrange("p t (g k) -> p t g k", g=4),
                                        in1=bc_last(pen[:], 8), op=ALU.add), ["lg", "pen"], ["elm"])
            V(lambda e: e.tensor_reduce(out=m1[:], in_=elm[:], axis=AX.X, op=ALU.max), ["elm"], ["m1"])
            V(lambda e: e.tensor_tensor(out=mk1[:], in0=elm[:], in1=bc_last(m1[:], 32), op=ALU.is_equal), ["elm", "m1"], ["mk1"])
            V(lambda e: e.scalar_tensor_tensor(out=elm2[:], in0=mk1[:], scalar=-1e30, in1=elm[:], op0=ALU.mult, op1=ALU.add), ["mk1", "elm"], ["elm2"])
            V(lambda e: e.tensor_reduce(out=m2[:], in_=elm2[:], axis=AX.X, op=ALU.max), ["elm2"], ["m2"])
            V(lambda e: e.tensor_tensor(out=mk2[:], in0=elm2[:], in1=bc_last(m2[:], 32), op=ALU.is_equal), ["elm2", "m2"], ["mk2"])
            V(lambda e: e.tensor_tensor(out=dd[:], in0=m2[:], in1=m1[:], op=ALU.subtract), ["m1", "m2"], ["dd"])
            P.op("scalar", lambda e: e.activation(out=ee[:], in_=dd[:], func=AF.Exp), reads=["dd"], writes=["ee"])
            V(lambda e: e.tensor_scalar(out=rr[:], in0=ee[:], scalar1=1.0, scalar2=None, op0=ALU.add), ["ee"], ["rr"])
            V(lambda e: e.reciprocal(out=rr[:], in_=rr[:]), ["rr"], ["rr"])
            V(lambda e: e.tensor_tensor(out=wA[:], in0=gse[:], in1=rr[:], op=ALU.mult), ["gse", "rr"], ["wA"])
            V(lambda e: e.tensor_tensor(out=wB[:], in0=wA[:], in1=ee[:], op=ALU.mult), ["wA", "ee"], ["wB"])
            V(lambda e: e.tensor_tensor(out=Mb[:], in0=mk1[:], in1=mk2[:], op=ALU.add), ["mk1", "mk2"], ["Mb"])

        def rout2b(b):
            bb = b % 2
            tok0 = b * BT

            def V(fn, reads, writes):
                P.op("vector", fn, reads=reads, writes=writes)

            for tl in range(4):
                P.op("tensor", lambda e, tl=tl: e.matmul(A0[:, tl * 32:(tl + 1) * 32], lhsT=utri[:], rhs=Mb[:, tl, :], start=True, stop=(tl == 0)),
                     reads=["utri", "Mb"], writes=["A0"])
                for t2 in range(tl):
                    P.op("tensor", lambda e, tl=tl, t2=t2: e.matmul(A0[:, tl * 32:(tl + 1) * 32], lhsT=ones[:], rhs=Mb[:, t2, :], start=False, stop=(t2 == tl - 1)),
                         reads=["ones", "Mb"], writes=["A0"])
            for tl in range(4):
                P.op("tensor", lambda e, tl=tl: e.matmul(A1[:, 0:32], lhsT=ones[:], rhs=Mb[:, tl, :], start=(tl == 0), stop=(tl == 3)),
                     reads=["ones", "Mb"], writes=["A1"])
            V(lambda e: e.tensor_tensor(out=pos[:], in0=A0[:, 0:128].rearrange("p (t n) -> p t n", t=4), in1=bc_mid(cnt[:], 4), op=ALU.add),
              ["A0", "cnt"], ["pos"])
            V(lambda e: e.tensor_tensor(out=cnt[:], in0=cnt[:], in1=A1[:, 0:32], op=ALU.add), ["A1", "cnt", "pos"], ["cnt"])
            V(lambda e: e.tensor_scalar(out=okm[:], in0=pos[:], scalar1=float(CAP), scalar2=None, op0=ALU.is_lt), ["pos"], ["okm"])
            V(lambda e: e.tensor_tensor(out=slot[:], in0=pos[:], in1=bc_mid(ecap[:], 4), op=ALU.add), ["pos", "ecap"], ["slot"])
            V(lambda e: e.tensor_scalar(out=tmp[:], in0=okm[:], scalar1=-1.0e6, scalar2=1.0e6, op0=ALU.mult, op1=ALU.add), ["okm"], ["tmp"])
            V(lambda e: e.tensor_tensor(out=slot[:], in0=slot[:], in1=tmp[:], op=ALU.add), ["slot", "tmp"], ["slot"])
            V(lambda e: e.tensor_scalar(out=slot[:], in0=slot[:], scalar1=float(NSLOT), scalar2=None, op0=ALU.min), ["slot"], ["slot"])
            for k, mk in ((0, mk1), (1, mk2)):
                V(lambda e, mk=mk: e.tensor_tensor(out=tmp[:], in0=mk[:], in1=slot[:], op=ALU.mult), ["mk1", "mk2", "slot"], ["tmp"])
                V(lambda e, k=k: e.tensor_reduce(out=dsel[:, :, k], in_=tmp[:], axis=AX.X, op=ALU.add), ["tmp"], [("dsel", k)])
                V(lambda e, mk=mk: e.tensor_tensor(out=tmp[:], in0=mk[:], in1=okm[:], op=ALU.mult), ["mk1", "mk2", "okm", ("dsel", k)], ["tmp"])
                V(lambda e, k=k: e.tensor_reduce(out=oksel[:, :, k], in_=tmp[:], axis=AX.X, op=ALU.add), ["tmp"], [("oksel", k)])
            tb = 4 * b
            V(lambda e, tb=tb: e.tensor_copy(out=dtab[:, tb:tb + 4, :], in_=dsel[:]), [("dsel", 0), ("dsel", 1)], [("dtab", b)])
            V(lambda e, tb=tb: e.tensor_tensor(out=wtab[:, tb:tb + 4, 0], in0=wA[:], in1=oksel[:, :, 0], op=ALU.mult), ["wA", ("oksel", 0)], [("wtab", b, 0)])
            V(lambda e, tb=tb: e.tensor_tensor(out=wtab[:, tb:tb + 4, 1], in0=wB[:], in1=oksel[:, :, 1], op=ALU.mult), ["wB", ("oksel", 1)], [("wtab", b, 1)])
            for tl in range(4):
                t = 4 * b + tl
                for k in range(2):
                    P.op("gpsimd", lambda e, t=t, k=k, tl=tl, bb=bb: e.indirect_dma_start(
                        out=xs_d[:, :], out_offset=bass.IndirectOffsetOnAxis(ap=dtab[:, t, k:k + 1], axis=0),
                        in_=n2[bb][:, tl, :], in_offset=None),
                        reads=[("n2", bb, tl), ("dtab", b)], writes=[("xs_d", t, k)], dma=True)

        gates2(0)
        for b in range(NB):
            hsec2(b)
            rout2a(b)
            if b + 1 < NB:
                gates2(b + 1)
            if b == NB - 2 and 3 in phases:
                for i in range(2):
                    expert_weight_loads(i, pre3[i]["w1"], pre3[i]["w3"], pre3[i]["w2"], [("w1p", i), ("w3p", i), ("w2p", i)],
                                        extra_writes=WGKEYS + ["wpa", "wpc"])
            rout2b(b)
        if debug:
            P.op("sync", lambda e: e.dma_start(out=dtab_o.ap(), in_=dtab[:].rearrange("p t k -> p (t k)")),
                 reads=[("dtab", b) for b in range(NB)], dma=True, is_out=True)
            P.op("sync", lambda e: e.dma_start(out=wtab_o.ap(), in_=wtab[:].rearrange("p t k -> p (t k)")),
                 reads=[("wtab", b, k) for b in range(NB) for k in range(2)], dma=True, is_out=True)
        P.barrier()
    if 2 in phases:
        phase2()
    sb.reset(base_mark)

    TOP = SBAlloc.HI - 20 * 1024
    p4w = {"wpg": nc.alloc_sbuf_tensor_at("wpg_top", [128, 8, D], BF16, offset=TOP),
           "wpp": nc.alloc_sbuf_tensor_at("wpp_top", [128, 2, D], BF16, offset=TOP + 16 * 1024)}

    def p4_weight_loads():
        wpg_v = wpg_d.ap().rearrange("(c p) n -> p c n", p=128)
        for c in range(0, 8, 4):
            P.op("gpsimd", lambda e, c=c: e.dma_start(out=p4w["wpg"][:, c:c + 4, :], in_=wpg_v[:, c:c + 4, :]), writes=[("wpg", c)], dma=True)
        P.op("gpsimd", lambda e: e.dma_start(out=p4w["wpp"][:], in_=wpp_d.ap().rearrange("(c p) n -> p c n", p=128)), writes=["wpp"], dma=True)

    def phase3():
        NWB = 3
        w1b, w3b, w2b = [], [], []
        for i in range(NWB):
            w1b.append(sb.alloc("w1b%d" % i, [128, 8, 512], BF16))
            w3b.append(sb.alloc("w3b%d" % i, [128, 8, 512], BF16))
            w2b.append(sb.alloc("w2b%d" % i, [128, 4, D], BF16))
        assert sb.off["w1b0"] == BASE and sb.off["w2b1"] == BASE + 24576 + 16384
        preloaded = 2 if 2 in phases else 0
        xr = [sb.alloc("xr%d" % i, [128, 3, D], BF16) for i in range(3)]
        xsT = [sb.alloc("xsT%d" % i, [128, 8, CAP], BF16) for i in range(2)]
        s1 = [sb.alloc("s1%d" % i, [128, CAP], F32) for i in range(2)]
        hdn = [sb.alloc("hdn%d" % i, [128, 4, CAP], BF16) for i in range(2)]
        yb = [sb.alloc("yb%d" % i, [128, D], BF16) for i in range(3)]
        HACC = [(A0, "A0", A1, "A1"), (A2, "A2", B0, ("B0", 0))]
        yctr = [0]
        P.op("gpsimd", lambda e: e.memset(yb[0][:], 0.0), writes=[("yb", 0, 0), ("yb", 0, 1)])
        P.op("sync", lambda e: e.dma_start(out=ys_d.ap()[NSLOT:NSLOT + 128, :], in_=yb[0][:]),
             reads=[("yb", 0, 0), ("yb", 0, 1)], writes=["ys_trash"], dma=True)
        def wload(ex):
            wb_ = ex % NWB
            if ex < preloaded:
                return
            expert_weight_loads(ex, w1b[wb_], w3b[wb_], w2b[wb_], [("w1b", wb_), ("w3b", wb_), ("w2b", wb_)])

        def xsload(ex):
            e3 = ex % 3
            P.op("sync", lambda e, ex=ex, e3=e3: e.dma_start(
                out=xr[e3][:], in_=xs_d.ap()[ex * CAP:(ex + 1) * CAP, :].rearrange("(r p) d -> p r d", p=128)),
                writes=[("xr", e3)], dma=True)

        def xsT_group(ex, r):
            eb = ex % 2
            e3 = ex % 3
            transpose_tile(xr[e3][:, r, :], 8, xsT[eb][:, :, r * 128:(r + 1) * 128], ("xr", e3), ("xsT", eb, r),
                           evac=("scalar" if r % 2 == 0 else "vector"), interleave=8)

        xsload(0)
        wload(0)
        xsload(1)
        wload(1)
        for r in range(3):
            xsT_group(0, r)
        for ex in range(NE):
            eb = ex % 2
            wb_ = ex % NWB
            if ex + 2 < NE:
                wload(ex + 2)
                xsload(ex + 2)
            if ex == 2:
                p4_weight_loads()
            xk = [("xsT", eb, r) for r in range(3)]
            for f in range(4):
                a1, k1, a3, k3 = HACC[f % 2]
                z = f % 2
                for (dst, dkey, wsrc, wkey) in ((a1, k1, w1b, "w1b"), (a3, k3, w3b, "w3b")):
                    for c in range(8):
                        P.op("tensor", lambda e, c=c, dst=dst, wsrc=wsrc, f=f, eb=eb, wb_=wb_: e.matmul(
                            dst[:, 0:CAP], lhsT=wsrc[wb_][:, c, f * 128:(f + 1) * 128], rhs=xsT[eb][:, c, :], start=(c == 0), stop=(c == 7)),
                            reads=[(wkey, wb_)] + xk, writes=[dkey])
                P.op("scalar", lambda e, a1=a1, z=z: e.activation(out=s1[z][:], in_=a1[:, 0:CAP], func=AF.Silu), reads=[k1], writes=[("s1", z)])
                P.op("vector", lambda e, a3=a3, z=z, f=f, eb=eb: e.tensor_tensor(out=hdn[eb][:, f, :], in0=s1[z][:], in1=a3[:, 0:CAP], op=ALU.mult),
                     reads=[("s1", z), k3], writes=[("hdn", eb, f)])
            hk_ = [("hdn", eb, f) for f in range(4)]
            for r in range(3):
                if ex + 1 < NE:
                    xsT_group(ex + 1, r)
                ys_ = yctr[0] % 3
                yctr[0] += 1
                for half in range(2):
                    for f in range(4):
                        P.op("tensor", lambda e, f=f, half=half, r=r, eb=eb, wb_=wb_: e.matmul(
                            B1[:, half * 512:(half + 1) * 512], lhsT=hdn[eb][:, f, r * 128:(r + 1) * 128], rhs=w2b[wb_][:, f, half * 512:(half + 1) * 512],
                            start=(f == 0), stop=(f == 3)),
                            reads=hk_ + [("w2b", wb_)], writes=[("B1", half)])
                    if half == 0:
                        P.op("scalar", lambda e, ys_=ys_: e.activation(out=yb[ys_][:, 0:512], in_=B1[:, 0:512], func=AF.Copy),
                             reads=[("B1", 0)], writes=[("yb", ys_, 0)])
                    else:
                        P.op("vector", lambda e, ys_=ys_: e.tensor_copy(out=yb[ys_][:, 512:1024], in_=B1[:, 512:1024]),
                             reads=[("B1", 1)], writes=[("yb", ys_, 1)])
                row0 = ex * CAP + r * 128
                P.op("sync", lambda e, row0=row0, ys_=ys_: e.dma_start(out=ys_d.ap()[row0:row0 + 128, :], in_=yb[ys_][:]),
                     reads=[("yb", ys_, 0), ("yb", ys_, 1)], writes=[("ys_d", ex, r)], dma=True)
        P.barrier()
    if 3 in phases:
        phase3()
    sb.reset(base_mark)

    def phase4():
        wpg, wpp = p4w["wpg"], p4w["wpp"]
        gple = sb.alloc("gple", [128, D], F32)
        bpg = sb.alloc("bpg", [128, D], F32)
        hb = [sb.alloc("hb%d" % i, [128, D], F32) for i in range(3)]
        y1 = [sb.alloc("y1%d" % i, [128, D], BF16) for i in range(3)]
        y2 = [sb.alloc("y2%d" % i, [128, D], BF16) for i in range(3)]
        pin = [sb.alloc("pin%d" % i, [128, 256], F32) for i in range(3)]
        pbf = [sb.alloc("pbf%d" % i, [128, 256], BF16) for i in range(2)]
        ppT = [sb.alloc("ppT%d" % i, [128, 2, 128], BF16) for i in range(2)]
        n3 = [sb.alloc("n3%d" % i, [128, D], BF16) for i in range(2)]
        n3T = [sb.alloc("n3T%d" % i, [128, 8, 128], BF16) for i in range(2)]
        gz = [sb.alloc("gz%d" % i, [128, D], F32) for i in range(2)]
        ob = [sb.alloc("ob%d" % i, [128, D], F32) for i in range(2)]
        ld("sync", gple[:], gple_d.ap().partition_broadcast(128), "gple")
        ld("sync", bpg[:], bpg_d.ap().partition_broadcast(128), "bpg")
        def loads4(t):
            h3 = t % 3
            P.op("sync", lambda e, t=t, h3=h3: e.dma_start(out=hb[h3][:], in_=h_d.ap()[t * 128:(t + 1) * 128, :]), writes=[("hb", h3)], dma=True)
            P.op("sync", lambda e, t=t, h3=h3: e.dma_start(out=pin[h3][:], in_=p_d.ap()[t * 128:(t + 1) * 128, :]), writes=[("pin", h3)], dma=True)
            for (yy, ykey, k) in ((y1, "y1", 0), (y2, "y2", 1)):
                P.op("gpsimd", lambda e, yy=yy, k=k, t=t, h3=h3: e.indirect_dma_start(
                    out=yy[h3][:, :], out_offset=None, in_=ys_d[:, :], in_offset=bass.IndirectOffsetOnAxis(ap=dtab[:, t, k:k + 1], axis=0)), reads=["dtab_all"], writes=[(ykey, h3)], dma=True)

        def S1a(t):
            h3 = t % 3
            z = t % 2
            P.op("vector", lambda e, h3=h3, t=t: e.scalar_tensor_tensor(out=hb[h3][:], in0=y1[h3][:], scalar=wtab[:, t, 0:1], in1=hb[h3][:],
                                                                       op0=ALU.mult, op1=ALU.add), reads=[("y1", h3), ("hb", h3)], writes=[("hb", h3)])
            P.op("vector", lambda e, h3=h3, t=t: e.scalar_tensor_tensor(out=hb[h3][:], in0=y2[h3][:], scalar=wtab[:, t, 1:2], in1=hb[h3][:],
                                                                       op0=ALU.mult, op1=ALU.add), reads=[("y2", h3), ("hb", h3)], writes=[("hb", h3)])
            rmsnorm_tile(hb[h3][:], gple[:], n3[z][:], ("hb", h3), "gple", ("n3", z))
            P.op("scalar", lambda e, z=z, h3=h3: e.activation(out=pbf[z][:], in_=pin[h3][:], func=AF.Copy), reads=[("pin", h3)], writes=[("pbf", z)])

        def S1b(t):
            z = t % 2
            transpose_tile(n3[z], 8, n3T[z][:], ("n3", z), ("n3T", z))
            transpose_tile(pbf[z], 2, ppT[z][:], ("pbf", z), ("ppT", z))

        GACC = [((A0, "A0"), (A2, "A2")), ((A1, "A1"), (B0, ("B0", 0)))]

        def S2mm(t, half):
            z = t % 2
            (ga, gk), (pa_, pk) = GACC[half]
            for c in range(8):
                P.op("tensor", lambda e, c=c, half=half, ga=ga, z=z: e.matmul(
                    ga[:], lhsT=n3T[z][:, c, :], rhs=wpg[:, c, half * 512:(half + 1) * 512], start=(c == 0), stop=(c == 7)),
                    reads=[("n3T", z), ("wpg", 0), ("wpg", 4)], writes=[gk])
            for c in range(2):
                P.op("tensor", lambda e, c=c, half=half, pa_=pa_, z=z: e.matmul(
                    pa_[:, 0:512], lhsT=ppT[z][:, c, :], rhs=wpp[:, c, half * 512:(half + 1) * 512], start=(c == 0), stop=(c == 1)),
                    reads=[("ppT", z), "wpp"], writes=[pk])

        def S2tail(t):
            z = t % 2
            h3 = t % 3
            hsl = [slice(0, 512), slice(512, 1024)]
            for half in range(2):
                (ga, gk), (pa_, pk) = GACC[half]
                hs = hsl[half]
                P.op("vector", lambda e, ga=ga, z=z, hs=hs: e.tensor_tensor(out=gz[z][:, hs], in0=ga[:], in1=bpg[:, hs], op=ALU.add),
                     reads=[gk, "bpg"], writes=[("gz", z, half)])
                P.op("scalar", lambda e, z=z, hs=hs: e.activation(out=gz[z][:, hs], in_=gz[z][:, hs], func=AF.Tanh, scale=0.5),
                     reads=[("gz", z, half)], writes=[("gz", z, half)])
            for half in range(2):
                (ga, gk), (pa_, pk) = GACC[half]
                hs = hsl[half]
                P.op("vector", lambda e, pa_=pa_, z=z, hs=hs: e.scalar_tensor_tensor(out=gz[z][:, hs], in0=gz[z][:, hs], scalar=1.0, in1=pa_[:, 0:512],
                                                                                    op0=ALU.add, op1=ALU.mult), reads=[("gz", z, half), pk], writes=[("gz", z, half)])
                P.op("vector", lambda e, z=z, hs=hs, h3=h3: e.scalar_tensor_tensor(out=ob[z][:, hs], in0=gz[z][:, hs], scalar=0.5, in1=hb[h3][:, hs],
                                                                                  op0=ALU.mult, op1=ALU.add), reads=[("gz", z, half), ("hb", h3)], writes=[("ob", z, half)])

        loads4(0)
        loads4(1)
        S1a(0)
        S1b(0)
        for t in range(NT):
            z = t % 2
            if t + 2 < NT:
                loads4(t + 2)
            if t + 1 < NT:
                S1a(t + 1)
            S2mm(t, 0)
            S2mm(t, 1)
            S2tail(t)
            if t + 1 < NT:
                S1b(t + 1)
            P.op("sync", lambda e, t=t, z=z: e.dma_start(out=out_d.ap()[t * 128:(t + 1) * 128, :], in_=ob[z][:]),
                 reads=[("ob", z, 0), ("ob", z, 1)], dma=True, is_out=True)
    if 4 in phases:
        phase4()
    P.emit()
    return nc, P


def _sel_tables():
    f = np.arange(128)[:, None, None]
    j = np.arange(8)[None, :, None]
    m = np.arange(128)[None, None, :]
    hit = ((m // 8) == (2 * j + f // 64)).astype(np.float32)
    selA = (hit / 64.0).reshape(128, 8 * 128).astype(ml_dtypes.bfloat16)
    selB = (hit.transpose(2, 1, 0) / 8.0).reshape(128, 8 * 128).astype(ml_dtypes.bfloat16)
    return np.ascontiguousarray(selA), np.ascontiguousarray(selB)


def _host_layout(inp):
    f = lambda a: np.ascontiguousarray(np.asarray(a, dtype=np.float32))
    bf = ml_dtypes.bfloat16
    b_in = f(inp["b_in"])[0]
    rel = f(inp["rel_bias"])[0]
    jj = np.arange(5)[::-1][:, None, None]
    kk = np.arange(128)[None, :, None]
    qq = np.arange(128)[None, None, :]
    dist = qq - kk + 128 * (4 - jj)
    idx = np.clip(dist, -63, 256) + 63
    cdiff = (qq // 64) - (kk // 64) + 2 * (4 - jj)
    mask = ((cdiff >= 0) & (cdiff <= 8)).astype(np.float32)
    rbT = rel[:, idx]
    rbT = np.ascontiguousarray(rbT.transpose(2, 0, 1, 3)).reshape(128, NH * 5 * 128)
    maskT = np.ascontiguousarray(mask.transpose(1, 0, 2)).reshape(128, 5 * 128)
    cwv = f(inp["conv_w"])[0]
    cw = np.ascontiguousarray(cwv.reshape(3, 4, 128).transpose(2, 1, 0)).reshape(128, 12)
    cb = np.ascontiguousarray(f(inp["conv_b"])[0].reshape(4, 128).T)
    gq = f(inp["g_q"])[0]
    gk = f(inp["g_k"])[0]
    shared = {
        "g_mix": f(inp["g_mix"]),
        "w_in": f(inp["w_in"])[0],
        "bcol": np.ascontiguousarray(b_in.reshape(40, 128).T),
        "gqk": np.ascontiguousarray(np.stack([np.tile(gq, 2), np.tile(gk, 2)], axis=1)),
        "bv": np.ascontiguousarray(b_in[1024:1536].reshape(1, 512)),
        "rbT": rbT, "maskT": maskT, "cw": cw, "cb": cb,
        "w_pa": f(inp["w_pa"])[0], "w_pc": f(inp["w_pc"])[0], "w_o": f(inp["w_o"])[0],
        "g_ffn": f(inp["g_ffn"]),
        "w_rg": np.ascontiguousarray(np.concatenate([f(inp["w_group"])[0], f(inp["w_router"])[0]], axis=1)),
        "b_rg": np.ascontiguousarray(np.concatenate([f(inp["b_group"])[0], f(inp["b_router"])[0]])[None, :]),
        "w1": f(inp["w1"])[0], "w3": f(inp["w3"])[0], "w2": f(inp["w2"])[0],
        "g_ple": f(inp["g_ple"]), "w_pg": f(inp["w_ple_gate"])[0], "b_pg": f(inp["b_ple_gate"]),
        "w_pp": f(inp["w_ple_proj"])[0],
        "ident": np.eye(128, dtype=np.float32).astype(bf),
        "utri": np.triu(np.ones((128, 128), np.float32), 1).astype(bf),
        "ones": np.ones((128, 128), np.float32).astype(bf),
        "bdiag": (np.kron(np.eye(2, dtype=np.float32), np.ones((64, 64), np.float32)) / 64.0).astype(bf),
        "ecap": np.ascontiguousarray(np.broadcast_to((np.arange(NE, dtype=np.float32) * CAP)[None, :], (128, NE))),
        "selA": _sel_tables()[0], "selB": _sel_tables()[1],
    }
    x = f(inp["x"])
    p = f(inp["p"])[0]
    maps = []
    for c in range(NCORES):
        m = dict(shared)
        m["x"] = x[c]
        m["p"] = p[c]
        maps.append(m)
    return maps


_CACHE = {}


def kernel(**inputs):
    if "nc" not in _CACHE:
        _CACHE["nc"] = build(debug=False)[0]
    nc = _CACHE["nc"]
    maps = _host_layout(inputs)
    res = run_bass_kernel_spmd(nc, maps, core_ids=list(range(NCORES)))
    out = np.stack([np.asarray(res.results[c]["out"], dtype=np.float32) for c in range(NCORES)], axis=0)
    return out
```

```python
import numpy as np
import ml_dtypes
import concourse.bass as bass
import concourse.mybir as mybir
from concourse.bass_utils import run_bass_kernel_spmd

F32 = mybir.dt.float32
BF16 = mybir.dt.bfloat16
I32 = mybir.dt.int32
ALU = mybir.AluOpType
AF = mybir.ActivationFunctionType
AX = mybir.AxisListType

NCORES = 8
S = 4096
D = 1024
NT = S // 128
BT = 512
NB = S // BT
NH = 8
DH = 64
NE = 32
CAP = 384
NSLOT = NE * CAP
KR = 12
EPS = 1e-6

ENGS = ("sync", "scalar", "vector", "gpsimd", "tensor")
NDMASEM = 24
SAME_ENG_WINDOW = 10 ** 9


class Op:
    __slots__ = ("idx", "eng", "fn", "reads", "writes", "dma", "deps", "sig",
                 "sem", "val", "clock", "epos", "barrier")


class Prog:
    def __init__(self, nc):
        self.nc = nc
        self.ops = []
        self.last_w = {}
        self.readers = {}
        self.out_ops = []
        self.last_barrier = None
        self.since_barrier = []

    def op(self, eng, fn, reads=(), writes=(), dma=False, is_out=False):
        o = Op()
        o.idx = len(self.ops)
        o.eng = eng
        o.fn = fn
        o.dma = dma
        o.barrier = False
        o.reads = tuple(reads)
        o.writes = tuple(writes)
        deps = set()
        for k in o.reads:
            w = self.last_w.get(k)
            if w is not None:
                deps.add(w)
        for k in o.writes:
            w = self.last_w.get(k)
            if w is not None:
                deps.add(w)
            for r in self.readers.get(k, ()):
                deps.add(r)
        for k in o.writes:
            self.last_w[k] = o.idx
            self.readers[k] = []
        for k in o.reads:
            if k not in o.writes:
                self.readers.setdefault(k, []).append(o.idx)
        if self.last_barrier is not None:
            deps.add(self.last_barrier)
        deps.discard(o.idx)
        o.deps = sorted(deps)
        o.sig = False
        self.ops.append(o)
        self.since_barrier.append(o.idx)
        if is_out:
            self.out_ops.append(o.idx)
        return o.idx

    def barrier(self):
        o = Op()
        o.idx = len(self.ops)
        o.eng = "sync"
        o.fn = "BARRIER"
        o.dma = False
        o.barrier = True
        o.reads = ()
        o.writes = ()
        last = {}
        deps = []
        for i in self.since_barrier:
            p = self.ops[i]
            if p.dma:
                deps.append(i)
            else:
                last[p.eng] = i
        deps.extend(last.values())
        if self.last_barrier is not None:
            deps.append(self.last_barrier)
        o.deps = sorted(set(deps))
        o.sig = True
        self.ops.append(o)
        self.last_barrier = o.idx
        self.since_barrier = []
        self.last_w = {}
        self.readers = {}

    def emit(self):
        nc = self.nc
        ops = self.ops
        epos = {e: 0 for e in ENGS}
        for o in ops:
            o.epos = epos[o.eng]
            epos[o.eng] += 1
        fin = Op()
        fin.idx = len(ops)
        fin.eng = "sync"
        fin.fn = None
        fin.dma = False
        fin.barrier = False
        fin.reads = ()
        fin.writes = ()
        fin.deps = list(self.out_ops)
        fin.sig = False
        fin.epos = epos["sync"]
        ops = ops + [fin]
        for o in ops:
            nd = []
            for d in o.deps:
                do = ops[d]
                if do.eng == o.eng and not do.dma and not o.barrier:
                    if o.eng == "tensor" and not o.dma:
                        continue
                    if o.dma:
                        pass
                    elif o.epos - do.epos > SAME_ENG_WINDOW:
                        continue
                nd.append(d)
            o.deps = nd
            for d in nd:
                ops[d].sig = True
        sems = {}
        dma_engs = set(o.eng for o in ops if o.dma)
        for e in ENGS:
            sems[("c", e)] = nc.alloc_semaphore("c_" + e)
            if e in dma_engs:
                for i in range(NDMASEM):
                    sems[("d", e, i)] = nc.alloc_semaphore("d_%s_%d" % (e, i))
        ccount = {e: 0 for e in ENGS}
        dcount = {e: 0 for e in ENGS}
        dma_prev = {}
        for o in ops:
            if o.dma:
                k = dcount[o.eng]
                dcount[o.eng] += 1
                slot = k % NDMASEM
                o.sem = ("d", o.eng, slot)
                o.val = 16 * (k // NDMASEM + 1)
                prev = dma_prev.get((o.eng, slot))
                if prev is not None and prev not in o.deps:
                    o.deps.append(prev)
                dma_prev[(o.eng, slot)] = o.idx
            elif o.sig:
                ccount[o.eng] += 1
                o.sem = ("c", o.eng)
                o.val = ccount[o.eng]
            else:
                o.sem = None
                o.val = 0
        known = {e: {} for e in ENGS}
        streams = {e: [] for e in ENGS}
        for o in ops:
            kn = known[o.eng]
            wm = {}
            for d in sorted(o.deps, reverse=True):
                do = ops[d]
                if kn.get(do.sem, 0) >= do.val:
                    continue
                if wm.get(do.sem, 0) < do.val:
                    wm[do.sem] = do.val
                for s, v in do.clock.items():
                    if kn.get(s, 0) < v:
                        kn[s] = v
            o.clock = dict(kn)
            if o.sem is not None:
                o.clock[o.sem] = o.val
            streams[o.eng].append((o, list(wm.items())))
        self.n_waits = sum(len(w) for st in streams.values() for _, w in st)
        self.counts = (dict(ccount), dict(dcount))

        def run_stream(eng_name):
            def body(eng):
                for o, waits in streams[eng_name]:
                    for s, v in waits:
                        eng.wait_ge(sems[s], v)
                    if o.fn is None:
                        continue
                    if o.barrier:
                        eng.sem_inc(sems[o.sem], 1)
                        continue
                    ins = o.fn(eng)
                    if o.sem is not None:
                        ins.then_inc(sems[o.sem], 16 if o.dma else 1)
            return body

        with nc.Block() as block:
            for e in ENGS:
                if streams[e]:
                    getattr(block, e)(run_stream(e))


class SBAlloc:
    LO = 16512
    HI = 229344

    def __init__(self, nc):
        self.nc = nc
        self.cur = self.LO
        self.n = 0

    def alloc(self, name, shape, dt):
        esz = {F32: 4, BF16: 2, I32: 4}[dt]
        nbytes = esz
        for s in shape[1:]:
            nbytes *= s
        off = (self.cur + 31) // 32 * 32
        assert off + nbytes <= self.HI, "SBUF overflow at %s: need %d have %d" % (name, nbytes, self.HI - off)
        self.n += 1
        t = self.nc.alloc_sbuf_tensor_at("%s_%d" % (name, self.n), list(shape), dt, offset=off)
        self.cur = off + nbytes
        self.off = getattr(self, "off", {})
        self.off[name] = off
        return t

    def alloc_alias(self, name, shape, dt, of):
        self.n += 1
        return self.nc.alloc_sbuf_tensor_at("%s_%d" % (name, self.n), list(shape), dt, offset=self.off[of])

    def mark(self):
        return self.cur

    def reset(self, m):
        self.cur = m


def bc_last(ap, n):
    shp = list(ap.shape)
    return ap.unsqueeze(len(shp)).broadcast_to(shp + [n])


def bc_mid(ap, n):
    shp = list(ap.shape)
    return ap.unsqueeze(1).broadcast_to([shp[0], n] + shp[1:])


def build(debug=False, phases=(1, 2, 3, 4)):
    nc = bass.Bass("TRN2", target_bir_lowering=False)
    P = Prog(nc)
    sb = SBAlloc(nc)

    def din(name, shape, dt=F32):
        return nc.dram_tensor(name, list(shape), dt, kind="ExternalInput")

    def dscr(name, shape, dt):
        return nc.dram_tensor(name, list(shape), dt, kind="ExternalOutput" if debug else "Internal")

    x_d = din("x", [S, D])
    p_d = din("p", [S, 256])
    gmix_d = din("g_mix", [1, D])
    win_d = din("w_in", [D, 5120])
    bcol_d = din("bcol", [128, 40])
    gqk_d = din("gqk", [128, 2])
    bv_d = din("bv", [1, 512])
    rbT_d = din("rbT", [128, NH * 5 * 128])
    maskT_d = din("maskT", [128, 5 * 128])
    cw_d = din("cw", [128, 12])
    cb_d = din("cb", [128, 4])
    wpa_d = din("w_pa", [512, D])
    wpc_d = din("w_pc", [512, D])
    wo_d = din("w_o", [D, D])
    gffn_d = din("g_ffn", [1, D])
    wrg_d = din("w_rg", [D, 36])
    brg_d = din("b_rg", [1, 36])
    w1_d = din("w1", [NE, D, 512])
    w3_d = din("w3", [NE, D, 512])
    w2_d = din("w2", [NE, 512, D])
    gple_d = din("g_ple", [1, D])
    wpg_d = din("w_pg", [D, D])
    bpg_d = din("b_pg", [1, D])
    wpp_d = din("w_pp", [256, D])
    ident_d = din("ident", [128, 128], BF16)
    utri_d = din("utri", [128, 128], BF16)
    ones_d = din("ones", [128, 128], BF16)
    bdiag_d = din("bdiag", [128, 128], BF16)
    ecap_d = din("ecap", [128, NE])
    selA_d = din("selA", [128, 8 * 128], BF16)
    selB_d = din("selB", [128, 8 * 128], BF16)
    out_d = nc.dram_tensor("out", [S, D], F32, kind="ExternalOutput")

    nT_d = dscr("nT_s", [D, S], BF16)
    yaT_d = dscr("yaT_s", [512, S], BF16)
    ycT_d = dscr("ycT_s", [512, S], BF16)
    h_d = dscr("h_s", [S, D], F32)
    xs_d = dscr("xs_s", [NSLOT + 128, D], BF16)
    ys_d = dscr("ys_s", [NSLOT + 128, D], BF16)
    if debug:
        dtab_o = nc.dram_tensor("dtab_o", [128, NT * 2], I32, kind="ExternalOutput")
        wtab_o = nc.dram_tensor("wtab_o", [128, NT * 2], F32, kind="ExternalOutput")

    pT = nc.alloc_psum_tensor("pT", [128, 8, 128], BF16)
    A0 = nc.alloc_psum_tensor("A0", [128, 512], F32)
    A1 = nc.alloc_psum_tensor("A1", [128, 512], F32)
    A2 = nc.alloc_psum_tensor("A2", [128, 512], F32)
    B0 = nc.alloc_psum_tensor("B0", [128, 1024], F32)
    B1 = nc.alloc_psum_tensor("B1", [128, 1024], F32)

    ident = sb.alloc("ident", [128, 128], BF16)
    utri = sb.alloc("utri", [128, 128], BF16)
    ones = sb.alloc("ones", [128, 128], BF16)
    bdiag = sb.alloc("bdiag", [128, 128], BF16)
    ecap = sb.alloc("ecap", [128, NE], F32)
    bcol = sb.alloc("bcol", [128, 40], F32)
    hbcol = sb.alloc("hbcol", [128, 40], F32)
    mhalf = sb.alloc("mhalf", [128, 8], F32)
    epsc = sb.alloc("epsc", [128, 8], F32)
    dtab = sb.alloc("dtab", [128, NT, 2], I32)
    wtab = sb.alloc("wtab", [128, NT, 2], F32)
    cnt = sb.alloc("cnt", [128, NE], F32)
    ss = sb.alloc("ss", [128, 8], F32)
    rs = sb.alloc("rs", [128, 8], F32)
    junk = sb.alloc("junk", [128, D], BF16)

    def ld(eng, dst, src, key):
        P.op(eng, lambda e: e.dma_start(out=dst, in_=src), writes=[key], dma=True)

    ld("sync", ident[:], ident_d.ap(), "ident")
    ld("sync", utri[:], utri_d.ap(), "utri")
    ld("sync", ones[:], ones_d.ap(), "ones")
    ld("sync", bdiag[:], bdiag_d.ap(), "bdiag")
    ld("sync", ecap[:], ecap_d.ap(), "ecap")
    ld("sync", bcol[:], bcol_d.ap(), "bcol")
    P.op("vector", lambda e: e.tensor_scalar(out=hbcol[:], in0=bcol[:], scalar1=0.5, scalar2=None, op0=ALU.mult),
         reads=["bcol"], writes=["hbcol"])
    P.op("gpsimd", lambda e: e.memset(mhalf[:], -0.5), writes=["mhalf"])
    P.op("gpsimd", lambda e: e.memset(epsc[:], EPS), writes=["epsc"])
    P.op("gpsimd", lambda e: e.memset(cnt[:], 0.0), writes=["cnt"])
    P.op("gpsimd", lambda e: e.memset(wtab[:], 0.0), writes=["wtab"])

    nrm_ctr = [0]

    def rmsnorm_tile(src, g_bc, dst_bf, src_key, g_key, dst_key):
        i = nrm_ctr[0] % 8
        nrm_ctr[0] += 1
        ssk, rsk = ("ss", i), ("rs", i)
        P.op("scalar", lambda e: e.activation(out=junk[:], in_=src, func=AF.Square, accum_out=ss[:, i:i + 1]),
             reads=[src_key], writes=["junk", ssk])
        P.op("vector", lambda e: e.tensor_scalar(out=rs[:, i:i + 1], in0=ss[:, i:i + 1], scalar1=1.0 / D, scalar2=EPS,
                                                  op0=ALU.mult, op1=ALU.add), reads=[ssk], writes=[rsk])
        P.op("gpsimd", lambda e: e.tensor_tensor(out=rs[:, i:i + 1], in0=rs[:, i:i + 1], in1=mhalf[:, 0:1], op=ALU.pow),
             reads=[rsk, "mhalf"], writes=[rsk])
        P.op("vector", lambda e: e.scalar_tensor_tensor(out=dst_bf, in0=src, scalar=rs[:, i:i + 1], in1=g_bc,
                                                         op0=ALU.mult, op1=ALU.mult),
             reads=[src_key, rsk, g_key], writes=[dst_key])

    def transpose_tile(src_bf, nchunk, dst, src_key, dst_key, evac="scalar", interleave=0):
        for c in range(nchunk):
            if interleave:
                src_c = src_bf.rearrange("t (p c) -> t c p", c=interleave)[:, c, :]
            else:
                src_c = src_bf[:, c * 128:(c + 1) * 128]
            P.op("tensor", lambda e, c=c, src_c=src_c: e.transpose(out=pT[:, c, :], in_=src_c, identity=ident[:]),
                 reads=(list(src_key) if isinstance(src_key, list) else [src_key]) + ["ident"], writes=[("pT", c)])
        if evac == "scalar":
            P.op("scalar", lambda e: e.activation(out=dst, in_=pT[:, 0:nchunk, :], func=AF.Copy),
                 reads=[("pT", c) for c in range(nchunk)], writes=[dst_key])
        else:
            P.op("vector", lambda e: e.tensor_copy(out=dst, in_=pT[:, 0:nchunk, :]),
                 reads=[("pT", c) for c in range(nchunk)], writes=[dst_key])

    _breg = {}

    def breg(e):
        if "r" not in _breg:
            _breg["r"] = e.to_reg(NSLOT - 1)
        return _breg["r"]

    base_mark = sb.mark()
    BASE = (base_mark + 31) // 32 * 32
    pre2 = {"Wg": nc.alloc_sbuf_tensor_at("Wg_pre", [128, 8, 2048], BF16, offset=BASE),
            "wpa": nc.alloc_sbuf_tensor_at("wpa_pre", [128, 4, D], BF16, offset=BASE + 32768),
            "wpc": nc.alloc_sbuf_tensor_at("wpc_pre", [128, 4, D], BF16, offset=BASE + 40960)}
    pre3 = [{"w1": nc.alloc_sbuf_tensor_at("w1_pre%d" % i, [128, 8, 512], BF16, offset=BASE + i * 24576),
             "w3": nc.alloc_sbuf_tensor_at("w3_pre%d" % i, [128, 8, 512], BF16, offset=BASE + i * 24576 + 8192),
             "w2": nc.alloc_sbuf_tensor_at("w2_pre%d" % i, [128, 4, D], BF16, offset=BASE + i * 24576 + 16384)} for i in range(2)]
    WGKEYS = [("Wg", g0 + q4 * 256) for q4 in range(4) for g0 in (0, 1024)]

    def p2_weight_loads(Wg, wpa, wpc, extra_writes=()):
        win_v2 = win_d.ap().rearrange("(c p) n -> p c n", p=128)
        ew = list(extra_writes)

        def wg_load(q4):
            for g0 in (0, 1024):
                c0_ = g0 + q4 * 256
                P.op("gpsimd", lambda e, c0_=c0_: e.dma_start(out=Wg[:, :, c0_:c0_ + 256], in_=win_v2[:, :, 3072 + c0_:3072 + c0_ + 256]),
                     writes=[("Wg", c0_)] + ew, dma=True)

        wg_load(0)
        P.op("gpsimd", lambda e: e.dma_start(out=wpa[:], in_=wpa_d.ap().rearrange("(c p) n -> p c n", p=128)), writes=["wpa"] + ew, dma=True)
        P.op("gpsimd", lambda e: e.dma_start(out=wpc[:], in_=wpc_d.ap().rearrange("(c p) n -> p c n", p=128)), writes=["wpc"] + ew, dma=True)
        for q4 in range(1, 4):
            wg_load(q4)

    def expert_weight_loads(ex, w1t, w3t, w2t, keys, extra_writes=()):
        ew = list(extra_writes)
        P.op("gpsimd", lambda e: e.dma_start(out=w1t[:], in_=w1_d.ap()[ex].rearrange("(p c) f -> p c f", c=8)), writes=[keys[0]] + ew, dma=True)
        P.op("gpsimd", lambda e: e.dma_start(out=w3t[:], in_=w3_d.ap()[ex].rearrange("(p c) f -> p c f", c=8)), writes=[keys[1]] + ew, dma=True)
        P.op("gpsimd", lambda e: e.dma_start(out=w2t[:], in_=w2_d.ap()[ex].rearrange("(c p) f -> p c f", p=128)), writes=[keys[2]] + ew, dma=True)

    def phase1():
        Wa = sb.alloc("Wa", [128, 8, 3072], BF16)
        gmix = sb.alloc("gmix", [128, D], F32)
        gqk = sb.alloc("gqk", [128, 2], F32)
        bvb = sb.alloc("bvb", [128, 512], F32)
        cw = sb.alloc("cw", [128, 12], F32)
        cb = sb.alloc("cb", [128, 4], F32)
        expB = sb.alloc("expB", [128, NH, 5, 128], BF16)
        maskT = sb.alloc("maskT", [128, 5, 128], F32)
        kring = sb.alloc("kring", [128, 4, KR * 128], BF16)
        vring = sb.alloc("vring", [128, KR, NH, 65], BF16)
        xt = [sb.alloc("xt%d" % i, [128, D], F32) for i in range(2)]
        nb = [sb.alloc("nb%d" % i, [128, D], BF16) for i in range(2)]
        nTb = [sb.alloc("nTb%d" % i, [128, 8, BT], BF16) for i in range(2)]
        qT = [sb.alloc("qT%d" % i, [128, 4, BT], BF16) for i in range(2)]
        zq = [sb.alloc("zq%d" % i, [128, BT], F32) for i in range(8)]
        sq = [sb.alloc("sq%d" % i, [128, BT], BF16) for i in range(3)]
        rs = sb.alloc("rs_all", [128, BT], F32)
        r1b = sb.alloc("r1b", [128, BT], BF16)
        selA = sb.alloc("selA", [128, 8, 128], BF16)
        selB = sb.alloc("selB", [128, 8, 128], BF16)
        gw = sb.alloc("gw", [128, 1], F32)
        us = [sb.alloc("us%d" % i, [128, BT], F32) for i in range(2)]
        t1 = [sb.alloc("t1%d" % i, [128, BT], F32) for i in range(2)]
        cu = sb.alloc("cu", [128, 4, BT + 2], F32)
        ycT = [sb.alloc("ycT%d" % i, [128, 4, BT], BF16) for i in range(2)]
        yaT = [sb.alloc("yaT%d" % i, [128, 4, BT], BF16) for i in range(2)]
        pt = [sb.alloc("pt%d" % i, [128, 4, 128], BF16) for i in range(6)]
        rden = [sb.alloc("rden%d" % i, [128, 4], F32) for i in range(2)]
        ya = sb.alloc("ya", [128, 4, 512], BF16)

        win_v = win_d.ap().rearrange("(c p) n -> p c n", p=128)
        for (c0_, c1_) in ((0, 1024), (1024, 1536), (1536, 3072)):
            for c in range(0, 8, 2):
                P.op("gpsimd", lambda e, c=c, c0_=c0_, c1_=c1_: e.dma_start(out=Wa[:, c:c + 2, c0_:c1_], in_=win_v[:, c:c + 2, c0_:c1_]),
                     writes=[("Wa", c, c0_), ("Wa", c + 1, c0_)], dma=True)
        zt = sb.alloc("zt", [128, 2 * D], BF16)
        P.op("gpsimd", lambda e: e.memset(zt[:], 0.0), writes=["zt"])
        NR = (NSLOT + 128) // 128
        xs_z = xs_d.ap().rearrange("(p r) d -> p (r d)", p=128)
        zchunks = [(r0, min(r0 + 2, NR)) for r0 in range(0, NR, 2)]

        def zero_fill(k):
            for (r0, r1_) in zchunks[k::NB]:
                P.op("sync", lambda e, r0=r0, r1_=r1_: e.dma_start(out=xs_z[:, r0 * D:r1_ * D], in_=zt[:, 0:(r1_ - r0) * D]),
                     reads=["zt"], writes=[("xs_zero", r0)], dma=True)

        ld("sync", gmix[:], gmix_d.ap().partition_broadcast(128), "gmix")
        ld("sync", gqk[:], gqk_d.ap(), "gqk")
        ld("sync", selA[:], selA_d.ap().rearrange("p (j m) -> p j m", j=8), "selA")
        ld("sync", selB[:], selB_d.ap().rearrange("p (j m) -> p j m", j=8), "selB")
        ld("sync", bvb[:], bv_d.ap().partition_broadcast(128), "bvb")
        P.op("vector", lambda e: e.tensor_tensor(out=gw[:], in0=gqk[:, 0:1], in1=gqk[:, 1:2], op=ALU.mult), reads=["gqk"], writes=["gw"])
        ld("sync", cw[:], cw_d.ap(), "cw")
        ld("sync", cb[:], cb_d.ap(), "cb")
        ld("sync", maskT[:], maskT_d.ap().rearrange("p (j q) -> p j q", j=5), "maskT")
        rb_v = rbT_d.ap().rearrange("p (h n) -> p h n", h=NH)
        stg = [sb.alloc_alias("stg0", [128, 640], F32, "zq0"), sb.alloc_alias("stg1", [128, 640], F32, "zq2")]
        for h in range(NH):
            st = stg[h % 2]
            sk = [("zq", 2 * (h % 2)), ("zq", 2 * (h % 2) + 1)]
            P.op("sync", lambda e, h=h, st=st: e.dma_start(out=st[:], in_=rb_v[:, h, :]), writes=sk, dma=True)
            P.op("scalar", lambda e, st=st: e.activation(out=st[:], in_=st[:], func=AF.Exp), reads=sk, writes=sk)
            P.op("vector", lambda e, h=h, st=st: e.tensor_tensor(
                out=expB[:, h, :, :], in0=st[:].rearrange("p (j q) -> p j q", j=5), in1=maskT[:], op=ALU.mult),
                reads=sk + ["maskT"], writes=[("expB", h)])
        P.op("gpsimd", lambda e: e.memset(vring[:], 1.0), writes=[("v", s_) for s_ in range(KR)])
        P.op("gpsimd", lambda e: e.memset(cu[:], 0.0), writes=[("cu", ct) for ct in range(4)] + [("cuh", ct) for ct in range(4)])

        nT_v = nT_d.ap().rearrange("(c p) t -> p c t", p=128)
        yaT_v = yaT_d.ap().rearrange("(c p) t -> p c t", p=128)
        ycT_v = ycT_d.ap().rearrange("(c p) t -> p c t", p=128)
        acc_rot = [0]
        ACC = [(A0, "A0"), (A1, "A1")]

        def next_acc():
            a = ACC[acc_rot[0] % 2]
            acc_rot[0] += 1
            return a

        def A_norm(b, tl):
            t = 4 * b + tl
            s2 = t % 2
            P.op("sync", lambda e, t=t, s2=s2: e.dma_start(out=xt[s2][:], in_=x_d.ap()[t * 128:(t + 1) * 128, :]),
                 writes=[("xt", s2)], dma=True)
            rmsnorm_tile(xt[s2][:], gmix[:], nb[s2][:], ("xt", s2), "gmix", ("nb", s2))

        def A_tr(b, tl):
            t = 4 * b + tl
            s2 = t % 2
            bb = b % 2
            transpose_tile(nb[s2], 8, nTb[bb][:, :, tl * 128:(tl + 1) * 128], ("nb", s2), ("nTb", bb, tl))
            if tl == 3:
                P.op("sync", lambda e, bb=bb, b=b: e.dma_start(out=nT_v[:, :, b * BT:(b + 1) * BT], in_=nTb[bb][:]),
                     reads=[("nTb", bb, q_) for q_ in range(4)], writes=[("nT_d", b)], dma=True)

        def secC(b):
            bb = b % 2
            tok0 = b * BT
            nkeys = [("nTb", bb, tl) for tl in range(4)]
            PACC = [(A0[:], "A0"), (A1[:], "A1"), (B0[:, 0:512], ("B0", 0))]
            prot = [0]

            def nacc():
                a = PACC[prot[0] % 3]
                prot[0] += 1
                return a

            def proj(j):
                acc, akey = nacc()
                for c in range(8):
                    P.op("tensor", lambda e, j=j, c=c, acc=acc: e.matmul(
                        acc, lhsT=Wa[:, c, j * 128:(j + 1) * 128], rhs=nTb[bb][:, c, :], start=(c == 0), stop=(c == 7)),
                        reads=[("Wa", c, 0)] + nkeys, writes=[akey])
                z = j % 3
                P.op("scalar", lambda e, j=j, acc=acc: e.activation(out=zq[j][:], in_=acc, func=AF.Identity, bias=bcol[:, j:j + 1]),
                     reads=[akey, "bcol"], writes=[("zq", j)])
                P.op("gpsimd", lambda e, z=z, j=j: e.tensor_tensor(out=sq[z][:], in0=zq[j][:], in1=zq[j][:], op=ALU.mult),
                     reads=[("zq", j)], writes=[("sq", z)])

            def msacc(j):
                z = j % 3
                P.op("tensor", lambda e, z=z, j=j: e.matmul(A2[:], lhsT=selA[:, j, :], rhs=sq[z][:], start=(j == 0), stop=(j == 7)),
                     reads=[("sq", z), "selA"], writes=["A2"])

            def vproj():
                for tl in range(4):
                    t = 4 * b + tl
                    sl = t % KR
                    acc, akey = nacc()
                    for c in range(8):
                        P.op("tensor", lambda e, c=c, tl=tl, acc=acc: e.matmul(
                            acc, lhsT=nTb[bb][:, c, tl * 128:(tl + 1) * 128], rhs=Wa[:, c, 1024:1536], start=(c == 0), stop=(c == 7)),
                            reads=[("Wa", c, 1024), ("nTb", bb, tl)], writes=[akey])
                    P.op("vector", lambda e, sl=sl, acc=acc: e.tensor_tensor(
                        out=vring[:, sl, :, 0:64], in0=acc.rearrange("p (h d) -> p h d", h=NH),
                        in1=bvb[:].rearrange("p (h d) -> p h d", h=NH), op=ALU.add),
                        reads=[akey, "bvb"], writes=[("v", sl)])

            def fin(j):
                acc, akey = nacc()
                P.op("tensor", lambda e, j=j, acc=acc: e.matmul(acc, lhsT=selB[:, j, :], rhs=r1b[:], start=True, stop=True),
                     reads=["selB", "r1b"], writes=[akey])
                if j < 4:
                    P.op("vector", lambda e, j=j, acc=acc: e.tensor_tensor(out=qT[bb][:, j, :], in0=zq[j][:], in1=acc, op=ALU.mult),
                         reads=[("zq", j), akey], writes=[("qT", bb, j)])
                else:
                    hp = j - 4
                    sl0 = (4 * b) % KR
                    P.op("vector", lambda e, hp=hp, j=j, sl0=sl0, acc=acc: e.scalar_tensor_tensor(
                        out=kring[:, hp, sl0 * 128:(sl0 + 4) * 128], in0=zq[j][:], scalar=gw[:, 0:1], in1=acc, op0=ALU.mult, op1=ALU.mult),
                        reads=[("zq", j), akey, "gw"], writes=[("k", hp, sl0 + q_) for q_ in range(4)])

            proj(0)
            for j in range(8):
                if j + 1 < 8:
                    proj(j + 1)
                msacc(j)
            P.op("scalar", lambda e: e.activation(out=rs[:], in_=A2[:], func=AF.Sqrt, bias=epsc[:, 0:1]), reads=["A2", "epsc"], writes=["rs"])
            P.op("vector", lambda e: e.reciprocal(out=rs[:], in_=rs[:]), reads=["rs"], writes=["rs"])
            P.op("scalar", lambda e: e.activation(out=r1b[:], in_=rs[:], func=AF.Copy), reads=["rs"], writes=["r1b"])
            vproj()
            for j in range(8):
                fin(j)
        def secC3(b):
            bb = b % 2
            tok0 = b * BT
            nkeys = [("nTb", bb, tl) for tl in range(4)]
            SETS = [((A0[:], ["A0"]), (A1[:], ["A1"]), (A2[:], ["A2"])),
                    ((B0[:, 0:512], [("B0", 0)]), (B0[:, 512:1024], [("B0", 4)]), (B1[:, 0:512], [("B1h", 0)]))]
            for ct in range(4):
                z = ct % 2
                (pu, ku), (pb, kb), (pc, kc) = SETS[ct % 2]
                for (dst, dkey, col0) in ((pu, ku, 1536), (pb, kb, 2048), (pc, kc, 2560)):
                    for c in range(8):
                        P.op("tensor", lambda e, c=c, dst=dst, col0=col0, ct=ct: e.matmul(
                            dst, lhsT=Wa[:, c, col0 + ct * 128:col0 + (ct + 1) * 128], rhs=nTb[bb][:, c, :],
                            start=(c == 0), stop=(c == 7)),
                            reads=[("Wa", c, 1536)] + nkeys, writes=dkey)
                ju, jb, jc = 12 + ct, 16 + ct, 20 + ct
                P.op("scalar", lambda e, z=z, ju=ju, pu=pu: e.activation(out=us[z][:], in_=pu, func=AF.Identity, bias=bcol[:, ju:ju + 1]),
                     reads=ku + ["bcol"], writes=[("us", z)])
                P.op("vector", lambda e, z=z, jc=jc, ct=ct, pc=pc: e.scalar_tensor_tensor(
                    out=cu[:, ct, 2:BT + 2], in0=pc, scalar=bcol[:, jc:jc + 1], in1=us[z][:], op0=ALU.add, op1=ALU.mult),
                    reads=kc + ["bcol", ("us", z)], writes=[("cu", ct)])
                P.op("scalar", lambda e, z=z, ct=ct: e.activation(out=t1[z][:], in_=cu[:, ct, 2:BT + 2], func=AF.Identity,
                                                               scale=cw[:, ct * 3 + 2:ct * 3 + 3], bias=cb[:, ct:ct + 1]),
                     reads=[("cu", ct), "cw", "cb"], writes=[("t1", z)])
                P.op("vector", lambda e, z=z, ct=ct: e.scalar_tensor_tensor(
                    out=t1[z][:], in0=cu[:, ct, 1:BT + 1], scalar=cw[:, ct * 3 + 1:ct * 3 + 2], in1=t1[z][:], op0=ALU.mult, op1=ALU.add),
                    reads=[("cu", ct), ("cuh", ct), "cw", ("t1", z)], writes=[("t1", z)])
                P.op("vector", lambda e, z=z, ct=ct: e.scalar_tensor_tensor(
                    out=t1[z][:], in0=cu[:, ct, 0:BT], scalar=cw[:, ct * 3:ct * 3 + 1], in1=t1[z][:], op0=ALU.mult, op1=ALU.add),
                    reads=[("cu", ct), ("cuh", ct), "cw", ("t1", z)], writes=[("t1", z)])
                P.op("vector", lambda e, z=z, jb=jb, ct=ct, pb=pb: e.scalar_tensor_tensor(
                    out=ycT[bb][:, ct, :], in0=pb, scalar=bcol[:, jb:jb + 1], in1=t1[z][:], op0=ALU.add, op1=ALU.mult),
                    reads=kb + ["bcol", ("t1", z)], writes=[("ycT", bb, ct)])
                P.op("vector", lambda e, ct=ct: e.tensor_copy(out=cu[:, ct, 0:2], in_=cu[:, ct, BT:BT + 2]),
                     reads=[("cu", ct)], writes=[("cuh", ct)])
            P.op("sync", lambda e, tok0=tok0: e.dma_start(out=ycT_v[:, :, tok0:tok0 + BT], in_=ycT[bb][:]),
                 reads=[("ycT", bb, ct) for ct in range(4)], writes=[("ycT_d", b)], dma=True)

        def secD(b):
            bb = b % 2
            tok0 = b * BT
            nxt = b + 1 < NB
            units = []
            for h in range(NH):
                for m in range(8):
                    kt = 4 * b - 4 + m
                    if kt < 0:
                        continue
                    units.append((h, m, kt, max(m - 4, 0), min(m, 3)))
            SPS = [(A0[:], "A0"), (A1[:], "A1"), (A2[:], "A2"), (B0[:, 0:512], ("B0", 0)), (B0[:, 512:1024], ("B0", 4))]
            LA = 4

            def QKEXP(u):
                h, m, kt, tlo, thi = units[u]
                hp, r0 = h // 2, (h % 2) * 64
                nq = thi - tlo + 1
                sp, sk = SPS[u % 5]
                pz = u % 6
                sl = kt % KR
                P.op("tensor", lambda e, hp=hp, r0=r0, sl=sl, sp=sp, tlo=tlo, thi=thi: e.matmul(
                    sp[:, 0:(thi - tlo + 1) * 128], lhsT=kring[r0:r0 + 64, hp, sl * 128:(sl + 1) * 128],
                    rhs=qT[bb][r0:r0 + 64, hp, tlo * 128:(thi + 1) * 128], start=True, stop=True),
                    reads=[("k", hp, sl), ("qT", bb, hp)], writes=[sk])
                P.op("scalar", lambda e, pz=pz, sp=sp, nq=nq: e.activation(
                    out=pt[pz][:, 0:nq, :], in_=sp[:, 0:nq * 128].rearrange("p (j q) -> p j q", q=128), func=AF.Exp, scale=DH ** -0.5),
                    reads=[sk], writes=[("pt", pz)])
                rlo = 4 - m + tlo
                P.op("vector", lambda e, pz=pz, nq=nq, h=h, rlo=rlo: e.tensor_tensor(
                    out=pt[pz][:, 0:nq, :], in0=pt[pz][:, 0:nq, :], in1=expB[:, h, rlo:rlo + nq, :], op=ALU.mult),
                    reads=[("pt", pz), ("expB", h)], writes=[("pt", pz)])

            def PV(u):
                h, m, kt, tlo, thi = units[u]
                pz = u % 6
                sl = kt % KR
                hb2 = h % 2
                first = (u == 0) or units[u - 1][0] != h
                last_u = (u + 1 == len(units)) or units[u + 1][0] != h
                if first:
                    P.op("tensor", lambda e, hb2=hb2: e.matmul(
                        B1[:, hb2 * 512:hb2 * 512 + 260], lhsT=zt[:, 0:128], rhs=zt[:, 0:260], start=True, stop=False),
                        reads=["zt"], writes=[("B1h", hb2)])
                for tl in range(tlo, thi + 1):
                    c0 = hb2 * 512 + tl * 65
                    P.op("tensor", lambda e, h=h, tl=tl, tlo=tlo, sl=sl, pz=pz, c0=c0, fin=(last_u and tl == thi): e.matmul(
                        B1[:, c0:c0 + 65], lhsT=pt[pz][:, tl - tlo, :], rhs=vring[:, sl, h, :],
                        start=False, stop=fin),
                        reads=[("pt", pz), ("v", sl)], writes=[("B1h", hb2)])

            def FINH(h):
                hb2 = h % 2
                Bv = B1[:, hb2 * 512:hb2 * 512 + 260].rearrange("p (t d) -> p t d", d=65)
                P.op("vector", lambda e, hb2=hb2, Bv=Bv: e.reciprocal(out=rden[hb2][:], in_=Bv[:, :, 64]),
                     reads=[("B1h", hb2)], writes=[("rden", hb2)])
                P.op("vector", lambda e, hb2=hb2, Bv=Bv, h=h: e.tensor_tensor(
                    out=ya[:, :, h * 64:(h + 1) * 64], in0=Bv[:, :, 0:64], in1=bc_last(rden[hb2][:], 64), op=ALU.mult),
                    reads=[("B1h", hb2), ("rden", hb2)], writes=[("ya", h)])

            if nxt:
                A_norm(b + 1, 0)
            for u in range(min(LA, len(units))):
                QKEXP(u)
            secC3(b)
            zero_fill(b)
            if b == NB - 1 and 2 in phases:
                p2_weight_loads(pre2["Wg"], pre2["wpa"], pre2["wpc"],
                                extra_writes=[("Wa", c, c0_) for c in range(8) for c0_ in (0, 1024, 1536)])
            for u in range(len(units)):
                if u + LA < len(units):
                    QKEXP(u + LA)
                PV(u)
                h = units[u][0]
                if u + 1 == len(units) or units[u + 1][0] != h:
                    FINH(h)
                    if nxt and h % 2 == 1:
                        tl = h // 2
                        A_tr(b + 1, tl)
                        if tl + 1 < 4:
                            A_norm(b + 1, tl + 1)
            for tl in range(4):
                transpose_tile(ya[:, tl, :], 4, yaT[bb][:, :, tl * 128:(tl + 1) * 128], [("ya", h) for h in range(NH)], ("yaT", bb, tl))
            P.op("sync", lambda e, tok0=tok0: e.dma_start(out=yaT_v[:, :, tok0:tok0 + BT], in_=yaT[bb][:]),
                 reads=[("yaT", bb, tl) for tl in range(4)], writes=[("yaT_d", b)], dma=True)

        for tl in range(4):
            A_norm(0, tl)
            A_tr(0, tl)
        for b in range(NB):
            secC(b)
            secD(b)
        P.barrier()
    if 1 in phases:
        phase1()
    sb.reset(base_mark)

    def phase2():
        Wg = sb.alloc("Wg", [128, 8, 2048], BF16)
        wpa = sb.alloc("wpa", [128, 4, D], BF16)
        wpc = sb.alloc("wpc", [128, 4, D], BF16)
        wo = sb.alloc("wo", [128, 8, D], BF16)
        wrg = sb.alloc("wrg", [128, 8, 36], BF16)
        brg = sb.alloc("brg", [128, 36], F32)
        gffn = sb.alloc("gffn", [128, D], F32)
        nTb = [sb.alloc("nTb%d" % i, [128, 8, BT], BF16) for i in range(2)]
        yaT = [sb.alloc("yaT%d" % i, [128, 4, BT], BF16) for i in range(2)]
        ycT = [sb.alloc("ycT%d" % i, [128, 4, BT], BF16) for i in range(2)]
        xt = [sb.alloc("xt%d" % i, [128, D], F32) for i in range(4)]
        tA = [sb.alloc("tA%d" % i, [128, BT], F32) for i in range(2)]
        tC = [sb.alloc("tC%d" % i, [128, BT], F32) for i in range(2)]
        mA = [sb.alloc("mA%d" % i, [128, BT], F32) for i in range(2)]
        mC = [sb.alloc("mC%d" % i, [128, BT], F32) for i in range(2)]
        mT = [sb.alloc("mT%d" % i, [128, 8, BT], BF16) for i in range(2)]
        ht = [sb.alloc("ht%d" % i, [128, D], F32) for i in range(3)]
        n2 = [sb.alloc("n2%d" % i, [128, 4, D], BF16) for i in range(2)]
        n2T = [sb.alloc("n2T%d" % i, [128, 8, 128], BF16) for i in range(2)]
        lg = sb.alloc("lg", [128, 4, 36], F32)
        gmax = sb.alloc("gmax", [128, 4], F32)
        gmask = sb.alloc("gmask", [128, 4, 4], F32)
        gex = sb.alloc("gex", [128, 4, 4], F32)
        gse = sb.alloc("gse", [128, 4], F32)
        pen = sb.alloc("pen", [128, 4, 4], F32)
        elm = sb.alloc("elm", [128, 4, 32], F32)
        elm2 = sb.alloc("elm2", [128, 4, 32], F32)
        m1 = sb.alloc("m1", [128, 4], F32)
        m2 = sb.alloc("m2", [128, 4], F32)
        mk1 = sb.alloc("mk1", [128, 4, 32], F32)
        mk2 = sb.alloc("mk2", [128, 4, 32], F32)
        Mb = sb.alloc("Mb", [128, 4, 32], BF16)
        dd = sb.alloc("dd", [128, 4], F32)
        ee = sb.alloc("ee", [128, 4], F32)
        rr = sb.alloc("rr", [128, 4], F32)
        wA = sb.alloc("wA", [128, 4], F32)
        wB = sb.alloc("wB", [128, 4], F32)
        pos = sb.alloc("pos", [128, 4, 32], F32)
        okm = sb.alloc("okm", [128, 4, 32], F32)
        slot = sb.alloc("slot", [128, 4, 32], F32)
        tmp = sb.alloc("tmp", [128, 4, 32], F32)
        dsel = sb.alloc("dsel", [128, 4, 2], F32)
        oksel = sb.alloc("oksel", [128, 4, 2], F32)

        assert sb.off["Wg"] == BASE and sb.off["wpa"] == BASE + 32768 and sb.off["wpc"] == BASE + 40960
        if 1 not in phases:
            p2_weight_loads(Wg, wpa, wpc)
        wo_v = wo_d.ap().rearrange("(c p) n -> p c n", p=128)
        for c in range(0, 8, 4):
            P.op("gpsimd", lambda e, c=c: e.dma_start(out=wo[:, c:c + 4, :], in_=wo_v[:, c:c + 4, :]), writes=[("wo", c)], dma=True)
        P.op("gpsimd", lambda e: e.dma_start(out=wrg[:], in_=wrg_d.ap().rearrange("(c p) n -> p c n", p=128)), writes=["wrg"], dma=True)
        ld("sync", brg[:], brg_d.ap().partition_broadcast(128), "brg")
        ld("sync", gffn[:], gffn_d.ap().partition_broadcast(128), "gffn")

        nT_v = nT_d.ap().rearrange("(c p) t -> p c t", p=128)
        yaT_v = yaT_d.ap().rearrange("(c p) t -> p c t", p=128)
        ycT_v = ycT_d.ap().rearrange("(c p) t -> p c t", p=128)
        wokeys = [("wo", 0), ("wo", 4)]
        xctr = [0]
        hctr = [0]

        def loads2(b):
            bb = b % 2
            tok0 = b * BT
            P.op("sync", lambda e, bb=bb, tok0=tok0: e.dma_start(out=nTb[bb][:], in_=nT_v[:, :, tok0:tok0 + BT]), writes=[("nTb", bb)], dma=True)
            P.op("sync", lambda e, bb=bb, tok0=tok0: e.dma_start(out=yaT[bb][:], in_=yaT_v[:, :, tok0:tok0 + BT]), writes=[("yaT", bb)], dma=True)
            P.op("sync", lambda e, bb=bb, tok0=tok0: e.dma_start(out=ycT[bb][:], in_=ycT_v[:, :, tok0:tok0 + BT]), writes=[("ycT", bb)], dma=True)

        def xload(t):
            P.op("sync", lambda e, t=t: e.dma_start(out=xt[t % 4][:], in_=x_d.ap()[t * 128:(t + 1) * 128, :]), writes=[("xt", t % 4)], dma=True)

        loads2(0)
        for t_ in range(3):
            xload(t_)
        def gates2(b):
            bb = b % 2
            tok0 = b * BT
            if b + 1 < NB:
                loads2(b + 1)
            for j in range(8):
                z = j % 2
                for (dst, dkey, col0) in ((A0, "A0", 0), (A1, "A1", 1024)):
                    for c in range(8):
                        P.op("tensor", lambda e, c=c, dst=dst, col0=col0, j=j, bb=bb: e.matmul(
                            dst[:], lhsT=Wg[:, c, col0 + j * 128:col0 + (j + 1) * 128], rhs=nTb[bb][:, c, :], start=(c == 0), stop=(c == 7)),
                            reads=[("Wg", col0 + (j // 2) * 256), ("nTb", bb)], writes=[dkey])
                for (dst, dkey, wsrc, wkey, asrc, akey) in ((A2, "A2", wpa, "wpa", yaT, "yaT"), (B0, ("B0", 0), wpc, "wpc", ycT, "ycT")):
                    for c in range(4):
                        P.op("tensor", lambda e, c=c, dst=dst, wsrc=wsrc, asrc=asrc, j=j, bb=bb: e.matmul(
                            dst[:, 0:512], lhsT=wsrc[:, c, j * 128:(j + 1) * 128], rhs=asrc[bb][:, c, :], start=(c == 0), stop=(c == 3)),
                            reads=[wkey, (akey, bb)], writes=[dkey])
                P.op("scalar", lambda e, z=z, j=j: e.activation(out=tA[z][:], in_=A0[:], func=AF.Tanh, scale=0.5, bias=hbcol[:, 24 + j:25 + j]),
                     reads=["A0", "hbcol"], writes=[("tA", z)])
                P.op("scalar", lambda e, z=z, j=j: e.activation(out=tC[z][:], in_=A1[:], func=AF.Tanh, scale=0.5, bias=hbcol[:, 32 + j:33 + j]),
                     reads=["A1", "hbcol"], writes=[("tC", z)])
                P.op("vector", lambda e, z=z: e.scalar_tensor_tensor(out=mA[z][:], in0=tA[z][:], scalar=1.0, in1=A2[:], op0=ALU.add, op1=ALU.mult),
                     reads=[("tA", z), "A2"], writes=[("mA", z)])
                P.op("vector", lambda e, z=z: e.scalar_tensor_tensor(out=mC[z][:], in0=tC[z][:], scalar=1.0, in1=B0[:, 0:512], op0=ALU.add, op1=ALU.mult),
                     reads=[("tC", z), ("B0", 0)], writes=[("mC", z)])
                P.op("vector", lambda e, z=z, j=j, bb=bb: e.tensor_tensor(out=mT[bb][:, j, :], in0=mA[z][:], in1=mC[z][:], op=ALU.add),
                     reads=[("mA", z), ("mC", z)], writes=[("mT", bb, j)])

        def hsec2(b):
            bb = b % 2
            tok0 = b * BT
            mkeys = [("mT", bb, j) for j in range(8)]
            def hmm(tl):
                t = 4 * b + tl
                xs_ = t % 4
                hs_ = t % 3
                if t + 3 < NT:
                    xload(t + 3)
                HB = [(B1[:, 0:512], ("B1", 0)), (B1[:, 512:1024], ("B1", 1))] if tl % 2 == 0 else [(A0[:], "A0"), (A1[:], "A1")]
                for half in range(2):
                    hacc, hkey = HB[half]
                    for j in range(8):
                        P.op("tensor", lambda e, j=j, half=half, tl=tl, bb=bb, hacc=hacc: e.matmul(
                            hacc, lhsT=mT[bb][:, j, tl * 128:(tl + 1) * 128], rhs=wo[:, j, half * 512:(half + 1) * 512],
                            start=(j == 0), stop=(j == 7)),
                            reads=mkeys + wokeys, writes=[hkey])
                    P.op("vector", lambda e, half=half, xs_=xs_, hs_=hs_, hacc=hacc: e.scalar_tensor_tensor(
                        out=ht[hs_][:, half * 512:(half + 1) * 512], in0=hacc, scalar=0.5,
                        in1=xt[xs_][:, half * 512:(half + 1) * 512], op0=ALU.mult, op1=ALU.add),
                        reads=[hkey, ("xt", xs_)] + ([("ht", hs_)] if half == 1 else []), writes=[("ht", hs_)])
                P.op("sync", lambda e, t=t, hs_=hs_: e.dma_start(out=h_d.ap()[t * 128:(t + 1) * 128, :], in_=ht[hs_][:]),
                     reads=[("ht", hs_)], writes=[("h_d", t)], dma=True)

            def hnorm(tl):
                t = 4 * b + tl
                hs_ = t % 3
                rmsnorm_tile(ht[hs_][:], gffn[:], n2[bb][:, tl, :], ("ht", hs_), "gffn", ("n2", bb, tl))

            def htr(tl):
                t = 4 * b + tl
                z2 = t % 2
                transpose_tile(n2[bb][:, tl, :], 8, n2T[z2][:], ("n2", bb, tl), ("n2T", z2))
                for c in range(8):
                    P.op("tensor", lambda e, c=c, tl=tl, z2=z2: e.matmul(
                        B0[:, 512 + tl * 36:512 + (tl + 1) * 36], lhsT=n2T[z2][:, c, :], rhs=wrg[:, c, :], start=(c == 0), stop=(c == 7)),
                        reads=[("n2T", z2), "wrg"], writes=[("lgp", tl)])

            hmm(0)
            hnorm(0)
            for tl in range(4):
                if tl + 1 < 4:
                    hmm(tl + 1)
                htr(tl)
                if tl + 1 < 4:
                    hnorm(tl + 1)

        def rout2a(b):
            bb = b % 2
            tok0 = b * BT
            lgp = B0[:, 512:512 + 144].rearrange("p (t n) -> p t n", t=4)
            R = []

            def V(fn, reads, writes):
                P.op("vector", fn, reads=reads, writes=writes)

            V(lambda e: e.tensor_tensor(out=lg[:], in0=lgp, in1=bc_mid(brg[:], 4), op=ALU.add),
              [("lgp", tl) for tl in range(4)] + ["brg"], ["lg"])
            V(lambda e: e.tensor_reduce(out=gmax[:], in_=lg[:, :, 0:4], axis=AX.X, op=ALU.max), ["lg"], ["gmax"])
            V(lambda e: e.tensor_tensor(out=gmask[:], in0=lg[:, :, 0:4], in1=bc_last(gmax[:], 4), op=ALU.is_equal), ["lg", "gmax"], ["gmask"])
            V(lambda e: e.tensor_tensor(out=gex[:], in0=lg[:, :, 0:4], in1=bc_last(gmax[:], 4), op=ALU.subtract), ["lg", "gmax"], ["gex"])
            P.op("scalar", lambda e: e.activation(out=gex[:], in_=gex[:], func=AF.Exp), reads=["gex"], writes=["gex"])
            V(lambda e: e.tensor_reduce(out=gse[:], in_=gex[:], axis=AX.X, op=ALU.add), ["gex"], ["gse"])
            V(lambda e: e.reciprocal(out=gse[:], in_=gse[:]), ["gse"], ["gse"])
            V(lambda e: e.tensor_scalar(out=pen[:], in0=gmask[:], scalar1=1.0, scalar2=1e30, op0=ALU.subtract, op1=ALU.mult), ["gmask"], ["pen"])
            V(lambda e: e.tensor_tensor(out=elm[:].rearrange("p t (g k) -> p t g k", g=4),
                                        in0=lg[:, :, 4:36].rearrange("p t (g k) -> p t g k", g=4),
                                        in1=bc_last(pen[:], 8), op=ALU.add), ["lg", "pen"], ["elm"])
            V(lambda e: e.tensor_reduce(out=m1[:], in_=elm[:], axis=AX.X, op=ALU.max), ["elm"], ["m1"])
            V(lambda e: e.tensor_tensor(out=mk1[:], in0=elm[:], in1=bc_last(m1[:], 32), op=ALU.is_equal), ["elm", "m1"], ["mk1"])
            V(lambda e: e.scalar_tensor_tensor(out=elm2[:], in0=mk1[:], scalar=-1e30, in1=elm[:], op0=ALU.mult, op1=ALU.add), ["mk1", "elm"], ["elm2"])
            V(lambda e: e.tensor_reduce(out=m2[:], in_=elm2[:], axis=AX.X, op=ALU.max), ["elm2"], ["m2"])
            V(lambda e: e.tensor_tensor(out=mk2[:], in0=elm2[:], in1=bc_last(m2[:], 32), op=ALU.is_equal), ["elm2", "m2"], ["mk2"])
            V(lambda e: e.tensor_tensor(out=dd[:], in0=m2[:], in1=m1[:], op=ALU.subtract), ["m1", "m2"], ["dd"])
            P.op("scalar", lambda e: e.activation(out=ee[:], in_=dd[:], func=AF.Exp), reads=["dd"], writes=["ee"])
            V(lambda e: e.tensor_scalar(out=rr[:], in0=ee[:], scalar1=1.0, scalar2=None, op0=ALU.add), ["ee"], ["rr"])
            V(lambda e: e.reciprocal(out=rr[:], in_=rr[:]), ["rr"], ["rr"])
            V(lambda e: e.tensor_tensor(out=wA[:], in0=gse[:], in1=rr[:], op=ALU.mult), ["gse", "rr"], ["wA"])
            V(lambda e: e.tensor_tensor(out=wB[:], in0=wA[:], in1=ee[:], op=ALU.mult), ["wA", "ee"], ["wB"])
            V(lambda e: e.tensor_tensor(out=Mb[:], in0=mk1[:], in1=mk2[:], op=ALU.add), ["mk1", "mk2"], ["Mb"])

        def rout2b(b):
            bb = b % 2
            tok0 = b * BT

            def V(fn, reads, writes):
                P.op("vector", fn, reads=reads, writes=writes)

            for tl in range(4):
                P.op("tensor", lambda e, tl=tl: e.matmul(A0[:, tl * 32:(tl + 1) * 32], lhsT=utri[:], rhs=Mb[:, tl, :], start=True, stop=(tl == 0)),
                     reads=["utri", "Mb"], writes=["A0"])
                for t2 in range(tl):
                    P.op("tensor", lambda e, tl=tl, t2=t2: e.matmul(A0[:, tl * 32:(tl + 1) * 32], lhsT=ones[:], rhs=Mb[:, t2, :], start=False, stop=(t2 == tl - 1)),
                         reads=["ones", "Mb"], writes=["A0"])
            for tl in range(4):
                P.op("tensor", lambda e, tl=tl: e.matmul(A1[:, 0:32], lhsT=ones[:], rhs=Mb[:, tl, :], start=(tl == 0), stop=(tl == 3)),
                     reads=["ones", "Mb"], writes=["A1"])
            V(lambda e: e.tensor_tensor(out=pos[:], in0=A0[:, 0:128].rearrange("p (t n) -> p t n", t=4), in1=bc_mid(cnt[:], 4), op=ALU.add),
              ["A0", "cnt"], ["pos"])
            V(lambda e: e.tensor_tensor(out=cnt[:], in0=cnt[:], in1=A1[:, 0:32], op=ALU.add), ["A1", "cnt", "pos"], ["cnt"])
            V(lambda e: e.tensor_scalar(out=okm[:], in0=pos[:], scalar1=float(CAP), scalar2=None, op0=ALU.is_lt), ["pos"], ["okm"])
            V(lambda e: e.tensor_tensor(out=slot[:], in0=pos[:], in1=bc_mid(ecap[:], 4), op=ALU.add), ["pos", "ecap"], ["slot"])
            V(lambda e: e.tensor_scalar(out=tmp[:], in0=okm[:], scalar1=-1.0e6, scalar2=1.0e6, op0=ALU.mult, op1=ALU.add), ["okm"], ["tmp"])
            V(lambda e: e.tensor_tensor(out=slot[:], in0=slot[:], in1=tmp[:], op=ALU.add), ["slot", "tmp"], ["slot"])
            V(lambda e: e.tensor_scalar(out=slot[:], in0=slot[:], scalar1=float(NSLOT), scalar2=None, op0=ALU.min), ["slot"], ["slot"])
            for k, mk in ((0, mk1), (1, mk2)):
                V(lambda e, mk=mk: e.tensor_tensor(out=tmp[:], in0=mk[:], in1=slot[:], op=ALU.mult), ["mk1", "mk2", "slot"], ["tmp"])
                V(lambda e, k=k: e.tensor_reduce(out=dsel[:, :, k], in_=tmp[:], axis=AX.X, op=ALU.add), ["tmp"], [("dsel", k)])
                V(lambda e, mk=mk: e.tensor_tensor(out=tmp[:], in0=mk[:], in1=okm[:], op=ALU.mult), ["mk1", "mk2", "okm", ("dsel", k)], ["tmp"])
                V(lambda e, k=k: e.tensor_reduce(out=oksel[:, :, k], in_=tmp[:], axis=AX.X, op=ALU.add), ["tmp"], [("oksel", k)])
            tb = 4 * b
            V(lambda e, tb=tb: e.tensor_copy(out=dtab[:, tb:tb + 4, :], in_=dsel[:]), [("dsel", 0), ("dsel", 1)], [("dtab", b)])
            V(lambda e, tb=tb: e.tensor_tensor(out=wtab[:, tb:tb + 4, 0], in0=wA[:], in1=oksel[:, :, 0], op=ALU.mult), ["wA", ("oksel", 0)], [("wtab", b, 0)])
            V(lambda e, tb=tb: e.tensor_tensor(out=wtab[:, tb:tb + 4, 1], in0=wB[:], in1=oksel[:, :, 1], op=ALU.mult), ["wB", ("oksel", 1)], [("wtab", b, 1)])
            for tl in range(4):
                t = 4 * b + tl
                for k in range(2):
                    P.op("gpsimd", lambda e, t=t, k=k, tl=tl, bb=bb: e.indirect_dma_start(
                        out=xs_d[:, :], out_offset=bass.IndirectOffsetOnAxis(ap=dtab[:, t, k:k + 1], axis=0),
                        in_=n2[bb][:, tl, :], in_offset=None),
                        reads=[("n2", bb, tl), ("dtab", b)], writes=[("xs_d", t, k)], dma=True)

        gates2(0)
        for b in range(NB):
            hsec2(b)
            if b == NB - 1 and 3 in phases:
                for i in range(2):
                    expert_weight_loads(i, pre3[i]["w1"], pre3[i]["w3"], pre3[i]["w2"], [("w1p", i), ("w3p", i), ("w2p", i)],
                                        extra_writes=WGKEYS + ["wpa", "wpc"])
            rout2a(b)
            if b + 1 < NB:
                gates2(b + 1)
            rout2b(b)
        if debug:
            P.op("sync", lambda e: e.dma_start(out=dtab_o.ap(), in_=dtab[:].rearrange("p t k -> p (t k)")),
                 reads=[("dtab", b) for b in range(NB)], dma=True, is_out=True)
            P.op("sync", lambda e: e.dma_start(out=wtab_o.ap(), in_=wtab[:].rearrange("p t k -> p (t k)")),
                 reads=[("wtab", b, k) for b in range(NB) for k in range(2)], dma=True, is_out=True)
        P.barrier()
    if 2 in phases:
        phase2()
    sb.reset(base_mark)

    TOP = SBAlloc.HI - 20 * 1024
    p4w = {"wpg": nc.alloc_sbuf_tensor_at("wpg_top", [128, 8, D], BF16, offset=TOP),
           "wpp": nc.alloc_sbuf_tensor_at("wpp_top", [128, 2, D], BF16, offset=TOP + 16 * 1024)}

    def p4_weight_loads():
        wpg_v = wpg_d.ap().rearrange("(c p) n -> p c n", p=128)
        for c in range(0, 8, 4):
            P.op("gpsimd", lambda e, c=c: e.dma_start(out=p4w["wpg"][:, c:c + 4, :], in_=wpg_v[:, c:c + 4, :]), writes=[("wpg", c)], dma=True)
        P.op("gpsimd", lambda e: e.dma_start(out=p4w["wpp"][:], in_=wpp_d.ap().rearrange("(c p) n -> p c n", p=128)), writes=["wpp"], dma=True)

    def phase3():
        NWB = 3
        w1b, w3b, w2b = [], [], []
        for i in range(NWB):
            w1b.append(sb.alloc("w1b%d" % i, [128, 8, 512], BF16))
            w3b.append(sb.alloc("w3b%d" % i, [128, 8, 512], BF16))
            w2b.append(sb.alloc("w2b%d" % i, [128, 4, D], BF16))
        assert sb.off["w1b0"] == BASE and sb.off["w2b1"] == BASE + 24576 + 16384
        preloaded = 2 if 2 in phases else 0
        xr = [sb.alloc("xr%d" % i, [128, 3, D], BF16) for i in range(3)]
        xsT = [sb.alloc("xsT%d" % i, [128, 8, CAP], BF16) for i in range(2)]
        s1 = [sb.alloc("s1%d" % i, [128, CAP], F32) for i in range(2)]
        hdn = [sb.alloc("hdn%d" % i, [128, 4, CAP], BF16) for i in range(2)]
        yb = [sb.alloc("yb%d" % i, [128, D], BF16) for i in range(3)]
        HACC = [(A0, "A0", A1, "A1"), (A2, "A2", B0, ("B0", 0))]
        yctr = [0]
        P.op("gpsimd", lambda e: e.memset(yb[0][:], 0.0), writes=[("yb", 0, 0), ("yb", 0, 1)])
        P.op("sync", lambda e: e.dma_start(out=ys_d.ap()[NSLOT:NSLOT + 128, :], in_=yb[0][:]),
             reads=[("yb", 0, 0), ("yb", 0, 1)], writes=["ys_trash"], dma=True)
        def wload(ex):
            wb_ = ex % NWB
            if ex < preloaded:
                return
            expert_weight_loads(ex, w1b[wb_], w3b[wb_], w2b[wb_], [("w1b", wb_), ("w3b", wb_), ("w2b", wb_)])

        def xsload(ex):
            e3 = ex % 3
            P.op("sync", lambda e, ex=ex, e3=e3: e.dma_start(
                out=xr[e3][:], in_=xs_d.ap()[ex * CAP:(ex + 1) * CAP, :].rearrange("(r p) d -> p r d", p=128)),
                writes=[("xr", e3)], dma=True)

        def xsT_group(ex, r):
            eb = ex % 2
            e3 = ex % 3
            transpose_tile(xr[e3][:, r, :], 8, xsT[eb][:, :, r * 128:(r + 1) * 128], ("xr", e3), ("xsT", eb, r),
                           evac=("scalar" if r % 2 == 0 else "vector"), interleave=8)

        xsload(0)
        wload(0)
        xsload(1)
        wload(1)
        for r in range(3):
            xsT_group(0, r)
        for ex in range(NE):
            eb = ex % 2
            wb_ = ex % NWB
            if ex + 2 < NE:
                wload(ex + 2)
                xsload(ex + 2)
            if ex == 2:
                p4_weight_loads()
            xk = [("xsT", eb, r) for r in range(3)]
            for f in range(4):
                a1, k1, a3, k3 = HACC[f % 2]
                z = f % 2
                for (dst, dkey, wsrc, wkey) in ((a1, k1, w1b, "w1b"), (a3, k3, w3b, "w3b")):
                    for c in range(8):
                        P.op("tensor", lambda e, c=c, dst=dst, wsrc=wsrc, f=f, eb=eb, wb_=wb_: e.matmul(
                            dst[:, 0:CAP], lhsT=wsrc[wb_][:, c, f * 128:(f + 1) * 128], rhs=xsT[eb][:, c, :], start=(c == 0), stop=(c == 7)),
                            reads=[(wkey, wb_)] + xk, writes=[dkey])
                P.op("scalar", lambda e, a1=a1, z=z: e.activation(out=s1[z][:], in_=a1[:, 0:CAP], func=AF.Silu), reads=[k1], writes=[("s1", z)])
                P.op("vector", lambda e, a3=a3, z=z, f=f, eb=eb: e.tensor_tensor(out=hdn[eb][:, f, :], in0=s1[z][:], in1=a3[:, 0:CAP], op=ALU.mult),
                     reads=[("s1", z), k3], writes=[("hdn", eb, f)])
            hk_ = [("hdn", eb, f) for f in range(4)]
            for r in range(3):
                if ex + 1 < NE:
                    xsT_group(ex + 1, r)
                ys_ = yctr[0] % 3
                yctr[0] += 1
                for half in range(2):
                    for f in range(4):
                        P.op("tensor", lambda e, f=f, half=half, r=r, eb=eb, wb_=wb_: e.matmul(
                            B1[:, half * 512:(half + 1) * 512], lhsT=hdn[eb][:, f, r * 128:(r + 1) * 128], rhs=w2b[wb_][:, f, half * 512:(half + 1) * 512],
                            start=(f == 0), stop=(f == 3)),
                            reads=hk_ + [("w2b", wb_)], writes=[("B1", half)])
                    if half == 0:
                        P.op("scalar", lambda e, ys_=ys_: e.activation(out=yb[ys_][:, 0:512], in_=B1[:, 0:512], func=AF.Copy),
                             reads=[("B1", 0)], writes=[("yb", ys_, 0)])
                    else:
                        P.op("vector", lambda e, ys_=ys_: e.tensor_copy(out=yb[ys_][:, 512:1024], in_=B1[:, 512:1024]),
                             reads=[("B1", 1)], writes=[("yb", ys_, 1)])
                row0 = ex * CAP + r * 128
                P.op("sync", lambda e, row0=row0, ys_=ys_: e.dma_start(out=ys_d.ap()[row0:row0 + 128, :], in_=yb[ys_][:]),
                     reads=[("yb", ys_, 0), ("yb", ys_, 1)], writes=[("ys_d", ex, r)], dma=True)
        P.barrier()
    if 3 in phases:
        phase3()
    sb.reset(base_mark)

    def phase4():
        wpg, wpp = p4w["wpg"], p4w["wpp"]
        gple = sb.alloc("gple", [128, D], F32)
        bpg = sb.alloc("bpg", [128, D], F32)
        hb = [sb.alloc("hb%d" % i, [128, D], F32) for i in range(3)]
        y1 = [sb.alloc("y1%d" % i, [128, D], BF16) for i in range(3)]
        y2 = [sb.alloc("y2%d" % i, [128, D], BF16) for i in range(3)]
        pin = [sb.alloc("pin%d" % i, [128, 256], F32) for i in range(3)]
        pbf = [sb.alloc("pbf%d" % i, [128, 256], BF16) for i in range(2)]
        ppT = [sb.alloc("ppT%d" % i, [128, 2, 128], BF16) for i in range(2)]
        n3 = [sb.alloc("n3%d" % i, [128, D], BF16) for i in range(2)]
        n3T = [sb.alloc("n3T%d" % i, [128, 8, 128], BF16) for i in range(2)]
        gz = [sb.alloc("gz%d" % i, [128, D], F32) for i in range(2)]
        ob = [sb.alloc("ob%d" % i, [128, D], F32) for i in range(2)]
        ld("sync", gple[:], gple_d.ap().partition_broadcast(128), "gple")
        ld("sync", bpg[:], bpg_d.ap().partition_broadcast(128), "bpg")
        def loads4(t):
            h3 = t % 3
            P.op("sync", lambda e, t=t, h3=h3: e.dma_start(out=hb[h3][:], in_=h_d.ap()[t * 128:(t + 1) * 128, :]), writes=[("hb", h3)], dma=True)
            P.op("sync", lambda e, t=t, h3=h3: e.dma_start(out=pin[h3][:], in_=p_d.ap()[t * 128:(t + 1) * 128, :]), writes=[("pin", h3)], dma=True)
            for (yy, ykey, k) in ((y1, "y1", 0), (y2, "y2", 1)):
                P.op("gpsimd", lambda e, yy=yy, k=k, t=t, h3=h3: e.indirect_dma_start(
                    out=yy[h3][:, :], out_offset=None, in_=ys_d[:, :], in_offset=bass.IndirectOffsetOnAxis(ap=dtab[:, t, k:k + 1], axis=0)), reads=["dtab_all"], writes=[(ykey, h3)], dma=True)

        def S1a(t):
            h3 = t % 3
            z = t % 2
            P.op("vector", lambda e, h3=h3, t=t: e.scalar_tensor_tensor(out=hb[h3][:], in0=y1[h3][:], scalar=wtab[:, t, 0:1], in1=hb[h3][:],
                                                                       op0=ALU.mult, op1=ALU.add), reads=[("y1", h3), ("hb", h3)], writes=[("hb", h3)])
            P.op("vector", lambda e, h3=h3, t=t: e.scalar_tensor_tensor(out=hb[h3][:], in0=y2[h3][:], scalar=wtab[:, t, 1:2], in1=hb[h3][:],
                                                                       op0=ALU.mult, op1=ALU.add), reads=[("y2", h3), ("hb", h3)], writes=[("hb", h3)])
            rmsnorm_tile(hb[h3][:], gple[:], n3[z][:], ("hb", h3), "gple", ("n3", z))
            P.op("scalar", lambda e, z=z, h3=h3: e.activation(out=pbf[z][:], in_=pin[h3][:], func=AF.Copy), reads=[("pin", h3)], writes=[("pbf", z)])

        def S1b(t):
            z = t % 2
            transpose_tile(n3[z], 8, n3T[z][:], ("n3", z), ("n3T", z))
            transpose_tile(pbf[z], 2, ppT[z][:], ("pbf", z), ("ppT", z))

        GACC = [((A0, "A0"), (A2, "A2")), ((A1, "A1"), (B0, ("B0", 0)))]

        def S2mm(t, half):
            z = t % 2
            (ga, gk), (pa_, pk) = GACC[half]
            for c in range(8):
                P.op("tensor", lambda e, c=c, half=half, ga=ga, z=z: e.matmul(
                    ga[:], lhsT=n3T[z][:, c, :], rhs=wpg[:, c, half * 512:(half + 1) * 512], start=(c == 0), stop=(c == 7)),
                    reads=[("n3T", z), ("wpg", 0), ("wpg", 4)], writes=[gk])
            for c in range(2):
                P.op("tensor", lambda e, c=c, half=half, pa_=pa_, z=z: e.matmul(
                    pa_[:, 0:512], lhsT=ppT[z][:, c, :], rhs=wpp[:, c, half * 512:(half + 1) * 512], start=(c == 0), stop=(c == 1)),
                    reads=[("ppT", z), "wpp"], writes=[pk])

        def S2tail(t):
            z = t % 2
            h3 = t % 3
            hsl = [slice(0, 512), slice(512, 1024)]
            for half in range(2):
                (ga, gk), (pa_, pk) = GACC[half]
                hs = hsl[half]
                P.op("vector", lambda e, ga=ga, z=z, hs=hs: e.tensor_tensor(out=gz[z][:, hs], in0=ga[:], in1=bpg[:, hs], op=ALU.add),
                     reads=[gk, "bpg"], writes=[("gz", z, half)])
                P.op("scalar", lambda e, z=z, hs=hs: e.activation(out=gz[z][:, hs], in_=gz[z][:, hs], func=AF.Tanh, scale=0.5),
                     reads=[("gz", z, half)], writes=[("gz", z, half)])
            for half in range(2):
                (ga, gk), (pa_, pk) = GACC[half]
                hs = hsl[half]
                P.op("vector", lambda e, pa_=pa_, z=z, hs=hs: e.scalar_tensor_tensor(out=gz[z][:, hs], in0=gz[z][:, hs], scalar=1.0, in1=pa_[:, 0:512],
                                                                                    op0=ALU.add, op1=ALU.mult), reads=[("gz", z, half), pk], writes=[("gz", z, half)])
                P.op("vector", lambda e, z=z, hs=hs, h3=h3: e.scalar_tensor_tensor(out=ob[z][:, hs], in0=gz[z][:, hs], scalar=0.5, in1=hb[h3][:, hs],
                                                                                  op0=ALU.mult, op1=ALU.add), reads=[("gz", z, half), ("hb", h3)], writes=[("ob", z, half)])

        loads4(0)
        loads4(1)
        S1a(0)
        S1b(0)
        for t in range(NT):
            z = t % 2
            if t + 2 < NT:
                loads4(t + 2)
            if t + 1 < NT:
                S1a(t + 1)
            S2mm(t, 0)
            S2mm(t, 1)
            S2tail(t)
            if t + 1 < NT:
                S1b(t + 1)
            P.op("sync", lambda e, t=t, z=z: e.dma_start(out=out_d.ap()[t * 128:(t + 1) * 128, :], in_=ob[z][:]),
                 reads=[("ob", z, 0), ("ob", z, 1)], dma=True, is_out=True)
    if 4 in phases:
        phase4()
    P.emit()
    return nc, P


def _sel_tables():
    f = np.arange(128)[:, None, None]
    j = np.arange(8)[None, :, None]
    m = np.arange(128)[None, None, :]
    hit = ((m // 8) == (2 * j + f // 64)).astype(np.float32)
    selA = (hit / 64.0).reshape(128, 8 * 128).astype(ml_dtypes.bfloat16)
    selB = (hit.transpose(2, 1, 0) / 8.0).reshape(128, 8 * 128).astype(ml_dtypes.bfloat16)
    return np.ascontiguousarray(selA), np.ascontiguousarray(selB)


def _host_layout(inp):
    f = lambda a: np.ascontiguousarray(np.asarray(a, dtype=np.float32))
    bf = ml_dtypes.bfloat16
    b_in = f(inp["b_in"])[0]
    rel = f(inp["rel_bias"])[0]
    jj = np.arange(5)[::-1][:, None, None]
    kk = np.arange(128)[None, :, None]
    qq = np.arange(128)[None, None, :]
    dist = qq - kk + 128 * (4 - jj)
    idx = np.clip(dist, -63, 256) + 63
    cdiff = (qq // 64) - (kk // 64) + 2 * (4 - jj)
    mask = ((cdiff >= 0) & (cdiff <= 8)).astype(np.float32)
    rbT = rel[:, idx]
    rbT = np.ascontiguousarray(rbT.transpose(2, 0, 1, 3)).reshape(128, NH * 5 * 128)
    maskT = np.ascontiguousarray(mask.transpose(1, 0, 2)).reshape(128, 5 * 128)
    cwv = f(inp["conv_w"])[0]
    cw = np.ascontiguousarray(cwv.reshape(3, 4, 128).transpose(2, 1, 0)).reshape(128, 12)
    cb = np.ascontiguousarray(f(inp["conv_b"])[0].reshape(4, 128).T)
    gq = f(inp["g_q"])[0]
    gk = f(inp["g_k"])[0]
    shared = {
        "g_mix": f(inp["g_mix"]),
        "w_in": f(inp["w_in"])[0],
        "bcol": np.ascontiguousarray(b_in.reshape(40, 128).T),
        "gqk": np.ascontiguousarray(np.stack([np.tile(gq, 2), np.tile(gk, 2)], axis=1)),
        "bv": np.ascontiguousarray(b_in[1024:1536].reshape(1, 512)),
        "rbT": rbT, "maskT": maskT, "cw": cw, "cb": cb,
        "w_pa": f(inp["w_pa"])[0], "w_pc": f(inp["w_pc"])[0], "w_o": f(inp["w_o"])[0],
        "g_ffn": f(inp["g_ffn"]),
        "w_rg": np.ascontiguousarray(np.concatenate([f(inp["w_group"])[0], f(inp["w_router"])[0]], axis=1)),
        "b_rg": np.ascontiguousarray(np.concatenate([f(inp["b_group"])[0], f(inp["b_router"])[0]])[None, :]),
        "w1": f(inp["w1"])[0], "w3": f(inp["w3"])[0], "w2": f(inp["w2"])[0],
        "g_ple": f(inp["g_ple"]), "w_pg": f(inp["w_ple_gate"])[0], "b_pg": f(inp["b_ple_gate"]),
        "w_pp": f(inp["w_ple_proj"])[0],
        "ident": np.eye(128, dtype=np.float32).astype(bf),
        "utri": np.triu(np.ones((128, 128), np.float32), 1).astype(bf),
        "ones": np.ones((128, 128), np.float32).astype(bf),
        "bdiag": (np.kron(np.eye(2, dtype=np.float32), np.ones((64, 64), np.float32)) / 64.0).astype(bf),
        "ecap": np.ascontiguousarray(np.broadcast_to((np.arange(NE, dtype=np.float32) * CAP)[None, :], (128, NE))),
        "selA": _sel_tables()[0], "selB": _sel_tables()[1],
    }
    x = f(inp["x"])
    p = f(inp["p"])[0]
    maps = []
    for c in range(NCORES):
        m = dict(shared)
        m["x"] = x[c]
        m["p"] = p[c]
        maps.append(m)
    return maps


_CACHE = {}


def kernel(**inputs):
    if "nc" not in _CACHE:
        _CACHE["nc"] = build(debug=False)[0]
    nc = _CACHE["nc"]
    maps = _host_layout(inputs)
    res = run_bass_kernel_spmd(nc, maps, core_ids=list(range(NCORES)))
    out = np.stack([np.asarray(res.results[c]["out"], dtype=np.float32) for c in range(NCORES)], axis=0)
    return out
```

```python
import numpy as np
import ml_dtypes
import concourse.bass as bass
import concourse.mybir as mybir
from concourse.bass_utils import run_bass_kernel_spmd

F32 = mybir.dt.float32
BF16 = mybir.dt.bfloat16
I32 = mybir.dt.int32
ALU = mybir.AluOpType
AF = mybir.ActivationFunctionType
AX = mybir.AxisListType

NCORES = 8
S = 4096
D = 1024
NT = S // 128
BT = 512
NB = S // BT
NH = 8
DH = 64
NE = 32
CAP = 384
NSLOT = NE * CAP
KR = 12
EPS = 1e-6

ENGS = ("sync", "scalar", "vector", "gpsimd", "tensor")
NDMASEM = 24
SAME_ENG_WINDOW = 10 ** 9


class Op:
    __slots__ = ("idx", "eng", "fn", "reads", "writes", "dma", "deps", "sig",
                 "sem", "val", "clock", "epos", "barrier")


class Prog:
    def __init__(self, nc):
        self.nc = nc
        self.ops = []
        self.last_w = {}
        self.readers = {}
        self.out_ops = []
        self.last_barrier = None
        self.since_barrier = []

    def op(self, eng, fn, reads=(), writes=(), dma=False, is_out=False):
        o = Op()
        o.idx = len(self.ops)
        o.eng = eng
        o.fn = fn
        o.dma = dma
        o.barrier = False
        o.reads = tuple(reads)
        o.writes = tuple(writes)
        deps = set()
        for k in o.reads:
            w = self.last_w.get(k)
            if w is not None:
                deps.add(w)
        for k in o.writes:
            w = self.last_w.get(k)
            if w is not None:
                deps.add(w)
            for r in self.readers.get(k, ()):
                deps.add(r)
        for k in o.writes:
            self.last_w[k] = o.idx
            self.readers[k] = []
        for k in o.reads:
            if k not in o.writes:
                self.readers.setdefault(k, []).append(o.idx)
        if self.last_barrier is not None:
            deps.add(self.last_barrier)
        deps.discard(o.idx)
        o.deps = sorted(deps)
        o.sig = False
        self.ops.append(o)
        self.since_barrier.append(o.idx)
        if is_out:
            self.out_ops.append(o.idx)
        return o.idx

    def barrier(self):
        o = Op()
        o.idx = len(self.ops)
        o.eng = "sync"
        o.fn = "BARRIER"
        o.dma = False
        o.barrier = True
        o.reads = ()
        o.writes = ()
        last = {}
        deps = []
        for i in self.since_barrier:
            p = self.ops[i]
            if p.dma:
                deps.append(i)
            else:
                last[p.eng] = i
        deps.extend(last.values())
        if self.last_barrier is not None:
            deps.append(self.last_barrier)
        o.deps = sorted(set(deps))
        o.sig = True
        self.ops.append(o)
        self.last_barrier = o.idx
        self.since_barrier = []
        self.last_w = {}
        self.readers = {}

    def emit(self):
        nc = self.nc
        ops = self.ops
        epos = {e: 0 for e in ENGS}
        for o in ops:
            o.epos = epos[o.eng]
            epos[o.eng] += 1
        fin = Op()
        fin.idx = len(ops)
        fin.eng = "sync"
        fin.fn = None
        fin.dma = False
        fin.barrier = False
        fin.reads = ()
        fin.writes = ()
        fin.deps = list(self.out_ops)
        fin.sig = False
        fin.epos = epos["sync"]
        ops = ops + [fin]
        for o in ops:
            nd = []
            for d in o.deps:
                do = ops[d]
                if do.eng == o.eng and not do.dma and not o.barrier:
                    if o.eng == "tensor" and not o.dma:
                        continue
                    if o.dma:
                        pass
                    elif o.epos - do.epos > SAME_ENG_WINDOW:
                        continue
                nd.append(d)
            o.deps = nd
            for d in nd:
                ops[d].sig = True
        sems = {}
        dma_engs = set(o.eng for o in ops if o.dma)
        for e in ENGS:
            sems[("c", e)] = nc.alloc_semaphore("c_" + e)
            if e in dma_engs:
                for i in range(NDMASEM):
                    sems[("d", e, i)] = nc.alloc_semaphore("d_%s_%d" % (e, i))
        ccount = {e: 0 for e in ENGS}
        dcount = {e: 0 for e in ENGS}
        dma_prev = {}
        for o in ops:
            if o.dma:
                k = dcount[o.eng]
                dcount[o.eng] += 1
                slot = k % NDMASEM
                o.sem = ("d", o.eng, slot)
                o.val = 16 * (k // NDMASEM + 1)
                prev = dma_prev.get((o.eng, slot))
                if prev is not None and prev not in o.deps:
                    o.deps.append(prev)
                dma_prev[(o.eng, slot)] = o.idx
            elif o.sig:
                ccount[o.eng] += 1
                o.sem = ("c", o.eng)
                o.val = ccount[o.eng]
            else:
                o.sem = None
                o.val = 0
        known = {e: {} for e in ENGS}
        streams = {e: [] for e in ENGS}
        for o in ops:
            kn = known[o.eng]
            wm = {}
            for d in sorted(o.deps, reverse=True):
                do = ops[d]
                if kn.get(do.sem, 0) >= do.val:
                    continue
                if wm.get(do.sem, 0) < do.val:
                    wm[do.sem] = do.val
                for s, v in do.clock.items():
                    if kn.get(s, 0) < v:
                        kn[s] = v
            o.clock = dict(kn)
            if o.sem is not None:
                o.clock[o.sem] = o.val
            streams[o.eng].append((o, list(wm.items())))
        self.n_waits = sum(len(w) for st in streams.values() for _, w in st)
        self.counts = (dict(ccount), dict(dcount))

        def run_stream(eng_name):
            def body(eng):
                for o, waits in streams[eng_name]:
                    for s, v in waits:
                        eng.wait_ge(sems[s], v)
                    if o.fn is None:
                        continue
                    if o.barrier:
                        eng.sem_inc(sems[o.sem], 1)
                        continue
                    ins = o.fn(eng)
                    if o.sem is not None:
                        ins.then_inc(sems[o.sem], 16 if o.dma else 1)
            return body

        with nc.Block() as block:
            for e in ENGS:
                if streams[e]:
                    getattr(block, e)(run_stream(e))


class SBAlloc:
    LO = 16512
    HI = 229344

    def __init__(self, nc):
        self.nc = nc
        self.cur = self.LO
        self.n = 0

    def alloc(self, name, shape, dt):
        esz = {F32: 4, BF16: 2, I32: 4}[dt]
        nbytes = esz
        for s in shape[1:]:
            nbytes *= s
        off = (self.cur + 31) // 32 * 32
        assert off + nbytes <= self.HI, "SBUF overflow at %s: need %d have %d" % (name, nbytes, self.HI - off)
        self.n += 1
        t = self.nc.alloc_sbuf_tensor_at("%s_%d" % (name, self.n), list(shape), dt, offset=off)
        self.cur = off + nbytes
        self.off = getattr(self, "off", {})
        self.off[name] = off
        return t

    def alloc_alias(self, name, shape, dt, of):
        self.n += 1
        return self.nc.alloc_sbuf_tensor_at("%s_%d" % (name, self.n), list(shape), dt, offset=self.off[of])

    def mark(self):
        return self.cur

    def reset(self, m):
        self.cur = m


def bc_last(ap, n):
    shp = list(ap.shape)
    return ap.unsqueeze(len(shp)).broadcast_to(shp + [n])


def bc_mid(ap, n):
    shp = list(ap.shape)
    return ap.unsqueeze(1).broadcast_to([shp[0], n] + shp[1:])


def build(debug=False, phases=(1, 2, 3, 4)):
    nc = bass.Bass("TRN2", target_bir_lowering=False)
    P = Prog(nc)
    sb = SBAlloc(nc)

    def din(name, shape, dt=F32):
        return nc.dram_tensor(name, list(shape), dt, kind="ExternalInput")

    def dscr(name, shape, dt):
        return nc.dram_tensor(name, list(shape), dt, kind="ExternalOutput" if debug else "Internal")

    x_d = din("x", [S, D])
    p_d = din("p", [S, 256])
    gmix_d = din("g_mix", [1, D])
    win_d = din("w_in", [D, 5120])
    bcol_d = din("bcol", [128, 40])
    gqk_d = din("gqk", [128, 2])
    bv_d = din("bv", [1, 512])
    rbT_d = din("rbT", [128, NH * 5 * 128])
    maskT_d = din("maskT", [128, 5 * 128])
    cw_d = din("cw", [128, 12])
    cb_d = din("cb", [128, 4])
    wpa_d = din("w_pa", [512, D])
    wpc_d = din("w_pc", [512, D])
    wo_d = din("w_o", [D, D])
    gffn_d = din("g_ffn", [1, D])
    wrg_d = din("w_rg", [D, 36])
    brg_d = din("b_rg", [1, 36])
    w1_d = din("w1", [NE, D, 512])
    w3_d = din("w3", [NE, D, 512])
    w2_d = din("w2", [NE, 512, D])
    gple_d = din("g_ple", [1, D])
    wpg_d = din("w_pg", [D, D])
    bpg_d = din("b_pg", [1, D])
    wpp_d = din("w_pp", [256, D])
    ident_d = din("ident", [128, 128], BF16)
    utri_d = din("utri", [128, 128], BF16)
    ones_d = din("ones", [128, 128], BF16)
    bdiag_d = din("bdiag", [128, 128], BF16)
    ecap_d = din("ecap", [128, NE])
    selA_d = din("selA", [128, 8 * 128], BF16)
    selB_d = din("selB", [128, 8 * 128], BF16)
    out_d = nc.dram_tensor("out", [S, D], F32, kind="ExternalOutput")

    nT_d = dscr("nT_s", [D, S], BF16)
    yaT_d = dscr("yaT_s", [512, S], BF16)
    ycT_d = dscr("ycT_s", [512, S], BF16)
    h_d = dscr("h_s", [S, D], F32)
    xs_d = dscr("xs_s", [NSLOT + 128, D], BF16)
    ys_d = dscr("ys_s", [NSLOT + 128, D], BF16)
    if debug:
        dtab_o = nc.dram_tensor("dtab_o", [128, NT * 2], I32, kind="ExternalOutput")
        wtab_o = nc.dram_tensor("wtab_o", [128, NT * 2], F32, kind="ExternalOutput")

    pT = nc.alloc_psum_tensor("pT", [128, 8, 128], BF16)
    A0 = nc.alloc_psum_tensor("A0", [128, 512], F32)
    A1 = nc.alloc_psum_tensor("A1", [128, 512], F32)
    A2 = nc.alloc_psum_tensor("A2", [128, 512], F32)
    B0 = nc.alloc_psum_tensor("B0", [128, 1024], F32)
    B1 = nc.alloc_psum_tensor("B1", [128, 1024], F32)

    ident = sb.alloc("ident", [128, 128], BF16)
    utri = sb.alloc("utri", [128, 128], BF16)
    ones = sb.alloc("ones", [128, 128], BF16)
    bdiag = sb.alloc("bdiag", [128, 128], BF16)
    ecap = sb.alloc("ecap", [128, NE], F32)
    bcol = sb.alloc("bcol", [128, 40], F32)
    hbcol = sb.alloc("hbcol", [128, 40], F32)
    mhalf = sb.alloc("mhalf", [128, 8], F32)
    epsc = sb.alloc("epsc", [128, 8], F32)
    dtab = sb.alloc("dtab", [128, NT, 2], I32)
    wtab = sb.alloc("wtab", [128, NT, 2], F32)
    cnt = sb.alloc("cnt", [128, NE], F32)
    ss = sb.alloc("ss", [128, 8], F32)
    rs = sb.alloc("rs", [128, 8], F32)
    junk = sb.alloc("junk", [128, D], BF16)

    def ld(eng, dst, src, key):
        P.op(eng, lambda e: e.dma_start(out=dst, in_=src), writes=[key], dma=True)

    ld("sync", ident[:], ident_d.ap(), "ident")
    ld("sync", utri[:], utri_d.ap(), "utri")
    ld("sync", ones[:], ones_d.ap(), "ones")
    ld("sync", bdiag[:], bdiag_d.ap(), "bdiag")
    ld("sync", ecap[:], ecap_d.ap(), "ecap")
    ld("sync", bcol[:], bcol_d.ap(), "bcol")
    P.op("vector", lambda e: e.tensor_scalar(out=hbcol[:], in0=bcol[:], scalar1=0.5, scalar2=None, op0=ALU.mult),
         reads=["bcol"], writes=["hbcol"])
    P.op("gpsimd", lambda e: e.memset(mhalf[:], -0.5), writes=["mhalf"])
    P.op("gpsimd", lambda e: e.memset(epsc[:], EPS), writes=["epsc"])
    P.op("gpsimd", lambda e: e.memset(cnt[:], 0.0), writes=["cnt"])
    P.op("gpsimd", lambda e: e.memset(wtab[:], 0.0), writes=["wtab"])

    nrm_ctr = [0]

    def rmsnorm_tile(src, g_bc, dst_bf, src_key, g_key, dst_key):
        i = nrm_ctr[0] % 8
        nrm_ctr[0] += 1
        ssk, rsk = ("ss", i), ("rs", i)
        P.op("scalar", lambda e: e.activation(out=junk[:], in_=src, func=AF.Square, accum_out=ss[:, i:i + 1]),
             reads=[src_key], writes=["junk", ssk])
        P.op("vector", lambda e: e.tensor_scalar(out=rs[:, i:i + 1], in0=ss[:, i:i + 1], scalar1=1.0 / D, scalar2=EPS,
                                                  op0=ALU.mult, op1=ALU.add), reads=[ssk], writes=[rsk])
        P.op("gpsimd", lambda e: e.tensor_tensor(out=rs[:, i:i + 1], in0=rs[:, i:i + 1], in1=mhalf[:, 0:1], op=ALU.pow),
             reads=[rsk, "mhalf"], writes=[rsk])
        P.op("vector", lambda e: e.scalar_tensor_tensor(out=dst_bf, in0=src, scalar=rs[:, i:i + 1], in1=g_bc,
                                                         op0=ALU.mult, op1=ALU.mult),
             reads=[src_key, rsk, g_key], writes=[dst_key])

    def transpose_tile(src_bf, nchunk, dst, src_key, dst_key, evac="scalar", interleave=0):
        for c in range(nchunk):
            if interleave:
                src_c = src_bf.rearrange("t (p c) -> t c p", c=interleave)[:, c, :]
            else:
                src_c = src_bf[:, c * 128:(c + 1) * 128]
            P.op("tensor", lambda e, c=c, src_c=src_c: e.transpose(out=pT[:, c, :], in_=src_c, identity=ident[:]),
                 reads=(list(src_key) if isinstance(src_key, list) else [src_key]) + ["ident"], writes=[("pT", c)])
        if evac == "scalar":
            P.op("scalar", lambda e: e.activation(out=dst, in_=pT[:, 0:nchunk, :], func=AF.Copy),
                 reads=[("pT", c) for c in range(nchunk)], writes=[dst_key])
        else:
            P.op("vector", lambda e: e.tensor_copy(out=dst, in_=pT[:, 0:nchunk, :]),
                 reads=[("pT", c) for c in range(nchunk)], writes=[dst_key])

    _breg = {}

    def breg(e):
        if "r" not in _breg:
            _breg["r"] = e.to_reg(NSLOT - 1)
        return _breg["r"]

    base_mark = sb.mark()
    BASE = (base_mark + 31) // 32 * 32
    pre2 = {"Wg": nc.alloc_sbuf_tensor_at("Wg_pre", [128, 8, 2048], BF16, offset=BASE),
            "wpa": nc.alloc_sbuf_tensor_at("wpa_pre", [128, 4, D], BF16, offset=BASE + 32768),
            "wpc": nc.alloc_sbuf_tensor_at("wpc_pre", [128, 4, D], BF16, offset=BASE + 40960)}
    pre3 = [{"w1": nc.alloc_sbuf_tensor_at("w1_pre%d" % i, [128, 8, 512], BF16, offset=BASE + i * 24576),
             "w3": nc.alloc_sbuf_tensor_at("w3_pre%d" % i, [128, 8, 512], BF16, offset=BASE + i * 24576 + 8192),
             "w2": nc.alloc_sbuf_tensor_at("w2_pre%d" % i, [128, 4, D], BF16, offset=BASE + i * 24576 + 16384)} for i in range(2)]
    WGKEYS = [("Wg", g0 + q4 * 256) for q4 in range(4) for g0 in (0, 1024)]

    def p2_weight_loads(Wg, wpa, wpc, extra_writes=()):
        win_v2 = win_d.ap().rearrange("(c p) n -> p c n", p=128)
        ew = list(extra_writes)
        if ew:
            for c in range(0, 8, 2):
                P.op("gpsimd", lambda e, c=c: e.dma_start(out=Wg[:, c:c + 2, :], in_=win_v2[:, c:c + 2, 3072:5120]),
                     writes=WGKEYS + ew, dma=True)
            P.op("gpsimd", lambda e: e.dma_start(out=wpa[:], in_=wpa_d.ap().rearrange("(c p) n -> p c n", p=128)), writes=["wpa"] + ew, dma=True)
            P.op("gpsimd", lambda e: e.dma_start(out=wpc[:], in_=wpc_d.ap().rearrange("(c p) n -> p c n", p=128)), writes=["wpc"] + ew, dma=True)
            return

        def wg_load(q4):
            for g0 in (0, 1024):
                c0_ = g0 + q4 * 256
                P.op("gpsimd", lambda e, c0_=c0_: e.dma_start(out=Wg[:, :, c0_:c0_ + 256], in_=win_v2[:, :, 3072 + c0_:3072 + c0_ + 256]),
                     writes=[("Wg", c0_)] + ew, dma=True)

        wg_load(0)
        P.op("gpsimd", lambda e: e.dma_start(out=wpa[:], in_=wpa_d.ap().rearrange("(c p) n -> p c n", p=128)), writes=["wpa"] + ew, dma=True)
        P.op("gpsimd", lambda e: e.dma_start(out=wpc[:], in_=wpc_d.ap().rearrange("(c p) n -> p c n", p=128)), writes=["wpc"] + ew, dma=True)
        for q4 in range(1, 4):
            wg_load(q4)

    def expert_weight_loads(ex, w1t, w3t, w2t, keys, extra_writes=()):
        ew = list(extra_writes)
        P.op("gpsimd", lambda e: e.dma_start(out=w1t[:], in_=w1_d.ap()[ex].rearrange("(p c) f -> p c f", c=8)), writes=[keys[0]] + ew, dma=True)
        P.op("gpsimd", lambda e: e.dma_start(out=w3t[:], in_=w3_d.ap()[ex].rearrange("(p c) f -> p c f", c=8)), writes=[keys[1]] + ew, dma=True)
        P.op("gpsimd", lambda e: e.dma_start(out=w2t[:], in_=w2_d.ap()[ex].rearrange("(c p) f -> p c f", p=128)), writes=[keys[2]] + ew, dma=True)

    def phase1():
        Wa = sb.alloc("Wa", [128, 8, 3072], BF16)
        gmix = sb.alloc("gmix", [128, D], F32)
        gqk = sb.alloc("gqk", [128, 2], F32)
        bvb = sb.alloc("bvb", [128, 512], F32)
        cw = sb.alloc("cw", [128, 12], F32)
        cb = sb.alloc("cb", [128, 4], F32)
        expB = sb.alloc("expB", [128, NH, 5, 128], BF16)
        maskT = sb.alloc("maskT", [128, 5, 128], F32)
        kring = sb.alloc("kring", [128, 4, KR * 128], BF16)
        vring = sb.alloc("vring", [128, KR, NH, 65], BF16)
        xt = [sb.alloc("xt%d" % i, [128, D], F32) for i in range(2)]
        nb = [sb.alloc("nb%d" % i, [128, D], BF16) for i in range(2)]
        nTb = [sb.alloc("nTb%d" % i, [128, 8, BT], BF16) for i in range(2)]
        qT = [sb.alloc("qT%d" % i, [128, 4, BT], BF16) for i in range(2)]
        zq = [sb.alloc("zq%d" % i, [128, BT], F32) for i in range(8)]
        sq = [sb.alloc("sq%d" % i, [128, BT], BF16) for i in range(3)]
        rs = sb.alloc("rs_all", [128, BT], F32)
        r1b = sb.alloc("r1b", [128, BT], BF16)
        selA = sb.alloc("selA", [128, 8, 128], BF16)
        selB = sb.alloc("selB", [128, 8, 128], BF16)
        gw = sb.alloc("gw", [128, 1], F32)
        us = [sb.alloc("us%d" % i, [128, BT], F32) for i in range(2)]
        t1 = [sb.alloc("t1%d" % i, [128, BT], F32) for i in range(2)]
        cu = sb.alloc("cu", [128, 4, BT + 2], F32)
        ycT = [sb.alloc("ycT%d" % i, [128, 4, BT], BF16) for i in range(2)]
        yaT = [sb.alloc("yaT%d" % i, [128, 4, BT], BF16) for i in range(2)]
        pt = [sb.alloc("pt%d" % i, [128, 4, 128], BF16) for i in range(6)]
        rden = [sb.alloc("rden%d" % i, [128, 4], F32) for i in range(2)]
        ya = sb.alloc("ya", [128, 4, 512], BF16)

        win_v = win_d.ap().rearrange("(c p) n -> p c n", p=128)
        for (c0_, c1_) in ((0, 1024), (1024, 1536), (1536, 3072)):
            for c in range(0, 8, 2):
                P.op("gpsimd", lambda e, c=c, c0_=c0_, c1_=c1_: e.dma_start(out=Wa[:, c:c + 2, c0_:c1_], in_=win_v[:, c:c + 2, c0_:c1_]),
                     writes=[("Wa", c, c0_), ("Wa", c + 1, c0_)], dma=True)
        zt = sb.alloc("zt", [128, 2 * D], BF16)
        P.op("gpsimd", lambda e: e.memset(zt[:], 0.0), writes=["zt"])
        NR = (NSLOT + 128) // 128
        xs_z = xs_d.ap().rearrange("(p r) d -> p (r d)", p=128)
        zchunks = [(r0, min(r0 + 2, NR)) for r0 in range(0, NR, 2)]

        def zero_fill(k):
            for (r0, r1_) in zchunks[k::NB]:
                P.op("sync", lambda e, r0=r0, r1_=r1_: e.dma_start(out=xs_z[:, r0 * D:r1_ * D], in_=zt[:, 0:(r1_ - r0) * D]),
                     reads=["zt"], writes=[("xs_zero", r0)], dma=True)

        ld("sync", gmix[:], gmix_d.ap().partition_broadcast(128), "gmix")
        ld("sync", gqk[:], gqk_d.ap(), "gqk")
        ld("sync", selA[:], selA_d.ap().rearrange("p (j m) -> p j m", j=8), "selA")
        ld("sync", selB[:], selB_d.ap().rearrange("p (j m) -> p j m", j=8), "selB")
        ld("sync", bvb[:], bv_d.ap().partition_broadcast(128), "bvb")
        P.op("vector", lambda e: e.tensor_tensor(out=gw[:], in0=gqk[:, 0:1], in1=gqk[:, 1:2], op=ALU.mult), reads=["gqk"], writes=["gw"])
        ld("sync", cw[:], cw_d.ap(), "cw")
        ld("sync", cb[:], cb_d.ap(), "cb")
        ld("sync", maskT[:], maskT_d.ap().rearrange("p (j q) -> p j q", j=5), "maskT")
        rb_v = rbT_d.ap().rearrange("p (h n) -> p h n", h=NH)
        stg = [sb.alloc_alias("stg0", [128, 640], F32, "zq0"), sb.alloc_alias("stg1", [128, 640], F32, "zq2")]
        for h in range(NH):
            st = stg[h % 2]
            sk = [("zq", 2 * (h % 2)), ("zq", 2 * (h % 2) + 1)]
            P.op("sync", lambda e, h=h, st=st: e.dma_start(out=st[:], in_=rb_v[:, h, :]), writes=sk, dma=True)
            P.op("scalar", lambda e, st=st: e.activation(out=st[:], in_=st[:], func=AF.Exp), reads=sk, writes=sk)
            P.op("vector", lambda e, h=h, st=st: e.tensor_tensor(
                out=expB[:, h, :, :], in0=st[:].rearrange("p (j q) -> p j q", j=5), in1=maskT[:], op=ALU.mult),
                reads=sk + ["maskT"], writes=[("expB", h)])
        P.op("gpsimd", lambda e: e.memset(vring[:], 1.0), writes=[("v", s_) for s_ in range(KR)])
        P.op("gpsimd", lambda e: e.memset(cu[:], 0.0), writes=[("cu", ct) for ct in range(4)] + [("cuh", ct) for ct in range(4)])

        nT_v = nT_d.ap().rearrange("(c p) t -> p c t", p=128)
        yaT_v = yaT_d.ap().rearrange("(c p) t -> p c t", p=128)
        ycT_v = ycT_d.ap().rearrange("(c p) t -> p c t", p=128)
        acc_rot = [0]
        ACC = [(A0, "A0"), (A1, "A1")]

        def next_acc():
            a = ACC[acc_rot[0] % 2]
            acc_rot[0] += 1
            return a

        def A_norm(b, tl):
            t = 4 * b + tl
            s2 = t % 2
            P.op("sync", lambda e, t=t, s2=s2: e.dma_start(out=xt[s2][:], in_=x_d.ap()[t * 128:(t + 1) * 128, :]),
                 writes=[("xt", s2)], dma=True)
            rmsnorm_tile(xt[s2][:], gmix[:], nb[s2][:], ("xt", s2), "gmix", ("nb", s2))

        def A_tr(b, tl):
            t = 4 * b + tl
            s2 = t % 2
            bb = b % 2
            transpose_tile(nb[s2], 8, nTb[bb][:, :, tl * 128:(tl + 1) * 128], ("nb", s2), ("nTb", bb, tl))
            if tl == 3:
                P.op("sync", lambda e, bb=bb, b=b: e.dma_start(out=nT_v[:, :, b * BT:(b + 1) * BT], in_=nTb[bb][:]),
                     reads=[("nTb", bb, q_) for q_ in range(4)], writes=[("nT_d", b)], dma=True)

        def secC(b):
            bb = b % 2
            tok0 = b * BT
            nkeys = [("nTb", bb, tl) for tl in range(4)]
            PACC = [(A0[:], "A0"), (A1[:], "A1"), (B0[:, 0:512], ("B0", 0))]
            prot = [0]

            def nacc():
                a = PACC[prot[0] % 3]
                prot[0] += 1
                return a

            def proj(j):
                acc, akey = nacc()
                for c in range(8):
                    P.op("tensor", lambda e, j=j, c=c, acc=acc: e.matmul(
                        acc, lhsT=Wa[:, c, j * 128:(j + 1) * 128], rhs=nTb[bb][:, c, :], start=(c == 0), stop=(c == 7)),
                        reads=[("Wa", c, 0)] + nkeys, writes=[akey])
                z = j % 3
                P.op("scalar", lambda e, j=j, acc=acc: e.activation(out=zq[j][:], in_=acc, func=AF.Identity, bias=bcol[:, j:j + 1]),
                     reads=[akey, "bcol"], writes=[("zq", j)])
                P.op("gpsimd", lambda e, z=z, j=j: e.tensor_tensor(out=sq[z][:], in0=zq[j][:], in1=zq[j][:], op=ALU.mult),
                     reads=[("zq", j)], writes=[("sq", z)])

            def msacc(j):
                z = j % 3
                P.op("tensor", lambda e, z=z, j=j: e.matmul(A2[:], lhsT=selA[:, j, :], rhs=sq[z][:], start=(j == 0), stop=(j == 7)),
                     reads=[("sq", z), "selA"], writes=["A2"])

            def vproj():
                for tl in range(4):
                    t = 4 * b + tl
                    sl = t % KR
                    acc, akey = nacc()
                    for c in range(8):
                        P.op("tensor", lambda e, c=c, tl=tl, acc=acc: e.matmul(
                            acc, lhsT=nTb[bb][:, c, tl * 128:(tl + 1) * 128], rhs=Wa[:, c, 1024:1536], start=(c == 0), stop=(c == 7)),
                            reads=[("Wa", c, 1024), ("nTb", bb, tl)], writes=[akey])
                    P.op("vector", lambda e, sl=sl, acc=acc: e.tensor_tensor(
                        out=vring[:, sl, :, 0:64], in0=acc.rearrange("p (h d) -> p h d", h=NH),
                        in1=bvb[:].rearrange("p (h d) -> p h d", h=NH), op=ALU.add),
                        reads=[akey, "bvb"], writes=[("v", sl)])

            def fin(j):
                acc, akey = nacc()
                P.op("tensor", lambda e, j=j, acc=acc: e.matmul(acc, lhsT=selB[:, j, :], rhs=r1b[:], start=True, stop=True),
                     reads=["selB", "r1b"], writes=[akey])
                if j < 4:
                    P.op("vector", lambda e, j=j, acc=acc: e.tensor_tensor(out=qT[bb][:, j, :], in0=zq[j][:], in1=acc, op=ALU.mult),
                         reads=[("zq", j), akey], writes=[("qT", bb, j)])
                else:
                    hp = j - 4
                    sl0 = (4 * b) % KR
                    P.op("vector", lambda e, hp=hp, j=j, sl0=sl0, acc=acc: e.scalar_tensor_tensor(
                        out=kring[:, hp, sl0 * 128:(sl0 + 4) * 128], in0=zq[j][:], scalar=gw[:, 0:1], in1=acc, op0=ALU.mult, op1=ALU.mult),
                        reads=[("zq", j), akey, "gw"], writes=[("k", hp, sl0 + q_) for q_ in range(4)])

            proj(0)
            for j in range(8):
                if j + 1 < 8:
                    proj(j + 1)
                msacc(j)
            P.op("scalar", lambda e: e.activation(out=rs[:], in_=A2[:], func=AF.Sqrt, bias=epsc[:, 0:1]), reads=["A2", "epsc"], writes=["rs"])
            P.op("vector", lambda e: e.reciprocal(out=rs[:], in_=rs[:]), reads=["rs"], writes=["rs"])
            P.op("scalar", lambda e: e.activation(out=r1b[:], in_=rs[:], func=AF.Copy), reads=["rs"], writes=["r1b"])
            vproj()
            for j in range(8):
                fin(j)
        def secC3(b):
            bb = b % 2
            tok0 = b * BT
            nkeys = [("nTb", bb, tl) for tl in range(4)]
            SETS = [((A0[:], ["A0"]), (A1[:], ["A1"]), (A2[:], ["A2"])),
                    ((B0[:, 0:512], [("B0", 0)]), (B0[:, 512:1024], [("B0", 4)]), (B1[:, 0:512], [("B1h", 0)]))]
            for ct in range(4):
                z = ct % 2
                (pu, ku), (pb, kb), (pc, kc) = SETS[ct % 2]
                for (dst, dkey, col0) in ((pu, ku, 1536), (pb, kb, 2048), (pc, kc, 2560)):
                    for c in range(8):
                        P.op("tensor", lambda e, c=c, dst=dst, col0=col0, ct=ct: e.matmul(
                            dst, lhsT=Wa[:, c, col0 + ct * 128:col0 + (ct + 1) * 128], rhs=nTb[bb][:, c, :],
                            start=(c == 0), stop=(c == 7)),
                            reads=[("Wa", c, 1536)] + nkeys, writes=dkey)
                ju, jb, jc = 12 + ct, 16 + ct, 20 + ct
                P.op("scalar", lambda e, z=z, ju=ju, pu=pu: e.activation(out=us[z][:], in_=pu, func=AF.Identity, bias=bcol[:, ju:ju + 1]),
                     reads=ku + ["bcol"], writes=[("us", z)])
                P.op("vector", lambda e, z=z, jc=jc, ct=ct, pc=pc: e.scalar_tensor_tensor(
                    out=cu[:, ct, 2:BT + 2], in0=pc, scalar=bcol[:, jc:jc + 1], in1=us[z][:], op0=ALU.add, op1=ALU.mult),
                    reads=kc + ["bcol", ("us", z)], writes=[("cu", ct)])
                P.op("scalar", lambda e, z=z, ct=ct: e.activation(out=t1[z][:], in_=cu[:, ct, 2:BT + 2], func=AF.Identity,
                                                               scale=cw[:, ct * 3 + 2:ct * 3 + 3], bias=cb[:, ct:ct + 1]),
                     reads=[("cu", ct), "cw", "cb"], writes=[("t1", z)])
                P.op("vector", lambda e, z=z, ct=ct: e.scalar_tensor_tensor(
                    out=t1[z][:], in0=cu[:, ct, 1:BT + 1], scalar=cw[:, ct * 3 + 1:ct * 3 + 2], in1=t1[z][:], op0=ALU.mult, op1=ALU.add),
                    reads=[("cu", ct), ("cuh", ct), "cw", ("t1", z)], writes=[("t1", z)])
                P.op("vector", lambda e, z=z, ct=ct: e.scalar_tensor_tensor(
                    out=t1[z][:], in0=cu[:, ct, 0:BT], scalar=cw[:, ct * 3:ct * 3 + 1], in1=t1[z][:], op0=ALU.mult, op1=ALU.add),
                    reads=[("cu", ct), ("cuh", ct), "cw", ("t1", z)], writes=[("t1", z)])
                P.op("vector", lambda e, z=z, jb=jb, ct=ct, pb=pb: e.scalar_tensor_tensor(
                    out=ycT[bb][:, ct, :], in0=pb, scalar=bcol[:, jb:jb + 1], in1=t1[z][:], op0=ALU.add, op1=ALU.mult),
                    reads=kb + ["bcol", ("t1", z)], writes=[("ycT", bb, ct)])
                P.op("vector", lambda e, ct=ct: e.tensor_copy(out=cu[:, ct, 0:2], in_=cu[:, ct, BT:BT + 2]),
                     reads=[("cu", ct)], writes=[("cuh", ct)])
            P.op("sync", lambda e, tok0=tok0: e.dma_start(out=ycT_v[:, :, tok0:tok0 + BT], in_=ycT[bb][:]),
                 reads=[("ycT", bb, ct) for ct in range(4)], writes=[("ycT_d", b)], dma=True)

        def secD(b):
            bb = b % 2
            tok0 = b * BT
            nxt = b + 1 < NB
            units = []
            for h in range(NH):
                for m in range(8):
                    kt = 4 * b - 4 + m
                    if kt < 0:
                        continue
                    units.append((h, m, kt, max(m - 4, 0), min(m, 3)))
            SPS = [(A0[:], "A0"), (A1[:], "A1"), (A2[:], "A2"), (B0[:, 0:512], ("B0", 0)), (B0[:, 512:1024], ("B0", 4))]
            LA = 4

            def QKEXP(u):
                h, m, kt, tlo, thi = units[u]
                hp, r0 = h // 2, (h % 2) * 64
                nq = thi - tlo + 1
                sp, sk = SPS[u % 5]
                pz = u % 6
                sl = kt % KR
                P.op("tensor", lambda e, hp=hp, r0=r0, sl=sl, sp=sp, tlo=tlo, thi=thi: e.matmul(
                    sp[:, 0:(thi - tlo + 1) * 128], lhsT=kring[r0:r0 + 64, hp, sl * 128:(sl + 1) * 128],
                    rhs=qT[bb][r0:r0 + 64, hp, tlo * 128:(thi + 1) * 128], start=True, stop=True),
                    reads=[("k", hp, sl), ("qT", bb, hp)], writes=[sk])
                P.op("scalar", lambda e, pz=pz, sp=sp, nq=nq: e.activation(
                    out=pt[pz][:, 0:nq, :], in_=sp[:, 0:nq * 128].rearrange("p (j q) -> p j q", q=128), func=AF.Exp, scale=DH ** -0.5),
                    reads=[sk], writes=[("pt", pz)])
                rlo = 4 - m + tlo
                P.op("vector", lambda e, pz=pz, nq=nq, h=h, rlo=rlo: e.tensor_tensor(
                    out=pt[pz][:, 0:nq, :], in0=pt[pz][:, 0:nq, :], in1=expB[:, h, rlo:rlo + nq, :], op=ALU.mult),
                    reads=[("pt", pz), ("expB", h)], writes=[("pt", pz)])

            def PV(u):
                h, m, kt, tlo, thi = units[u]
                pz = u % 6
                sl = kt % KR
                hb2 = h % 2
                first = (u == 0) or units[u - 1][0] != h
                last_u = (u + 1 == len(units)) or units[u + 1][0] != h
                if first:
                    P.op("tensor", lambda e, hb2=hb2: e.matmul(
                        B1[:, hb2 * 512:hb2 * 512 + 260], lhsT=zt[:, 0:128], rhs=zt[:, 0:260], start=True, stop=False),
                        reads=["zt"], writes=[("B1h", hb2)])
                for tl in range(tlo, thi + 1):
                    c0 = hb2 * 512 + tl * 65
                    P.op("tensor", lambda e, h=h, tl=tl, tlo=tlo, sl=sl, pz=pz, c0=c0, fin=(last_u and tl == thi): e.matmul(
                        B1[:, c0:c0 + 65], lhsT=pt[pz][:, tl - tlo, :], rhs=vring[:, sl, h, :],
                        start=False, stop=fin),
                        reads=[("pt", pz), ("v", sl)], writes=[("B1h", hb2)])

            def FINH(h):
                hb2 = h % 2
                Bv = B1[:, hb2 * 512:hb2 * 512 + 260].rearrange("p (t d) -> p t d", d=65)
                P.op("vector", lambda e, hb2=hb2, Bv=Bv: e.reciprocal(out=rden[hb2][:], in_=Bv[:, :, 64]),
                     reads=[("B1h", hb2)], writes=[("rden", hb2)])
                P.op("vector", lambda e, hb2=hb2, Bv=Bv, h=h: e.tensor_tensor(
                    out=ya[:, :, h * 64:(h + 1) * 64], in0=Bv[:, :, 0:64], in1=bc_last(rden[hb2][:], 64), op=ALU.mult),
                    reads=[("B1h", hb2), ("rden", hb2)], writes=[("ya", h)])

            if nxt:
                A_norm(b + 1, 0)
            for u in range(min(LA, len(units))):
                QKEXP(u)
            secC3(b)
            zero_fill(b)
            if b == NB - 1 and 2 in phases:
                p2_weight_loads(pre2["Wg"], pre2["wpa"], pre2["wpc"],
                                extra_writes=[("Wa", c, c0_) for c in range(8) for c0_ in (0, 1024, 1536)])
            for u in range(len(units)):
                if u + LA < len(units):
                    QKEXP(u + LA)
                PV(u)
                h = units[u][0]
                if u + 1 == len(units) or units[u + 1][0] != h:
                    FINH(h)
                    if nxt and h % 2 == 1:
                        tl = h // 2
                        A_tr(b + 1, tl)
                        if tl + 1 < 4:
                            A_norm(b + 1, tl + 1)
            for tl in range(4):
                transpose_tile(ya[:, tl, :], 4, yaT[bb][:, :, tl * 128:(tl + 1) * 128], [("ya", h) for h in range(NH)], ("yaT", bb, tl))
            P.op("sync", lambda e, tok0=tok0: e.dma_start(out=yaT_v[:, :, tok0:tok0 + BT], in_=yaT[bb][:]),
                 reads=[("yaT", bb, tl) for tl in range(4)], writes=[("yaT_d", b)], dma=True)

        for tl in range(4):
            A_norm(0, tl)
            A_tr(0, tl)
        for b in range(NB):
            secC(b)
            secD(b)
        P.barrier()
    if 1 in phases:
        phase1()
    sb.reset(base_mark)

    def phase2():
        Wg = sb.alloc("Wg", [128, 8, 2048], BF16)
        wpa = sb.alloc("wpa", [128, 4, D], BF16)
        wpc = sb.alloc("wpc", [128, 4, D], BF16)
        wo = sb.alloc("wo", [128, 8, D], BF16)
        wrg = sb.alloc("wrg", [128, 8, 36], BF16)
        brg = sb.alloc("brg", [128, 36], F32)
        gffn = sb.alloc("gffn", [128, D], F32)
        nTb = [sb.alloc("nTb%d" % i, [128, 8, BT], BF16) for i in range(2)]
        yaT = [sb.alloc("yaT%d" % i, [128, 4, BT], BF16) for i in range(2)]
        ycT = [sb.alloc("ycT%d" % i, [128, 4, BT], BF16) for i in range(2)]
        xt = [sb.alloc("xt%d" % i, [128, D], F32) for i in range(4)]
        tA = [sb.alloc("tA%d" % i, [128, BT], F32) for i in range(2)]
        tC = [sb.alloc("tC%d" % i, [128, BT], F32) for i in range(2)]
        mA = [sb.alloc("mA%d" % i, [128, BT], F32) for i in range(2)]
        mC = [sb.alloc("mC%d" % i, [128, BT], F32) for i in range(2)]
        mT = [sb.alloc("mT%d" % i, [128, 8, BT], BF16) for i in range(2)]
        ht = [sb.alloc("ht%d" % i, [128, D], F32) for i in range(3)]
        n2 = [sb.alloc("n2%d" % i, [128, 4, D], BF16) for i in range(2)]
        n2T = [sb.alloc("n2T%d" % i, [128, 8, 128], BF16) for i in range(2)]
        lg = sb.alloc("lg", [128, 4, 36], F32)
        gmax = sb.alloc("gmax", [128, 4], F32)
        gmask = sb.alloc("gmask", [128, 4, 4], F32)
        gex = sb.alloc("gex", [128, 4, 4], F32)
        gse = sb.alloc("gse", [128, 4], F32)
        pen = sb.alloc("pen", [128, 4, 4], F32)
        elm = sb.alloc("elm", [128, 4, 32], F32)
        elm2 = sb.alloc("elm2", [128, 4, 32], F32)
        m1 = sb.alloc("m1", [128, 4], F32)
        m2 = sb.alloc("m2", [128, 4], F32)
        mk1 = sb.alloc("mk1", [128, 4, 32], F32)
        mk2 = sb.alloc("mk2", [128, 4, 32], F32)
        Mb = sb.alloc("Mb", [128, 4, 32], BF16)
        dd = sb.alloc("dd", [128, 4], F32)
        ee = sb.alloc("ee", [128, 4], F32)
        rr = sb.alloc("rr", [128, 4], F32)
        wA = sb.alloc("wA", [128, 4], F32)
        wB = sb.alloc("wB", [128, 4], F32)
        pos = sb.alloc("pos", [128, 4, 32], F32)
        okm = sb.alloc("okm", [128, 4, 32], F32)
        slot = sb.alloc("slot", [128, 4, 32], F32)
        tmp = sb.alloc("tmp", [128, 4, 32], F32)
        dsel = sb.alloc("dsel", [128, 4, 2], F32)
        oksel = sb.alloc("oksel", [128, 4, 2], F32)

        assert sb.off["Wg"] == BASE and sb.off["wpa"] == BASE + 32768 and sb.off["wpc"] == BASE + 40960
        if 1 not in phases:
            p2_weight_loads(Wg, wpa, wpc)
        wo_v = wo_d.ap().rearrange("(c p) n -> p c n", p=128)
        for c in range(0, 8, 4):
            P.op("gpsimd", lambda e, c=c: e.dma_start(out=wo[:, c:c + 4, :], in_=wo_v[:, c:c + 4, :]), writes=[("wo", c)], dma=True)
        P.op("gpsimd", lambda e: e.dma_start(out=wrg[:], in_=wrg_d.ap().rearrange("(c p) n -> p c n", p=128)), writes=["wrg"], dma=True)
        ld("sync", brg[:], brg_d.ap().partition_broadcast(128), "brg")
        ld("sync", gffn[:], gffn_d.ap().partition_broadcast(128), "gffn")

        nT_v = nT_d.ap().rearrange("(c p) t -> p c t", p=128)
        yaT_v = yaT_d.ap().rearrange("(c p) t -> p c t", p=128)
        ycT_v = ycT_d.ap().rearrange("(c p) t -> p c t", p=128)
        wokeys = [("wo", 0), ("wo", 4)]
        xctr = [0]
        hctr = [0]

        def loads2(b):
            bb = b % 2
            tok0 = b * BT
            P.op("sync", lambda e, bb=bb, tok0=tok0: e.dma_start(out=nTb[bb][:], in_=nT_v[:, :, tok0:tok0 + BT]), writes=[("nTb", bb)], dma=True)
            P.op("sync", lambda e, bb=bb, tok0=tok0: e.dma_start(out=yaT[bb][:], in_=yaT_v[:, :, tok0:tok0 + BT]), writes=[("yaT", bb)], dma=True)
            P.op("sync", lambda e, bb=bb, tok0=tok0: e.dma_start(out=ycT[bb][:], in_=ycT_v[:, :, tok0:tok0 + BT]), writes=[("ycT", bb)], dma=True)

        def xload(t):
            P.op("sync", lambda e, t=t: e.dma_start(out=xt[t % 4][:], in_=x_d.ap()[t * 128:(t + 1) * 128, :]), writes=[("xt", t % 4)], dma=True)

        loads2(0)
        for t_ in range(3):
            xload(t_)
        def gates2(b):
            bb = b % 2
            tok0 = b * BT
            if b + 1 < NB:
                loads2(b + 1)
            for j in range(8):
                z = j % 2
                for (dst, dkey, col0) in ((A0, "A0", 0), (A1, "A1", 1024)):
                    for c in range(8):
                        P.op("tensor", lambda e, c=c, dst=dst, col0=col0, j=j, bb=bb: e.matmul(
                            dst[:], lhsT=Wg[:, c, col0 + j * 128:col0 + (j + 1) * 128], rhs=nTb[bb][:, c, :], start=(c == 0), stop=(c == 7)),
                            reads=[("Wg", col0 + (j // 2) * 256), ("nTb", bb)], writes=[dkey])
                for (dst, dkey, wsrc, wkey, asrc, akey) in ((A2, "A2", wpa, "wpa", yaT, "yaT"), (B0, ("B0", 0), wpc, "wpc", ycT, "ycT")):
                    for c in range(4):
                        P.op("tensor", lambda e, c=c, dst=dst, wsrc=wsrc, asrc=asrc, j=j, bb=bb: e.matmul(
                            dst[:, 0:512], lhsT=wsrc[:, c, j * 128:(j + 1) * 128], rhs=asrc[bb][:, c, :], start=(c == 0), stop=(c == 3)),
                            reads=[wkey, (akey, bb)], writes=[dkey])
                P.op("scalar", lambda e, z=z, j=j: e.activation(out=tA[z][:], in_=A0[:], func=AF.Tanh, scale=0.5, bias=hbcol[:, 24 + j:25 + j]),
                     reads=["A0", "hbcol"], writes=[("tA", z)])
                P.op("scalar", lambda e, z=z, j=j: e.activation(out=tC[z][:], in_=A1[:], func=AF.Tanh, scale=0.5, bias=hbcol[:, 32 + j:33 + j]),
                     reads=["A1", "hbcol"], writes=[("tC", z)])
                P.op("vector", lambda e, z=z: e.scalar_tensor_tensor(out=mA[z][:], in0=tA[z][:], scalar=1.0, in1=A2[:], op0=ALU.add, op1=ALU.mult),
                     reads=[("tA", z), "A2"], writes=[("mA", z)])
                P.op("vector", lambda e, z=z: e.scalar_tensor_tensor(out=mC[z][:], in0=tC[z][:], scalar=1.0, in1=B0[:, 0:512], op0=ALU.add, op1=ALU.mult),
                     reads=[("tC", z), ("B0", 0)], writes=[("mC", z)])
                P.op("vector", lambda e, z=z, j=j, bb=bb: e.tensor_tensor(out=mT[bb][:, j, :], in0=mA[z][:], in1=mC[z][:], op=ALU.add),
                     reads=[("mA", z), ("mC", z)], writes=[("mT", bb, j)])

        def hsec2(b):
            bb = b % 2
            tok0 = b * BT
            mkeys = [("mT", bb, j) for j in range(8)]
            def hmm(tl):
                t = 4 * b + tl
                xs_ = t % 4
                hs_ = t % 3
                if t + 3 < NT:
                    xload(t + 3)
                HB = [(B1[:, 0:512], ("B1", 0)), (B1[:, 512:1024], ("B1", 1))] if tl % 2 == 0 else [(A0[:], "A0"), (A1[:], "A1")]
                for half in range(2):
                    hacc, hkey = HB[half]
                    for j in range(8):
                        P.op("tensor", lambda e, j=j, half=half, tl=tl, bb=bb, hacc=hacc: e.matmul(
                            hacc, lhsT=mT[bb][:, j, tl * 128:(tl + 1) * 128], rhs=wo[:, j, half * 512:(half + 1) * 512],
                            start=(j == 0), stop=(j == 7)),
                            reads=mkeys + wokeys, writes=[hkey])
                    P.op("vector", lambda e, half=half, xs_=xs_, hs_=hs_, hacc=hacc: e.scalar_tensor_tensor(
                        out=ht[hs_][:, half * 512:(half + 1) * 512], in0=hacc, scalar=0.5,
                        in1=xt[xs_][:, half * 512:(half + 1) * 512], op0=ALU.mult, op1=ALU.add),
                        reads=[hkey, ("xt", xs_)] + ([("ht", hs_)] if half == 1 else []), writes=[("ht", hs_)])
                P.op("sync", lambda e, t=t, hs_=hs_: e.dma_start(out=h_d.ap()[t * 128:(t + 1) * 128, :], in_=ht[hs_][:]),
                     reads=[("ht", hs_)], writes=[("h_d", t)], dma=True)

            def hnorm(tl):
                t = 4 * b + tl
                hs_ = t % 3
                rmsnorm_tile(ht[hs_][:], gffn[:], n2[bb][:, tl, :], ("ht", hs_), "gffn", ("n2", bb, tl))

            def htr(tl):
                t = 4 * b + tl
                z2 = t % 2
                transpose_tile(n2[bb][:, tl, :], 8, n2T[z2][:], ("n2", bb, tl), ("n2T", z2))
                for c in range(8):
                    P.op("tensor", lambda e, c=c, tl=tl, z2=z2: e.matmul(
                        B0[:, 512 + tl * 36:512 + (tl + 1) * 36], lhsT=n2T[z2][:, c, :], rhs=wrg[:, c, :], start=(c == 0), stop=(c == 7)),
                        reads=[("n2T", z2), "wrg"], writes=[("lgp", tl)])

            hmm(0)
            hnorm(0)
            for tl in range(4):
                if tl + 1 < 4:
                    hmm(tl + 1)
                htr(tl)
                if tl + 1 < 4:
                    hnorm(tl + 1)

        def rout2a(b):
            bb = b % 2
            tok0 = b * BT
            lgp = B0[:, 512:512 + 144].rearrange("p (t n) -> p t n", t=4)
            R = []

            def V(fn, reads, writes):
                P.op("vector", fn, reads=reads, writes=writes)

            V(lambda e: e.tensor_tensor(out=lg[:], in0=lgp, in1=bc_mid(brg[:], 4), op=ALU.add),
              [("lgp", tl) for tl in range(4)] + ["brg"], ["lg"])
            V(lambda e: e.tensor_reduce(out=gmax[:], in_=lg[:, :, 0:4], axis=AX.X, op=ALU.max), ["lg"], ["gmax"])
            V(lambda e: e.tensor_tensor(out=gmask[:], in0=lg[:, :, 0:4], in1=bc_last(gmax[:], 4), op=ALU.is_equal), ["lg", "gmax"], ["gmask"])
            V(lambda e: e.tensor_tensor(out=gex[:], in0=lg[:, :, 0:4], in1=bc_last(gmax[:], 4), op=ALU.subtract), ["lg", "gmax"], ["gex"])
            P.op("scalar", lambda e: e.activation(out=gex[:], in_=gex[:], func=AF.Exp), reads=["gex"], writes=["gex"])
            V(lambda e: e.tensor_reduce(out=gse[:], in_=gex[:], axis=AX.X, op=ALU.add), ["gex"], ["gse"])
            V(lambda e: e.reciprocal(out=gse[:], in_=gse[:]), ["gse"], ["gse"])
            V(lambda e: e.tensor_scalar(out=pen[:], in0=gmask[:], scalar1=1.0, scalar2=1e30, op0=ALU.subtract, op1=ALU.mult), ["gmask"], ["pen"])
            V(lambda e: e.tensor_tensor(out=elm[:].rearrange("p t (g k) -> p t g k", g=4),
                                        in0=lg[:, :, 4:36].rearrange("p t (g k) -> p t g k", g=4),
                                        in1=bc_last(pen[:], 8), op=ALU.add), ["lg", "pen"], ["elm"])
            V(lambda e: e.tensor_reduce(out=m1[:], in_=elm[:], axis=AX.X, op=ALU.max), ["elm"], ["m1"])
            V(lambda e: e.tensor_tensor(out=mk1[:], in0=elm[:], in1=bc_last(m1[:], 32), op=ALU.is_equal), ["elm", "m1"], ["mk1"])
            V(lambda e: e.scalar_tensor_tensor(out=elm2[:], in0=mk1[:], scalar=-1e30, in1=elm[:], op0=ALU.mult, op1=ALU.add), ["mk1", "elm"], ["elm2"])
            V(lambda e: e.tensor_reduce(out=m2[:], in_=elm2[:], axis=AX.X, op=ALU.max), ["elm2"], ["m2"])
            V(lambda e: e.tensor_tensor(out=mk2[:], in0=elm2[:], in1=bc_last(m2[:], 32), op=ALU.is_equal), ["elm2", "m2"], ["mk2"])
            V(lambda e: e.tensor_tensor(out=dd[:], in0=m2[:], in1=m1[:], op=ALU.subtract), ["m1", "m2"], ["dd"])
            P.op("scalar", lambda e: e.activation(out=ee[:], in_=dd[:], func=AF.Exp), reads=["dd"], writes=["ee"])
            V(lambda e: e.tensor_scalar(out=rr[:], in0=ee[:], scalar1=1.0, scalar2=None, op0=ALU.add), ["ee"], ["rr"])
            V(lambda e: e.reciprocal(out=rr[:], in_=rr[:]), ["rr"], ["rr"])
            V(lambda e: e.tensor_tensor(out=wA[:], in0=gse[:], in1=rr[:], op=ALU.mult), ["gse", "rr"], ["wA"])
            V(lambda e: e.tensor_tensor(out=wB[:], in0=wA[:], in1=ee[:], op=ALU.mult), ["wA", "ee"], ["wB"])
            V(lambda e: e.tensor_tensor(out=Mb[:], in0=mk1[:], in1=mk2[:], op=ALU.add), ["mk1", "mk2"], ["Mb"])

        def rout2b(b):
            bb = b % 2
            tok0 = b * BT

            def V(fn, reads, writes):
                P.op("vector", fn, reads=reads, writes=writes)

            for tl in range(4):
                P.op("tensor", lambda e, tl=tl: e.matmul(A0[:, tl * 32:(tl + 1) * 32], lhsT=utri[:], rhs=Mb[:, tl, :], start=True, stop=(tl == 0)),
                     reads=["utri", "Mb"], writes=["A0"])
                for t2 in range(tl):
                    P.op("tensor", lambda e, tl=tl, t2=t2: e.matmul(A0[:, tl * 32:(tl + 1) * 32], lhsT=ones[:], rhs=Mb[:, t2, :], start=False, stop=(t2 == tl - 1)),
                         reads=["ones", "Mb"], writes=["A0"])
            for tl in range(4):
                P.op("tensor", lambda e, tl=tl: e.matmul(A1[:, 0:32], lhsT=ones[:], rhs=Mb[:, tl, :], start=(tl == 0), stop=(tl == 3)),
                     reads=["ones", "Mb"], writes=["A1"])
            V(lambda e: e.tensor_tensor(out=pos[:], in0=A0[:, 0:128].rearrange("p (t n) -> p t n", t=4), in1=bc_mid(cnt[:], 4), op=ALU.add),
              ["A0", "cnt"], ["pos"])
            V(lambda e: e.tensor_tensor(out=cnt[:], in0=cnt[:], in1=A1[:, 0:32], op=ALU.add), ["A1", "cnt", "pos"], ["cnt"])
            V(lambda e: e.tensor_scalar(out=okm[:], in0=pos[:], scalar1=float(CAP), scalar2=None, op0=ALU.is_lt), ["pos"], ["okm"])
            V(lambda e: e.tensor_tensor(out=slot[:], in0=pos[:], in1=bc_mid(ecap[:], 4), op=ALU.add), ["pos", "ecap"], ["slot"])
            V(lambda e: e.tensor_scalar(out=tmp[:], in0=okm[:], scalar1=-1.0e6, scalar2=1.0e6, op0=ALU.mult, op1=ALU.add), ["okm"], ["tmp"])
            V(lambda e: e.tensor_tensor(out=slot[:], in0=slot[:], in1=tmp[:], op=ALU.add), ["slot", "tmp"], ["slot"])
            V(lambda e: e.tensor_scalar(out=slot[:], in0=slot[:], scalar1=float(NSLOT), scalar2=None, op0=ALU.min), ["slot"], ["slot"])
            for k, mk in ((0, mk1), (1, mk2)):
                V(lambda e, mk=mk: e.tensor_tensor(out=tmp[:], in0=mk[:], in1=slot[:], op=ALU.mult), ["mk1", "mk2", "slot"], ["tmp"])
                V(lambda e, k=k: e.tensor_reduce(out=dsel[:, :, k], in_=tmp[:], axis=AX.X, op=ALU.add), ["tmp"], [("dsel", k)])
                V(lambda e, mk=mk: e.tensor_tensor(out=tmp[:], in0=mk[:], in1=okm[:], op=ALU.mult), ["mk1", "mk2", "okm", ("dsel", k)], ["tmp"])
                V(lambda e, k=k: e.tensor_reduce(out=oksel[:, :, k], in_=tmp[:], axis=AX.X, op=ALU.add), ["tmp"], [("oksel", k)])
            tb = 4 * b
            V(lambda e, tb=tb: e.tensor_copy(out=dtab[:, tb:tb + 4, :], in_=dsel[:]), [("dsel", 0), ("dsel", 1)], [("dtab", b)])
            V(lambda e, tb=tb: e.tensor_tensor(out=wtab[:, tb:tb + 4, 0], in0=wA[:], in1=oksel[:, :, 0], op=ALU.mult), ["wA", ("oksel", 0)], [("wtab", b, 0)])
            V(lambda e, tb=tb: e.tensor_tensor(out=wtab[:, tb:tb + 4, 1], in0=wB[:], in1=oksel[:, :, 1], op=ALU.mult), ["wB", ("oksel", 1)], [("wtab", b, 1)])
            for tl in range(4):
                t = 4 * b + tl
                for k in range(2):
                    P.op("gpsimd", lambda e, t=t, k=k, tl=tl, bb=bb: e.indirect_dma_start(
                        out=xs_d[:, :], out_offset=bass.IndirectOffsetOnAxis(ap=dtab[:, t, k:k + 1], axis=0),
                        in_=n2[bb][:, tl, :], in_offset=None),
                        reads=[("n2", bb, tl), ("dtab", b)], writes=[("xs_d", t, k)], dma=True)

        gates2(0)
        for b in range(NB):
            hsec2(b)
            if b == NB - 1 and 3 in phases:
                for i in range(2):
                    expert_weight_loads(i, pre3[i]["w1"], pre3[i]["w3"], pre3[i]["w2"], [("w1p", i), ("w3p", i), ("w2p", i)],
                                        extra_writes=WGKEYS + ["wpa", "wpc"])
            rout2a(b)
            if b + 1 < NB:
                gates2(b + 1)
            rout2b(b)
        if debug:
            P.op("sync", lambda e: e.dma_start(out=dtab_o.ap(), in_=dtab[:].rearrange("p t k -> p (t k)")),
                 reads=[("dtab", b) for b in range(NB)], dma=True, is_out=True)
            P.op("sync", lambda e: e.dma_start(out=wtab_o.ap(), in_=wtab[:].rearrange("p t k -> p (t k)")),
                 reads=[("wtab", b, k) for b in range(NB) for k in range(2)], dma=True, is_out=True)
        P.barrier()
    if 2 in phases:
        phase2()
    sb.reset(base_mark)

    TOP = SBAlloc.HI - 20 * 1024
    p4w = {"wpg": nc.alloc_sbuf_tensor_at("wpg_top", [128, 8, D], BF16, offset=TOP),
           "wpp": nc.alloc_sbuf_tensor_at("wpp_top", [128, 2, D], BF16, offset=TOP + 16 * 1024)}

    def p4_weight_loads():
        wpg_v = wpg_d.ap().rearrange("(c p) n -> p c n", p=128)
        for c in range(0, 8, 4):
            P.op("gpsimd", lambda e, c=c: e.dma_start(out=p4w["wpg"][:, c:c + 4, :], in_=wpg_v[:, c:c + 4, :]), writes=[("wpg", c)], dma=True)
        P.op("gpsimd", lambda e: e.dma_start(out=p4w["wpp"][:], in_=wpp_d.ap().rearrange("(c p) n -> p c n", p=128)), writes=["wpp"], dma=True)

    def phase3():
        NWB = 3
        w1b, w3b, w2b = [], [], []
        for i in range(NWB):
            w1b.append(sb.alloc("w1b%d" % i, [128, 8, 512], BF16))
            w3b.append(sb.alloc("w3b%d" % i, [128, 8, 512], BF16))
            w2b.append(sb.alloc("w2b%d" % i, [128, 4, D], BF16))
        assert sb.off["w1b0"] == BASE and sb.off["w2b1"] == BASE + 24576 + 16384
        preloaded = 2 if 2 in phases else 0
        xr = [sb.alloc("xr%d" % i, [128, 3, D], BF16) for i in range(3)]
        xsT = [sb.alloc("xsT%d" % i, [128, 8, CAP], BF16) for i in range(2)]
        s1 = [sb.alloc("s1%d" % i, [128, CAP], F32) for i in range(2)]
        hdn = [sb.alloc("hdn%d" % i, [128, 4, CAP], BF16) for i in range(2)]
        yb = [sb.alloc("yb%d" % i, [128, D], BF16) for i in range(3)]
        HACC = [(A0, "A0", A1, "A1"), (A2, "A2", B0, ("B0", 0))]
        yctr = [0]
        P.op("gpsimd", lambda e: e.memset(yb[0][:], 0.0), writes=[("yb", 0, 0), ("yb", 0, 1)])
        P.op("sync", lambda e: e.dma_start(out=ys_d.ap()[NSLOT:NSLOT + 128, :], in_=yb[0][:]),
             reads=[("yb", 0, 0), ("yb", 0, 1)], writes=["ys_trash"], dma=True)
        def wload(ex):
            wb_ = ex % NWB
            if ex < preloaded:
                return
            expert_weight_loads(ex, w1b[wb_], w3b[wb_], w2b[wb_], [("w1b", wb_), ("w3b", wb_), ("w2b", wb_)])

        def xsload(ex):
            e3 = ex % 3
            P.op("sync", lambda e, ex=ex, e3=e3: e.dma_start(
                out=xr[e3][:], in_=xs_d.ap()[ex * CAP:(ex + 1) * CAP, :].rearrange("(r p) d -> p r d", p=128)),
                writes=[("xr", e3)], dma=True)

        def xsT_group(ex, r):
            eb = ex % 2
            e3 = ex % 3
            transpose_tile(xr[e3][:, r, :], 8, xsT[eb][:, :, r * 128:(r + 1) * 128], ("xr", e3), ("xsT", eb, r),
                           evac=("scalar" if r % 2 == 0 else "vector"), interleave=8)

        xsload(0)
        wload(0)
        xsload(1)
        wload(1)
        for r in range(3):
            xsT_group(0, r)
        for ex in range(NE):
            eb = ex % 2
            wb_ = ex % NWB
            if ex + 2 < NE:
                wload(ex + 2)
                xsload(ex + 2)
            if ex == 2:
                p4_weight_loads()
            xk = [("xsT", eb, r) for r in range(3)]
            for f in range(4):
                a1, k1, a3, k3 = HACC[f % 2]
                z = f % 2
                for (dst, dkey, wsrc, wkey) in ((a1, k1, w1b, "w1b"), (a3, k3, w3b, "w3b")):
                    for c in range(8):
                        P.op("tensor", lambda e, c=c, dst=dst, wsrc=wsrc, f=f, eb=eb, wb_=wb_: e.matmul(
                            dst[:, 0:CAP], lhsT=wsrc[wb_][:, c, f * 128:(f + 1) * 128], rhs=xsT[eb][:, c, :], start=(c == 0), stop=(c == 7)),
                            reads=[(wkey, wb_)] + xk, writes=[dkey])
                P.op("scalar", lambda e, a1=a1, z=z: e.activation(out=s1[z][:], in_=a1[:, 0:CAP], func=AF.Silu), reads=[k1], writes=[("s1", z)])
                P.op("vector", lambda e, a3=a3, z=z, f=f, eb=eb: e.tensor_tensor(out=hdn[eb][:, f, :], in0=s1[z][:], in1=a3[:, 0:CAP], op=ALU.mult),
                     reads=[("s1", z), k3], writes=[("hdn", eb, f)])
            hk_ = [("hdn", eb, f) for f in range(4)]
            for r in range(3):
                if ex + 1 < NE:
                    xsT_group(ex + 1, r)
                ys_ = yctr[0] % 3
                yctr[0] += 1
                for half in range(2):
                    for f in range(4):
                        P.op("tensor", lambda e, f=f, half=half, r=r, eb=eb, wb_=wb_: e.matmul(
                            B1[:, half * 512:(half + 1) * 512], lhsT=hdn[eb][:, f, r * 128:(r + 1) * 128], rhs=w2b[wb_][:, f, half * 512:(half + 1) * 512],
                            start=(f == 0), stop=(f == 3)),
                            reads=hk_ + [("w2b", wb_)], writes=[("B1", half)])
                    if half == 0:
                        P.op("scalar", lambda e, ys_=ys_: e.activation(out=yb[ys_][:, 0:512], in_=B1[:, 0:512], func=AF.Copy),
                             reads=[("B1", 0)], writes=[("yb", ys_, 0)])
                    else:
                        P.op("vector", lambda e, ys_=ys_: e.tensor_copy(out=yb[ys_][:, 512:1024], in_=B1[:, 512:1024]),
                             reads=[("B1", 1)], writes=[("yb", ys_, 1)])
                row0 = ex * CAP + r * 128
                P.op("sync", lambda e, row0=row0, ys_=ys_: e.dma_start(out=ys_d.ap()[row0:row0 + 128, :], in_=yb[ys_][:]),
                     reads=[("yb", ys_, 0), ("yb", ys_, 1)], writes=[("ys_d", ex, r)], dma=True)
        P.barrier()
    if 3 in phases:
        phase3()
    sb.reset(base_mark)

    def phase4():
        wpg, wpp = p4w["wpg"], p4w["wpp"]
        gple = sb.alloc("gple", [128, D], F32)
        bpg = sb.alloc("bpg", [128, D], F32)
        hb = [sb.alloc("hb%d" % i, [128, D], F32) for i in range(3)]
        y1 = [sb.alloc("y1%d" % i, [128, D], BF16) for i in range(3)]
        y2 = [sb.alloc("y2%d" % i, [128, D], BF16) for i in range(3)]
        pin = [sb.alloc("pin%d" % i, [128, 256], F32) for i in range(3)]
        pbf = [sb.alloc("pbf%d" % i, [128, 256], BF16) for i in range(2)]
        ppT = [sb.alloc("ppT%d" % i, [128, 2, 128], BF16) for i in range(2)]
        n3 = [sb.alloc("n3%d" % i, [128, D], BF16) for i in range(2)]
        n3T = [sb.alloc("n3T%d" % i, [128, 8, 128], BF16) for i in range(2)]
        gz = [sb.alloc("gz%d" % i, [128, D], F32) for i in range(2)]
        ob = [sb.alloc("ob%d" % i, [128, D], F32) for i in range(2)]
        ld("sync", gple[:], gple_d.ap().partition_broadcast(128), "gple")
        ld("sync", bpg[:], bpg_d.ap().partition_broadcast(128), "bpg")
        def loads4(t):
            h3 = t % 3
            P.op("sync", lambda e, t=t, h3=h3: e.dma_start(out=hb[h3][:], in_=h_d.ap()[t * 128:(t + 1) * 128, :]), writes=[("hb", h3)], dma=True)
            P.op("sync", lambda e, t=t, h3=h3: e.dma_start(out=pin[h3][:], in_=p_d.ap()[t * 128:(t + 1) * 128, :]), writes=[("pin", h3)], dma=True)
            for (yy, ykey, k) in ((y1, "y1", 0), (y2, "y2", 1)):
                P.op("gpsimd", lambda e, yy=yy, k=k, t=t, h3=h3: e.indirect_dma_start(
                    out=yy[h3][:, :], out_offset=None, in_=ys_d[:, :], in_offset=bass.IndirectOffsetOnAxis(ap=dtab[:, t, k:k + 1], axis=0)), reads=["dtab_all"], writes=[(ykey, h3)], dma=True)

        def S1a(t):
            h3 = t % 3
            z = t % 2
            P.op("vector", lambda e, h3=h3, t=t: e.scalar_tensor_tensor(out=hb[h3][:], in0=y1[h3][:], scalar=wtab[:, t, 0:1], in1=hb[h3][:],
                                                                       op0=ALU.mult, op1=ALU.add), reads=[("y1", h3), ("hb", h3)], writes=[("hb", h3)])
            P.op("vector", lambda e, h3=h3, t=t: e.scalar_tensor_tensor(out=hb[h3][:], in0=y2[h3][:], scalar=wtab[:, t, 1:2], in1=hb[h3][:],
                                                                       op0=ALU.mult, op1=ALU.add), reads=[("y2", h3), ("hb", h3)], writes=[("hb", h3)])
            rmsnorm_tile(hb[h3][:], gple[:], n3[z][:], ("hb", h3), "gple", ("n3", z))
            P.op("scalar", lambda e, z=z, h3=h3: e.activation(out=pbf[z][:], in_=pin[h3][:], func=AF.Copy), reads=[("pin", h3)], writes=[("pbf", z)])

        def S1b(t):
            z = t % 2
            transpose_tile(n3[z], 8, n3T[z][:], ("n3", z), ("n3T", z))
            transpose_tile(pbf[z], 2, ppT[z][:], ("pbf", z), ("ppT", z))

        GACC = [((A0, "A0"), (A2, "A2")), ((A1, "A1"), (B0, ("B0", 0)))]

        def S2mm(t, half):
            z = t % 2
            (ga, gk), (pa_, pk) = GACC[half]
            for c in range(8):
                P.op("tensor", lambda e, c=c, half=half, ga=ga, z=z: e.matmul(
                    ga[:], lhsT=n3T[z][:, c, :], rhs=wpg[:, c, half * 512:(half + 1) * 512], start=(c == 0), stop=(c == 7)),
                    reads=[("n3T", z), ("wpg", 0), ("wpg", 4)], writes=[gk])
            for c in range(2):
                P.op("tensor", lambda e, c=c, half=half, pa_=pa_, z=z: e.matmul(
                    pa_[:, 0:512], lhsT=ppT[z][:, c, :], rhs=wpp[:, c, half * 512:(half + 1) * 512], start=(c == 0), stop=(c == 1)),
                    reads=[("ppT", z), "wpp"], writes=[pk])

        def S2tail(t):
            z = t % 2
            h3 = t % 3
            hsl = [slice(0, 512), slice(512, 1024)]
            for half in range(2):
                (ga, gk), (pa_, pk) = GACC[half]
                hs = hsl[half]
                P.op("vector", lambda e, ga=ga, z=z, hs=hs: e.tensor_tensor(out=gz[z][:, hs], in0=ga[:], in1=bpg[:, hs], op=ALU.add),
                     reads=[gk, "bpg"], writes=[("gz", z, half)])
                P.op("scalar", lambda e, z=z, hs=hs: e.activation(out=gz[z][:, hs], in_=gz[z][:, hs], func=AF.Tanh, scale=0.5),
                     reads=[("gz", z, half)], writes=[("gz", z, half)])
            for half in range(2):
                (ga, gk), (pa_, pk) = GACC[half]
                hs = hsl[half]
                P.op("vector", lambda e, pa_=pa_, z=z, hs=hs: e.scalar_tensor_tensor(out=gz[z][:, hs], in0=gz[z][:, hs], scalar=1.0, in1=pa_[:, 0:512],
                                                                                    op0=ALU.add, op1=ALU.mult), reads=[("gz", z, half), pk], writes=[("gz", z, half)])
                P.op("vector", lambda e, z=z, hs=hs, h3=h3: e.scalar_tensor_tensor(out=ob[z][:, hs], in0=gz[z][:, hs], scalar=0.5, in1=hb[h3][:, hs],
                                                                                  op0=ALU.mult, op1=ALU.add), reads=[("gz", z, half), ("hb", h3)], writes=[("ob", z, half)])

        loads4(0)
        loads4(1)
        S1a(0)
        S1b(0)
        for t in range(NT):
            z = t % 2
            if t + 2 < NT:
                loads4(t + 2)
            if t + 1 < NT:
                S1a(t + 1)
            S2mm(t, 0)
            S2mm(t, 1)
            S2tail(t)
            if t + 1 < NT:
                S1b(t + 1)
            P.op("sync", lambda e, t=t, z=z: e.dma_start(out=out_d.ap()[t * 128:(t + 1) * 128, :], in_=ob[z][:]),
                 reads=[("ob", z, 0), ("ob", z, 1)], dma=True, is_out=True)
    if 4 in phases:
        phase4()
    P.emit()
    return nc, P


def _sel_tables():
    f = np.arange(128)[:, None, None]
    j = np.arange(8)[None, :, None]
    m = np.arange(128)[None, None, :]
    hit = ((m // 8) == (2 * j + f // 64)).astype(np.float32)
    selA = (hit / 64.0).reshape(128, 8 * 128).astype(ml_dtypes.bfloat16)
    selB = (hit.transpose(2, 1, 0) / 8.0).reshape(128, 8 * 128).astype(ml_dtypes.bfloat16)
    return np.ascontiguousarray(selA), np.ascontiguousarray(selB)


def _host_layout(inp):
    f = lambda a: np.ascontiguousarray(np.asarray(a, dtype=np.float32))
    bf = ml_dtypes.bfloat16
    b_in = f(inp["b_in"])[0]
    rel = f(inp["rel_bias"])[0]
    jj = np.arange(5)[::-1][:, None, None]
    kk = np.arange(128)[None, :, None]
    qq = np.arange(128)[None, None, :]
    dist = qq - kk + 128 * (4 - jj)
    idx = np.clip(dist, -63, 256) + 63
    cdiff = (qq // 64) - (kk // 64) + 2 * (4 - jj)
    mask = ((cdiff >= 0) & (cdiff <= 8)).astype(np.float32)
    rbT = rel[:, idx]
    rbT = np.ascontiguousarray(rbT.transpose(2, 0, 1, 3)).reshape(128, NH * 5 * 128)
    maskT = np.ascontiguousarray(mask.transpose(1, 0, 2)).reshape(128, 5 * 128)
    cwv = f(inp["conv_w"])[0]
    cw = np.ascontiguousarray(cwv.reshape(3, 4, 128).transpose(2, 1, 0)).reshape(128, 12)
    cb = np.ascontiguousarray(f(inp["conv_b"])[0].reshape(4, 128).T)
    gq = f(inp["g_q"])[0]
    gk = f(inp["g_k"])[0]
    shared = {
        "g_mix": f(inp["g_mix"]),
        "w_in": f(inp["w_in"])[0],
        "bcol": np.ascontiguousarray(b_in.reshape(40, 128).T),
        "gqk": np.ascontiguousarray(np.stack([np.tile(gq, 2), np.tile(gk, 2)], axis=1)),
        "bv": np.ascontiguousarray(b_in[1024:1536].reshape(1, 512)),
        "rbT": rbT, "maskT": maskT, "cw": cw, "cb": cb,
        "w_pa": f(inp["w_pa"])[0], "w_pc": f(inp["w_pc"])[0], "w_o": f(inp["w_o"])[0],
        "g_ffn": f(inp["g_ffn"]),
        "w_rg": np.ascontiguousarray(np.concatenate([f(inp["w_group"])[0], f(inp["w_router"])[0]], axis=1)),
        "b_rg": np.ascontiguousarray(np.concatenate([f(inp["b_group"])[0], f(inp["b_router"])[0]])[None, :]),
        "w1": f(inp["w1"])[0], "w3": f(inp["w3"])[0], "w2": f(inp["w2"])[0],
        "g_ple": f(inp["g_ple"]), "w_pg": f(inp["w_ple_gate"])[0], "b_pg": f(inp["b_ple_gate"]),
        "w_pp": f(inp["w_ple_proj"])[0],
        "ident": np.eye(128, dtype=np.float32).astype(bf),
        "utri": np.triu(np.ones((128, 128), np.float32), 1).astype(bf),
        "ones": np.ones((128, 128), np.float32).astype(bf),
        "bdiag": (np.kron(np.eye(2, dtype=np.float32), np.ones((64, 64), np.float32)) / 64.0).astype(bf),
        "ecap": np.ascontiguousarray(np.broadcast_to((np.arange(NE, dtype=np.float32) * CAP)[None, :], (128, NE))),
        "selA": _sel_tables()[0], "selB": _sel_tables()[1],
    }
    x = f(inp["x"])
    p = f(inp["p"])[0]
    maps = []
    for c in range(NCORES):
        m = dict(shared)
        m["x"] = x[c]
        m["p"] = p[c]
        maps.append(m)
    return maps


_CACHE = {}


def kernel(**inputs):
    if "nc" not in _CACHE:
        _CACHE["nc"] = build(debug=False)[0]
    nc = _CACHE["nc"]
    maps = _host_layout(inputs)
    res = run_bass_kernel_spmd(nc, maps, core_ids=list(range(NCORES)))
    out = np.stack([np.asarray(res.results[c]["out"], dtype=np.float32) for c in range(NCORES)], axis=0)
    return out
```

```python
import numpy as np
import ml_dtypes
import concourse.bass as bass
import concourse.mybir as mybir
from concourse.bass_utils import run_bass_kernel_spmd

F32 = mybir.dt.float32
BF16 = mybir.dt.bfloat16
I32 = mybir.dt.int32
ALU = mybir.AluOpType
AF = mybir.ActivationFunctionType
AX = mybir.AxisListType

NCORES = 8
S = 4096
D = 1024
NT = S // 128
BT = 512
NB = S // BT
NH = 8
DH = 64
NE = 32
CAP = 384
NSLOT = NE * CAP
KR = 12
EPS = 1e-6

ENGS = ("sync", "scalar", "vector", "gpsimd", "tensor")
NDMASEM = 24
SAME_ENG_WINDOW = 10 ** 9


class Op:
    __slots__ = ("idx", "eng", "fn", "reads", "writes", "dma", "deps", "sig",
                 "sem", "val", "clock", "epos", "barrier")


class Prog:
    def __init__(self, nc):
        self.nc = nc
        self.ops = []
        self.last_w = {}
        self.readers = {}
        self.out_ops = []
        self.last_barrier = None
        self.since_barrier = []

    def op(self, eng, fn, reads=(), writes=(), dma=False, is_out=False):
        o = Op()
        o.idx = len(self.ops)
        o.eng = eng
        o.fn = fn
        o.dma = dma
        o.barrier = False
        o.reads = tuple(reads)
        o.writes = tuple(writes)
        deps = set()
        for k in o.reads:
            w = self.last_w.get(k)
            if w is not None:
                deps.add(w)
        for k in o.writes:
            w = self.last_w.get(k)
            if w is not None:
                deps.add(w)
            for r in self.readers.get(k, ()):
                deps.add(r)
        for k in o.writes:
            self.last_w[k] = o.idx
            self.readers[k] = []
        for k in o.reads:
            if k not in o.writes:
                self.readers.setdefault(k, []).append(o.idx)
        if self.last_barrier is not None:
            deps.add(self.last_barrier)
        deps.discard(o.idx)
        o.deps = sorted(deps)
        o.sig = False
        self.ops.append(o)
        self.since_barrier.append(o.idx)
        if is_out:
            self.out_ops.append(o.idx)
        return o.idx

    def barrier(self):
        o = Op()
        o.idx = len(self.ops)
        o.eng = "sync"
        o.fn = "BARRIER"
        o.dma = False
        o.barrier = True
        o.reads = ()
        o.writes = ()
        last = {}
        deps = []
        for i in self.since_barrier:
            p = self.ops[i]
            if p.dma:
                deps.append(i)
            else:
                last[p.eng] = i
        deps.extend(last.values())
        if self.last_barrier is not None:
            deps.append(self.last_barrier)
        o.deps = sorted(set(deps))
        o.sig = True
        self.ops.append(o)
        self.last_barrier = o.idx
        self.since_barrier = []
        self.last_w = {}
        self.readers = {}

    def emit(self):
        nc = self.nc
        ops = self.ops
        epos = {e: 0 for e in ENGS}
        for o in ops:
            o.epos = epos[o.eng]
            epos[o.eng] += 1
        fin = Op()
        fin.idx = len(ops)
        fin.eng = "sync"
        fin.fn = None
        fin.dma = False
        fin.barrier = False
        fin.reads = ()
        fin.writes = ()
        fin.deps = list(self.out_ops)
        fin.sig = False
        fin.epos = epos["sync"]
        ops = ops + [fin]
        for o in ops:
            nd = []
            for d in o.deps:
                do = ops[d]
                if do.eng == o.eng and not do.dma and not o.barrier:
                    if o.eng == "tensor" and not o.dma:
                        continue
                    if o.dma:
                        pass
                    elif o.epos - do.epos > SAME_ENG_WINDOW:
                        continue
                nd.append(d)
            o.deps = nd
            for d in nd:
                ops[d].sig = True
        sems = {}
        dma_engs = set(o.eng for o in ops if o.dma)
        for e in ENGS:
            sems[("c", e)] = nc.alloc_semaphore("c_" + e)
            if e in dma_engs:
                for i in range(NDMASEM):
                    sems[("d", e, i)] = nc.alloc_semaphore("d_%s_%d" % (e, i))
        ccount = {e: 0 for e in ENGS}
        dcount = {e: 0 for e in ENGS}
        dma_prev = {}
        for o in ops:
            if o.dma:
                k = dcount[o.eng]
                dcount[o.eng] += 1
                slot = k % NDMASEM
                o.sem = ("d", o.eng, slot)
                o.val = 16 * (k // NDMASEM + 1)
                prev = dma_prev.get((o.eng, slot))
                if prev is not None and prev not in o.deps:
                    o.deps.append(prev)
                dma_prev[(o.eng, slot)] = o.idx
            elif o.sig:
                ccount[o.eng] += 1
                o.sem = ("c", o.eng)
                o.val = ccount[o.eng]
            else:
                o.sem = None
                o.val = 0
        known = {e: {} for e in ENGS}
        streams = {e: [] for e in ENGS}
        for o in ops:
            kn = known[o.eng]
            wm = {}
            for d in sorted(o.deps, reverse=True):
                do = ops[d]
                if kn.get(do.sem, 0) >= do.val:
                    continue
                if wm.get(do.sem, 0) < do.val:
                    wm[do.sem] = do.val
                for s, v in do.clock.items():
                    if kn.get(s, 0) < v:
                        kn[s] = v
            o.clock = dict(kn)
            if o.sem is not None:
                o.clock[o.sem] = o.val
            streams[o.eng].append((o, list(wm.items())))
        self.n_waits = sum(len(w) for st in streams.values() for _, w in st)
        self.counts = (dict(ccount), dict(dcount))

        def run_stream(eng_name):
            def body(eng):
                for o, waits in streams[eng_name]:
                    for s, v in waits:
                        eng.wait_ge(sems[s], v)
                    if o.fn is None:
                        continue
                    if o.barrier:
                        eng.sem_inc(sems[o.sem], 1)
                        continue
                    ins = o.fn(eng)
                    if o.sem is not None:
                        ins.then_inc(sems[o.sem], 16 if o.dma else 1)
            return body

        with nc.Block() as block:
            for e in ENGS:
                if streams[e]:
                    getattr(block, e)(run_stream(e))


class SBAlloc:
    LO = 16512
    HI = 229344

    def __init__(self, nc):
        self.nc = nc
        self.cur = self.LO
        self.n = 0

    def alloc(self, name, shape, dt):
        esz = {F32: 4, BF16: 2, I32: 4}[dt]
        nbytes = esz
        for s in shape[1:]:
            nbytes *= s
        off = (self.cur + 31) // 32 * 32
        assert off + nbytes <= self.HI, "SBUF overflow at %s: need %d have %d" % (name, nbytes, self.HI - off)
        self.n += 1
        t = self.nc.alloc_sbuf_tensor_at("%s_%d" % (name, self.n), list(shape), dt, offset=off)
        self.cur = off + nbytes
        self.off = getattr(self, "off", {})
        self.off[name] = off
        return t

    def alloc_alias(self, name, shape, dt, of):
        self.n += 1
        return self.nc.alloc_sbuf_tensor_at("%s_%d" % (name, self.n), list(shape), dt, offset=self.off[of])

    def mark(self):
        return self.cur

    def reset(self, m):
        self.cur = m


def bc_last(ap, n):
    shp = list(ap.shape)
    return ap.unsqueeze(len(shp)).broadcast_to(shp + [n])


def bc_mid(ap, n):
    shp = list(ap.shape)
    return ap.unsqueeze(1).broadcast_to([shp[0], n] + shp[1:])


def build(debug=False, phases=(1, 2, 3, 4)):
    nc = bass.Bass("TRN2", target_bir_lowering=False)
    P = Prog(nc)
    sb = SBAlloc(nc)

    def din(name, shape, dt=F32):
        return nc.dram_tensor(name, list(shape), dt, kind="ExternalInput")

    def dscr(name, shape, dt):
        return nc.dram_tensor(name, list(shape), dt, kind="ExternalOutput" if debug else "Internal")

    x_d = din("x", [S, D])
    p_d = din("p", [S, 256])
    gmix_d = din("g_mix", [1, D])
    win_d = din("w_in", [D, 5120])
    bcol_d = din("bcol", [128, 40])
    gqk_d = din("gqk", [128, 2])
    bv_d = din("bv", [1, 512])
    rbT_d = din("rbT", [128, NH * 5 * 128])
    maskT_d = din("maskT", [128, 5 * 128])
    cw_d = din("cw", [128, 12])
    cb_d = din("cb", [128, 4])
    wpa_d = din("w_pa", [512, D])
    wpc_d = din("w_pc", [512, D])
    wo_d = din("w_o", [D, D])
    gffn_d = din("g_ffn", [1, D])
    wrg_d = din("w_rg", [D, 36])
    brg_d = din("b_rg", [1, 36])
    w1_d = din("w1", [NE, D, 512])
    w3_d = din("w3", [NE, D, 512])
    w2_d = din("w2", [NE, 512, D])
    gple_d = din("g_ple", [1, D])
    wpg_d = din("w_pg", [D, D])
    bpg_d = din("b_pg", [1, D])
    wpp_d = din("w_pp", [256, D])
    ident_d = din("ident", [128, 128], BF16)
    utri_d = din("utri", [128, 128], BF16)
    ones_d = din("ones", [128, 128], BF16)
    bdiag_d = din("bdiag", [128, 128], BF16)
    ecap_d = din("ecap", [128, NE])
    selA_d = din("selA", [128, 8 * 128], BF16)
    selB_d = din("selB", [128, 8 * 128], BF16)
    out_d = nc.dram_tensor("out", [S, D], F32, kind="ExternalOutput")

    nT_d = dscr("nT_s", [D, S], BF16)
    yaT_d = dscr("yaT_s", [512, S], BF16)
    ycT_d = dscr("ycT_s", [512, S], BF16)
    h_d = dscr("h_s", [S, D], F32)
    xs_d = dscr("xs_s", [NSLOT + 128, D], BF16)
    ys_d = dscr("ys_s", [NSLOT + 128, D], BF16)
    if debug:
        dtab_o = nc.dram_tensor("dtab_o", [128, NT * 2], I32, kind="ExternalOutput")
        wtab_o = nc.dram_tensor("wtab_o", [128, NT * 2], F32, kind="ExternalOutput")

    pT = nc.alloc_psum_tensor("pT", [128, 8, 128], BF16)
    A0 = nc.alloc_psum_tensor("A0", [128, 512], F32)
    A1 = nc.alloc_psum_tensor("A1", [128, 512], F32)
    A2 = nc.alloc_psum_tensor("A2", [128, 512], F32)
    B0 = nc.alloc_psum_tensor("B0", [128, 1024], F32)
    B1 = nc.alloc_psum_tensor("B1", [128, 1024], F32)

    ident = sb.alloc("ident", [128, 128], BF16)
    utri = sb.alloc("utri", [128, 128], BF16)
    ones = sb.alloc("ones", [128, 128], BF16)
    bdiag = sb.alloc("bdiag", [128, 128], BF16)
    ecap = sb.alloc("ecap", [128, NE], F32)
    bcol = sb.alloc("bcol", [128, 40], F32)
    hbcol = sb.alloc("hbcol", [128, 40], F32)
    mhalf = sb.alloc("mhalf", [128, 8], F32)
    epsc = sb.alloc("epsc", [128, 8], F32)
    dtab = sb.alloc("dtab", [128, NT, 2], I32)
    wtab = sb.alloc("wtab", [128, NT, 2], F32)
    cnt = sb.alloc("cnt", [128, NE], F32)
    ss = sb.alloc("ss", [128, 8], F32)
    rs = sb.alloc("rs", [128, 8], F32)
    junk = sb.alloc("junk", [128, D], BF16)

    def ld(eng, dst, src, key):
        P.op(eng, lambda e: e.dma_start(out=dst, in_=src), writes=[key], dma=True)

    ld("sync", ident[:], ident_d.ap(), "ident")
    ld("sync", utri[:], utri_d.ap(), "utri")
    ld("sync", ones[:], ones_d.ap(), "ones")
    ld("sync", bdiag[:], bdiag_d.ap(), "bdiag")
    ld("sync", ecap[:], ecap_d.ap(), "ecap")
    ld("sync", bcol[:], bcol_d.ap(), "bcol")
    P.op("vector", lambda e: e.tensor_scalar(out=hbcol[:], in0=bcol[:], scalar1=0.5, scalar2=None, op0=ALU.mult),
         reads=["bcol"], writes=["hbcol"])
    P.op("gpsimd", lambda e: e.memset(mhalf[:], -0.5), writes=["mhalf"])
    P.op("gpsimd", lambda e: e.memset(epsc[:], EPS), writes=["epsc"])
    P.op("gpsimd", lambda e: e.memset(cnt[:], 0.0), writes=["cnt"])
    P.op("gpsimd", lambda e: e.memset(wtab[:], 0.0), writes=["wtab"])

    nrm_ctr = [0]

    def rmsnorm_tile(src, g_bc, dst_bf, src_key, g_key, dst_key, pool=True):
        i = nrm_ctr[0] % 8
        nrm_ctr[0] += 1
        ssk, rsk = ("ss", i), ("rs", i)
        P.op("scalar", lambda e: e.activation(out=junk[:], in_=src, func=AF.Square, accum_out=ss[:, i:i + 1]),
             reads=[src_key], writes=["junk", ssk])
        P.op("vector", lambda e: e.tensor_scalar(out=rs[:, i:i + 1], in0=ss[:, i:i + 1], scalar1=1.0 / D, scalar2=EPS,
                                                  op0=ALU.mult, op1=ALU.add), reads=[ssk], writes=[rsk])
        if pool:
            P.op("gpsimd", lambda e: e.tensor_tensor(out=rs[:, i:i + 1], in0=rs[:, i:i + 1], in1=mhalf[:, 0:1], op=ALU.pow),
                 reads=[rsk, "mhalf"], writes=[rsk])
        else:
            P.op("scalar", lambda e: e.activation(out=rs[:, i:i + 1], in_=rs[:, i:i + 1], func=AF.Sqrt), reads=[rsk], writes=[rsk])
            P.op("vector", lambda e: e.reciprocal(out=rs[:, i:i + 1], in_=rs[:, i:i + 1]), reads=[rsk], writes=[rsk])
        P.op("vector", lambda e: e.scalar_tensor_tensor(out=dst_bf, in0=src, scalar=rs[:, i:i + 1], in1=g_bc,
                                                         op0=ALU.mult, op1=ALU.mult),
             reads=[src_key, rsk, g_key], writes=[dst_key])

    def transpose_tile(src_bf, nchunk, dst, src_key, dst_key, evac="scalar", interleave=0):
        for c in range(nchunk):
            if interleave:
                src_c = src_bf.rearrange("t (p c) -> t c p", c=interleave)[:, c, :]
            else:
                src_c = src_bf[:, c * 128:(c + 1) * 128]
            P.op("tensor", lambda e, c=c, src_c=src_c: e.transpose(out=pT[:, c, :], in_=src_c, identity=ident[:]),
                 reads=(list(src_key) if isinstance(src_key, list) else [src_key]) + ["ident"], writes=[("pT", c)])
        if evac == "scalar":
            P.op("scalar", lambda e: e.activation(out=dst, in_=pT[:, 0:nchunk, :], func=AF.Copy),
                 reads=[("pT", c) for c in range(nchunk)], writes=[dst_key])
        else:
            P.op("vector", lambda e: e.tensor_copy(out=dst, in_=pT[:, 0:nchunk, :]),
                 reads=[("pT", c) for c in range(nchunk)], writes=[dst_key])

    _breg = {}

    def breg(e):
        if "r" not in _breg:
            _breg["r"] = e.to_reg(NSLOT - 1)
        return _breg["r"]

    base_mark = sb.mark()
    BASE = (base_mark + 31) // 32 * 32
    pre2 = {"Wg": nc.alloc_sbuf_tensor_at("Wg_pre", [128, 8, 2048], BF16, offset=BASE),
            "wpa": nc.alloc_sbuf_tensor_at("wpa_pre", [128, 4, D], BF16, offset=BASE + 32768),
            "wpc": nc.alloc_sbuf_tensor_at("wpc_pre", [128, 4, D], BF16, offset=BASE + 40960)}
    pre3 = [{"w1": nc.alloc_sbuf_tensor_at("w1_pre%d" % i, [128, 8, 512], BF16, offset=BASE + i * 24576),
             "w3": nc.alloc_sbuf_tensor_at("w3_pre%d" % i, [128, 8, 512], BF16, offset=BASE + i * 24576 + 8192),
             "w2": nc.alloc_sbuf_tensor_at("w2_pre%d" % i, [128, 4, D], BF16, offset=BASE + i * 24576 + 16384)} for i in range(2)]
    WGKEYS = [("Wg", g0 + q4 * 256) for q4 in range(4) for g0 in (0, 1024)]

    def p2_weight_loads(Wg, wpa, wpc, extra_writes=()):
        win_v2 = win_d.ap().rearrange("(c p) n -> p c n", p=128)
        ew = list(extra_writes)
        if ew:
            for c in range(0, 8, 2):
                P.op("gpsimd", lambda e, c=c: e.dma_start(out=Wg[:, c:c + 2, :], in_=win_v2[:, c:c + 2, 3072:5120]),
                     writes=WGKEYS + ew, dma=True)
            P.op("gpsimd", lambda e: e.dma_start(out=wpa[:], in_=wpa_d.ap().rearrange("(c p) n -> p c n", p=128)), writes=["wpa"] + ew, dma=True)
            P.op("gpsimd", lambda e: e.dma_start(out=wpc[:], in_=wpc_d.ap().rearrange("(c p) n -> p c n", p=128)), writes=["wpc"] + ew, dma=True)
            return

        def wg_load(q4):
            for g0 in (0, 1024):
                c0_ = g0 + q4 * 256
                P.op("gpsimd", lambda e, c0_=c0_: e.dma_start(out=Wg[:, :, c0_:c0_ + 256], in_=win_v2[:, :, 3072 + c0_:3072 + c0_ + 256]),
                     writes=[("Wg", c0_)] + ew, dma=True)

        wg_load(0)
        P.op("gpsimd", lambda e: e.dma_start(out=wpa[:], in_=wpa_d.ap().rearrange("(c p) n -> p c n", p=128)), writes=["wpa"] + ew, dma=True)
        P.op("gpsimd", lambda e: e.dma_start(out=wpc[:], in_=wpc_d.ap().rearrange("(c p) n -> p c n", p=128)), writes=["wpc"] + ew, dma=True)
        for q4 in range(1, 4):
            wg_load(q4)

    def expert_weight_loads(ex, w1t, w3t, w2t, keys, extra_writes=()):
        ew = list(extra_writes)
        P.op("gpsimd", lambda e: e.dma_start(out=w1t[:], in_=w1_d.ap()[ex].rearrange("(p c) f -> p c f", c=8)), writes=[keys[0]] + ew, dma=True)
        P.op("gpsimd", lambda e: e.dma_start(out=w3t[:], in_=w3_d.ap()[ex].rearrange("(p c) f -> p c f", c=8)), writes=[keys[1]] + ew, dma=True)
        P.op("gpsimd", lambda e: e.dma_start(out=w2t[:], in_=w2_d.ap()[ex].rearrange("(c p) f -> p c f", p=128)), writes=[keys[2]] + ew, dma=True)

    def phase1():
        Wa = sb.alloc("Wa", [128, 8, 3072], BF16)
        gmix = sb.alloc("gmix", [128, D], F32)
        gqk = sb.alloc("gqk", [128, 2], F32)
        bvb = sb.alloc("bvb", [128, 512], F32)
        cw = sb.alloc("cw", [128, 12], F32)
        cb = sb.alloc("cb", [128, 4], F32)
        expB = sb.alloc("expB", [128, NH, 5, 128], BF16)
        maskT = sb.alloc("maskT", [128, 5, 128], F32)
        kring = sb.alloc("kring", [128, 4, KR * 128], BF16)
        vring = sb.alloc("vring", [128, KR, NH, 65], BF16)
        xt = [sb.alloc("xt%d" % i, [128, D], F32) for i in range(2)]
        nb = [sb.alloc("nb%d" % i, [128, D], BF16) for i in range(2)]
        nTb = [sb.alloc("nTb%d" % i, [128, 8, BT], BF16) for i in range(2)]
        qT = [sb.alloc("qT%d" % i, [128, 4, BT], BF16) for i in range(2)]
        zq = [sb.alloc("zq%d" % i, [128, BT], F32) for i in range(8)]
        sq = [sb.alloc("sq%d" % i, [128, BT], BF16) for i in range(3)]
        rs = sb.alloc("rs_all", [128, BT], F32)
        r1b = sb.alloc("r1b", [128, BT], BF16)
        selA = sb.alloc("selA", [128, 8, 128], BF16)
        selB = sb.alloc("selB", [128, 8, 128], BF16)
        gw = sb.alloc("gw", [128, 1], F32)
        us = [sb.alloc("us%d" % i, [128, BT], F32) for i in range(2)]
        t1 = [sb.alloc("t1%d" % i, [128, BT], F32) for i in range(2)]
        cu = sb.alloc("cu", [128, 4, BT + 2], F32)
        ycT = [sb.alloc("ycT%d" % i, [128, 4, BT], BF16) for i in range(2)]
        yaT = [sb.alloc("yaT%d" % i, [128, 4, BT], BF16) for i in range(2)]
        pt = [sb.alloc("pt%d" % i, [128, 4, 128], BF16) for i in range(6)]
        rden = [sb.alloc("rden%d" % i, [128, 4], F32) for i in range(2)]
        ya = sb.alloc("ya", [128, 4, 512], BF16)

        win_v = win_d.ap().rearrange("(c p) n -> p c n", p=128)
        for (c0_, c1_) in ((0, 1024), (1024, 1536), (1536, 3072)):
            for c in range(0, 8, 2):
                P.op("gpsimd", lambda e, c=c, c0_=c0_, c1_=c1_: e.dma_start(out=Wa[:, c:c + 2, c0_:c1_], in_=win_v[:, c:c + 2, c0_:c1_]),
                     writes=[("Wa", c, c0_), ("Wa", c + 1, c0_)], dma=True)
        zt = sb.alloc("zt", [128, 2 * D], BF16)
        P.op("gpsimd", lambda e: e.memset(zt[:], 0.0), writes=["zt"])
        NR = (NSLOT + 128) // 128
        xs_z = xs_d.ap().rearrange("(p r) d -> p (r d)", p=128)
        zchunks = [(r0, min(r0 + 2, NR)) for r0 in range(0, NR, 2)]

        def zero_fill(k):
            for (r0, r1_) in zchunks[k::NB]:
                P.op("sync", lambda e, r0=r0, r1_=r1_: e.dma_start(out=xs_z[:, r0 * D:r1_ * D], in_=zt[:, 0:(r1_ - r0) * D]),
                     reads=["zt"], writes=[("xs_zero", r0)], dma=True)

        ld("sync", gmix[:], gmix_d.ap().partition_broadcast(128), "gmix")
        ld("sync", gqk[:], gqk_d.ap(), "gqk")
        ld("sync", selA[:], selA_d.ap().rearrange("p (j m) -> p j m", j=8), "selA")
        ld("sync", selB[:], selB_d.ap().rearrange("p (j m) -> p j m", j=8), "selB")
        ld("sync", bvb[:], bv_d.ap().partition_broadcast(128), "bvb")
        P.op("vector", lambda e: e.tensor_tensor(out=gw[:], in0=gqk[:, 0:1], in1=gqk[:, 1:2], op=ALU.mult), reads=["gqk"], writes=["gw"])
        ld("sync", cw[:], cw_d.ap(), "cw")
        ld("sync", cb[:], cb_d.ap(), "cb")
        ld("sync", maskT[:], maskT_d.ap().rearrange("p (j q) -> p j q", j=5), "maskT")
        rb_v = rbT_d.ap().rearrange("p (h n) -> p h n", h=NH)
        stg = [sb.alloc_alias("stg0", [128, 640], F32, "zq0"), sb.alloc_alias("stg1", [128, 640], F32, "zq2")]
        for h in range(NH):
            st = stg[h % 2]
            sk = [("zq", 2 * (h % 2)), ("zq", 2 * (h % 2) + 1)]
            P.op("sync", lambda e, h=h, st=st: e.dma_start(out=st[:], in_=rb_v[:, h, :]), writes=sk, dma=True)
            P.op("scalar", lambda e, st=st: e.activation(out=st[:], in_=st[:], func=AF.Exp), reads=sk, writes=sk)
            P.op("vector", lambda e, h=h, st=st: e.tensor_tensor(
                out=expB[:, h, :, :], in0=st[:].rearrange("p (j q) -> p j q", j=5), in1=maskT[:], op=ALU.mult),
                reads=sk + ["maskT"], writes=[("expB", h)])
        P.op("gpsimd", lambda e: e.memset(vring[:], 1.0), writes=[("v", s_) for s_ in range(KR)])
        P.op("gpsimd", lambda e: e.memset(cu[:], 0.0), writes=[("cu", ct) for ct in range(4)] + [("cuh", ct) for ct in range(4)])

        nT_v = nT_d.ap().rearrange("(c p) t -> p c t", p=128)
        yaT_v = yaT_d.ap().rearrange("(c p) t -> p c t", p=128)
        ycT_v = ycT_d.ap().rearrange("(c p) t -> p c t", p=128)
        acc_rot = [0]
        ACC = [(A0, "A0"), (A1, "A1")]

        def next_acc():
            a = ACC[acc_rot[0] % 2]
            acc_rot[0] += 1
            return a

        def A_norm(b, tl):
            t = 4 * b + tl
            s2 = t % 2
            P.op("sync", lambda e, t=t, s2=s2: e.dma_start(out=xt[s2][:], in_=x_d.ap()[t * 128:(t + 1) * 128, :]),
                 writes=[("xt", s2)], dma=True)
            rmsnorm_tile(xt[s2][:], gmix[:], nb[s2][:], ("xt", s2), "gmix", ("nb", s2))

        def A_tr(b, tl):
            t = 4 * b + tl
            s2 = t % 2
            bb = b % 2
            transpose_tile(nb[s2], 8, nTb[bb][:, :, tl * 128:(tl + 1) * 128], ("nb", s2), ("nTb", bb, tl))
            if tl == 3:
                P.op("sync", lambda e, bb=bb, b=b: e.dma_start(out=nT_v[:, :, b * BT:(b + 1) * BT], in_=nTb[bb][:]),
                     reads=[("nTb", bb, q_) for q_ in range(4)], writes=[("nT_d", b)], dma=True)

        def secC(b):
            bb = b % 2
            tok0 = b * BT
            nkeys = [("nTb", bb, tl) for tl in range(4)]
            PACC = [(A0[:], "A0"), (A1[:], "A1"), (B0[:, 0:512], ("B0", 0))]
            prot = [0]

            def nacc():
                a = PACC[prot[0] % 3]
                prot[0] += 1
                return a

            def proj(j):
                acc, akey = nacc()
                for c in range(8):
                    P.op("tensor", lambda e, j=j, c=c, acc=acc: e.matmul(
                        acc, lhsT=Wa[:, c, j * 128:(j + 1) * 128], rhs=nTb[bb][:, c, :], start=(c == 0), stop=(c == 7)),
                        reads=[("Wa", c, 0)] + nkeys, writes=[akey])
                z = j % 3
                P.op("scalar", lambda e, j=j, acc=acc: e.activation(out=zq[j][:], in_=acc, func=AF.Identity, bias=bcol[:, j:j + 1]),
                     reads=[akey, "bcol"], writes=[("zq", j)])
                P.op("gpsimd", lambda e, z=z, j=j: e.tensor_tensor(out=sq[z][:], in0=zq[j][:], in1=zq[j][:], op=ALU.mult),
                     reads=[("zq", j)], writes=[("sq", z)])

            def msacc(j):
                z = j % 3
                P.op("tensor", lambda e, z=z, j=j: e.matmul(A2[:], lhsT=selA[:, j, :], rhs=sq[z][:], start=(j == 0), stop=(j == 7)),
                     reads=[("sq", z), "selA"], writes=["A2"])

            def vproj():
                for tl in range(4):
                    t = 4 * b + tl
                    sl = t % KR
                    acc, akey = nacc()
                    for c in range(8):
                        P.op("tensor", lambda e, c=c, tl=tl, acc=acc: e.matmul(
                            acc, lhsT=nTb[bb][:, c, tl * 128:(tl + 1) * 128], rhs=Wa[:, c, 1024:1536], start=(c == 0), stop=(c == 7)),
                            reads=[("Wa", c, 1024), ("nTb", bb, tl)], writes=[akey])
                    P.op("vector", lambda e, sl=sl, acc=acc: e.tensor_tensor(
                        out=vring[:, sl, :, 0:64], in0=acc.rearrange("p (h d) -> p h d", h=NH),
                        in1=bvb[:].rearrange("p (h d) -> p h d", h=NH), op=ALU.add),
                        reads=[akey, "bvb"], writes=[("v", sl)])

            def fin(j):
                acc, akey = nacc()
                P.op("tensor", lambda e, j=j, acc=acc: e.matmul(acc, lhsT=selB[:, j, :], rhs=r1b[:], start=True, stop=True),
                     reads=["selB", "r1b"], writes=[akey])
                if j < 4:
                    P.op("vector", lambda e, j=j, acc=acc: e.tensor_tensor(out=qT[bb][:, j, :], in0=zq[j][:], in1=acc, op=ALU.mult),
                         reads=[("zq", j), akey], writes=[("qT", bb, j)])
                else:
                    hp = j - 4
                    sl0 = (4 * b) % KR
                    P.op("vector", lambda e, hp=hp, j=j, sl0=sl0, acc=acc: e.scalar_tensor_tensor(
                        out=kring[:, hp, sl0 * 128:(sl0 + 4) * 128], in0=zq[j][:], scalar=gw[:, 0:1], in1=acc, op0=ALU.mult, op1=ALU.mult),
                        reads=[("zq", j), akey, "gw"], writes=[("k", hp, sl0 + q_) for q_ in range(4)])

            proj(0)
            for j in range(8):
                if j + 1 < 8:
                    proj(j + 1)
                msacc(j)
            P.op("scalar", lambda e: e.activation(out=rs[:], in_=A2[:], func=AF.Sqrt, bias=epsc[:, 0:1]), reads=["A2", "epsc"], writes=["rs"])
            P.op("vector", lambda e: e.reciprocal(out=rs[:], in_=rs[:]), reads=["rs"], writes=["rs"])
            P.op("scalar", lambda e: e.activation(out=r1b[:], in_=rs[:], func=AF.Copy), reads=["rs"], writes=["r1b"])
            vproj()
            for j in range(8):
                fin(j)
        def secC3(b):
            bb = b % 2
            tok0 = b * BT
            nkeys = [("nTb", bb, tl) for tl in range(4)]
            SETS = [((A0[:], ["A0"]), (A1[:], ["A1"]), (A2[:], ["A2"])),
                    ((B0[:, 0:512], [("B0", 0)]), (B0[:, 512:1024], [("B0", 4)]), (B1[:, 0:512], [("B1h", 0)]))]
            for ct in range(4):
                z = ct % 2
                (pu, ku), (pb, kb), (pc, kc) = SETS[ct % 2]
                for (dst, dkey, col0) in ((pu, ku, 1536), (pb, kb, 2048), (pc, kc, 2560)):
                    for c in range(8):
                        P.op("tensor", lambda e, c=c, dst=dst, col0=col0, ct=ct: e.matmul(
                            dst, lhsT=Wa[:, c, col0 + ct * 128:col0 + (ct + 1) * 128], rhs=nTb[bb][:, c, :],
                            start=(c == 0), stop=(c == 7)),
                            reads=[("Wa", c, 1536)] + nkeys, writes=dkey)
                ju, jb, jc = 12 + ct, 16 + ct, 20 + ct
                P.op("scalar", lambda e, z=z, ju=ju, pu=pu: e.activation(out=us[z][:], in_=pu, func=AF.Identity, bias=bcol[:, ju:ju + 1]),
                     reads=ku + ["bcol"], writes=[("us", z)])
                P.op("vector", lambda e, z=z, jc=jc, ct=ct, pc=pc: e.scalar_tensor_tensor(
                    out=cu[:, ct, 2:BT + 2], in0=pc, scalar=bcol[:, jc:jc + 1], in1=us[z][:], op0=ALU.add, op1=ALU.mult),
                    reads=kc + ["bcol", ("us", z)], writes=[("cu", ct)])
                P.op("scalar", lambda e, z=z, ct=ct: e.activation(out=t1[z][:], in_=cu[:, ct, 2:BT + 2], func=AF.Identity,
                                                               scale=cw[:, ct * 3 + 2:ct * 3 + 3], bias=cb[:, ct:ct + 1]),
                     reads=[("cu", ct), "cw", "cb"], writes=[("t1", z)])
                P.op("vector", lambda e, z=z, ct=ct: e.scalar_tensor_tensor(
                    out=t1[z][:], in0=cu[:, ct, 1:BT + 1], scalar=cw[:, ct * 3 + 1:ct * 3 + 2], in1=t1[z][:], op0=ALU.mult, op1=ALU.add),
                    reads=[("cu", ct), ("cuh", ct), "cw", ("t1", z)], writes=[("t1", z)])
                P.op("vector", lambda e, z=z, ct=ct: e.scalar_tensor_tensor(
                    out=t1[z][:], in0=cu[:, ct, 0:BT], scalar=cw[:, ct * 3:ct * 3 + 1], in1=t1[z][:], op0=ALU.mult, op1=ALU.add),
                    reads=[("cu", ct), ("cuh", ct), "cw", ("t1", z)], writes=[("t1", z)])
                P.op("vector", lambda e, z=z, jb=jb, ct=ct, pb=pb: e.scalar_tensor_tensor(
                    out=ycT[bb][:, ct, :], in0=pb, scalar=bcol[:, jb:jb + 1], in1=t1[z][:], op0=ALU.add, op1=ALU.mult),
                    reads=kb + ["bcol", ("t1", z)], writes=[("ycT", bb, ct)])
                P.op("vector", lambda e, ct=ct: e.tensor_copy(out=cu[:, ct, 0:2], in_=cu[:, ct, BT:BT + 2]),
                     reads=[("cu", ct)], writes=[("cuh", ct)])
            P.op("sync", lambda e, tok0=tok0: e.dma_start(out=ycT_v[:, :, tok0:tok0 + BT], in_=ycT[bb][:]),
                 reads=[("ycT", bb, ct) for ct in range(4)], writes=[("ycT_d", b)], dma=True)

        def secD(b):
            bb = b % 2
            tok0 = b * BT
            nxt = b + 1 < NB
            units = []
            for h in range(NH):
                for m in range(8):
                    kt = 4 * b - 4 + m
                    if kt < 0:
                        continue
                    units.append((h, m, kt, max(m - 4, 0), min(m, 3)))
            SPS = [(A0[:], "A0"), (A1[:], "A1"), (A2[:], "A2"), (B0[:, 0:512], ("B0", 0)), (B0[:, 512:1024], ("B0", 4))]
            LA = 4

            def QKEXP(u):
                h, m, kt, tlo, thi = units[u]
                hp, r0 = h // 2, (h % 2) * 64
                nq = thi - tlo + 1
                sp, sk = SPS[u % 5]
                pz = u % 6
                sl = kt % KR
                P.op("tensor", lambda e, hp=hp, r0=r0, sl=sl, sp=sp, tlo=tlo, thi=thi: e.matmul(
                    sp[:, 0:(thi - tlo + 1) * 128], lhsT=kring[r0:r0 + 64, hp, sl * 128:(sl + 1) * 128],
                    rhs=qT[bb][r0:r0 + 64, hp, tlo * 128:(thi + 1) * 128], start=True, stop=True),
                    reads=[("k", hp, sl), ("qT", bb, hp)], writes=[sk])
                P.op("scalar", lambda e, pz=pz, sp=sp, nq=nq: e.activation(
                    out=pt[pz][:, 0:nq, :], in_=sp[:, 0:nq * 128].rearrange("p (j q) -> p j q", q=128), func=AF.Exp, scale=DH ** -0.5),
                    reads=[sk], writes=[("pt", pz)])
                rlo = 4 - m + tlo
                P.op("vector", lambda e, pz=pz, nq=nq, h=h, rlo=rlo: e.tensor_tensor(
                    out=pt[pz][:, 0:nq, :], in0=pt[pz][:, 0:nq, :], in1=expB[:, h, rlo:rlo + nq, :], op=ALU.mult),
                    reads=[("pt", pz), ("expB", h)], writes=[("pt", pz)])

            def PV(u):
                h, m, kt, tlo, thi = units[u]
                pz = u % 6
                sl = kt % KR
                hb2 = h % 2
                first = (u == 0) or units[u - 1][0] != h
                last_u = (u + 1 == len(units)) or units[u + 1][0] != h
                if first:
                    P.op("tensor", lambda e, hb2=hb2: e.matmul(
                        B1[:, hb2 * 512:hb2 * 512 + 260], lhsT=zt[:, 0:128], rhs=zt[:, 0:260], start=True, stop=False),
                        reads=["zt"], writes=[("B1h", hb2)])
                for tl in range(tlo, thi + 1):
                    c0 = hb2 * 512 + tl * 65
                    P.op("tensor", lambda e, h=h, tl=tl, tlo=tlo, sl=sl, pz=pz, c0=c0, fin=(last_u and tl == thi): e.matmul(
                        B1[:, c0:c0 + 65], lhsT=pt[pz][:, tl - tlo, :], rhs=vring[:, sl, h, :],
                        start=False, stop=fin),
                        reads=[("pt", pz), ("v", sl)], writes=[("B1h", hb2)])

            def FINH(h):
                hb2 = h % 2
                Bv = B1[:, hb2 * 512:hb2 * 512 + 260].rearrange("p (t d) -> p t d", d=65)
                P.op("vector", lambda e, hb2=hb2, Bv=Bv: e.reciprocal(out=rden[hb2][:], in_=Bv[:, :, 64]),
                     reads=[("B1h", hb2)], writes=[("rden", hb2)])
                P.op("vector", lambda e, hb2=hb2, Bv=Bv, h=h: e.tensor_tensor(
                    out=ya[:, :, h * 64:(h + 1) * 64], in0=Bv[:, :, 0:64], in1=bc_last(rden[hb2][:], 64), op=ALU.mult),
                    reads=[("B1h", hb2), ("rden", hb2)], writes=[("ya", h)])

            if nxt:
                A_norm(b + 1, 0)
            for u in range(min(LA, len(units))):
                QKEXP(u)
            secC3(b)
            zero_fill(b)
            if b == NB - 1 and 2 in phases:
                p2_weight_loads(pre2["Wg"], pre2["wpa"], pre2["wpc"],
                                extra_writes=[("Wa", c, c0_) for c in range(8) for c0_ in (0, 1024, 1536)])
            for u in range(len(units)):
                if u + LA < len(units):
                    QKEXP(u + LA)
                PV(u)
                h = units[u][0]
                if u + 1 == len(units) or units[u + 1][0] != h:
                    FINH(h)
                    if nxt and h % 2 == 1:
                        tl = h // 2
                        A_tr(b + 1, tl)
                        if tl + 1 < 4:
                            A_norm(b + 1, tl + 1)
            for tl in range(4):
                transpose_tile(ya[:, tl, :], 4, yaT[bb][:, :, tl * 128:(tl + 1) * 128], [("ya", h) for h in range(NH)], ("yaT", bb, tl))
            P.op("sync", lambda e, tok0=tok0: e.dma_start(out=yaT_v[:, :, tok0:tok0 + BT], in_=yaT[bb][:]),
                 reads=[("yaT", bb, tl) for tl in range(4)], writes=[("yaT_d", b)], dma=True)

        for tl in range(4):
            A_norm(0, tl)
            A_tr(0, tl)
        for b in range(NB):
            secC(b)
            secD(b)
        P.barrier()
    if 1 in phases:
        phase1()
    sb.reset(base_mark)

    def phase2():
        Wg = sb.alloc("Wg", [128, 8, 2048], BF16)
        wpa = sb.alloc("wpa", [128, 4, D], BF16)
        wpc = sb.alloc("wpc", [128, 4, D], BF16)
        wo = sb.alloc("wo", [128, 8, D], BF16)
        wrg = sb.alloc("wrg", [128, 8, 36], BF16)
        brg = sb.alloc("brg", [128, 36], F32)
        gffn = sb.alloc("gffn", [128, D], F32)
        nTb = [sb.alloc("nTb%d" % i, [128, 8, BT], BF16) for i in range(2)]
        yaT = [sb.alloc("yaT%d" % i, [128, 4, BT], BF16) for i in range(2)]
        ycT = [sb.alloc("ycT%d" % i, [128, 4, BT], BF16) for i in range(2)]
        xt = [sb.alloc("xt%d" % i, [128, D], F32) for i in range(4)]
        tA = [sb.alloc("tA%d" % i, [128, BT], F32) for i in range(2)]
        tC = [sb.alloc("tC%d" % i, [128, BT], F32) for i in range(2)]
        mA = [sb.alloc("mA%d" % i, [128, BT], F32) for i in range(2)]
        mC = [sb.alloc("mC%d" % i, [128, BT], F32) for i in range(2)]
        mT = [sb.alloc("mT%d" % i, [128, 8, BT], BF16) for i in range(2)]
        ht = [sb.alloc("ht%d" % i, [128, D], F32) for i in range(3)]
        n2 = [sb.alloc("n2%d" % i, [128, 4, D], BF16) for i in range(2)]
        n2T = [sb.alloc("n2T%d" % i, [128, 8, 128], BF16) for i in range(2)]
        lg = sb.alloc("lg", [128, 4, 36], F32)
        gmax = sb.alloc("gmax", [128, 4], F32)
        gmask = sb.alloc("gmask", [128, 4, 4], F32)
        gex = sb.alloc("gex", [128, 4, 4], F32)
        gse = sb.alloc("gse", [128, 4], F32)
        pen = sb.alloc("pen", [128, 4, 4], F32)
        elm = sb.alloc("elm", [128, 4, 32], F32)
        elm2 = sb.alloc("elm2", [128, 4, 32], F32)
        m1 = sb.alloc("m1", [128, 4], F32)
        m2 = sb.alloc("m2", [128, 4], F32)
        mk1 = sb.alloc("mk1", [128, 4, 32], F32)
        mk2 = sb.alloc("mk2", [128, 4, 32], F32)
        Mb = sb.alloc("Mb", [128, 4, 32], BF16)
        dd = sb.alloc("dd", [128, 4], F32)
        ee = sb.alloc("ee", [128, 4], F32)
        rr = sb.alloc("rr", [128, 4], F32)
        wA = sb.alloc("wA", [128, 4], F32)
        wB = sb.alloc("wB", [128, 4], F32)
        pos = sb.alloc("pos", [128, 4, 32], F32)
        okm = sb.alloc("okm", [128, 4, 32], F32)
        slot = sb.alloc("slot", [128, 4, 32], F32)
        tmp = sb.alloc("tmp", [128, 4, 32], F32)
        dsel = sb.alloc("dsel", [128, 4, 2], F32)
        oksel = sb.alloc("oksel", [128, 4, 2], F32)

        assert sb.off["Wg"] == BASE and sb.off["wpa"] == BASE + 32768 and sb.off["wpc"] == BASE + 40960
        if 1 not in phases:
            p2_weight_loads(Wg, wpa, wpc)
        wo_v = wo_d.ap().rearrange("(c p) n -> p c n", p=128)
        for c in range(0, 8, 4):
            P.op("gpsimd", lambda e, c=c: e.dma_start(out=wo[:, c:c + 4, :], in_=wo_v[:, c:c + 4, :]), writes=[("wo", c)], dma=True)
        P.op("gpsimd", lambda e: e.dma_start(out=wrg[:], in_=wrg_d.ap().rearrange("(c p) n -> p c n", p=128)), writes=["wrg"], dma=True)
        ld("sync", brg[:], brg_d.ap().partition_broadcast(128), "brg")
        ld("sync", gffn[:], gffn_d.ap().partition_broadcast(128), "gffn")

        nT_v = nT_d.ap().rearrange("(c p) t -> p c t", p=128)
        yaT_v = yaT_d.ap().rearrange("(c p) t -> p c t", p=128)
        ycT_v = ycT_d.ap().rearrange("(c p) t -> p c t", p=128)
        wokeys = [("wo", 0), ("wo", 4)]
        xctr = [0]
        hctr = [0]

        def loads2(b):
            bb = b % 2
            tok0 = b * BT
            P.op("sync", lambda e, bb=bb, tok0=tok0: e.dma_start(out=nTb[bb][:], in_=nT_v[:, :, tok0:tok0 + BT]), writes=[("nTb", bb)], dma=True)
            P.op("sync", lambda e, bb=bb, tok0=tok0: e.dma_start(out=yaT[bb][:], in_=yaT_v[:, :, tok0:tok0 + BT]), writes=[("yaT", bb)], dma=True)
            P.op("sync", lambda e, bb=bb, tok0=tok0: e.dma_start(out=ycT[bb][:], in_=ycT_v[:, :, tok0:tok0 + BT]), writes=[("ycT", bb)], dma=True)

        def xload(t):
            P.op("sync", lambda e, t=t: e.dma_start(out=xt[t % 4][:], in_=x_d.ap()[t * 128:(t + 1) * 128, :]), writes=[("xt", t % 4)], dma=True)

        loads2(0)
        for t_ in range(3):
            xload(t_)
        def gates2(b):
            bb = b % 2
            tok0 = b * BT
            if b + 1 < NB:
                loads2(b + 1)
            for j in range(8):
                z = j % 2
                for (dst, dkey, col0) in ((A0, "A0", 0), (A1, "A1", 1024)):
                    for c in range(8):
                        P.op("tensor", lambda e, c=c, dst=dst, col0=col0, j=j, bb=bb: e.matmul(
                            dst[:], lhsT=Wg[:, c, col0 + j * 128:col0 + (j + 1) * 128], rhs=nTb[bb][:, c, :], start=(c == 0), stop=(c == 7)),
                            reads=[("Wg", col0 + (j // 2) * 256), ("nTb", bb)], writes=[dkey])
                for (dst, dkey, wsrc, wkey, asrc, akey) in ((A2, "A2", wpa, "wpa", yaT, "yaT"), (B0, ("B0", 0), wpc, "wpc", ycT, "ycT")):
                    for c in range(4):
                        P.op("tensor", lambda e, c=c, dst=dst, wsrc=wsrc, asrc=asrc, j=j, bb=bb: e.matmul(
                            dst[:, 0:512], lhsT=wsrc[:, c, j * 128:(j + 1) * 128], rhs=asrc[bb][:, c, :], start=(c == 0), stop=(c == 3)),
                            reads=[wkey, (akey, bb)], writes=[dkey])
                P.op("scalar", lambda e, z=z, j=j: e.activation(out=tA[z][:], in_=A0[:], func=AF.Tanh, scale=0.5, bias=hbcol[:, 24 + j:25 + j]),
                     reads=["A0", "hbcol"], writes=[("tA", z)])
                P.op("scalar", lambda e, z=z, j=j: e.activation(out=tC[z][:], in_=A1[:], func=AF.Tanh, scale=0.5, bias=hbcol[:, 32 + j:33 + j]),
                     reads=["A1", "hbcol"], writes=[("tC", z)])
                P.op("vector", lambda e, z=z: e.scalar_tensor_tensor(out=mA[z][:], in0=tA[z][:], scalar=1.0, in1=A2[:], op0=ALU.add, op1=ALU.mult),
                     reads=[("tA", z), "A2"], writes=[("mA", z)])
                P.op("vector", lambda e, z=z: e.scalar_tensor_tensor(out=mC[z][:], in0=tC[z][:], scalar=1.0, in1=B0[:, 0:512], op0=ALU.add, op1=ALU.mult),
                     reads=[("tC", z), ("B0", 0)], writes=[("mC", z)])
                P.op("vector", lambda e, z=z, j=j, bb=bb: e.tensor_tensor(out=mT[bb][:, j, :], in0=mA[z][:], in1=mC[z][:], op=ALU.add),
                     reads=[("mA", z), ("mC", z)], writes=[("mT", bb, j)])

        def hsec2(b):
            bb = b % 2
            tok0 = b * BT
            mkeys = [("mT", bb, j) for j in range(8)]
            def hmm(tl):
                t = 4 * b + tl
                xs_ = t % 4
                hs_ = t % 3
                if t + 3 < NT:
                    xload(t + 3)
                HB = [(B1[:, 0:512], ("B1", 0)), (B1[:, 512:1024], ("B1", 1))] if tl % 2 == 0 else [(A0[:], "A0"), (A1[:], "A1")]
                for half in range(2):
                    hacc, hkey = HB[half]
                    for j in range(8):
                        P.op("tensor", lambda e, j=j, half=half, tl=tl, bb=bb, hacc=hacc: e.matmul(
                            hacc, lhsT=mT[bb][:, j, tl * 128:(tl + 1) * 128], rhs=wo[:, j, half * 512:(half + 1) * 512],
                            start=(j == 0), stop=(j == 7)),
                            reads=mkeys + wokeys, writes=[hkey])
                    P.op("vector", lambda e, half=half, xs_=xs_, hs_=hs_, hacc=hacc: e.scalar_tensor_tensor(
                        out=ht[hs_][:, half * 512:(half + 1) * 512], in0=hacc, scalar=0.5,
                        in1=xt[xs_][:, half * 512:(half + 1) * 512], op0=ALU.mult, op1=ALU.add),
                        reads=[hkey, ("xt", xs_)] + ([("ht", hs_)] if half == 1 else []), writes=[("ht", hs_)])
                P.op("sync", lambda e, t=t, hs_=hs_: e.dma_start(out=h_d.ap()[t * 128:(t + 1) * 128, :], in_=ht[hs_][:]),
                     reads=[("ht", hs_)], writes=[("h_d", t)], dma=True)

            def hnorm(tl):
                t = 4 * b + tl
                hs_ = t % 3
                rmsnorm_tile(ht[hs_][:], gffn[:], n2[bb][:, tl, :], ("ht", hs_), "gffn", ("n2", bb, tl),
                             pool=not (b == NB - 1 and 3 in phases))

            def htr(tl):
                t = 4 * b + tl
                z2 = t % 2
                transpose_tile(n2[bb][:, tl, :], 8, n2T[z2][:], ("n2", bb, tl), ("n2T", z2))
                for c in range(8):
                    P.op("tensor", lambda e, c=c, tl=tl, z2=z2: e.matmul(
                        B0[:, 512 + tl * 36:512 + (tl + 1) * 36], lhsT=n2T[z2][:, c, :], rhs=wrg[:, c, :], start=(c == 0), stop=(c == 7)),
                        reads=[("n2T", z2), "wrg"], writes=[("lgp", tl)])

            hmm(0)
            hnorm(0)
            for tl in range(4):
                if tl + 1 < 4:
                    hmm(tl + 1)
                htr(tl)
                if tl + 1 < 4:
                    hnorm(tl + 1)

        def rout2a(b):
            bb = b % 2
            tok0 = b * BT
            lgp = B0[:, 512:512 + 144].rearrange("p (t n) -> p t n", t=4)
            R = []

            def V(fn, reads, writes):
                P.op("vector", fn, reads=reads, writes=writes)

            V(lambda e: e.tensor_tensor(out=lg[:], in0=lgp, in1=bc_mid(brg[:], 4), op=ALU.add),
              [("lgp", tl) for tl in range(4)] + ["brg"], ["lg"])
            V(lambda e: e.tensor_reduce(out=gmax[:], in_=lg[:, :, 0:4], axis=AX.X, op=ALU.max), ["lg"], ["gmax"])
            V(lambda e: e.tensor_tensor(out=gmask[:], in0=lg[:, :, 0:4], in1=bc_last(gmax[:], 4), op=ALU.is_equal), ["lg", "gmax"], ["gmask"])
            V(lambda e: e.tensor_tensor(out=gex[:], in0=lg[:, :, 0:4], in1=bc_last(gmax[:], 4), op=ALU.subtract), ["lg", "gmax"], ["gex"])
            P.op("scalar", lambda e: e.activation(out=gex[:], in_=gex[:], func=AF.Exp), reads=["gex"], writes=["gex"])
            V(lambda e: e.tensor_reduce(out=gse[:], in_=gex[:], axis=AX.X, op=ALU.add), ["gex"], ["gse"])
            V(lambda e: e.reciprocal(out=gse[:], in_=gse[:]), ["gse"], ["gse"])
            V(lambda e: e.tensor_scalar(out=pen[:], in0=gmask[:], scalar1=1.0, scalar2=1e30, op0=ALU.subtract, op1=ALU.mult), ["gmask"], ["pen"])
            V(lambda e: e.tensor_tensor(out=elm[:].rearrange("p t (g k) -> p t g k", g=4),
                                        in0=lg[:, :, 4:36].rearrange("p t (g k) -> p t g k", g=4),
                                        in1=bc_last(pen[:], 8), op=ALU.add), ["lg", "pen"], ["elm"])
            V(lambda e: e.tensor_reduce(out=m1[:], in_=elm[:], axis=AX.X, op=ALU.max), ["elm"], ["m1"])
            V(lambda e: e.tensor_tensor(out=mk1[:], in0=elm[:], in1=bc_last(m1[:], 32), op=ALU.is_equal), ["elm", "m1"], ["mk1"])
            V(lambda e: e.scalar_tensor_tensor(out=elm2[:], in0=mk1[:], scalar=-1e30, in1=elm[:], op0=ALU.mult, op1=ALU.add), ["mk1", "elm"], ["elm2"])
            V(lambda e: e.tensor_reduce(out=m2[:], in_=elm2[:], axis=AX.X, op=ALU.max), ["elm2"], ["m2"])
            V(lambda e: e.tensor_tensor(out=mk2[:], in0=elm2[:], in1=bc_last(m2[:], 32), op=ALU.is_equal), ["elm2", "m2"], ["mk2"])
            V(lambda e: e.tensor_tensor(out=dd[:], in0=m2[:], in1=m1[:], op=ALU.subtract), ["m1", "m2"], ["dd"])
            P.op("scalar", lambda e: e.activation(out=ee[:], in_=dd[:], func=AF.Exp), reads=["dd"], writes=["ee"])
            V(lambda e: e.tensor_scalar(out=rr[:], in0=ee[:], scalar1=1.0, scalar2=None, op0=ALU.add), ["ee"], ["rr"])
            V(lambda e: e.reciprocal(out=rr[:], in_=rr[:]), ["rr"], ["rr"])
            V(lambda e: e.tensor_tensor(out=wA[:], in0=gse[:], in1=rr[:], op=ALU.mult), ["gse", "rr"], ["wA"])
            V(lambda e: e.tensor_tensor(out=wB[:], in0=wA[:], in1=ee[:], op=ALU.mult), ["wA", "ee"], ["wB"])
            V(lambda e: e.tensor_tensor(out=Mb[:], in0=mk1[:], in1=mk2[:], op=ALU.add), ["mk1", "mk2"], ["Mb"])

        def rout2b(b):
            bb = b % 2
            tok0 = b * BT

            def V(fn, reads, writes):
                P.op("vector", fn, reads=reads, writes=writes)

            for tl in range(4):
                P.op("tensor", lambda e, tl=tl: e.matmul(A0[:, tl * 32:(tl + 1) * 32], lhsT=utri[:], rhs=Mb[:, tl, :], start=True, stop=(tl == 0)),
                     reads=["utri", "Mb"], writes=["A0"])
                for t2 in range(tl):
                    P.op("tensor", lambda e, tl=tl, t2=t2: e.matmul(A0[:, tl * 32:(tl + 1) * 32], lhsT=ones[:], rhs=Mb[:, t2, :], start=False, stop=(t2 == tl - 1)),
                         reads=["ones", "Mb"], writes=["A0"])
            for tl in range(4):
                P.op("tensor", lambda e, tl=tl: e.matmul(A1[:, 0:32], lhsT=ones[:], rhs=Mb[:, tl, :], start=(tl == 0), stop=(tl == 3)),
                     reads=["ones", "Mb"], writes=["A1"])
            V(lambda e: e.tensor_tensor(out=pos[:], in0=A0[:, 0:128].rearrange("p (t n) -> p t n", t=4), in1=bc_mid(cnt[:], 4), op=ALU.add),
              ["A0", "cnt"], ["pos"])
            V(lambda e: e.tensor_tensor(out=cnt[:], in0=cnt[:], in1=A1[:, 0:32], op=ALU.add), ["A1", "cnt", "pos"], ["cnt"])
            V(lambda e: e.tensor_scalar(out=okm[:], in0=pos[:], scalar1=float(CAP), scalar2=None, op0=ALU.is_lt), ["pos"], ["okm"])
            V(lambda e: e.tensor_tensor(out=slot[:], in0=pos[:], in1=bc_mid(ecap[:], 4), op=ALU.add), ["pos", "ecap"], ["slot"])
            V(lambda e: e.tensor_scalar(out=tmp[:], in0=okm[:], scalar1=-1.0e6, scalar2=1.0e6, op0=ALU.mult, op1=ALU.add), ["okm"], ["tmp"])
            V(lambda e: e.tensor_tensor(out=slot[:], in0=slot[:], in1=tmp[:], op=ALU.add), ["slot", "tmp"], ["slot"])
            V(lambda e: e.tensor_scalar(out=slot[:], in0=slot[:], scalar1=float(NSLOT), scalar2=None, op0=ALU.min), ["slot"], ["slot"])
            for k, mk in ((0, mk1), (1, mk2)):
                V(lambda e, mk=mk: e.tensor_tensor(out=tmp[:], in0=mk[:], in1=slot[:], op=ALU.mult), ["mk1", "mk2", "slot"], ["tmp"])
                V(lambda e, k=k: e.tensor_reduce(out=dsel[:, :, k], in_=tmp[:], axis=AX.X, op=ALU.add), ["tmp"], [("dsel", k)])
                V(lambda e, mk=mk: e.tensor_tensor(out=tmp[:], in0=mk[:], in1=okm[:], op=ALU.mult), ["mk1", "mk2", "okm", ("dsel", k)], ["tmp"])
                V(lambda e, k=k: e.tensor_reduce(out=oksel[:, :, k], in_=tmp[:], axis=AX.X, op=ALU.add), ["tmp"], [("oksel", k)])
            tb = 4 * b
            V(lambda e, tb=tb: e.tensor_copy(out=dtab[:, tb:tb + 4, :], in_=dsel[:]), [("dsel", 0), ("dsel", 1)], [("dtab", b)])
            V(lambda e, tb=tb: e.tensor_tensor(out=wtab[:, tb:tb + 4, 0], in0=wA[:], in1=oksel[:, :, 0], op=ALU.mult), ["wA", ("oksel", 0)], [("wtab", b, 0)])
            V(lambda e, tb=tb: e.tensor_tensor(out=wtab[:, tb:tb + 4, 1], in0=wB[:], in1=oksel[:, :, 1], op=ALU.mult), ["wB", ("oksel", 1)], [("wtab", b, 1)])
            for tl in range(4):
                t = 4 * b + tl
                for k in range(2):
                    P.op("gpsimd", lambda e, t=t, k=k, tl=tl, bb=bb: e.indirect_dma_start(
                        out=xs_d[:, :], out_offset=bass.IndirectOffsetOnAxis(ap=dtab[:, t, k:k + 1], axis=0),
                        in_=n2[bb][:, tl, :], in_offset=None),
                        reads=[("n2", bb, tl), ("dtab", b)], writes=[("xs_d", t, k)], dma=True)

        gates2(0)
        for b in range(NB):
            hsec2(b)
            rout2a(b)
            if b + 1 < NB:
                gates2(b + 1)
            rout2b(b)
            if b == NB - 2 and 3 in phases:
                for i in range(2):
                    expert_weight_loads(i, pre3[i]["w1"], pre3[i]["w3"], pre3[i]["w2"], [("w1p", i), ("w3p", i), ("w2p", i)],
                                        extra_writes=WGKEYS + ["wpa", "wpc"])
        if debug:
            P.op("sync", lambda e: e.dma_start(out=dtab_o.ap(), in_=dtab[:].rearrange("p t k -> p (t k)")),
                 reads=[("dtab", b) for b in range(NB)], dma=True, is_out=True)
            P.op("sync", lambda e: e.dma_start(out=wtab_o.ap(), in_=wtab[:].rearrange("p t k -> p (t k)")),
                 reads=[("wtab", b, k) for b in range(NB) for k in range(2)], dma=True, is_out=True)
        P.barrier()
    if 2 in phases:
        phase2()
    sb.reset(base_mark)

    TOP = SBAlloc.HI - 20 * 1024
    p4w = {"wpg": nc.alloc_sbuf_tensor_at("wpg_top", [128, 8, D], BF16, offset=TOP),
           "wpp": nc.alloc_sbuf_tensor_at("wpp_top", [128, 2, D], BF16, offset=TOP + 16 * 1024)}

    def p4_weight_loads():
        wpg_v = wpg_d.ap().rearrange("(c p) n -> p c n", p=128)
        for c in range(0, 8, 4):
            P.op("gpsimd", lambda e, c=c: e.dma_start(out=p4w["wpg"][:, c:c + 4, :], in_=wpg_v[:, c:c + 4, :]), writes=[("wpg", c)], dma=True)
        P.op("gpsimd", lambda e: e.dma_start(out=p4w["wpp"][:], in_=wpp_d.ap().rearrange("(c p) n -> p c n", p=128)), writes=["wpp"], dma=True)

    def phase3():
        NWB = 3
        w1b, w3b, w2b = [], [], []
        for i in range(NWB):
            w1b.append(sb.alloc("w1b%d" % i, [128, 8, 512], BF16))
            w3b.append(sb.alloc("w3b%d" % i, [128, 8, 512], BF16))
            w2b.append(sb.alloc("w2b%d" % i, [128, 4, D], BF16))
        assert sb.off["w1b0"] == BASE and sb.off["w2b1"] == BASE + 24576 + 16384
        preloaded = 2 if 2 in phases else 0
        xr = [sb.alloc("xr%d" % i, [128, 3, D], BF16) for i in range(3)]
        xsT = [sb.alloc("xsT%d" % i, [128, 8, CAP], BF16) for i in range(2)]
        s1 = [sb.alloc("s1%d" % i, [128, CAP], F32) for i in range(2)]
        hdn = [sb.alloc("hdn%d" % i, [128, 4, CAP], BF16) for i in range(2)]
        yb = [sb.alloc("yb%d" % i, [128, D], BF16) for i in range(3)]
        HACC = [(A0, "A0", A1, "A1"), (A2, "A2", B0, ("B0", 0))]
        yctr = [0]
        P.op("gpsimd", lambda e: e.memset(yb[0][:], 0.0), writes=[("yb", 0, 0), ("yb", 0, 1)])
        P.op("sync", lambda e: e.dma_start(out=ys_d.ap()[NSLOT:NSLOT + 128, :], in_=yb[0][:]),
             reads=[("yb", 0, 0), ("yb", 0, 1)], writes=["ys_trash"], dma=True)
        def wload(ex):
            wb_ = ex % NWB
            if ex < preloaded:
                return
            expert_weight_loads(ex, w1b[wb_], w3b[wb_], w2b[wb_], [("w1b", wb_), ("w3b", wb_), ("w2b", wb_)])

        def xsload(ex):
            e3 = ex % 3
            P.op("sync", lambda e, ex=ex, e3=e3: e.dma_start(
                out=xr[e3][:], in_=xs_d.ap()[ex * CAP:(ex + 1) * CAP, :].rearrange("(r p) d -> p r d", p=128)),
                writes=[("xr", e3)], dma=True)

        def xsT_group(ex, r):
            eb = ex % 2
            e3 = ex % 3
            transpose_tile(xr[e3][:, r, :], 8, xsT[eb][:, :, r * 128:(r + 1) * 128], ("xr", e3), ("xsT", eb, r),
                           evac=("scalar" if r % 2 == 0 else "vector"), interleave=8)

        xsload(0)
        wload(0)
        xsload(1)
        wload(1)
        for r in range(3):
            xsT_group(0, r)
        for ex in range(NE):
            eb = ex % 2
            wb_ = ex % NWB
            if ex + 2 < NE:
                wload(ex + 2)
                xsload(ex + 2)
            if ex == 2:
                p4_weight_loads()
            xk = [("xsT", eb, r) for r in range(3)]
            for f in range(4):
                a1, k1, a3, k3 = HACC[f % 2]
                z = f % 2
                for (dst, dkey, wsrc, wkey) in ((a1, k1, w1b, "w1b"), (a3, k3, w3b, "w3b")):
                    for c in range(8):
                        P.op("tensor", lambda e, c=c, dst=dst, wsrc=wsrc, f=f, eb=eb, wb_=wb_: e.matmul(
                            dst[:, 0:CAP], lhsT=wsrc[wb_][:, c, f * 128:(f + 1) * 128], rhs=xsT[eb][:, c, :], start=(c == 0), stop=(c == 7)),
                            reads=[(wkey, wb_)] + xk, writes=[dkey])
                P.op("scalar", lambda e, a1=a1, z=z: e.activation(out=s1[z][:], in_=a1[:, 0:CAP], func=AF.Silu), reads=[k1], writes=[("s1", z)])
                P.op("vector", lambda e, a3=a3, z=z, f=f, eb=eb: e.tensor_tensor(out=hdn[eb][:, f, :], in0=s1[z][:], in1=a3[:, 0:CAP], op=ALU.mult),
                     reads=[("s1", z), k3], writes=[("hdn", eb, f)])
            hk_ = [("hdn", eb, f) for f in range(4)]
            for r in range(3):
                if ex + 1 < NE:
                    xsT_group(ex + 1, r)
                ys_ = yctr[0] % 3
                yctr[0] += 1
                for half in range(2):
                    for f in range(4):
                        P.op("tensor", lambda e, f=f, half=half, r=r, eb=eb, wb_=wb_: e.matmul(
                            B1[:, half * 512:(half + 1) * 512], lhsT=hdn[eb][:, f, r * 128:(r + 1) * 128], rhs=w2b[wb_][:, f, half * 512:(half + 1) * 512],
                            start=(f == 0), stop=(f == 3)),
                            reads=hk_ + [("w2b", wb_)], writes=[("B1", half)])
                    if half == 0:
                        P.op("scalar", lambda e, ys_=ys_: e.activation(out=yb[ys_][:, 0:512], in_=B1[:, 0:512], func=AF.Copy),
                             reads=[("B1", 0)], writes=[("yb", ys_, 0)])
                    else:
                        P.op("vector", lambda e, ys_=ys_: e.tensor_copy(out=yb[ys_][:, 512:1024], in_=B1[:, 512:1024]),
                             reads=[("B1", 1)], writes=[("yb", ys_, 1)])
                row0 = ex * CAP + r * 128
                P.op("sync", lambda e, row0=row0, ys_=ys_: e.dma_start(out=ys_d.ap()[row0:row0 + 128, :], in_=yb[ys_][:]),
                     reads=[("yb", ys_, 0), ("yb", ys_, 1)], writes=[("ys_d", ex, r)], dma=True)
        P.barrier()
    if 3 in phases:
        phase3()
    sb.reset(base_mark)

    def phase4():
        wpg, wpp = p4w["wpg"], p4w["wpp"]
        gple = sb.alloc("gple", [128, D], F32)
        bpg = sb.alloc("bpg", [128, D], F32)
        hb = [sb.alloc("hb%d" % i, [128, D], F32) for i in range(3)]
        y1 = [sb.alloc("y1%d" % i, [128, D], BF16) for i in range(3)]
        y2 = [sb.alloc("y2%d" % i, [128, D], BF16) for i in range(3)]
        pin = [sb.alloc("pin%d" % i, [128, 256], F32) for i in range(3)]
        pbf = [sb.alloc("pbf%d" % i, [128, 256], BF16) for i in range(2)]
        ppT = [sb.alloc("ppT%d" % i, [128, 2, 128], BF16) for i in range(2)]
        n3 = [sb.alloc("n3%d" % i, [128, D], BF16) for i in range(2)]
        n3T = [sb.alloc("n3T%d" % i, [128, 8, 128], BF16) for i in range(2)]
        gz = [sb.alloc("gz%d" % i, [128, D], F32) for i in range(2)]
        ob = [sb.alloc("ob%d" % i, [128, D], F32) for i in range(2)]
        ld("sync", gple[:], gple_d.ap().partition_broadcast(128), "gple")
        ld("sync", bpg[:], bpg_d.ap().partition_broadcast(128), "bpg")
        def loads4(t):
            h3 = t % 3
            P.op("sync", lambda e, t=t, h3=h3: e.dma_start(out=hb[h3][:], in_=h_d.ap()[t * 128:(t + 1) * 128, :]), writes=[("hb", h3)], dma=True)
            P.op("sync", lambda e, t=t, h3=h3: e.dma_start(out=pin[h3][:], in_=p_d.ap()[t * 128:(t + 1) * 128, :]), writes=[("pin", h3)], dma=True)
            for (yy, ykey, k) in ((y1, "y1", 0), (y2, "y2", 1)):
                P.op("gpsimd", lambda e, yy=yy, k=k, t=t, h3=h3: e.indirect_dma_start(
                    out=yy[h3][:, :], out_offset=None, in_=ys_d[:, :], in_offset=bass.IndirectOffsetOnAxis(ap=dtab[:, t, k:k + 1], axis=0)), reads=["dtab_all"], writes=[(ykey, h3)], dma=True)

        def S1a(t):
            h3 = t % 3
            z = t % 2
            P.op("vector", lambda e, h3=h3, t=t: e.scalar_tensor_tensor(out=hb[h3][:], in0=y1[h3][:], scalar=wtab[:, t, 0:1], in1=hb[h3][:],
                                                                       op0=ALU.mult, op1=ALU.add), reads=[("y1", h3), ("hb", h3)], writes=[("hb", h3)])
            P.op("vector", lambda e, h3=h3, t=t: e.scalar_tensor_tensor(out=hb[h3][:], in0=y2[h3][:], scalar=wtab[:, t, 1:2], in1=hb[h3][:],
                                                                       op0=ALU.mult, op1=ALU.add), reads=[("y2", h3), ("hb", h3)], writes=[("hb", h3)])
            rmsnorm_tile(hb[h3][:], gple[:], n3[z][:], ("hb", h3), "gple", ("n3", z))
            P.op("scalar", lambda e, z=z, h3=h3: e.activation(out=pbf[z][:], in_=pin[h3][:], func=AF.Copy), reads=[("pin", h3)], writes=[("pbf", z)])

        def S1b(t):
            z = t % 2
            transpose_tile(n3[z], 8, n3T[z][:], ("n3", z), ("n3T", z))
            transpose_tile(pbf[z], 2, ppT[z][:], ("pbf", z), ("ppT", z))

        GACC = [((A0, "A0"), (A2, "A2")), ((A1, "A1"), (B0, ("B0", 0)))]

        def S2mm(t, half):
            z = t % 2
            (ga, gk), (pa_, pk) = GACC[half]
            for c in range(8):
                P.op("tensor", lambda e, c=c, half=half, ga=ga, z=z: e.matmul(
                    ga[:], lhsT=n3T[z][:, c, :], rhs=wpg[:, c, half * 512:(half + 1) * 512], start=(c == 0), stop=(c == 7)),
                    reads=[("n3T", z), ("wpg", 0), ("wpg", 4)], writes=[gk])
            for c in range(2):
                P.op("tensor", lambda e, c=c, half=half, pa_=pa_, z=z: e.matmul(
                    pa_[:, 0:512], lhsT=ppT[z][:, c, :], rhs=wpp[:, c, half * 512:(half + 1) * 512], start=(c == 0), stop=(c == 1)),
                    reads=[("ppT", z), "wpp"], writes=[pk])

        def S2tail(t):
            z = t % 2
            h3 = t % 3
            hsl = [slice(0, 512), slice(512, 1024)]
            for half in range(2):
                (ga, gk), (pa_, pk) = GACC[half]
                hs = hsl[half]
                P.op("vector", lambda e, ga=ga, z=z, hs=hs: e.tensor_tensor(out=gz[z][:, hs], in0=ga[:], in1=bpg[:, hs], op=ALU.add),
                     reads=[gk, "bpg"], writes=[("gz", z, half)])
                P.op("scalar", lambda e, z=z, hs=hs: e.activation(out=gz[z][:, hs], in_=gz[z][:, hs], func=AF.Tanh, scale=0.5),
                     reads=[("gz", z, half)], writes=[("gz", z, half)])
            for half in range(2):
                (ga, gk), (pa_, pk) = GACC[half]
                hs = hsl[half]
                P.op("vector", lambda e, pa_=pa_, z=z, hs=hs: e.scalar_tensor_tensor(out=gz[z][:, hs], in0=gz[z][:, hs], scalar=1.0, in1=pa_[:, 0:512],
                                                                                    op0=ALU.add, op1=ALU.mult), reads=[("gz", z, half), pk], writes=[("gz", z, half)])
                P.op("vector", lambda e, z=z, hs=hs, h3=h3: e.scalar_tensor_tensor(out=ob[z][:, hs], in0=gz[z][:, hs], scalar=0.5, in1=hb[h3][:, hs],
                                                                                  op0=ALU.mult, op1=ALU.add), reads=[("gz", z, half), ("hb", h3)], writes=[("ob", z, half)])

        loads4(0)
        loads4(1)
        S1a(0)
        S1b(0)
        for t in range(NT):
            z = t % 2
            if t + 2 < NT:
                loads4(t + 2)
            if t + 1 < NT:
                S1a(t + 1)
            S2mm(t, 0)
            S2mm(t, 1)
            S2tail(t)
            if t + 1 < NT:
                S1b(t + 1)
            P.op("sync", lambda e, t=t, z=z: e.dma_start(out=out_d.ap()[t * 128:(t + 1) * 128, :], in_=ob[z][:]),
                 reads=[("ob", z, 0), ("ob", z, 1)], dma=True, is_out=True)
    if 4 in phases:
        phase4()
    P.emit()
    return nc, P


def _sel_tables():
    f = np.arange(128)[:, None, None]
    j = np.arange(8)[None, :, None]
    m = np.arange(128)[None, None, :]
    hit = ((m // 8) == (2 * j + f // 64)).astype(np.float32)
    selA = (hit / 64.0).reshape(128, 8 * 128).astype(ml_dtypes.bfloat16)
    selB = (hit.transpose(2, 1, 0) / 8.0).reshape(128, 8 * 128).astype(ml_dtypes.bfloat16)
    return np.ascontiguousarray(selA), np.ascontiguousarray(selB)


def _host_layout(inp):
    f = lambda a: np.ascontiguousarray(np.asarray(a, dtype=np.float32))
    bf = ml_dtypes.bfloat16
    b_in = f(inp["b_in"])[0]
    rel = f(inp["rel_bias"])[0]
    jj = np.arange(5)[::-1][:, None, None]
    kk = np.arange(128)[None, :, None]
    qq = np.arange(128)[None, None, :]
    dist = qq - kk + 128 * (4 - jj)
    idx = np.clip(dist, -63, 256) + 63
    cdiff = (qq // 64) - (kk // 64) + 2 * (4 - jj)
    mask = ((cdiff >= 0) & (cdiff <= 8)).astype(np.float32)
    rbT = rel[:, idx]
    rbT = np.ascontiguousarray(rbT.transpose(2, 0, 1, 3)).reshape(128, NH * 5 * 128)
    maskT = np.ascontiguousarray(mask.transpose(1, 0, 2)).reshape(128, 5 * 128)
    cwv = f(inp["conv_w"])[0]
    cw = np.ascontiguousarray(cwv.reshape(3, 4, 128).transpose(2, 1, 0)).reshape(128, 12)
    cb = np.ascontiguousarray(f(inp["conv_b"])[0].reshape(4, 128).T)
    gq = f(inp["g_q"])[0]
    gk = f(inp["g_k"])[0]
    shared = {
        "g_mix": f(inp["g_mix"]),
        "w_in": f(inp["w_in"])[0],
        "bcol": np.ascontiguousarray(b_in.reshape(40, 128).T),
        "gqk": np.ascontiguousarray(np.stack([np.tile(gq, 2), np.tile(gk, 2)], axis=1)),
        "bv": np.ascontiguousarray(b_in[1024:1536].reshape(1, 512)),
        "rbT": rbT, "maskT": maskT, "cw": cw, "cb": cb,
        "w_pa": f(inp["w_pa"])[0], "w_pc": f(inp["w_pc"])[0], "w_o": f(inp["w_o"])[0],
        "g_ffn": f(inp["g_ffn"]),
        "w_rg": np.ascontiguousarray(np.concatenate([f(inp["w_group"])[0], f(inp["w_router"])[0]], axis=1)),
        "b_rg": np.ascontiguousarray(np.concatenate([f(inp["b_group"])[0], f(inp["b_router"])[0]])[None, :]),
        "w1": f(inp["w1"])[0], "w3": f(inp["w3"])[0], "w2": f(inp["w2"])[0],
        "g_ple": f(inp["g_ple"]), "w_pg": f(inp["w_ple_gate"])[0], "b_pg": f(inp["b_ple_gate"]),
        "w_pp": f(inp["w_ple_proj"])[0],
        "ident": np.eye(128, dtype=np.float32).astype(bf),
        "utri": np.triu(np.ones((128, 128), np.float32), 1).astype(bf),
        "ones": np.ones((128, 128), np.float32).astype(bf),
        "bdiag": (np.kron(np.eye(2, dtype=np.float32), np.ones((64, 64), np.float32)) / 64.0).astype(bf),
        "ecap": np.ascontiguousarray(np.broadcast_to((np.arange(NE, dtype=np.float32) * CAP)[None, :], (128, NE))),
        "selA": _sel_tables()[0], "selB": _sel_tables()[1],
    }
    x = f(inp["x"])
    p = f(inp["p"])[0]
    maps = []
    for c in range(NCORES):
        m = dict(shared)
        m["x"] = x[c]
        m["p"] = p[c]
        maps.append(m)
    return maps


_CACHE = {}


def kernel(**inputs):
    if "nc" not in _CACHE:
        _CACHE["nc"] = build(debug=False)[0]
    nc = _CACHE["nc"]
    maps = _host_layout(inputs)
    res = run_bass_kernel_spmd(nc, maps, core_ids=list(range(NCORES)))
    out = np.stack([np.asarray(res.results[c]["out"], dtype=np.float32) for c in range(NCORES)], axis=0)
    return out
```

```python
import numpy as np
import ml_dtypes
import concourse.bass as bass
import concourse.mybir as mybir
from concourse.bass_utils import run_bass_kernel_spmd

F32 = mybir.dt.float32
BF16 = mybir.dt.bfloat16
I32 = mybir.dt.int32
ALU = mybir.AluOpType
AF = mybir.ActivationFunctionType
AX = mybir.AxisListType

NCORES = 8
S = 4096
D = 1024
NT = S // 128
BT = 512
NB = S // BT
NH = 8
DH = 64
NE = 32
CAP = 384
NSLOT = NE * CAP
KR = 12
EPS = 1e-6

ENGS = ("sync", "scalar", "vector", "gpsimd", "tensor")
NDMASEM = 24
SAME_ENG_WINDOW = 10 ** 9


class Op:
    __slots__ = ("idx", "eng", "fn", "reads", "writes", "dma", "deps", "sig",
                 "sem", "val", "clock", "epos", "barrier")


class Prog:
    def __init__(self, nc):
        self.nc = nc
        self.ops = []
        self.last_w = {}
        self.readers = {}
        self.out_ops = []
        self.last_barrier = None
        self.since_barrier = []

    def op(self, eng, fn, reads=(), writes=(), dma=False, is_out=False):
        o = Op()
        o.idx = len(self.ops)
        o.eng = eng
        o.fn = fn
        o.dma = dma
        o.barrier = False
        o.reads = tuple(reads)
        o.writes = tuple(writes)
        deps = set()
        for k in o.reads:
            w = self.last_w.get(k)
            if w is not None:
                deps.add(w)
        for k in o.writes:
            w = self.last_w.get(k)
            if w is not None:
                deps.add(w)
            for r in self.readers.get(k, ()):
                deps.add(r)
        for k in o.writes:
            self.last_w[k] = o.idx
            self.readers[k] = []
        for k in o.reads:
            if k not in o.writes:
                self.readers.setdefault(k, []).append(o.idx)
        if self.last_barrier is not None:
            deps.add(self.last_barrier)
        deps.discard(o.idx)
        o.deps = sorted(deps)
        o.sig = False
        self.ops.append(o)
        self.since_barrier.append(o.idx)
        if is_out:
            self.out_ops.append(o.idx)
        return o.idx

    def barrier(self):
        o = Op()
        o.idx = len(self.ops)
        o.eng = "sync"
        o.fn = "BARRIER"
        o.dma = False
        o.barrier = True
        o.reads = ()
        o.writes = ()
        last = {}
        deps = []
        for i in self.since_barrier:
            p = self.ops[i]
            if p.dma:
                deps.append(i)
            else:
                last[p.eng] = i
        deps.extend(last.values())
        if self.last_barrier is not None:
            deps.append(self.last_barrier)
        o.deps = sorted(set(deps))
        o.sig = True
        self.ops.append(o)
        self.last_barrier = o.idx
        self.since_barrier = []
        self.last_w = {}
        self.readers = {}

    def emit(self):
        nc = self.nc
        ops = self.ops
        epos = {e: 0 for e in ENGS}
        for o in ops:
            o.epos = epos[o.eng]
            epos[o.eng] += 1
        fin = Op()
        fin.idx = len(ops)
        fin.eng = "sync"
        fin.fn = None
        fin.dma = False
        fin.barrier = False
        fin.reads = ()
        fin.writes = ()
        fin.deps = list(self.out_ops)
        fin.sig = False
        fin.epos = epos["sync"]
        ops = ops + [fin]
        for o in ops:
            nd = []
            for d in o.deps:
                do = ops[d]
                if do.eng == o.eng and not do.dma and not o.barrier:
                    if o.eng == "tensor" and not o.dma:
                        continue
                    if o.dma:
                        pass
                    elif o.epos - do.epos > SAME_ENG_WINDOW:
                        continue
                nd.append(d)
            o.deps = nd
            for d in nd:
                ops[d].sig = True
        sems = {}
        dma_engs = set(o.eng for o in ops if o.dma)
        for e in ENGS:
            sems[("c", e)] = nc.alloc_semaphore("c_" + e)
            if e in dma_engs:
                for i in range(NDMASEM):
                    sems[("d", e, i)] = nc.alloc_semaphore("d_%s_%d" % (e, i))
        ccount = {e: 0 for e in ENGS}
        dcount = {e: 0 for e in ENGS}
        dma_prev = {}
        for o in ops:
            if o.dma:
                k = dcount[o.eng]
                dcount[o.eng] += 1
                slot = k % NDMASEM
                o.sem = ("d", o.eng, slot)
                o.val = 16 * (k // NDMASEM + 1)
                prev = dma_prev.get((o.eng, slot))
                if prev is not None and prev not in o.deps:
                    o.deps.append(prev)
                dma_prev[(o.eng, slot)] = o.idx
            elif o.sig:
                ccount[o.eng] += 1
                o.sem = ("c", o.eng)
                o.val = ccount[o.eng]
            else:
                o.sem = None
                o.val = 0
        known = {e: {} for e in ENGS}
        streams = {e: [] for e in ENGS}
        for o in ops:
            kn = known[o.eng]
            wm = {}
            for d in sorted(o.deps, reverse=True):
                do = ops[d]
                if kn.get(do.sem, 0) >= do.val:
                    continue
                if wm.get(do.sem, 0) < do.val:
                    wm[do.sem] = do.val
                for s, v in do.clock.items():
                    if kn.get(s, 0) < v:
                        kn[s] = v
            o.clock = dict(kn)
            if o.sem is not None:
                o.clock[o.sem] = o.val
            streams[o.eng].append((o, list(wm.items())))
        self.n_waits = sum(len(w) for st in streams.values() for _, w in st)
        self.counts = (dict(ccount), dict(dcount))

        def run_stream(eng_name):
            def body(eng):
                for o, waits in streams[eng_name]:
                    for s, v in waits:
                        eng.wait_ge(sems[s], v)
                    if o.fn is None:
                        continue
                    if o.barrier:
                        eng.sem_inc(sems[o.sem], 1)
                        continue
                    ins = o.fn(eng)
                    if o.sem is not None:
                        ins.then_inc(sems[o.sem], 16 if o.dma else 1)
            return body

        with nc.Block() as block:
            for e in ENGS:
                if streams[e]:
                    getattr(block, e)(run_stream(e))


class SBAlloc:
    LO = 16512
    HI = 229344

    def __init__(self, nc):
        self.nc = nc
        self.cur = self.LO
        self.n = 0

    def alloc(self, name, shape, dt):
        esz = {F32: 4, BF16: 2, I32: 4}[dt]
        nbytes = esz
        for s in shape[1:]:
            nbytes *= s
        off = (self.cur + 31) // 32 * 32
        assert off + nbytes <= self.HI, "SBUF overflow at %s: need %d have %d" % (name, nbytes, self.HI - off)
        self.n += 1
        t = self.nc.alloc_sbuf_tensor_at("%s_%d" % (name, self.n), list(shape), dt, offset=off)
        self.cur = off + nbytes
        self.off = getattr(self, "off", {})
        self.off[name] = off
        return t

    def alloc_alias(self, name, shape, dt, of):
        self.n += 1
        return self.nc.alloc_sbuf_tensor_at("%s_%d" % (name, self.n), list(shape), dt, offset=self.off[of])

    def mark(self):
        return self.cur

    def reset(self, m):
        self.cur = m


def bc_last(ap, n):
    shp = list(ap.shape)
    return ap.unsqueeze(len(shp)).broadcast_to(shp + [n])


def bc_mid(ap, n):
    shp = list(ap.shape)
    return ap.unsqueeze(1).broadcast_to([shp[0], n] + shp[1:])


def build(debug=False, phases=(1, 2, 3, 4)):
    nc = bass.Bass("TRN2", target_bir_lowering=False)
    P = Prog(nc)
    sb = SBAlloc(nc)

    def din(name, shape, dt=F32):
        return nc.dram_tensor(name, list(shape), dt, kind="ExternalInput")

    def dscr(name, shape, dt):
        return nc.dram_tensor(name, list(shape), dt, kind="ExternalOutput" if debug else "Internal")

    x_d = din("x", [S, D])
    p_d = din("p", [S, 256])
    gmix_d = din("g_mix", [1, D])
    win_d = din("w_in", [D, 5120])
    bcol_d = din("bcol", [128, 40])
    gqk_d = din("gqk", [128, 2])
    bv_d = din("bv", [1, 512])
    rbT_d = din("rbT", [128, NH * 5 * 128])
    maskT_d = din("maskT", [128, 5 * 128])
    cw_d = din("cw", [128, 12])
    cb_d = din("cb", [128, 4])
    wpa_d = din("w_pa", [512, D])
    wpc_d = din("w_pc", [512, D])
    wo_d = din("w_o", [D, D])
    gffn_d = din("g_ffn", [1, D])
    wrg_d = din("w_rg", [D, 36])
    brg_d = din("b_rg", [1, 36])
    w1_d = din("w1", [NE, D, 512])
    w3_d = din("w3", [NE, D, 512])
    w2_d = din("w2", [NE, 512, D])
    gple_d = din("g_ple", [1, D])
    wpg_d = din("w_pg", [D, D])
    bpg_d = din("b_pg", [1, D])
    wpp_d = din("w_pp", [256, D])
    ident_d = din("ident", [128, 128], BF16)
    utri_d = din("utri", [128, 128], BF16)
    ones_d = din("ones", [128, 128], BF16)
    bdiag_d = din("bdiag", [128, 128], BF16)
    ecap_d = din("ecap", [128, NE])
    selA_d = din("selA", [128, 8 * 128], BF16)
    selB_d = din("selB", [128, 8 * 128], BF16)
    out_d = nc.dram_tensor("out", [S, D], F32, kind="ExternalOutput")

    nT_d = dscr("nT_s", [D, S], BF16)
    yaT_d = dscr("yaT_s", [512, S], BF16)
    ycT_d = dscr("ycT_s", [512, S], BF16)
    h_d = dscr("h_s", [S, D], F32)
    xs_d = dscr("xs_s", [NSLOT + 128, D], BF16)
    ys_d = dscr("ys_s", [NSLOT + 128, D], BF16)
    if debug:
        dtab_o = nc.dram_tensor("dtab_o", [128, NT * 2], I32, kind="ExternalOutput")
        wtab_o = nc.dram_tensor("wtab_o", [128, NT * 2], F32, kind="ExternalOutput")

    pT = nc.alloc_psum_tensor("pT", [128, 8, 128], BF16)
    A0 = nc.alloc_psum_tensor("A0", [128, 512], F32)
    A1 = nc.alloc_psum_tensor("A1", [128, 512], F32)
    A2 = nc.alloc_psum_tensor("A2", [128, 512], F32)
    B0 = nc.alloc_psum_tensor("B0", [128, 1024], F32)
    B1 = nc.alloc_psum_tensor("B1", [128, 1024], F32)

    ident = sb.alloc("ident", [128, 128], BF16)
    utri = sb.alloc("utri", [128, 128], BF16)
    ones = sb.alloc("ones", [128, 128], BF16)
    bdiag = sb.alloc("bdiag", [128, 128], BF16)
    ecap = sb.alloc("ecap", [128, NE], F32)
    bcol = sb.alloc("bcol", [128, 40], F32)
    hbcol = sb.alloc("hbcol", [128, 40], F32)
    mhalf = sb.alloc("mhalf", [128, 8], F32)
    epsc = sb.alloc("epsc", [128, 8], F32)
    dtab = sb.alloc("dtab", [128, NT, 2], I32)
    wtab = sb.alloc("wtab", [128, NT, 2], F32)
    cnt = sb.alloc("cnt", [128, NE], F32)
    ss = sb.alloc("ss", [128, 8], F32)
    rs = sb.alloc("rs", [128, 8], F32)
    junk = sb.alloc("junk", [128, D], BF16)

    def ld(eng, dst, src, key):
        P.op(eng, lambda e: e.dma_start(out=dst, in_=src), writes=[key], dma=True)

    ld("sync", ident[:], ident_d.ap(), "ident")
    ld("sync", utri[:], utri_d.ap(), "utri")
    ld("sync", ones[:], ones_d.ap(), "ones")
    ld("sync", bdiag[:], bdiag_d.ap(), "bdiag")
    ld("sync", ecap[:], ecap_d.ap(), "ecap")
    ld("sync", bcol[:], bcol_d.ap(), "bcol")
    P.op("vector", lambda e: e.tensor_scalar(out=hbcol[:], in0=bcol[:], scalar1=0.5, scalar2=None, op0=ALU.mult),
         reads=["bcol"], writes=["hbcol"])
    P.op("gpsimd", lambda e: e.memset(mhalf[:], -0.5), writes=["mhalf"])
    P.op("gpsimd", lambda e: e.memset(epsc[:], EPS), writes=["epsc"])
    P.op("gpsimd", lambda e: e.memset(cnt[:], 0.0), writes=["cnt"])
    P.op("gpsimd", lambda e: e.memset(wtab[:], 0.0), writes=["wtab"])

    nrm_ctr = [0]

    def rmsnorm_tile(src, g_bc, dst_bf, src_key, g_key, dst_key, pool=True):
        i = nrm_ctr[0] % 8
        nrm_ctr[0] += 1
        ssk, rsk = ("ss", i), ("rs", i)
        P.op("scalar", lambda e: e.activation(out=junk[:], in_=src, func=AF.Square, accum_out=ss[:, i:i + 1]),
             reads=[src_key], writes=["junk", ssk])
        P.op("vector", lambda e: e.tensor_scalar(out=rs[:, i:i + 1], in0=ss[:, i:i + 1], scalar1=1.0 / D, scalar2=EPS,
                                                  op0=ALU.mult, op1=ALU.add), reads=[ssk], writes=[rsk])
        if pool:
            P.op("gpsimd", lambda e: e.tensor_tensor(out=rs[:, i:i + 1], in0=rs[:, i:i + 1], in1=mhalf[:, 0:1], op=ALU.pow),
                 reads=[rsk, "mhalf"], writes=[rsk])
        else:
            P.op("scalar", lambda e: e.activation(out=rs[:, i:i + 1], in_=rs[:, i:i + 1], func=AF.Sqrt), reads=[rsk], writes=[rsk])
            P.op("vector", lambda e: e.reciprocal(out=rs[:, i:i + 1], in_=rs[:, i:i + 1]), reads=[rsk], writes=[rsk])
        P.op("vector", lambda e: e.scalar_tensor_tensor(out=dst_bf, in0=src, scalar=rs[:, i:i + 1], in1=g_bc,
                                                         op0=ALU.mult, op1=ALU.mult),
             reads=[src_key, rsk, g_key], writes=[dst_key])

    def transpose_tile(src_bf, nchunk, dst, src_key, dst_key, evac="scalar", interleave=0):
        for c in range(nchunk):
            if interleave:
                src_c = src_bf.rearrange("t (p c) -> t c p", c=interleave)[:, c, :]
            else:
                src_c = src_bf[:, c * 128:(c + 1) * 128]
            P.op("tensor", lambda e, c=c, src_c=src_c: e.transpose(out=pT[:, c, :], in_=src_c, identity=ident[:]),
                 reads=(list(src_key) if isinstance(src_key, list) else [src_key]) + ["ident"], writes=[("pT", c)])
        if evac == "scalar":
            P.op("scalar", lambda e: e.activation(out=dst, in_=pT[:, 0:nchunk, :], func=AF.Copy),
                 reads=[("pT", c) for c in range(nchunk)], writes=[dst_key])
        else:
            P.op("vector", lambda e: e.tensor_copy(out=dst, in_=pT[:, 0:nchunk, :]),
                 reads=[("pT", c) for c in range(nchunk)], writes=[dst_key])

    _breg = {}

    def breg(e):
        if "r" not in _breg:
            _breg["r"] = e.to_reg(NSLOT - 1)
        return _breg["r"]

    base_mark = sb.mark()
    BASE = (base_mark + 31) // 32 * 32
    pre2 = {"Wg": nc.alloc_sbuf_tensor_at("Wg_pre", [128, 8, 2048], BF16, offset=BASE),
            "wpa": nc.alloc_sbuf_tensor_at("wpa_pre", [128, 4, D], BF16, offset=BASE + 32768),
            "wpc": nc.alloc_sbuf_tensor_at("wpc_pre", [128, 4, D], BF16, offset=BASE + 40960)}
    pre3 = [{"w1": nc.alloc_sbuf_tensor_at("w1_pre%d" % i, [128, 8, 512], BF16, offset=BASE + i * 24576),
             "w3": nc.alloc_sbuf_tensor_at("w3_pre%d" % i, [128, 8, 512], BF16, offset=BASE + i * 24576 + 8192),
             "w2": nc.alloc_sbuf_tensor_at("w2_pre%d" % i, [128, 4, D], BF16, offset=BASE + i * 24576 + 16384)} for i in range(2)]
    WGKEYS = [("Wg", g0 + q4 * 256) for q4 in range(4) for g0 in (0, 1024)]

    def p2_weight_loads(Wg, wpa, wpc, extra_writes=()):
        win_v2 = win_d.ap().rearrange("(c p) n -> p c n", p=128)
        ew = list(extra_writes)
        if ew:
            for c in range(0, 8, 2):
                P.op("gpsimd", lambda e, c=c: e.dma_start(out=Wg[:, c:c + 2, :], in_=win_v2[:, c:c + 2, 3072:5120]),
                     writes=WGKEYS + ew, dma=True)
            P.op("gpsimd", lambda e: e.dma_start(out=wpa[:], in_=wpa_d.ap().rearrange("(c p) n -> p c n", p=128)), writes=["wpa"] + ew, dma=True)
            P.op("gpsimd", lambda e: e.dma_start(out=wpc[:], in_=wpc_d.ap().rearrange("(c p) n -> p c n", p=128)), writes=["wpc"] + ew, dma=True)
            return

        def wg_load(q4):
            for g0 in (0, 1024):
                c0_ = g0 + q4 * 256
                P.op("gpsimd", lambda e, c0_=c0_: e.dma_start(out=Wg[:, :, c0_:c0_ + 256], in_=win_v2[:, :, 3072 + c0_:3072 + c0_ + 256]),
                     writes=[("Wg", c0_)] + ew, dma=True)

        wg_load(0)
        P.op("gpsimd", lambda e: e.dma_start(out=wpa[:], in_=wpa_d.ap().rearrange("(c p) n -> p c n", p=128)), writes=["wpa"] + ew, dma=True)
        P.op("gpsimd", lambda e: e.dma_start(out=wpc[:], in_=wpc_d.ap().rearrange("(c p) n -> p c n", p=128)), writes=["wpc"] + ew, dma=True)
        for q4 in range(1, 4):
            wg_load(q4)

    def expert_weight_loads(ex, w1t, w3t, w2t, keys, extra_writes=()):
        ew = list(extra_writes)
        P.op("gpsimd", lambda e: e.dma_start(out=w1t[:], in_=w1_d.ap()[ex].rearrange("(p c) f -> p c f", c=8)), writes=[keys[0]] + ew, dma=True)
        P.op("gpsimd", lambda e: e.dma_start(out=w3t[:], in_=w3_d.ap()[ex].rearrange("(p c) f -> p c f", c=8)), writes=[keys[1]] + ew, dma=True)
        P.op("gpsimd", lambda e: e.dma_start(out=w2t[:], in_=w2_d.ap()[ex].rearrange("(c p) f -> p c f", p=128)), writes=[keys[2]] + ew, dma=True)

    def phase1():
        Wa = sb.alloc("Wa", [128, 8, 3072], BF16)
        gmix = sb.alloc("gmix", [128, D], F32)
        gqk = sb.alloc("gqk", [128, 2], F32)
        bvb = sb.alloc("bvb", [128, 512], F32)
        cw = sb.alloc("cw", [128, 12], F32)
        cb = sb.alloc("cb", [128, 4], F32)
        expB = sb.alloc("expB", [128, NH, 5, 128], BF16)
        maskT = sb.alloc("maskT", [128, 5, 128], F32)
        kring = sb.alloc("kring", [128, 4, KR * 128], BF16)
        vring = sb.alloc("vring", [128, KR, NH, 65], BF16)
        xt = [sb.alloc("xt%d" % i, [128, D], F32) for i in range(2)]
        nb = [sb.alloc("nb%d" % i, [128, D], BF16) for i in range(2)]
        nTb = [sb.alloc("nTb%d" % i, [128, 8, BT], BF16) for i in range(2)]
        qT = [sb.alloc("qT%d" % i, [128, 4, BT], BF16) for i in range(2)]
        zq = [sb.alloc("zq%d" % i, [128, BT], F32) for i in range(8)]
        sq = [sb.alloc("sq%d" % i, [128, BT], BF16) for i in range(3)]
        rs = sb.alloc("rs_all", [128, BT], F32)
        r1b = sb.alloc("r1b", [128, BT], BF16)
        selA = sb.alloc("selA", [128, 8, 128], BF16)
        selB = sb.alloc("selB", [128, 8, 128], BF16)
        gw = sb.alloc("gw", [128, 1], F32)
        us = [sb.alloc("us%d" % i, [128, BT], F32) for i in range(2)]
        t1 = [sb.alloc("t1%d" % i, [128, BT], F32) for i in range(2)]
        cu = sb.alloc("cu", [128, 4, BT + 2], F32)
        ycT = [sb.alloc("ycT%d" % i, [128, 4, BT], BF16) for i in range(2)]
        yaT = [sb.alloc("yaT%d" % i, [128, 4, BT], BF16) for i in range(2)]
        pt = [sb.alloc("pt%d" % i, [128, 4, 128], BF16) for i in range(6)]
        rden = [sb.alloc("rden%d" % i, [128, 4], F32) for i in range(2)]
        ya = sb.alloc("ya", [128, 4, 512], BF16)

        win_v = win_d.ap().rearrange("(c p) n -> p c n", p=128)
        for (c0_, c1_) in ((0, 1024), (1024, 1536), (1536, 3072)):
            for c in range(0, 8, 2):
                P.op("gpsimd", lambda e, c=c, c0_=c0_, c1_=c1_: e.dma_start(out=Wa[:, c:c + 2, c0_:c1_], in_=win_v[:, c:c + 2, c0_:c1_]),
                     writes=[("Wa", c, c0_), ("Wa", c + 1, c0_)], dma=True)
        zt = sb.alloc("zt", [128, 2 * D], BF16)
        P.op("gpsimd", lambda e: e.memset(zt[:], 0.0), writes=["zt"])
        NR = (NSLOT + 128) // 128
        xs_z = xs_d.ap().rearrange("(p r) d -> p (r d)", p=128)
        zchunks = [(r0, min(r0 + 2, NR)) for r0 in range(0, NR, 2)]

        def zero_fill(k):
            for (r0, r1_) in zchunks[k::NB]:
                P.op("sync", lambda e, r0=r0, r1_=r1_: e.dma_start(out=xs_z[:, r0 * D:r1_ * D], in_=zt[:, 0:(r1_ - r0) * D]),
                     reads=["zt"], writes=[("xs_zero", r0)], dma=True)

        ld("sync", gmix[:], gmix_d.ap().partition_broadcast(128), "gmix")
        ld("sync", gqk[:], gqk_d.ap(), "gqk")
        ld("sync", selA[:], selA_d.ap().rearrange("p (j m) -> p j m", j=8), "selA")
        ld("sync", selB[:], selB_d.ap().rearrange("p (j m) -> p j m", j=8), "selB")
        ld("sync", bvb[:], bv_d.ap().partition_broadcast(128), "bvb")
        P.op("vector", lambda e: e.tensor_tensor(out=gw[:], in0=gqk[:, 0:1], in1=gqk[:, 1:2], op=ALU.mult), reads=["gqk"], writes=["gw"])
        ld("sync", cw[:], cw_d.ap(), "cw")
        ld("sync", cb[:], cb_d.ap(), "cb")
        ld("sync", maskT[:], maskT_d.ap().rearrange("p (j q) -> p j q", j=5), "maskT")
        rb_v = rbT_d.ap().rearrange("p (h n) -> p h n", h=NH)
        stg = [sb.alloc_alias("stg0", [128, 640], F32, "zq0"), sb.alloc_alias("stg1", [128, 640], F32, "zq2")]
        for h in range(NH):
            st = stg[h % 2]
            sk = [("zq", 2 * (h % 2)), ("zq", 2 * (h % 2) + 1)]
            P.op("sync", lambda e, h=h, st=st: e.dma_start(out=st[:], in_=rb_v[:, h, :]), writes=sk, dma=True)
            P.op("scalar", lambda e, st=st: e.activation(out=st[:], in_=st[:], func=AF.Exp), reads=sk, writes=sk)
            P.op("vector", lambda e, h=h, st=st: e.tensor_tensor(
                out=expB[:, h, :, :], in0=st[:].rearrange("p (j q) -> p j q", j=5), in1=maskT[:], op=ALU.mult),
                reads=sk + ["maskT"], writes=[("expB", h)])
        P.op("gpsimd", lambda e: e.memset(vring[:], 1.0), writes=[("v", s_) for s_ in range(KR)])
        P.op("gpsimd", lambda e: e.memset(cu[:], 0.0), writes=[("cu", ct) for ct in range(4)] + [("cuh", ct) for ct in range(4)])

        nT_v = nT_d.ap().rearrange("(c p) t -> p c t", p=128)
        yaT_v = yaT_d.ap().rearrange("(c p) t -> p c t", p=128)
        ycT_v = ycT_d.ap().rearrange("(c p) t -> p c t", p=128)
        acc_rot = [0]
        ACC = [(A0, "A0"), (A1, "A1")]

        def next_acc():
            a = ACC[acc_rot[0] % 2]
            acc_rot[0] += 1
            return a

        def A_norm(b, tl):
            t = 4 * b + tl
            s2 = t % 2
            P.op("sync", lambda e, t=t, s2=s2: e.dma_start(out=xt[s2][:], in_=x_d.ap()[t * 128:(t + 1) * 128, :]),
                 writes=[("xt", s2)], dma=True)
            rmsnorm_tile(xt[s2][:], gmix[:], nb[s2][:], ("xt", s2), "gmix", ("nb", s2))

        def A_tr(b, tl):
            t = 4 * b + tl
            s2 = t % 2
            bb = b % 2
            transpose_tile(nb[s2], 8, nTb[bb][:, :, tl * 128:(tl + 1) * 128], ("nb", s2), ("nTb", bb, tl))
            if tl == 3:
                P.op("sync", lambda e, bb=bb, b=b: e.dma_start(out=nT_v[:, :, b * BT:(b + 1) * BT], in_=nTb[bb][:]),
                     reads=[("nTb", bb, q_) for q_ in range(4)], writes=[("nT_d", b)], dma=True)

        def secC(b):
            bb = b % 2
            tok0 = b * BT
            nkeys = [("nTb", bb, tl) for tl in range(4)]
            PACC = [(A0[:], "A0"), (A1[:], "A1"), (B0[:, 0:512], ("B0", 0))]
            prot = [0]

            def nacc():
                a = PACC[prot[0] % 3]
                prot[0] += 1
                return a

            def proj(j):
                acc, akey = nacc()
                for c in range(8):
                    P.op("tensor", lambda e, j=j, c=c, acc=acc: e.matmul(
                        acc, lhsT=Wa[:, c, j * 128:(j + 1) * 128], rhs=nTb[bb][:, c, :], start=(c == 0), stop=(c == 7)),
                        reads=[("Wa", c, 0)] + nkeys, writes=[akey])
                z = j % 3
                P.op("scalar", lambda e, j=j, acc=acc: e.activation(out=zq[j][:], in_=acc, func=AF.Identity, bias=bcol[:, j:j + 1]),
                     reads=[akey, "bcol"], writes=[("zq", j)])
                P.op("gpsimd", lambda e, z=z, j=j: e.tensor_tensor(out=sq[z][:], in0=zq[j][:], in1=zq[j][:], op=ALU.mult),
                     reads=[("zq", j)], writes=[("sq", z)])

            def msacc(j):
                z = j % 3
                P.op("tensor", lambda e, z=z, j=j: e.matmul(A2[:], lhsT=selA[:, j, :], rhs=sq[z][:], start=(j == 0), stop=(j == 7)),
                     reads=[("sq", z), "selA"], writes=["A2"])

            def vproj():
                for tl in range(4):
                    t = 4 * b + tl
                    sl = t % KR
                    acc, akey = nacc()
                    for c in range(8):
                        P.op("tensor", lambda e, c=c, tl=tl, acc=acc: e.matmul(
                            acc, lhsT=nTb[bb][:, c, tl * 128:(tl + 1) * 128], rhs=Wa[:, c, 1024:1536], start=(c == 0), stop=(c == 7)),
                            reads=[("Wa", c, 1024), ("nTb", bb, tl)], writes=[akey])
                    P.op("vector", lambda e, sl=sl, acc=acc: e.tensor_tensor(
                        out=vring[:, sl, :, 0:64], in0=acc.rearrange("p (h d) -> p h d", h=NH),
                        in1=bvb[:].rearrange("p (h d) -> p h d", h=NH), op=ALU.add),
                        reads=[akey, "bvb"], writes=[("v", sl)])

            def fin(j):
                acc, akey = nacc()
                P.op("tensor", lambda e, j=j, acc=acc: e.matmul(acc, lhsT=selB[:, j, :], rhs=r1b[:], start=True, stop=True),
                     reads=["selB", "r1b"], writes=[akey])
                if j < 4:
                    P.op("vector", lambda e, j=j, acc=acc: e.tensor_tensor(out=qT[bb][:, j, :], in0=zq[j][:], in1=acc, op=ALU.mult),
                         reads=[("zq", j), akey], writes=[("qT", bb, j)])
                else:
                    hp = j - 4
                    sl0 = (4 * b) % KR
                    P.op("vector", lambda e, hp=hp, j=j, sl0=sl0, acc=acc: e.scalar_tensor_tensor(
                        out=kring[:, hp, sl0 * 128:(sl0 + 4) * 128], in0=zq[j][:], scalar=gw[:, 0:1], in1=acc, op0=ALU.mult, op1=ALU.mult),
                        reads=[("zq", j), akey, "gw"], writes=[("k", hp, sl0 + q_) for q_ in range(4)])

            proj(0)
            for j in range(8):
                if j + 1 < 8:
                    proj(j + 1)
                msacc(j)
            P.op("scalar", lambda e: e.activation(out=rs[:], in_=A2[:], func=AF.Sqrt, bias=epsc[:, 0:1]), reads=["A2", "epsc"], writes=["rs"])
            P.op("vector", lambda e: e.reciprocal(out=rs[:], in_=rs[:]), reads=["rs"], writes=["rs"])
            P.op("scalar", lambda e: e.activation(out=r1b[:], in_=rs[:], func=AF.Copy), reads=["rs"], writes=["r1b"])
            vproj()
            for j in range(8):
                fin(j)
        def secC3(b):
            bb = b % 2
            tok0 = b * BT
            nkeys = [("nTb", bb, tl) for tl in range(4)]
            SETS = [((A0[:], ["A0"]), (A1[:], ["A1"]), (A2[:], ["A2"])),
                    ((B0[:, 0:512], [("B0", 0)]), (B0[:, 512:1024], [("B0", 4)]), (B1[:, 0:512], [("B1h", 0)]))]
            for ct in range(4):
                z = ct % 2
                (pu, ku), (pb, kb), (pc, kc) = SETS[ct % 2]
                for (dst, dkey, col0) in ((pu, ku, 1536), (pb, kb, 2048), (pc, kc, 2560)):
                    for c in range(8):
                        P.op("tensor", lambda e, c=c, dst=dst, col0=col0, ct=ct: e.matmul(
                            dst, lhsT=Wa[:, c, col0 + ct * 128:col0 + (ct + 1) * 128], rhs=nTb[bb][:, c, :],
                            start=(c == 0), stop=(c == 7)),
                            reads=[("Wa", c, 1536)] + nkeys, writes=dkey)
                ju, jb, jc = 12 + ct, 16 + ct, 20 + ct
                P.op("scalar", lambda e, z=z, ju=ju, pu=pu: e.activation(out=us[z][:], in_=pu, func=AF.Identity, bias=bcol[:, ju:ju + 1]),
                     reads=ku + ["bcol"], writes=[("us", z)])
                P.op("vector", lambda e, z=z, jc=jc, ct=ct, pc=pc: e.scalar_tensor_tensor(
                    out=cu[:, ct, 2:BT + 2], in0=pc, scalar=bcol[:, jc:jc + 1], in1=us[z][:], op0=ALU.add, op1=ALU.mult),
                    reads=kc + ["bcol", ("us", z)], writes=[("cu", ct)])
                P.op("scalar", lambda e, z=z, ct=ct: e.activation(out=t1[z][:], in_=cu[:, ct, 2:BT + 2], func=AF.Identity,
                                                               scale=cw[:, ct * 3 + 2:ct * 3 + 3], bias=cb[:, ct:ct + 1]),
                     reads=[("cu", ct), "cw", "cb"], writes=[("t1", z)])
                P.op("vector", lambda e, z=z, ct=ct: e.scalar_tensor_tensor(
                    out=t1[z][:], in0=cu[:, ct, 1:BT + 1], scalar=cw[:, ct * 3 + 1:ct * 3 + 2], in1=t1[z][:], op0=ALU.mult, op1=ALU.add),
                    reads=[("cu", ct), ("cuh", ct), "cw", ("t1", z)], writes=[("t1", z)])
                P.op("vector", lambda e, z=z, ct=ct: e.scalar_tensor_tensor(
                    out=t1[z][:], in0=cu[:, ct, 0:BT], scalar=cw[:, ct * 3:ct * 3 + 1], in1=t1[z][:], op0=ALU.mult, op1=ALU.add),
                    reads=[("cu", ct), ("cuh", ct), "cw", ("t1", z)], writes=[("t1", z)])
                P.op("vector", lambda e, z=z, jb=jb, ct=ct, pb=pb: e.scalar_tensor_tensor(
                    out=ycT[bb][:, ct, :], in0=pb, scalar=bcol[:, jb:jb + 1], in1=t1[z][:], op0=ALU.add, op1=ALU.mult),
                    reads=kb + ["bcol", ("t1", z)], writes=[("ycT", bb, ct)])
                P.op("vector", lambda e, ct=ct: e.tensor_copy(out=cu[:, ct, 0:2], in_=cu[:, ct, BT:BT + 2]),
                     reads=[("cu", ct)], writes=[("cuh", ct)])
            P.op("sync", lambda e, tok0=tok0: e.dma_start(out=ycT_v[:, :, tok0:tok0 + BT], in_=ycT[bb][:]),
                 reads=[("ycT", bb, ct) for ct in range(4)], writes=[("ycT_d", b)], dma=True)

        def secD(b):
            bb = b % 2
            tok0 = b * BT
            nxt = b + 1 < NB
            units = []
            for h in range(NH):
                for m in range(8):
                    kt = 4 * b - 4 + m
                    if kt < 0:
                        continue
                    units.append((h, m, kt, max(m - 4, 0), min(m, 3)))
            SPS = [(A0[:], "A0"), (A1[:], "A1"), (A2[:], "A2"), (B0[:, 0:512], ("B0", 0)), (B0[:, 512:1024], ("B0", 4))]
            LA = 4

            def QKEXP(u):
                h, m, kt, tlo, thi = units[u]
                hp, r0 = h // 2, (h % 2) * 64
                nq = thi - tlo + 1
                sp, sk = SPS[u % 5]
                pz = u % 6
                sl = kt % KR
                P.op("tensor", lambda e, hp=hp, r0=r0, sl=sl, sp=sp, tlo=tlo, thi=thi: e.matmul(
                    sp[:, 0:(thi - tlo + 1) * 128], lhsT=kring[r0:r0 + 64, hp, sl * 128:(sl + 1) * 128],
                    rhs=qT[bb][r0:r0 + 64, hp, tlo * 128:(thi + 1) * 128], start=True, stop=True),
                    reads=[("k", hp, sl), ("qT", bb, hp)], writes=[sk])
                P.op("scalar", lambda e, pz=pz, sp=sp, nq=nq: e.activation(
                    out=pt[pz][:, 0:nq, :], in_=sp[:, 0:nq * 128].rearrange("p (j q) -> p j q", q=128), func=AF.Exp, scale=DH ** -0.5),
                    reads=[sk], writes=[("pt", pz)])
                rlo = 4 - m + tlo
                P.op("vector", lambda e, pz=pz, nq=nq, h=h, rlo=rlo: e.tensor_tensor(
                    out=pt[pz][:, 0:nq, :], in0=pt[pz][:, 0:nq, :], in1=expB[:, h, rlo:rlo + nq, :], op=ALU.mult),
                    reads=[("pt", pz), ("expB", h)], writes=[("pt", pz)])

            def PV(u):
                h, m, kt, tlo, thi = units[u]
                pz = u % 6
                sl = kt % KR
                hb2 = h % 2
                first = (u == 0) or units[u - 1][0] != h
                last_u = (u + 1 == len(units)) or units[u + 1][0] != h
                if first:
                    P.op("tensor", lambda e, hb2=hb2: e.matmul(
                        B1[:, hb2 * 512:hb2 * 512 + 260], lhsT=zt[:, 0:128], rhs=zt[:, 0:260], start=True, stop=False),
                        reads=["zt"], writes=[("B1h", hb2)])
                for tl in range(tlo, thi + 1):
                    c0 = hb2 * 512 + tl * 65
                    P.op("tensor", lambda e, h=h, tl=tl, tlo=tlo, sl=sl, pz=pz, c0=c0, fin=(last_u and tl == thi): e.matmul(
                        B1[:, c0:c0 + 65], lhsT=pt[pz][:, tl - tlo, :], rhs=vring[:, sl, h, :],
                        start=False, stop=fin),
                        reads=[("pt", pz), ("v", sl)], writes=[("B1h", hb2)])

            def FINH(h):
                hb2 = h % 2
                Bv = B1[:, hb2 * 512:hb2 * 512 + 260].rearrange("p (t d) -> p t d", d=65)
                P.op("vector", lambda e, hb2=hb2, Bv=Bv: e.reciprocal(out=rden[hb2][:], in_=Bv[:, :, 64]),
                     reads=[("B1h", hb2)], writes=[("rden", hb2)])
                P.op("vector", lambda e, hb2=hb2, Bv=Bv, h=h: e.tensor_tensor(
                    out=ya[:, :, h * 64:(h + 1) * 64], in0=Bv[:, :, 0:64], in1=bc_last(rden[hb2][:], 64), op=ALU.mult),
                    reads=[("B1h", hb2), ("rden", hb2)], writes=[("ya", h)])

            if nxt:
                A_norm(b + 1, 0)
            for u in range(min(LA, len(units))):
                QKEXP(u)
            secC3(b)
            zero_fill(b)
            if b == NB - 1 and 2 in phases:
                p2_weight_loads(pre2["Wg"], pre2["wpa"], pre2["wpc"],
                                extra_writes=[("Wa", c, c0_) for c in range(8) for c0_ in (0, 1024, 1536)])
            for u in range(len(units)):
                if u + LA < len(units):
                    QKEXP(u + LA)
                PV(u)
                h = units[u][0]
                if u + 1 == len(units) or units[u + 1][0] != h:
                    FINH(h)
                    if nxt and h % 2 == 1:
                        tl = h // 2
                        A_tr(b + 1, tl)
                        if tl + 1 < 4:
                            A_norm(b + 1, tl + 1)
            for tl in range(4):
                transpose_tile(ya[:, tl, :], 4, yaT[bb][:, :, tl * 128:(tl + 1) * 128], [("ya", h) for h in range(NH)], ("yaT", bb, tl))
            P.op("sync", lambda e, tok0=tok0: e.dma_start(out=yaT_v[:, :, tok0:tok0 + BT], in_=yaT[bb][:]),
                 reads=[("yaT", bb, tl) for tl in range(4)], writes=[("yaT_d", b)], dma=True)

        for tl in range(4):
            A_norm(0, tl)
            A_tr(0, tl)
        for b in range(NB):
            secC(b)
            secD(b)
        P.barrier()
    if 1 in phases:
        phase1()
    sb.reset(base_mark)

    def phase2():
        Wg = sb.alloc("Wg", [128, 8, 2048], BF16)
        wpa = sb.alloc("wpa", [128, 4, D], BF16)
        wpc = sb.alloc("wpc", [128, 4, D], BF16)
        wo = sb.alloc("wo", [128, 8, D], BF16)
        wrg = sb.alloc("wrg", [128, 8, 36], BF16)
        brg = sb.alloc("brg", [128, 36], F32)
        gffn = sb.alloc("gffn", [128, D], F32)
        nTb = [sb.alloc("nTb%d" % i, [128, 8, BT], BF16) for i in range(2)]
        yaT = [sb.alloc("yaT%d" % i, [128, 4, BT], BF16) for i in range(2)]
        ycT = [sb.alloc("ycT%d" % i, [128, 4, BT], BF16) for i in range(2)]
        xt = [sb.alloc("xt%d" % i, [128, D], F32) for i in range(4)]
        tA = [sb.alloc("tA%d" % i, [128, BT], F32) for i in range(2)]
        tC = [sb.alloc("tC%d" % i, [128, BT], F32) for i in range(2)]
        mA = [sb.alloc("mA%d" % i, [128, BT], F32) for i in range(2)]
        mC = [sb.alloc("mC%d" % i, [128, BT], F32) for i in range(2)]
        mT = [sb.alloc("mT%d" % i, [128, 8, BT], BF16) for i in range(2)]
        ht = [sb.alloc("ht%d" % i, [128, D], F32) for i in range(3)]
        n2 = [sb.alloc("n2%d" % i, [128, 4, D], BF16) for i in range(2)]
        n2T = [sb.alloc("n2T%d" % i, [128, 8, 128], BF16) for i in range(2)]
        lg = sb.alloc("lg", [128, 4, 36], F32)
        gmax = sb.alloc("gmax", [128, 4], F32)
        gmask = sb.alloc("gmask", [128, 4, 4], F32)
        gex = sb.alloc("gex", [128, 4, 4], F32)
        gse = sb.alloc("gse", [128, 4], F32)
        pen = sb.alloc("pen", [128, 4, 4], F32)
        elm = sb.alloc("elm", [128, 4, 32], F32)
        elm2 = sb.alloc("elm2", [128, 4, 32], F32)
        m1 = sb.alloc("m1", [128, 4], F32)
        m2 = sb.alloc("m2", [128, 4], F32)
        mk1 = sb.alloc("mk1", [128, 4, 32], F32)
        mk2 = sb.alloc("mk2", [128, 4, 32], F32)
        Mb = sb.alloc("Mb", [128, 4, 32], BF16)
        dd = sb.alloc("dd", [128, 4], F32)
        ee = sb.alloc("ee", [128, 4], F32)
        rr = sb.alloc("rr", [128, 4], F32)
        wA = sb.alloc("wA", [128, 4], F32)
        wB = sb.alloc("wB", [128, 4], F32)
        pos = sb.alloc("pos", [128, 4, 32], F32)
        okm = sb.alloc("okm", [128, 4, 32], F32)
        slot = sb.alloc("slot", [128, 4, 32], F32)
        tmp = sb.alloc("tmp", [128, 4, 32], F32)
        dsel = sb.alloc("dsel", [128, 4, 2], F32)
        oksel = sb.alloc("oksel", [128, 4, 2], F32)

        assert sb.off["Wg"] == BASE and sb.off["wpa"] == BASE + 32768 and sb.off["wpc"] == BASE + 40960
        if 1 not in phases:
            p2_weight_loads(Wg, wpa, wpc)
        wo_v = wo_d.ap().rearrange("(c p) n -> p c n", p=128)
        for c in range(0, 8, 4):
            P.op("gpsimd", lambda e, c=c: e.dma_start(out=wo[:, c:c + 4, :], in_=wo_v[:, c:c + 4, :]), writes=[("wo", c)], dma=True)
        P.op("gpsimd", lambda e: e.dma_start(out=wrg[:], in_=wrg_d.ap().rearrange("(c p) n -> p c n", p=128)), writes=["wrg"], dma=True)
        ld("sync", brg[:], brg_d.ap().partition_broadcast(128), "brg")
        ld("sync", gffn[:], gffn_d.ap().partition_broadcast(128), "gffn")

        nT_v = nT_d.ap().rearrange("(c p) t -> p c t", p=128)
        yaT_v = yaT_d.ap().rearrange("(c p) t -> p c t", p=128)
        ycT_v = ycT_d.ap().rearrange("(c p) t -> p c t", p=128)
        wokeys = [("wo", 0), ("wo", 4)]
        xctr = [0]
        hctr = [0]

        def loads2(b):
            bb = b % 2
            tok0 = b * BT
            P.op("sync", lambda e, bb=bb, tok0=tok0: e.dma_start(out=nTb[bb][:], in_=nT_v[:, :, tok0:tok0 + BT]), writes=[("nTb", bb)], dma=True)
            P.op("sync", lambda e, bb=bb, tok0=tok0: e.dma_start(out=yaT[bb][:], in_=yaT_v[:, :, tok0:tok0 + BT]), writes=[("yaT", bb)], dma=True)
            P.op("sync", lambda e, bb=bb, tok0=tok0: e.dma_start(out=ycT[bb][:], in_=ycT_v[:, :, tok0:tok0 + BT]), writes=[("ycT", bb)], dma=True)

        def xload(t):
            P.op("sync", lambda e, t=t: e.dma_start(out=xt[t % 4][:], in_=x_d.ap()[t * 128:(t + 1) * 128, :]), writes=[("xt", t % 4)], dma=True)

        loads2(0)
        for t_ in range(3):
            xload(t_)
        def gates2(b):
            bb = b % 2
            tok0 = b * BT
            if b + 1 < NB:
                loads2(b + 1)
            for j in range(8):
                z = j % 2
                for (dst, dkey, col0) in ((A0, "A0", 0), (A1, "A1", 1024)):
                    for c in range(8):
                        P.op("tensor", lambda e, c=c, dst=dst, col0=col0, j=j, bb=bb: e.matmul(
                            dst[:], lhsT=Wg[:, c, col0 + j * 128:col0 + (j + 1) * 128], rhs=nTb[bb][:, c, :], start=(c == 0), stop=(c == 7)),
                            reads=[("Wg", col0 + (j // 2) * 256), ("nTb", bb)], writes=[dkey])
                for (dst, dkey, wsrc, wkey, asrc, akey) in ((A2, "A2", wpa, "wpa", yaT, "yaT"), (B0, ("B0", 0), wpc, "wpc", ycT, "ycT")):
                    for c in range(4):
                        P.op("tensor", lambda e, c=c, dst=dst, wsrc=wsrc, asrc=asrc, j=j, bb=bb: e.matmul(
                            dst[:, 0:512], lhsT=wsrc[:, c, j * 128:(j + 1) * 128], rhs=asrc[bb][:, c, :], start=(c == 0), stop=(c == 3)),
                            reads=[wkey, (akey, bb)], writes=[dkey])
                P.op("scalar", lambda e, z=z, j=j: e.activation(out=tA[z][:], in_=A0[:], func=AF.Tanh, scale=0.5, bias=hbcol[:, 24 + j:25 + j]),
                     reads=["A0", "hbcol"], writes=[("tA", z)])
                P.op("scalar", lambda e, z=z, j=j: e.activation(out=tC[z][:], in_=A1[:], func=AF.Tanh, scale=0.5, bias=hbcol[:, 32 + j:33 + j]),
                     reads=["A1", "hbcol"], writes=[("tC", z)])
                P.op("vector", lambda e, z=z: e.scalar_tensor_tensor(out=mA[z][:], in0=tA[z][:], scalar=1.0, in1=A2[:], op0=ALU.add, op1=ALU.mult),
                     reads=[("tA", z), "A2"], writes=[("mA", z)])
                P.op("vector", lambda e, z=z: e.scalar_tensor_tensor(out=mC[z][:], in0=tC[z][:], scalar=1.0, in1=B0[:, 0:512], op0=ALU.add, op1=ALU.mult),
                     reads=[("tC", z), ("B0", 0)], writes=[("mC", z)])
                P.op("vector", lambda e, z=z, j=j, bb=bb: e.tensor_tensor(out=mT[bb][:, j, :], in0=mA[z][:], in1=mC[z][:], op=ALU.add),
                     reads=[("mA", z), ("mC", z)], writes=[("mT", bb, j)])

        def hsec2(b):
            bb = b % 2
            tok0 = b * BT
            mkeys = [("mT", bb, j) for j in range(8)]
            def hmm(tl):
                t = 4 * b + tl
                xs_ = t % 4
                hs_ = t % 3
                if t + 3 < NT:
                    xload(t + 3)
                HB = [(B1[:, 0:512], ("B1", 0)), (B1[:, 512:1024], ("B1", 1))] if tl % 2 == 0 else [(A0[:], "A0"), (A1[:], "A1")]
                for half in range(2):
                    hacc, hkey = HB[half]
                    for j in range(8):
                        P.op("tensor", lambda e, j=j, half=half, tl=tl, bb=bb, hacc=hacc: e.matmul(
                            hacc, lhsT=mT[bb][:, j, tl * 128:(tl + 1) * 128], rhs=wo[:, j, half * 512:(half + 1) * 512],
                            start=(j == 0), stop=(j == 7)),
                            reads=mkeys + wokeys, writes=[hkey])
                    P.op("vector", lambda e, half=half, xs_=xs_, hs_=hs_, hacc=hacc: e.scalar_tensor_tensor(
                        out=ht[hs_][:, half * 512:(half + 1) * 512], in0=hacc, scalar=0.5,
                        in1=xt[xs_][:, half * 512:(half + 1) * 512], op0=ALU.mult, op1=ALU.add),
                        reads=[hkey, ("xt", xs_)] + ([("ht", hs_)] if half == 1 else []), writes=[("ht", hs_)])
                P.op("sync", lambda e, t=t, hs_=hs_: e.dma_start(out=h_d.ap()[t * 128:(t + 1) * 128, :], in_=ht[hs_][:]),
                     reads=[("ht", hs_)], writes=[("h_d", t)], dma=True)

            def hnorm(tl):
                t = 4 * b + tl
                hs_ = t % 3
                rmsnorm_tile(ht[hs_][:], gffn[:], n2[bb][:, tl, :], ("ht", hs_), "gffn", ("n2", bb, tl),
                             pool=not (b == NB - 1 and 3 in phases))

            def htr(tl):
                t = 4 * b + tl
                z2 = t % 2
                transpose_tile(n2[bb][:, tl, :], 8, n2T[z2][:], ("n2", bb, tl), ("n2T", z2))
                for c in range(8):
                    P.op("tensor", lambda e, c=c, tl=tl, z2=z2: e.matmul(
                        B0[:, 512 + tl * 36:512 + (tl + 1) * 36], lhsT=n2T[z2][:, c, :], rhs=wrg[:, c, :], start=(c == 0), stop=(c == 7)),
                        reads=[("n2T", z2), "wrg"], writes=[("lgp", tl)])

            hmm(0)
            hnorm(0)
            for tl in range(4):
                if tl + 1 < 4:
                    hmm(tl + 1)
                htr(tl)
                if tl + 1 < 4:
                    hnorm(tl + 1)

        def rout2a(b):
            bb = b % 2
            tok0 = b * BT
            lgp = B0[:, 512:512 + 144].rearrange("p (t n) -> p t n", t=4)
            R = []

            def V(fn, reads, writes):
                P.op("vector", fn, reads=reads, writes=writes)

            V(lambda e: e.tensor_tensor(out=lg[:], in0=lgp, in1=bc_mid(brg[:], 4), op=ALU.add),
              [("lgp", tl) for tl in range(4)] + ["brg"], ["lg"])
            V(lambda e: e.tensor_reduce(out=gmax[:], in_=lg[:, :, 0:4], axis=AX.X, op=ALU.max), ["lg"], ["gmax"])
            V(lambda e: e.tensor_tensor(out=gmask[:], in0=lg[:, :, 0:4], in1=bc_last(gmax[:], 4), op=ALU.is_equal), ["lg", "gmax"], ["gmask"])
            V(lambda e: e.tensor_tensor(out=gex[:], in0=lg[:, :, 0:4], in1=bc_last(gmax[:], 4), op=ALU.subtract), ["lg", "gmax"], ["gex"])
            P.op("scalar", lambda e: e.activation(out=gex[:], in_=gex[:], func=AF.Exp), reads=["gex"], writes=["gex"])
            V(lambda e: e.tensor_reduce(out=gse[:], in_=gex[:], axis=AX.X, op=ALU.add), ["gex"], ["gse"])
            V(lambda e: e.reciprocal(out=gse[:], in_=gse[:]), ["gse"], ["gse"])
            V(lambda e: e.tensor_scalar(out=pen[:], in0=gmask[:], scalar1=1.0, scalar2=1e30, op0=ALU.subtract, op1=ALU.mult), ["gmask"], ["pen"])
            V(lambda e: e.tensor_tensor(out=elm[:].rearrange("p t (g k) -> p t g k", g=4),
                                        in0=lg[:, :, 4:36].rearrange("p t (g k) -> p t g k", g=4),
                                        in1=bc_last(pen[:], 8), op=ALU.add), ["lg", "pen"], ["elm"])
            V(lambda e: e.tensor_reduce(out=m1[:], in_=elm[:], axis=AX.X, op=ALU.max), ["elm"], ["m1"])
            V(lambda e: e.tensor_tensor(out=mk1[:], in0=elm[:], in1=bc_last(m1[:], 32), op=ALU.is_equal), ["elm", "m1"], ["mk1"])
            V(lambda e: e.scalar_tensor_tensor(out=elm2[:], in0=mk1[:], scalar=-1e30, in1=elm[:], op0=ALU.mult, op1=ALU.add), ["mk1", "elm"], ["elm2"])
            V(lambda e: e.tensor_reduce(out=m2[:], in_=elm2[:], axis=AX.X, op=ALU.max), ["elm2"], ["m2"])
            V(lambda e: e.tensor_tensor(out=mk2[:], in0=elm2[:], in1=bc_last(m2[:], 32), op=ALU.is_equal), ["elm2", "m2"], ["mk2"])
            V(lambda e: e.tensor_tensor(out=dd[:], in0=m2[:], in1=m1[:], op=ALU.subtract), ["m1", "m2"], ["dd"])
            P.op("scalar", lambda e: e.activation(out=ee[:], in_=dd[:], func=AF.Exp), reads=["dd"], writes=["ee"])
            V(lambda e: e.tensor_scalar(out=rr[:], in0=ee[:], scalar1=1.0, scalar2=None, op0=ALU.add), ["ee"], ["rr"])
            V(lambda e: e.reciprocal(out=rr[:], in_=rr[:]), ["rr"], ["rr"])
            V(lambda e: e.tensor_tensor(out=wA[:], in0=gse[:], in1=rr[:], op=ALU.mult), ["gse", "rr"], ["wA"])
            V(lambda e: e.tensor_tensor(out=wB[:], in0=wA[:], in1=ee[:], op=ALU.mult), ["wA", "ee"], ["wB"])
            V(lambda e: e.tensor_tensor(out=Mb[:], in0=mk1[:], in1=mk2[:], op=ALU.add), ["mk1", "mk2"], ["Mb"])

        def rout2b(b):
            bb = b % 2
            tok0 = b * BT

            def V(fn, reads, writes):
                P.op("vector", fn, reads=reads, writes=writes)

            for tl in range(4):
                P.op("tensor", lambda e, tl=tl: e.matmul(A0[:, tl * 32:(tl + 1) * 32], lhsT=utri[:], rhs=Mb[:, tl, :], start=True, stop=(tl == 0)),
                     reads=["utri", "Mb"], writes=["A0"])
                for t2 in range(tl):
                    P.op("tensor", lambda e, tl=tl, t2=t2: e.matmul(A0[:, tl * 32:(tl + 1) * 32], lhsT=ones[:], rhs=Mb[:, t2, :], start=False, stop=(t2 == tl - 1)),
                         reads=["ones", "Mb"], writes=["A0"])
            for tl in range(4):
                P.op("tensor", lambda e, tl=tl: e.matmul(A1[:, 0:32], lhsT=ones[:], rhs=Mb[:, tl, :], start=(tl == 0), stop=(tl == 3)),
                     reads=["ones", "Mb"], writes=["A1"])
            V(lambda e: e.tensor_tensor(out=pos[:], in0=A0[:, 0:128].rearrange("p (t n) -> p t n", t=4), in1=bc_mid(cnt[:], 4), op=ALU.add),
              ["A0", "cnt"], ["pos"])
            V(lambda e: e.tensor_tensor(out=cnt[:], in0=cnt[:], in1=A1[:, 0:32], op=ALU.add), ["A1", "cnt", "pos"], ["cnt"])
            V(lambda e: e.tensor_scalar(out=okm[:], in0=pos[:], scalar1=float(CAP), scalar2=None, op0=ALU.is_lt), ["pos"], ["okm"])
            V(lambda e: e.tensor_tensor(out=slot[:], in0=pos[:], in1=bc_mid(ecap[:], 4), op=ALU.add), ["pos", "ecap"], ["slot"])
            V(lambda e: e.tensor_scalar(out=tmp[:], in0=okm[:], scalar1=-1.0e6, scalar2=1.0e6, op0=ALU.mult, op1=ALU.add), ["okm"], ["tmp"])
            V(lambda e: e.tensor_tensor(out=slot[:], in0=slot[:], in1=tmp[:], op=ALU.add), ["slot", "tmp"], ["slot"])
            V(lambda e: e.tensor_scalar(out=slot[:], in0=slot[:], scalar1=float(NSLOT), scalar2=None, op0=ALU.min), ["slot"], ["slot"])
            for k, mk in ((0, mk1), (1, mk2)):
                V(lambda e, mk=mk: e.tensor_tensor(out=tmp[:], in0=mk[:], in1=slot[:], op=ALU.mult), ["mk1", "mk2", "slot"], ["tmp"])
                V(lambda e, k=k: e.tensor_reduce(out=dsel[:, :, k], in_=tmp[:], axis=AX.X, op=ALU.add), ["tmp"], [("dsel", k)])
                V(lambda e, mk=mk: e.tensor_tensor(out=tmp[:], in0=mk[:], in1=okm[:], op=ALU.mult), ["mk1", "mk2", "okm", ("dsel", k)], ["tmp"])
                V(lambda e, k=k: e.tensor_reduce(out=oksel[:, :, k], in_=tmp[:], axis=AX.X, op=ALU.add), ["tmp"], [("oksel", k)])
            tb = 4 * b
            V(lambda e, tb=tb: e.tensor_copy(out=dtab[:, tb:tb + 4, :], in_=dsel[:]), [("dsel", 0), ("dsel", 1)], [("dtab", b)])
            V(lambda e, tb=tb: e.tensor_tensor(out=wtab[:, tb:tb + 4, 0], in0=wA[:], in1=oksel[:, :, 0], op=ALU.mult), ["wA", ("oksel", 0)], [("wtab", b, 0)])
            V(lambda e, tb=tb: e.tensor_tensor(out=wtab[:, tb:tb + 4, 1], in0=wB[:], in1=oksel[:, :, 1], op=ALU.mult), ["wB", ("oksel", 1)], [("wtab", b, 1)])
            for tl in range(4):
                t = 4 * b + tl
                for k in range(2):
                    P.op("gpsimd", lambda e, t=t, k=k, tl=tl, bb=bb: e.indirect_dma_start(
                        out=xs_d[:, :], out_offset=bass.IndirectOffsetOnAxis(ap=dtab[:, t, k:k + 1], axis=0),
                        in_=n2[bb][:, tl, :], in_offset=None),
                        reads=[("n2", bb, tl), ("dtab", b)], writes=[("xs_d", t, k)], dma=True)

        gates2(0)
        for b in range(NB):
            hsec2(b)
            rout2a(b)
            if b + 1 < NB:
                gates2(b + 1)
            rout2b(b)
            if b == NB - 2 and 3 in phases:
                for i in range(2):
                    expert_weight_loads(i, pre3[i]["w1"], pre3[i]["w3"], pre3[i]["w2"], [("w1p", i), ("w3p", i), ("w2p", i)],
                                        extra_writes=WGKEYS + ["wpa", "wpc"])
        if debug:
            P.op("sync", lambda e: e.dma_start(out=dtab_o.ap(), in_=dtab[:].rearrange("p t k -> p (t k)")),
                 reads=[("dtab", b) for b in range(NB)], dma=True, is_out=True)
            P.op("sync", lambda e: e.dma_start(out=wtab_o.ap(), in_=wtab[:].rearrange("p t k -> p (t k)")),
                 reads=[("wtab", b, k) for b in range(NB) for k in range(2)], dma=True, is_out=True)
        P.barrier()
    if 2 in phases:
        phase2()
    sb.reset(base_mark)

    TOP = SBAlloc.HI - 20 * 1024
    p4w = {"wpg": nc.alloc_sbuf_tensor_at("wpg_top", [128, 8, D], BF16, offset=TOP),
           "wpp": nc.alloc_sbuf_tensor_at("wpp_top", [128, 2, D], BF16, offset=TOP + 16 * 1024)}

    def p4_weight_loads():
        wpg_v = wpg_d.ap().rearrange("(c p) n -> p c n", p=128)
        for c in range(0, 8, 4):
            P.op("gpsimd", lambda e, c=c: e.dma_start(out=p4w["wpg"][:, c:c + 4, :], in_=wpg_v[:, c:c + 4, :]), writes=[("wpg", c)], dma=True)
        P.op("gpsimd", lambda e: e.dma_start(out=p4w["wpp"][:], in_=wpp_d.ap().rearrange("(c p) n -> p c n", p=128)), writes=["wpp"], dma=True)

    def phase3():
        NWB = 3
        w1b, w3b, w2b = [], [], []
        for i in range(NWB):
            w1b.append(sb.alloc("w1b%d" % i, [128, 8, 512], BF16))
            w3b.append(sb.alloc("w3b%d" % i, [128, 8, 512], BF16))
            w2b.append(sb.alloc("w2b%d" % i, [128, 4, D], BF16))
        assert sb.off["w1b0"] == BASE and sb.off["w2b1"] == BASE + 24576 + 16384
        preloaded = 2 if 2 in phases else 0
        xr = [sb.alloc("xr%d" % i, [128, 3, D], BF16) for i in range(3)]
        xsT = [sb.alloc("xsT%d" % i, [128, 8, CAP], BF16) for i in range(2)]
        s1 = [sb.alloc("s1%d" % i, [128, CAP], F32) for i in range(2)]
        hdn = [sb.alloc("hdn%d" % i, [128, 4, CAP], BF16) for i in range(2)]
        yb = [sb.alloc("yb%d" % i, [128, D], BF16) for i in range(3)]
        HACC = [(A0, "A0", A1, "A1"), (A2, "A2", B0, ("B0", 0))]
        yctr = [0]
        P.op("gpsimd", lambda e: e.memset(yb[0][:], 0.0), writes=[("yb", 0, 0), ("yb", 0, 1)])
        P.op("sync", lambda e: e.dma_start(out=ys_d.ap()[NSLOT:NSLOT + 128, :], in_=yb[0][:]),
             reads=[("yb", 0, 0), ("yb", 0, 1)], writes=["ys_trash"], dma=True)
        def wload(ex):
            wb_ = ex % NWB
            if ex < preloaded:
                return
            expert_weight_loads(ex, w1b[wb_], w3b[wb_], w2b[wb_], [("w1b", wb_), ("w3b", wb_), ("w2b", wb_)])

        def xsload(ex):
            e3 = ex % 3
            P.op("sync", lambda e, ex=ex, e3=e3: e.dma_start(
                out=xr[e3][:], in_=xs_d.ap()[ex * CAP:(ex + 1) * CAP, :].rearrange("(r p) d -> p r d", p=128)),
                writes=[("xr", e3)], dma=True)

        def xsT_group(ex, r):
            eb = ex % 2
            e3 = ex % 3
            transpose_tile(xr[e3][:, r, :], 8, xsT[eb][:, :, r * 128:(r + 1) * 128], ("xr", e3), ("xsT", eb, r),
                           evac=("scalar" if r % 2 == 0 else "vector"), interleave=8)

        xsload(0)
        wload(0)
        xsload(1)
        wload(1)
        for r in range(3):
            xsT_group(0, r)
        for ex in range(NE):
            eb = ex % 2
            wb_ = ex % NWB
            if ex + 2 < NE:
                wload(ex + 2)
                xsload(ex + 2)
            if ex == 2:
                p4_weight_loads()
            xk = [("xsT", eb, r) for r in range(3)]
            for f in range(4):
                a1, k1, a3, k3 = HACC[f % 2]
                z = f % 2
                for (dst, dkey, wsrc, wkey) in ((a1, k1, w1b, "w1b"), (a3, k3, w3b, "w3b")):
                    for c in range(8):
                        P.op("tensor", lambda e, c=c, dst=dst, wsrc=wsrc, f=f, eb=eb, wb_=wb_: e.matmul(
                            dst[:, 0:CAP], lhsT=wsrc[wb_][:, c, f * 128:(f + 1) * 128], rhs=xsT[eb][:, c, :], start=(c == 0), stop=(c == 7)),
                            reads=[(wkey, wb_)] + xk, writes=[dkey])
                P.op("scalar", lambda e, a1=a1, z=z: e.activation(out=s1[z][:], in_=a1[:, 0:CAP], func=AF.Silu), reads=[k1], writes=[("s1", z)])
                P.op("vector", lambda e, a3=a3, z=z, f=f, eb=eb: e.tensor_tensor(out=hdn[eb][:, f, :], in0=s1[z][:], in1=a3[:, 0:CAP], op=ALU.mult),
                     reads=[("s1", z), k3], writes=[("hdn", eb, f)])
            hk_ = [("hdn", eb, f) for f in range(4)]
            for r in range(3):
                if ex + 1 < NE:
                    xsT_group(ex + 1, r)
                ys_ = yctr[0] % 3
                yctr[0] += 1
                for half in range(2):
                    for f in range(4):
                        P.op("tensor", lambda e, f=f, half=half, r=r, eb=eb, wb_=wb_: e.matmul(
                            B1[:, half * 512:(half + 1) * 512], lhsT=hdn[eb][:, f, r * 128:(r + 1) * 128], rhs=w2b[wb_][:, f, half * 512:(half + 1) * 512],
                            start=(f == 0), stop=(f == 3)),
                            reads=hk_ + [("w2b", wb_)], writes=[("B1", half)])
                    if half == 0:
                        P.op("scalar", lambda e, ys_=ys_: e.activation(out=yb[ys_][:, 0:512], in_=B1[:, 0:512], func=AF.Copy),
                             reads=[("B1", 0)], writes=[("yb", ys_, 0)])
                    else:
                        P.op("vector", lambda e, ys_=ys_: e.tensor_copy(out=yb[ys_][:, 512:1024], in_=B1[:, 512:1024]),
                             reads=[("B1", 1)], writes=[("yb", ys_, 1)])
                row0 = ex * CAP + r * 128
                P.op("sync", lambda e, row0=row0, ys_=ys_: e.dma_start(out=ys_d.ap()[row0:row0 + 128, :], in_=yb[ys_][:]),
                     reads=[("yb", ys_, 0), ("yb", ys_, 1)], writes=[("ys_d", ex, r)], dma=True)
        P.barrier()
    if 3 in phases:
        phase3()
    sb.reset(base_mark)

    def phase4():
        wpg, wpp = p4w["wpg"], p4w["wpp"]
        gple = sb.alloc("gple", [128, D], F32)
        bpg_row = sb.alloc("bpg_row", [1, D], BF16)
        hb = [sb.alloc("hb%d" % i, [128, D], F32) for i in range(3)]
        y1 = [sb.alloc("y1%d" % i, [128, D], BF16) for i in range(3)]
        y2 = [sb.alloc("y2%d" % i, [128, D], BF16) for i in range(3)]
        pin = [sb.alloc("pin%d" % i, [128, 256], F32) for i in range(3)]
        pbf = [sb.alloc("pbf%d" % i, [128, 256], BF16) for i in range(2)]
        ppT = [sb.alloc("ppT%d" % i, [128, 2, 128], BF16) for i in range(2)]
        n3 = [sb.alloc("n3%d" % i, [128, D], BF16) for i in range(2)]
        n3T = [sb.alloc("n3T%d" % i, [128, 8, 128], BF16) for i in range(2)]
        gz = [sb.alloc("gz%d" % i, [128, D], F32) for i in range(2)]
        ob = [sb.alloc("ob%d" % i, [128, D], F32) for i in range(2)]
        ld("sync", gple[:], gple_d.ap().partition_broadcast(128), "gple")
        P.op("gpsimd", lambda e: e.dma_start(out=bpg_row[:], in_=bpg_d.ap()), writes=["bpg"], dma=True)
        def loads4(t):
            h3 = t % 3
            P.op("sync", lambda e, t=t, h3=h3: e.dma_start(out=hb[h3][:], in_=h_d.ap()[t * 128:(t + 1) * 128, :]), writes=[("hb", h3)], dma=True)
            P.op("sync", lambda e, t=t, h3=h3: e.dma_start(out=pin[h3][:], in_=p_d.ap()[t * 128:(t + 1) * 128, :]), writes=[("pin", h3)], dma=True)
            for (yy, ykey, k) in ((y1, "y1", 0), (y2, "y2", 1)):
                P.op("gpsimd", lambda e, yy=yy, k=k, t=t, h3=h3: e.indirect_dma_start(
                    out=yy[h3][:, :], out_offset=None, in_=ys_d[:, :], in_offset=bass.IndirectOffsetOnAxis(ap=dtab[:, t, k:k + 1], axis=0)), reads=["dtab_all"], writes=[(ykey, h3)], dma=True)

        def S1a(t):
            h3 = t % 3
            z = t % 2
            P.op("vector", lambda e, h3=h3, t=t: e.scalar_tensor_tensor(out=hb[h3][:], in0=y1[h3][:], scalar=wtab[:, t, 0:1], in1=hb[h3][:],
                                                                       op0=ALU.mult, op1=ALU.add), reads=[("y1", h3), ("hb", h3)], writes=[("hb", h3)])
            P.op("vector", lambda e, h3=h3, t=t: e.scalar_tensor_tensor(out=hb[h3][:], in0=y2[h3][:], scalar=wtab[:, t, 1:2], in1=hb[h3][:],
                                                                       op0=ALU.mult, op1=ALU.add), reads=[("y2", h3), ("hb", h3)], writes=[("hb", h3)])
            rmsnorm_tile(hb[h3][:], gple[:], n3[z][:], ("hb", h3), "gple", ("n3", z))
            P.op("scalar", lambda e, z=z, h3=h3: e.activation(out=pbf[z][:], in_=pin[h3][:], func=AF.Copy), reads=[("pin", h3)], writes=[("pbf", z)])

        def S1b(t):
            z = t % 2
            transpose_tile(n3[z], 8, n3T[z][:], ("n3", z), ("n3T", z))
            transpose_tile(pbf[z], 2, ppT[z][:], ("pbf", z), ("ppT", z))

        GACC = [((A0, "A0"), (A2, "A2")), ((A1, "A1"), (B0, ("B0", 0)))]

        def S2mm(t, half):
            z = t % 2
            (ga, gk), (pa_, pk) = GACC[half]
            for c in range(8):
                P.op("tensor", lambda e, c=c, half=half, ga=ga, z=z: e.matmul(
                    ga[:], lhsT=n3T[z][:, c, :], rhs=wpg[:, c, half * 512:(half + 1) * 512], start=(c == 0), stop=False),
                    reads=[("n3T", z), ("wpg", 0), ("wpg", 4)], writes=[gk])
            P.op("tensor", lambda e, half=half, ga=ga: e.matmul(
                ga[:], lhsT=ones[0:1, 0:128], rhs=bpg_row[0:1, half * 512:(half + 1) * 512], start=False, stop=True),
                reads=["ones", "bpg"], writes=[gk])
            for c in range(2):
                P.op("tensor", lambda e, c=c, half=half, pa_=pa_, z=z: e.matmul(
                    pa_[:, 0:512], lhsT=ppT[z][:, c, :], rhs=wpp[:, c, half * 512:(half + 1) * 512], start=(c == 0), stop=(c == 1)),
                    reads=[("ppT", z), "wpp"], writes=[pk])

        def S2tail(t):
            z = t % 2
            h3 = t % 3
            hsl = [slice(0, 512), slice(512, 1024)]
            for half in range(2):
                (ga, gk), (pa_, pk) = GACC[half]
                hs = hsl[half]
                P.op("scalar", lambda e, ga=ga, z=z, hs=hs: e.activation(out=gz[z][:, hs], in_=ga[:], func=AF.Tanh, scale=0.5),
                     reads=[gk], writes=[("gz", z, half)])
            for half in range(2):
                (ga, gk), (pa_, pk) = GACC[half]
                hs = hsl[half]
                P.op("vector", lambda e, pa_=pa_, z=z, hs=hs: e.scalar_tensor_tensor(out=gz[z][:, hs], in0=gz[z][:, hs], scalar=1.0, in1=pa_[:, 0:512],
                                                                                    op0=ALU.add, op1=ALU.mult), reads=[("gz", z, half), pk], writes=[("gz", z, half)])
                P.op("vector", lambda e, z=z, hs=hs, h3=h3: e.scalar_tensor_tensor(out=ob[z][:, hs], in0=gz[z][:, hs], scalar=0.5, in1=hb[h3][:, hs],
                                                                                  op0=ALU.mult, op1=ALU.add), reads=[("gz", z, half), ("hb", h3)], writes=[("ob", z, half)])

        loads4(0)
        loads4(1)
        S1a(0)
        S1b(0)
        for t in range(NT):
            z = t % 2
            if t + 2 < NT:
                loads4(t + 2)
            if t + 1 < NT:
                S1a(t + 1)
            S2mm(t, 0)
            S2mm(t, 1)
            S2tail(t)
            if t + 1 < NT:
                S1b(t + 1)
            P.op("sync", lambda e, t=t, z=z: e.dma_start(out=out_d.ap()[t * 128:(t + 1) * 128, :], in_=ob[z][:]),
                 reads=[("ob", z, 0), ("ob", z, 1)], dma=True, is_out=True)
    if 4 in phases:
        phase4()
    P.emit()
    return nc, P


def _sel_tables():
    f = np.arange(128)[:, None, None]
    j = np.arange(8)[None, :, None]
    m = np.arange(128)[None, None, :]
    hit = ((m // 8) == (2 * j + f // 64)).astype(np.float32)
    selA = (hit / 64.0).reshape(128, 8 * 128).astype(ml_dtypes.bfloat16)
    selB = (hit.transpose(2, 1, 0) / 8.0).reshape(128, 8 * 128).astype(ml_dtypes.bfloat16)
    return np.ascontiguousarray(selA), np.ascontiguousarray(selB)


def _host_layout(inp):
    f = lambda a: np.ascontiguousarray(np.asarray(a, dtype=np.float32))
    bf = ml_dtypes.bfloat16
    b_in = f(inp["b_in"])[0]
    rel = f(inp["rel_bias"])[0]
    jj = np.arange(5)[::-1][:, None, None]
    kk = np.arange(128)[None, :, None]
    qq = np.arange(128)[None, None, :]
    dist = qq - kk + 128 * (4 - jj)
    idx = np.clip(dist, -63, 256) + 63
    cdiff = (qq // 64) - (kk // 64) + 2 * (4 - jj)
    mask = ((cdiff >= 0) & (cdiff <= 8)).astype(np.float32)
    rbT = rel[:, idx]
    rbT = np.ascontiguousarray(rbT.transpose(2, 0, 1, 3)).reshape(128, NH * 5 * 128)
    maskT = np.ascontiguousarray(mask.transpose(1, 0, 2)).reshape(128, 5 * 128)
    cwv = f(inp["conv_w"])[0]
    cw = np.ascontiguousarray(cwv.reshape(3, 4, 128).transpose(2, 1, 0)).reshape(128, 12)
    cb = np.ascontiguousarray(f(inp["conv_b"])[0].reshape(4, 128).T)
    gq = f(inp["g_q"])[0]
    gk = f(inp["g_k"])[0]
    shared = {
        "g_mix": f(inp["g_mix"]),
        "w_in": f(inp["w_in"])[0],
        "bcol": np.ascontiguousarray(b_in.reshape(40, 128).T),
        "gqk": np.ascontiguousarray(np.stack([np.tile(gq, 2), np.tile(gk, 2)], axis=1)),
        "bv": np.ascontiguousarray(b_in[1024:1536].reshape(1, 512)),
        "rbT": rbT, "maskT": maskT, "cw": cw, "cb": cb,
        "w_pa": f(inp["w_pa"])[0], "w_pc": f(inp["w_pc"])[0], "w_o": f(inp["w_o"])[0],
        "g_ffn": f(inp["g_ffn"]),
        "w_rg": np.ascontiguousarray(np.concatenate([f(inp["w_group"])[0], f(inp["w_router"])[0]], axis=1)),
        "b_rg": np.ascontiguousarray(np.concatenate([f(inp["b_group"])[0], f(inp["b_router"])[0]])[None, :]),
        "w1": f(inp["w1"])[0], "w3": f(inp["w3"])[0], "w2": f(inp["w2"])[0],
        "g_ple": f(inp["g_ple"]), "w_pg": f(inp["w_ple_gate"])[0], "b_pg": f(inp["b_ple_gate"]),
        "w_pp": f(inp["w_ple_proj"])[0],
        "ident": np.eye(128, dtype=np.float32).astype(bf),
        "utri": np.triu(np.ones((128, 128), np.float32), 1).astype(bf),
        "ones": np.ones((128, 128), np.float32).astype(bf),
        "bdiag": (np.kron(np.eye(2, dtype=np.float32), np.ones((64, 64), np.float32)) / 64.0).astype(bf),
        "ecap": np.ascontiguousarray(np.broadcast_to((np.arange(NE, dtype=np.float32) * CAP)[None, :], (128, NE))),
        "selA": _sel_tables()[0], "selB": _sel_tables()[1],
    }
    x = f(inp["x"])
    p = f(inp["p"])[0]
    maps = []
    for c in range(NCORES):
        m = dict(shared)
        m["x"] = x[c]
        m["p"] = p[c]
        maps.append(m)
    return maps


_CACHE = {}


def kernel(**inputs):
    if "nc" not in _CACHE:
        _CACHE["nc"] = build(debug=False)[0]
    nc = _CACHE["nc"]
    maps = _host_layout(inputs)
    res = run_bass_kernel_spmd(nc, maps, core_ids=list(range(NCORES)))
    out = np.stack([np.asarray(res.results[c]["out"], dtype=np.float32) for c in range(NCORES)], axis=0)
    return out
```

```python
import numpy as np
import ml_dtypes
import concourse.bass as bass
import concourse.mybir as mybir
from concourse.bass_utils import run_bass_kernel_spmd

F32 = mybir.dt.float32
BF16 = mybir.dt.bfloat16
I32 = mybir.dt.int32
ALU = mybir.AluOpType
AF = mybir.ActivationFunctionType
AX = mybir.AxisListType

NCORES = 8
S = 4096
D = 1024
NT = S // 128
BT = 512
NB = S // BT
NH = 8
DH = 64
NE = 32
CAP = 384
NSLOT = NE * CAP
KR = 12
EPS = 1e-6

ENGS = ("sync", "scalar", "vector", "gpsimd", "tensor")
NDMASEM = 24
SAME_ENG_WINDOW = 10 ** 9


class Op:
    __slots__ = ("idx", "eng", "fn", "reads", "writes", "dma", "deps", "sig",
                 "sem", "val", "clock", "epos", "barrier")


class Prog:
    def __init__(self, nc):
        self.nc = nc
        self.ops = []
        self.last_w = {}
        self.readers = {}
        self.out_ops = []
        self.last_barrier = None
        self.since_barrier = []

    def op(self, eng, fn, reads=(), writes=(), dma=False, is_out=False):
        o = Op()
        o.idx = len(self.ops)
        o.eng = eng
        o.fn = fn
        o.dma = dma
        o.barrier = False
        o.reads = tuple(reads)
        o.writes = tuple(writes)
        deps = set()
        for k in o.reads:
            w = self.last_w.get(k)
            if w is not None:
                deps.add(w)
        for k in o.writes:
            w = self.last_w.get(k)
            if w is not None:
                deps.add(w)
            for r in self.readers.get(k, ()):
                deps.add(r)
        for k in o.writes:
            self.last_w[k] = o.idx
            self.readers[k] = []
        for k in o.reads:
            if k not in o.writes:
                self.readers.setdefault(k, []).append(o.idx)
        if self.last_barrier is not None:
            deps.add(self.last_barrier)
        deps.discard(o.idx)
        o.deps = sorted(deps)
        o.sig = False
        self.ops.append(o)
        self.since_barrier.append(o.idx)
        if is_out:
            self.out_ops.append(o.idx)
        return o.idx

    def barrier(self):
        o = Op()
        o.idx = len(self.ops)
        o.eng = "sync"
        o.fn = "BARRIER"
        o.dma = False
        o.barrier = True
        o.reads = ()
        o.writes = ()
        last = {}
        deps = []
        for i in self.since_barrier:
            p = self.ops[i]
            if p.dma:
                deps.append(i)
            else:
                last[p.eng] = i
        deps.extend(last.values())
        if self.last_barrier is not None:
            deps.append(self.last_barrier)
        o.deps = sorted(set(deps))
        o.sig = True
        self.ops.append(o)
        self.last_barrier = o.idx
        self.since_barrier = []
        self.last_w = {}
        self.readers = {}

    def emit(self):
        nc = self.nc
        ops = self.ops
        epos = {e: 0 for e in ENGS}
        for o in ops:
            o.epos = epos[o.eng]
            epos[o.eng] += 1
        fin = Op()
        fin.idx = len(ops)
        fin.eng = "sync"
        fin.fn = None
        fin.dma = False
        fin.barrier = False
        fin.reads = ()
        fin.writes = ()
        fin.deps = list(self.out_ops)
        fin.sig = False
        fin.epos = epos["sync"]
        ops = ops + [fin]
        for o in ops:
            nd = []
            for d in o.deps:
                do = ops[d]
                if do.eng == o.eng and not do.dma and not o.barrier:
                    if o.eng == "tensor" and not o.dma:
                        continue
                    if o.dma:
                        pass
                    elif o.epos - do.epos > SAME_ENG_WINDOW:
                        continue
                nd.append(d)
            o.deps = nd
            for d in nd:
                ops[d].sig = True
        sems = {}
        dma_engs = set(o.eng for o in ops if o.dma)
        for e in ENGS:
            sems[("c", e)] = nc.alloc_semaphore("c_" + e)
            if e in dma_engs:
                for i in range(NDMASEM):
                    sems[("d", e, i)] = nc.alloc_semaphore("d_%s_%d" % (e, i))
        ccount = {e: 0 for e in ENGS}
        dcount = {e: 0 for e in ENGS}
        dma_prev = {}
        for o in ops:
            if o.dma:
                k = dcount[o.eng]
                dcount[o.eng] += 1
                slot = k % NDMASEM
                o.sem = ("d", o.eng, slot)
                o.val = 16 * (k // NDMASEM + 1)
                prev = dma_prev.get((o.eng, slot))
                if prev is not None and prev not in o.deps:
                    o.deps.append(prev)
                dma_prev[(o.eng, slot)] = o.idx
            elif o.sig:
                ccount[o.eng] += 1
                o.sem = ("c", o.eng)
                o.val = ccount[o.eng]
            else:
                o.sem = None
                o.val = 0
        known = {e: {} for e in ENGS}
        streams = {e: [] for e in ENGS}
        for o in ops:
            kn = known[o.eng]
            wm = {}
            for d in sorted(o.deps, reverse=True):
                do = ops[d]
                if kn.get(do.sem, 0) >= do.val:
                    continue
                if wm.get(do.sem, 0) < do.val:
                    wm[do.sem] = do.val
                for s, v in do.clock.items():
                    if kn.get(s, 0) < v:
                        kn[s] = v
            o.clock = dict(kn)
            if o.sem is not None:
                o.clock[o.sem] = o.val
            streams[o.eng].append((o, list(wm.items())))
        self.n_waits = sum(len(w) for st in streams.values() for _, w in st)
        self.counts = (dict(ccount), dict(dcount))

        def run_stream(eng_name):
            def body(eng):
                for o, waits in streams[eng_name]:
                    for s, v in waits:
                        eng.wait_ge(sems[s], v)
                    if o.fn is None:
                        continue
                    if o.barrier:
                        eng.sem_inc(sems[o.sem], 1)
                        continue
                    ins = o.fn(eng)
                    if o.sem is not None:
                        ins.then_inc(sems[o.sem], 16 if o.dma else 1)
            return body

        with nc.Block() as block:
            for e in ENGS:
                if streams[e]:
                    getattr(block, e)(run_stream(e))


class SBAlloc:
    LO = 16512
    HI = 229344

    def __init__(self, nc):
        self.nc = nc
        self.cur = self.LO
        self.n = 0

    def alloc(self, name, shape, dt):
        esz = {F32: 4, BF16: 2, I32: 4}[dt]
        nbytes = esz
        for s in shape[1:]:
            nbytes *= s
        off = (self.cur + 31) // 32 * 32
        assert off + nbytes <= self.HI, "SBUF overflow at %s: need %d have %d" % (name, nbytes, self.HI - off)
        self.n += 1
        t = self.nc.alloc_sbuf_tensor_at("%s_%d" % (name, self.n), list(shape), dt, offset=off)
        self.cur = off + nbytes
        self.off = getattr(self, "off", {})
        self.off[name] = off
        return t

    def reserve(self, name, nbytes):
        off = (self.cur + 31) // 32 * 32
        assert off + nbytes <= self.HI
        self.off = getattr(self, "off", {})
        self.off[name] = off
        self.cur = off + nbytes
        return off

    def alloc_alias(self, name, shape, dt, of):
        self.n += 1
        return self.nc.alloc_sbuf_tensor_at("%s_%d" % (name, self.n), list(shape), dt, offset=self.off[of])

    def mark(self):
        return self.cur

    def reset(self, m):
        self.cur = m


def bc_last(ap, n):
    shp = list(ap.shape)
    return ap.unsqueeze(len(shp)).broadcast_to(shp + [n])


def bc_mid(ap, n):
    shp = list(ap.shape)
    return ap.unsqueeze(1).broadcast_to([shp[0], n] + shp[1:])


def build(debug=False, phases=(1, 2, 3, 4)):
    nc = bass.Bass("TRN2", target_bir_lowering=False)
    P = Prog(nc)
    sb = SBAlloc(nc)

    def din(name, shape, dt=F32):
        return nc.dram_tensor(name, list(shape), dt, kind="ExternalInput")

    def dscr(name, shape, dt):
        return nc.dram_tensor(name, list(shape), dt, kind="ExternalOutput" if debug else "Internal")

    x_d = din("x", [S, D])
    p_d = din("p", [S, 256])
    gmix_d = din("g_mix", [1, D])
    win_d = din("w_in", [D, 5120])
    bcol_d = din("bcol", [128, 40])
    gqk_d = din("gqk", [128, 2])
    bv_d = din("bv", [1, 512])
    rbT_d = din("rbT", [128, NH * 5 * 128])
    maskT_d = din("maskT", [128, 5 * 128])
    cw_d = din("cw", [128, 12])
    cb_d = din("cb", [128, 4])
    wpa_d = din("w_pa", [512, D])
    wpc_d = din("w_pc", [512, D])
    wo_d = din("w_o", [D, D])
    gffn_d = din("g_ffn", [1, D])
    wrg_d = din("w_rg", [D, 36])
    brg_d = din("b_rg", [1, 36])
    w1_d = din("w1", [NE, D, 512])
    w3_d = din("w3", [NE, D, 512])
    w2_d = din("w2", [NE, 512, D])
    gple_d = din("g_ple", [1, D])
    wpg_d = din("w_pg", [D, D])
    bpg_d = din("b_pg", [1, D])
    wpp_d = din("w_pp", [256, D])
    ident_d = din("ident", [128, 128], BF16)
    utri_d = din("utri", [128, 128], BF16)
    ones_d = din("ones", [128, 128], BF16)
    bdiag_d = din("bdiag", [128, 128], BF16)
    ecap_d = din("ecap", [128, NE])
    selA_d = din("selA", [128, 8 * 128], BF16)
    selB_d = din("selB", [128, 8 * 128], BF16)
    out_d = nc.dram_tensor("out", [S, D], F32, kind="ExternalOutput")

    nT_d = dscr("nT_s", [D, S], BF16)
    yaT_d = dscr("yaT_s", [512, S], BF16)
    ycT_d = dscr("ycT_s", [512, S], BF16)
    h_d = dscr("h_s", [S, D], F32)
    xs_d = dscr("xs_s", [NSLOT + 128, D], BF16)
    ys_d = dscr("ys_s", [NSLOT + 128, D], BF16)
    if debug:
        dtab_o = nc.dram_tensor("dtab_o", [128, NT * 2], I32, kind="ExternalOutput")
        wtab_o = nc.dram_tensor("wtab_o", [128, NT * 2], F32, kind="ExternalOutput")

    pT = nc.alloc_psum_tensor("pT", [128, 8, 128], BF16)
    A0 = nc.alloc_psum_tensor("A0", [128, 512], F32)
    A1 = nc.alloc_psum_tensor("A1", [128, 512], F32)
    A2 = nc.alloc_psum_tensor("A2", [128, 512], F32)
    B0 = nc.alloc_psum_tensor("B0", [128, 1024], F32)
    B1 = nc.alloc_psum_tensor("B1", [128, 1024], F32)

    ident = sb.alloc("ident", [128, 128], BF16)
    utri = sb.alloc("utri", [128, 128], BF16)
    ones = sb.alloc("ones", [128, 128], BF16)
    bdiag = sb.alloc("bdiag", [128, 128], BF16)
    ecap = sb.alloc("ecap", [128, NE], F32)
    bcol = sb.alloc("bcol", [128, 40], F32)
    hbcol = sb.alloc("hbcol", [128, 40], F32)
    mhalf = sb.alloc("mhalf", [128, 8], F32)
    epsc = sb.alloc("epsc", [128, 8], F32)
    dtab = sb.alloc("dtab", [128, NT, 2], I32)
    wtab = sb.alloc("wtab", [128, NT, 2], F32)
    cnt = sb.alloc("cnt", [128, NE], F32)
    ss = sb.alloc("ss", [128, 8], F32)
    rs = sb.alloc("rs", [128, 8], F32)
    junk = sb.alloc("junk", [128, D], BF16)

    def ld(eng, dst, src, key):
        P.op(eng, lambda e: e.dma_start(out=dst, in_=src), writes=[key], dma=True)

    ld("sync", ident[:], ident_d.ap(), "ident")
    ld("sync", utri[:], utri_d.ap(), "utri")
    ld("sync", ones[:], ones_d.ap(), "ones")
    ld("sync", bdiag[:], bdiag_d.ap(), "bdiag")
    ld("sync", ecap[:], ecap_d.ap(), "ecap")
    ld("sync", bcol[:], bcol_d.ap(), "bcol")
    P.op("vector", lambda e: e.tensor_scalar(out=hbcol[:], in0=bcol[:], scalar1=0.5, scalar2=None, op0=ALU.mult),
         reads=["bcol"], writes=["hbcol"])
    P.op("gpsimd", lambda e: e.memset(mhalf[:], -0.5), writes=["mhalf"])
    P.op("gpsimd", lambda e: e.memset(epsc[:], EPS), writes=["epsc"])
    P.op("gpsimd", lambda e: e.memset(cnt[:], 0.0), writes=["cnt"])
    P.op("gpsimd", lambda e: e.memset(wtab[:], 0.0), writes=["wtab"])

    nrm_ctr = [0]

    def rmsnorm_tile(src, g_bc, dst_bf, src_key, g_key, dst_key, pool=True):
        i = nrm_ctr[0] % 8
        nrm_ctr[0] += 1
        ssk, rsk = ("ss", i), ("rs", i)
        P.op("scalar", lambda e: e.activation(out=junk[:], in_=src, func=AF.Square, accum_out=ss[:, i:i + 1]),
             reads=[src_key], writes=["junk", ssk])
        P.op("vector", lambda e: e.tensor_scalar(out=rs[:, i:i + 1], in0=ss[:, i:i + 1], scalar1=1.0 / D, scalar2=EPS,
                                                  op0=ALU.mult, op1=ALU.add), reads=[ssk], writes=[rsk])
        if pool:
            P.op("gpsimd", lambda e: e.tensor_tensor(out=rs[:, i:i + 1], in0=rs[:, i:i + 1], in1=mhalf[:, 0:1], op=ALU.pow),
                 reads=[rsk, "mhalf"], writes=[rsk])
        else:
            P.op("scalar", lambda e: e.activation(out=rs[:, i:i + 1], in_=rs[:, i:i + 1], func=AF.Sqrt), reads=[rsk], writes=[rsk])
            P.op("vector", lambda e: e.reciprocal(out=rs[:, i:i + 1], in_=rs[:, i:i + 1]), reads=[rsk], writes=[rsk])
        P.op("vector", lambda e: e.scalar_tensor_tensor(out=dst_bf, in0=src, scalar=rs[:, i:i + 1], in1=g_bc,
                                                         op0=ALU.mult, op1=ALU.mult),
             reads=[src_key, rsk, g_key], writes=[dst_key])

    def transpose_tile(src_bf, nchunk, dst, src_key, dst_key, evac="scalar", interleave=0):
        for c in range(nchunk):
            if interleave:
                src_c = src_bf.rearrange("t (p c) -> t c p", c=interleave)[:, c, :]
            else:
                src_c = src_bf[:, c * 128:(c + 1) * 128]
            P.op("tensor", lambda e, c=c, src_c=src_c: e.transpose(out=pT[:, c, :], in_=src_c, identity=ident[:]),
                 reads=(list(src_key) if isinstance(src_key, list) else [src_key]) + ["ident"], writes=[("pT", c)])
        if evac == "scalar":
            P.op("scalar", lambda e: e.activation(out=dst, in_=pT[:, 0:nchunk, :], func=AF.Copy),
                 reads=[("pT", c) for c in range(nchunk)], writes=[dst_key])
        else:
            P.op("vector", lambda e: e.tensor_copy(out=dst, in_=pT[:, 0:nchunk, :]),
                 reads=[("pT", c) for c in range(nchunk)], writes=[dst_key])

    _breg = {}

    def breg(e):
        if "r" not in _breg:
            _breg["r"] = e.to_reg(NSLOT - 1)
        return _breg["r"]

    base_mark = sb.mark()
    BASE = (base_mark + 31) // 32 * 32
    pre2 = {"Wg": nc.alloc_sbuf_tensor_at("Wg_pre", [128, 8, 2048], BF16, offset=BASE),
            "wpa": nc.alloc_sbuf_tensor_at("wpa_pre", [128, 4, D], BF16, offset=BASE + 32768),
            "wpc": nc.alloc_sbuf_tensor_at("wpc_pre", [128, 4, D], BF16, offset=BASE + 40960)}
    pre3 = [{"w1": nc.alloc_sbuf_tensor_at("w1_pre%d" % i, [128, 8, 512], BF16, offset=BASE + i * 24576),
             "w3": nc.alloc_sbuf_tensor_at("w3_pre%d" % i, [128, 8, 512], BF16, offset=BASE + i * 24576 + 8192),
             "w2": nc.alloc_sbuf_tensor_at("w2_pre%d" % i, [128, 4, D], BF16, offset=BASE + i * 24576 + 16384)} for i in range(2)]
    WGKEYS = [("Wg", g0 + q4 * 256) for q4 in range(4) for g0 in (0, 1024)]

    def p2_weight_loads(Wg, wpa, wpc, extra_writes=()):
        win_v2 = win_d.ap().rearrange("(c p) n -> p c n", p=128)
        ew = list(extra_writes)
        if ew:
            for c in range(0, 8, 2):
                P.op("gpsimd", lambda e, c=c: e.dma_start(out=Wg[:, c:c + 2, :], in_=win_v2[:, c:c + 2, 3072:5120]),
                     writes=WGKEYS + ew, dma=True)
            P.op("gpsimd", lambda e: e.dma_start(out=wpa[:], in_=wpa_d.ap().rearrange("(c p) n -> p c n", p=128)), writes=["wpa"] + ew, dma=True)
            P.op("gpsimd", lambda e: e.dma_start(out=wpc[:], in_=wpc_d.ap().rearrange("(c p) n -> p c n", p=128)), writes=["wpc"] + ew, dma=True)
            return

        def wg_load(q4):
            for g0 in (0, 1024):
                c0_ = g0 + q4 * 256
                P.op("gpsimd", lambda e, c0_=c0_: e.dma_start(out=Wg[:, :, c0_:c0_ + 256], in_=win_v2[:, :, 3072 + c0_:3072 + c0_ + 256]),
                     writes=[("Wg", c0_)] + ew, dma=True)

        wg_load(0)
        P.op("gpsimd", lambda e: e.dma_start(out=wpa[:], in_=wpa_d.ap().rearrange("(c p) n -> p c n", p=128)), writes=["wpa"] + ew, dma=True)
        P.op("gpsimd", lambda e: e.dma_start(out=wpc[:], in_=wpc_d.ap().rearrange("(c p) n -> p c n", p=128)), writes=["wpc"] + ew, dma=True)
        for q4 in range(1, 4):
            wg_load(q4)

    def expert_weight_loads(ex, w1t, w3t, w2t, keys, extra_writes=()):
        ew = list(extra_writes)
        P.op("gpsimd", lambda e: e.dma_start(out=w1t[:], in_=w1_d.ap()[ex].rearrange("(p c) f -> p c f", c=8)), writes=[keys[0]] + ew, dma=True)
        P.op("gpsimd", lambda e: e.dma_start(out=w3t[:], in_=w3_d.ap()[ex].rearrange("(p c) f -> p c f", c=8)), writes=[keys[1]] + ew, dma=True)
        P.op("gpsimd", lambda e: e.dma_start(out=w2t[:], in_=w2_d.ap()[ex].rearrange("(c p) f -> p c f", p=128)), writes=[keys[2]] + ew, dma=True)

    def phase1():
        Wa = sb.alloc("Wa", [128, 8, 3072], BF16)
        gmix = sb.alloc("gmix", [128, D], F32)
        gqk = sb.alloc("gqk", [128, 2], F32)
        bvb = sb.alloc("bvb", [128, 512], F32)
        cw = sb.alloc("cw", [128, 12], F32)
        cb = sb.alloc("cb", [128, 4], F32)
        expB = sb.alloc("expB", [128, NH, 5, 128], BF16)
        maskT = sb.alloc("maskT", [128, 5, 128], F32)
        kring = sb.alloc("kring", [128, 4, KR * 128], BF16)
        vring = sb.alloc("vring", [128, KR, NH, 65], BF16)
        xt = [sb.alloc("xt%d" % i, [128, D], F32) for i in range(2)]
        nb = [sb.alloc("nb%d" % i, [128, D], BF16) for i in range(2)]
        nTb = [sb.alloc("nTb%d" % i, [128, 8, BT], BF16) for i in range(2)]
        qT = [sb.alloc("qT%d" % i, [128, 4, BT], BF16) for i in range(2)]
        zq = [sb.alloc("zq%d" % i, [128, BT], F32) for i in range(8)]
        sq = [sb.alloc("sq%d" % i, [128, BT], BF16) for i in range(3)]
        rs = sb.alloc("rs_all", [128, BT], F32)
        r1b = sb.alloc("r1b", [128, BT], BF16)
        selA = sb.alloc("selA", [128, 8, 128], BF16)
        selB = sb.alloc("selB", [128, 8, 128], BF16)
        gw = sb.alloc("gw", [128, 1], F32)
        us = [sb.alloc("us%d" % i, [128, BT], F32) for i in range(2)]
        t1 = [sb.alloc("t1%d" % i, [128, BT], F32) for i in range(2)]
        cu = sb.alloc("cu", [128, 4, BT + 2], F32)
        ycT = [sb.alloc("ycT%d" % i, [128, 4, BT], BF16) for i in range(2)]
        yaT = [sb.alloc("yaT%d" % i, [128, 4, BT], BF16) for i in range(2)]
        pt = [sb.alloc("pt%d" % i, [128, 4, 128], BF16) for i in range(6)]
        rden = [sb.alloc("rden%d" % i, [128, 4], F32) for i in range(2)]
        ya = sb.alloc("ya", [128, 4, 512], BF16)

        win_v = win_d.ap().rearrange("(c p) n -> p c n", p=128)
        for (c0_, c1_) in ((0, 1024), (1024, 1536), (1536, 3072)):
            for c in range(0, 8, 2):
                P.op("gpsimd", lambda e, c=c, c0_=c0_, c1_=c1_: e.dma_start(out=Wa[:, c:c + 2, c0_:c1_], in_=win_v[:, c:c + 2, c0_:c1_]),
                     writes=[("Wa", c, c0_), ("Wa", c + 1, c0_)], dma=True)
        zt = sb.alloc("zt", [128, 2 * D], BF16)
        P.op("gpsimd", lambda e: e.memset(zt[:], 0.0), writes=["zt"])
        NR = (NSLOT + 128) // 128
        xs_z = xs_d.ap().rearrange("(p r) d -> p (r d)", p=128)
        zchunks = [(r0, min(r0 + 2, NR)) for r0 in range(0, NR, 2)]

        def zero_fill(k):
            for (r0, r1_) in zchunks[k::NB]:
                P.op("sync", lambda e, r0=r0, r1_=r1_: e.dma_start(out=xs_z[:, r0 * D:r1_ * D], in_=zt[:, 0:(r1_ - r0) * D]),
                     reads=["zt"], writes=[("xs_zero", r0)], dma=True)

        ld("sync", gmix[:], gmix_d.ap().partition_broadcast(128), "gmix")
        ld("sync", gqk[:], gqk_d.ap(), "gqk")
        ld("sync", selA[:], selA_d.ap().rearrange("p (j m) -> p j m", j=8), "selA")
        ld("sync", selB[:], selB_d.ap().rearrange("p (j m) -> p j m", j=8), "selB")
        ld("sync", bvb[:], bv_d.ap().partition_broadcast(128), "bvb")
        P.op("vector", lambda e: e.tensor_tensor(out=gw[:], in0=gqk[:, 0:1], in1=gqk[:, 1:2], op=ALU.mult), reads=["gqk"], writes=["gw"])
        ld("sync", cw[:], cw_d.ap(), "cw")
        ld("sync", cb[:], cb_d.ap(), "cb")
        ld("sync", maskT[:], maskT_d.ap().rearrange("p (j q) -> p j q", j=5), "maskT")
        rb_v = rbT_d.ap().rearrange("p (h n) -> p h n", h=NH)
        def expB_setup():
            stg = [sb.alloc_alias("stg0", [128, 640], F32, "ycT1"), sb.alloc_alias("stg1", [128, 640], F32, "yaT1")]
            skeys = [[("ycT", 1, q_) for q_ in range(4)], [("yaT", 1, q_) for q_ in range(4)]]
            for h in range(NH):
                st = stg[h % 2]
                sk = skeys[h % 2]
                P.op("sync", lambda e, h=h, st=st: e.dma_start(out=st[:], in_=rb_v[:, h, :]), writes=sk, dma=True)
                P.op("scalar", lambda e, st=st: e.activation(out=st[:], in_=st[:], func=AF.Exp), reads=sk, writes=sk)
                P.op("vector", lambda e, h=h, st=st: e.tensor_tensor(
                    out=expB[:, h, :, :], in0=st[:].rearrange("p (j q) -> p j q", j=5), in1=maskT[:], op=ALU.mult),
                    reads=sk + ["maskT"], writes=[("expB", h)])

        P.op("gpsimd", lambda e: e.memset(vring[:], 1.0), writes=[("v", s_) for s_ in range(KR)])
        P.op("gpsimd", lambda e: e.memset(cu[:], 0.0), writes=[("cu", ct) for ct in range(4)] + [("cuh", ct) for ct in range(4)])

        nT_v = nT_d.ap().rearrange("(c p) t -> p c t", p=128)
        yaT_v = yaT_d.ap().rearrange("(c p) t -> p c t", p=128)
        ycT_v = ycT_d.ap().rearrange("(c p) t -> p c t", p=128)
        acc_rot = [0]
        ACC = [(A0, "A0"), (A1, "A1")]

        def next_acc():
            a = ACC[acc_rot[0] % 2]
            acc_rot[0] += 1
            return a

        def A_norm(b, tl):
            t = 4 * b + tl
            s2 = t % 2
            P.op("sync", lambda e, t=t, s2=s2: e.dma_start(out=xt[s2][:], in_=x_d.ap()[t * 128:(t + 1) * 128, :]),
                 writes=[("xt", s2)], dma=True)
            rmsnorm_tile(xt[s2][:], gmix[:], nb[s2][:], ("xt", s2), "gmix", ("nb", s2))

        def A_tr(b, tl):
            t = 4 * b + tl
            s2 = t % 2
            bb = b % 2
            transpose_tile(nb[s2], 8, nTb[bb][:, :, tl * 128:(tl + 1) * 128], ("nb", s2), ("nTb", bb, tl))
            if tl == 3:
                P.op("sync", lambda e, bb=bb, b=b: e.dma_start(out=nT_v[:, :, b * BT:(b + 1) * BT], in_=nTb[bb][:]),
                     reads=[("nTb", bb, q_) for q_ in range(4)], writes=[("nT_d", b)], dma=True)

        def secC(b):
            bb = b % 2
            tok0 = b * BT
            nkeys = [("nTb", bb, tl) for tl in range(4)]
            PACC = [(A0[:], "A0"), (A1[:], "A1"), (B0[:, 0:512], ("B0", 0))]
            prot = [0]

            def nacc():
                a = PACC[prot[0] % 3]
                prot[0] += 1
                return a

            def proj(j):
                acc, akey = nacc()
                for c in range(8):
                    P.op("tensor", lambda e, j=j, c=c, acc=acc: e.matmul(
                        acc, lhsT=Wa[:, c, j * 128:(j + 1) * 128], rhs=nTb[bb][:, c, :], start=(c == 0), stop=(c == 7)),
                        reads=[("Wa", c, 0)] + nkeys, writes=[akey])
                z = j % 3
                P.op("scalar", lambda e, j=j, acc=acc: e.activation(out=zq[j][:], in_=acc, func=AF.Identity, bias=bcol[:, j:j + 1]),
                     reads=[akey, "bcol"], writes=[("zq", j)])
                P.op("gpsimd", lambda e, z=z, j=j: e.tensor_tensor(out=sq[z][:], in0=zq[j][:], in1=zq[j][:], op=ALU.mult),
                     reads=[("zq", j)], writes=[("sq", z)])

            def msacc(j):
                z = j % 3
                P.op("tensor", lambda e, z=z, j=j: e.matmul(A2[:], lhsT=selA[:, j, :], rhs=sq[z][:], start=(j == 0), stop=(j == 7)),
                     reads=[("sq", z), "selA"], writes=["A2"])

            def vproj():
                for tl in range(4):
                    t = 4 * b + tl
                    sl = t % KR
                    acc, akey = nacc()
                    for c in range(8):
                        P.op("tensor", lambda e, c=c, tl=tl, acc=acc: e.matmul(
                            acc, lhsT=nTb[bb][:, c, tl * 128:(tl + 1) * 128], rhs=Wa[:, c, 1024:1536], start=(c == 0), stop=(c == 7)),
                            reads=[("Wa", c, 1024), ("nTb", bb, tl)], writes=[akey])
                    P.op("vector", lambda e, sl=sl, acc=acc: e.tensor_tensor(
                        out=vring[:, sl, :, 0:64], in0=acc.rearrange("p (h d) -> p h d", h=NH),
                        in1=bvb[:].rearrange("p (h d) -> p h d", h=NH), op=ALU.add),
                        reads=[akey, "bvb"], writes=[("v", sl)])

            def fin(j):
                acc, akey = nacc()
                P.op("tensor", lambda e, j=j, acc=acc: e.matmul(acc, lhsT=selB[:, j, :], rhs=r1b[:], start=True, stop=True),
                     reads=["selB", "r1b"], writes=[akey])
                if j < 4:
                    P.op("vector", lambda e, j=j, acc=acc: e.tensor_tensor(out=qT[bb][:, j, :], in0=zq[j][:], in1=acc, op=ALU.mult),
                         reads=[("zq", j), akey], writes=[("qT", bb, j)])
                else:
                    hp = j - 4
                    sl0 = (4 * b) % KR
                    P.op("vector", lambda e, hp=hp, j=j, sl0=sl0, acc=acc: e.scalar_tensor_tensor(
                        out=kring[:, hp, sl0 * 128:(sl0 + 4) * 128], in0=zq[j][:], scalar=gw[:, 0:1], in1=acc, op0=ALU.mult, op1=ALU.mult),
                        reads=[("zq", j), akey, "gw"], writes=[("k", hp, sl0 + q_) for q_ in range(4)])

            proj(0)
            for j in range(8):
                if j + 1 < 8:
                    proj(j + 1)
                msacc(j)
            P.op("scalar", lambda e: e.activation(out=rs[:], in_=A2[:], func=AF.Sqrt, bias=epsc[:, 0:1]), reads=["A2", "epsc"], writes=["rs"])
            P.op("vector", lambda e: e.reciprocal(out=rs[:], in_=rs[:]), reads=["rs"], writes=["rs"])
            P.op("scalar", lambda e: e.activation(out=r1b[:], in_=rs[:], func=AF.Copy), reads=["rs"], writes=["r1b"])
            vproj()
            for j in range(8):
                fin(j)
        def secC3(b):
            bb = b % 2
            tok0 = b * BT
            nkeys = [("nTb", bb, tl) for tl in range(4)]
            SETS = [((A0[:], ["A0"]), (A1[:], ["A1"]), (A2[:], ["A2"])),
                    ((B0[:, 0:512], [("B0", 0)]), (B0[:, 512:1024], [("B0", 4)]), (B1[:, 0:512], [("B1h", 0)]))]
            for ct in range(4):
                z = ct % 2
                (pu, ku), (pb, kb), (pc, kc) = SETS[ct % 2]
                for (dst, dkey, col0) in ((pu, ku, 1536), (pb, kb, 2048), (pc, kc, 2560)):
                    for c in range(8):
                        P.op("tensor", lambda e, c=c, dst=dst, col0=col0, ct=ct: e.matmul(
                            dst, lhsT=Wa[:, c, col0 + ct * 128:col0 + (ct + 1) * 128], rhs=nTb[bb][:, c, :],
                            start=(c == 0), stop=(c == 7)),
                            reads=[("Wa", c, 1536)] + nkeys, writes=dkey)
                ju, jb, jc = 12 + ct, 16 + ct, 20 + ct
                P.op("scalar", lambda e, z=z, ju=ju, pu=pu: e.activation(out=us[z][:], in_=pu, func=AF.Identity, bias=bcol[:, ju:ju + 1]),
                     reads=ku + ["bcol"], writes=[("us", z)])
                P.op("vector", lambda e, z=z, jc=jc, ct=ct, pc=pc: e.scalar_tensor_tensor(
                    out=cu[:, ct, 2:BT + 2], in0=pc, scalar=bcol[:, jc:jc + 1], in1=us[z][:], op0=ALU.add, op1=ALU.mult),
                    reads=kc + ["bcol", ("us", z)], writes=[("cu", ct)])
                P.op("scalar", lambda e, z=z, ct=ct: e.activation(out=t1[z][:], in_=cu[:, ct, 2:BT + 2], func=AF.Identity,
                                                               scale=cw[:, ct * 3 + 2:ct * 3 + 3], bias=cb[:, ct:ct + 1]),
                     reads=[("cu", ct), "cw", "cb"], writes=[("t1", z)])
                P.op("vector", lambda e, z=z, ct=ct: e.scalar_tensor_tensor(
                    out=t1[z][:], in0=cu[:, ct, 1:BT + 1], scalar=cw[:, ct * 3 + 1:ct * 3 + 2], in1=t1[z][:], op0=ALU.mult, op1=ALU.add),
                    reads=[("cu", ct), ("cuh", ct), "cw", ("t1", z)], writes=[("t1", z)])
                P.op("vector", lambda e, z=z, ct=ct: e.scalar_tensor_tensor(
                    out=t1[z][:], in0=cu[:, ct, 0:BT], scalar=cw[:, ct * 3:ct * 3 + 1], in1=t1[z][:], op0=ALU.mult, op1=ALU.add),
                    reads=[("cu", ct), ("cuh", ct), "cw", ("t1", z)], writes=[("t1", z)])
                P.op("vector", lambda e, z=z, jb=jb, ct=ct, pb=pb: e.scalar_tensor_tensor(
                    out=ycT[bb][:, ct, :], in0=pb, scalar=bcol[:, jb:jb + 1], in1=t1[z][:], op0=ALU.add, op1=ALU.mult),
                    reads=kb + ["bcol", ("t1", z)], writes=[("ycT", bb, ct)])
                P.op("vector", lambda e, ct=ct: e.tensor_copy(out=cu[:, ct, 0:2], in_=cu[:, ct, BT:BT + 2]),
                     reads=[("cu", ct)], writes=[("cuh", ct)])
            P.op("sync", lambda e, tok0=tok0: e.dma_start(out=ycT_v[:, :, tok0:tok0 + BT], in_=ycT[bb][:]),
                 reads=[("ycT", bb, ct) for ct in range(4)], writes=[("ycT_d", b)], dma=True)

        def secD(b):
            bb = b % 2
            tok0 = b * BT
            nxt = b + 1 < NB
            units = []
            for h in range(NH):
                for m in range(8):
                    kt = 4 * b - 4 + m
                    if kt < 0:
                        continue
                    units.append((h, m, kt, max(m - 4, 0), min(m, 3)))
            SPS = [(A0[:], "A0"), (A1[:], "A1"), (A2[:], "A2"), (B0[:, 0:512], ("B0", 0)), (B0[:, 512:1024], ("B0", 4))]
            LA = 4

            def QKEXP(u):
                h, m, kt, tlo, thi = units[u]
                hp, r0 = h // 2, (h % 2) * 64
                nq = thi - tlo + 1
                sp, sk = SPS[u % 5]
                pz = u % 6
                sl = kt % KR
                P.op("tensor", lambda e, hp=hp, r0=r0, sl=sl, sp=sp, tlo=tlo, thi=thi: e.matmul(
                    sp[:, 0:(thi - tlo + 1) * 128], lhsT=kring[r0:r0 + 64, hp, sl * 128:(sl + 1) * 128],
                    rhs=qT[bb][r0:r0 + 64, hp, tlo * 128:(thi + 1) * 128], start=True, stop=True),
                    reads=[("k", hp, sl), ("qT", bb, hp)], writes=[sk])
                P.op("scalar", lambda e, pz=pz, sp=sp, nq=nq: e.activation(
                    out=pt[pz][:, 0:nq, :], in_=sp[:, 0:nq * 128].rearrange("p (j q) -> p j q", q=128), func=AF.Exp, scale=DH ** -0.5),
                    reads=[sk], writes=[("pt", pz)])
                rlo = 4 - m + tlo
                P.op("vector", lambda e, pz=pz, nq=nq, h=h, rlo=rlo: e.tensor_tensor(
                    out=pt[pz][:, 0:nq, :], in0=pt[pz][:, 0:nq, :], in1=expB[:, h, rlo:rlo + nq, :], op=ALU.mult),
                    reads=[("pt", pz), ("expB", h)], writes=[("pt", pz)])

            def PV(u):
                h, m, kt, tlo, thi = units[u]
                pz = u % 6
                sl = kt % KR
                hb2 = h % 2
                first = (u == 0) or units[u - 1][0] != h
                last_u = (u + 1 == len(units)) or units[u + 1][0] != h
                if first:
                    P.op("tensor", lambda e, hb2=hb2: e.matmul(
                        B1[:, hb2 * 512:hb2 * 512 + 260], lhsT=zt[:, 0:128], rhs=zt[:, 0:260], start=True, stop=False),
                        reads=["zt"], writes=[("B1h", hb2)])
                for tl in range(tlo, thi + 1):
                    c0 = hb2 * 512 + tl * 65
                    P.op("tensor", lambda e, h=h, tl=tl, tlo=tlo, sl=sl, pz=pz, c0=c0, fin=(last_u and tl == thi): e.matmul(
                        B1[:, c0:c0 + 65], lhsT=pt[pz][:, tl - tlo, :], rhs=vring[:, sl, h, :],
                        start=False, stop=fin),
                        reads=[("pt", pz), ("v", sl)], writes=[("B1h", hb2)])

            def FINH(h):
                hb2 = h % 2
                Bv = B1[:, hb2 * 512:hb2 * 512 + 260].rearrange("p (t d) -> p t d", d=65)
                P.op("vector", lambda e, hb2=hb2, Bv=Bv: e.reciprocal(out=rden[hb2][:], in_=Bv[:, :, 64]),
                     reads=[("B1h", hb2)], writes=[("rden", hb2)])
                P.op("vector", lambda e, hb2=hb2, Bv=Bv, h=h: e.tensor_tensor(
                    out=ya[:, :, h * 64:(h + 1) * 64], in0=Bv[:, :, 0:64], in1=bc_last(rden[hb2][:], 64), op=ALU.mult),
                    reads=[("B1h", hb2), ("rden", hb2)], writes=[("ya", h)])

            if nxt:
                A_norm(b + 1, 0)
            for u in range(min(LA, len(units))):
                QKEXP(u)
            secC3(b)
            zero_fill(b)
            if b == NB - 1 and 2 in phases:
                p2_weight_loads(pre2["Wg"], pre2["wpa"], pre2["wpc"],
                                extra_writes=[("Wa", c, c0_) for c in range(8) for c0_ in (0, 1024, 1536)])
            for u in range(len(units)):
                if u + LA < len(units):
                    QKEXP(u + LA)
                PV(u)
                h = units[u][0]
                if u + 1 == len(units) or units[u + 1][0] != h:
                    FINH(h)
                    if nxt and h % 2 == 1:
                        tl = h // 2
                        A_tr(b + 1, tl)
                        if tl + 1 < 4:
                            A_norm(b + 1, tl + 1)
            for tl in range(4):
                transpose_tile(ya[:, tl, :], 4, yaT[bb][:, :, tl * 128:(tl + 1) * 128], [("ya", h) for h in range(NH)], ("yaT", bb, tl))
            P.op("sync", lambda e, tok0=tok0: e.dma_start(out=yaT_v[:, :, tok0:tok0 + BT], in_=yaT[bb][:]),
                 reads=[("yaT", bb, tl) for tl in range(4)], writes=[("yaT_d", b)], dma=True)

        for tl in range(4):
            A_norm(0, tl)
            A_tr(0, tl)
        expB_setup()
        for b in range(NB):
            secC(b)
            secD(b)
        P.barrier()
    if 1 in phases:
        phase1()
    sb.reset(base_mark)

    def phase2():
        sb.reserve("Wg", 32768)
        sb.reserve("wpa", 8192)
        sb.reserve("wpc", 8192)
        Wg, wpa, wpc = pre2["Wg"], pre2["wpa"], pre2["wpc"]
        wo = sb.alloc("wo", [128, 8, D], BF16)
        wrg = sb.alloc("wrg", [128, 8, 36], BF16)
        brg = sb.alloc("brg", [128, 36], F32)
        gffn = sb.alloc("gffn", [128, D], F32)
        nTb = [sb.alloc("nTb%d" % i, [128, 8, BT], BF16) for i in range(2)]
        yaT = [sb.alloc("yaT%d" % i, [128, 4, BT], BF16) for i in range(2)]
        ycT = [sb.alloc("ycT%d" % i, [128, 4, BT], BF16) for i in range(2)]
        xt = [sb.alloc("xt%d" % i, [128, D], F32) for i in range(4)]
        tA = [sb.alloc("tA%d" % i, [128, BT], F32) for i in range(2)]
        tC = [sb.alloc("tC%d" % i, [128, BT], F32) for i in range(2)]
        mA = [sb.alloc("mA%d" % i, [128, BT], F32) for i in range(2)]
        mC = [sb.alloc("mC%d" % i, [128, BT], F32) for i in range(2)]
        mT = [sb.alloc("mT%d" % i, [128, 8, BT], BF16) for i in range(2)]
        ht = [sb.alloc("ht%d" % i, [128, D], F32) for i in range(3)]
        n2 = [sb.alloc("n2%d" % i, [128, 4, D], BF16) for i in range(2)]
        n2T = [sb.alloc("n2T%d" % i, [128, 8, 128], BF16) for i in range(2)]
        lg = sb.alloc("lg", [128, 4, 36], F32)
        gmax = sb.alloc("gmax", [128, 4], F32)
        gmask = sb.alloc("gmask", [128, 4, 4], F32)
        gex = sb.alloc("gex", [128, 4, 4], F32)
        gse = sb.alloc("gse", [128, 4], F32)
        pen = sb.alloc("pen", [128, 4, 4], F32)
        elm = sb.alloc("elm", [128, 4, 32], F32)
        elm2 = sb.alloc("elm2", [128, 4, 32], F32)
        m1 = sb.alloc("m1", [128, 4], F32)
        m2 = sb.alloc("m2", [128, 4], F32)
        mk1 = sb.alloc("mk1", [128, 4, 32], F32)
        mk2 = sb.alloc("mk2", [128, 4, 32], F32)
        Mb = sb.alloc("Mb", [128, 4, 32], BF16)
        dd = sb.alloc("dd", [128, 4], F32)
        ee = sb.alloc("ee", [128, 4], F32)
        rr = sb.alloc("rr", [128, 4], F32)
        wA = sb.alloc("wA", [128, 4], F32)
        wB = sb.alloc("wB", [128, 4], F32)
        pos = sb.alloc("pos", [128, 4, 32], F32)
        okm = sb.alloc("okm", [128, 4, 32], F32)
        slot = sb.alloc("slot", [128, 4, 32], F32)
        tmp = sb.alloc("tmp", [128, 4, 32], F32)
        dsel = sb.alloc("dsel", [128, 4, 2], F32)
        oksel = sb.alloc("oksel", [128, 4, 2], F32)

        assert sb.off["Wg"] == BASE and sb.off["wpa"] == BASE + 32768 and sb.off["wpc"] == BASE + 40960
        if 1 not in phases:
            p2_weight_loads(Wg, wpa, wpc)
        wo_v = wo_d.ap().rearrange("(c p) n -> p c n", p=128)
        for c in range(0, 8, 4):
            P.op("gpsimd", lambda e, c=c: e.dma_start(out=wo[:, c:c + 4, :], in_=wo_v[:, c:c + 4, :]), writes=[("wo", c)], dma=True)
        P.op("gpsimd", lambda e: e.dma_start(out=wrg[:], in_=wrg_d.ap().rearrange("(c p) n -> p c n", p=128)), writes=["wrg"], dma=True)
        ld("sync", brg[:], brg_d.ap().partition_broadcast(128), "brg")
        ld("sync", gffn[:], gffn_d.ap().partition_broadcast(128), "gffn")

        nT_v = nT_d.ap().rearrange("(c p) t -> p c t", p=128)
        yaT_v = yaT_d.ap().rearrange("(c p) t -> p c t", p=128)
        ycT_v = ycT_d.ap().rearrange("(c p) t -> p c t", p=128)
        wokeys = [("wo", 0), ("wo", 4)]
        xctr = [0]
        hctr = [0]

        def loads2(b):
            bb = b % 2
            tok0 = b * BT
            P.op("sync", lambda e, bb=bb, tok0=tok0: e.dma_start(out=nTb[bb][:], in_=nT_v[:, :, tok0:tok0 + BT]), writes=[("nTb", bb)], dma=True)
            P.op("sync", lambda e, bb=bb, tok0=tok0: e.dma_start(out=yaT[bb][:], in_=yaT_v[:, :, tok0:tok0 + BT]), writes=[("yaT", bb)], dma=True)
            P.op("sync", lambda e, bb=bb, tok0=tok0: e.dma_start(out=ycT[bb][:], in_=ycT_v[:, :, tok0:tok0 + BT]), writes=[("ycT", bb)], dma=True)

        def xload(t):
            P.op("sync", lambda e, t=t: e.dma_start(out=xt[t % 4][:], in_=x_d.ap()[t * 128:(t + 1) * 128, :]), writes=[("xt", t % 4)], dma=True)

        loads2(0)
        for t_ in range(3):
            xload(t_)
        def gates2(b):
            bb = b % 2
            tok0 = b * BT
            if b + 1 < NB:
                loads2(b + 1)
            for j in range(8):
                z = j % 2
                for (dst, dkey, col0) in ((A0, "A0", 0), (A1, "A1", 1024)):
                    for c in range(8):
                        P.op("tensor", lambda e, c=c, dst=dst, col0=col0, j=j, bb=bb: e.matmul(
                            dst[:], lhsT=Wg[:, c, col0 + j * 128:col0 + (j + 1) * 128], rhs=nTb[bb][:, c, :], start=(c == 0), stop=(c == 7)),
                            reads=[("Wg", col0 + (j // 2) * 256), ("nTb", bb)], writes=[dkey])
                for (dst, dkey, wsrc, wkey, asrc, akey) in ((A2, "A2", wpa, "wpa", yaT, "yaT"), (B0, ("B0", 0), wpc, "wpc", ycT, "ycT")):
                    for c in range(4):
                        P.op("tensor", lambda e, c=c, dst=dst, wsrc=wsrc, asrc=asrc, j=j, bb=bb: e.matmul(
                            dst[:, 0:512], lhsT=wsrc[:, c, j * 128:(j + 1) * 128], rhs=asrc[bb][:, c, :], start=(c == 0), stop=(c == 3)),
                            reads=[wkey, (akey, bb)], writes=[dkey])
                P.op("scalar", lambda e, z=z, j=j: e.activation(out=tA[z][:], in_=A0[:], func=AF.Tanh, scale=0.5, bias=hbcol[:, 24 + j:25 + j]),
                     reads=["A0", "hbcol"], writes=[("tA", z)])
                P.op("scalar", lambda e, z=z, j=j: e.activation(out=tC[z][:], in_=A1[:], func=AF.Tanh, scale=0.5, bias=hbcol[:, 32 + j:33 + j]),
                     reads=["A1", "hbcol"], writes=[("tC", z)])
                P.op("vector", lambda e, z=z: e.scalar_tensor_tensor(out=mA[z][:], in0=tA[z][:], scalar=1.0, in1=A2[:], op0=ALU.add, op1=ALU.mult),
                     reads=[("tA", z), "A2"], writes=[("mA", z)])
                P.op("vector", lambda e, z=z: e.scalar_tensor_tensor(out=mC[z][:], in0=tC[z][:], scalar=1.0, in1=B0[:, 0:512], op0=ALU.add, op1=ALU.mult),
                     reads=[("tC", z), ("B0", 0)], writes=[("mC", z)])
                P.op("vector", lambda e, z=z, j=j, bb=bb: e.tensor_tensor(out=mT[bb][:, j, :], in0=mA[z][:], in1=mC[z][:], op=ALU.add),
                     reads=[("mA", z), ("mC", z)], writes=[("mT", bb, j)])

        def hsec2(b):
            bb = b % 2
            tok0 = b * BT
            mkeys = [("mT", bb, j) for j in range(8)]
            def hmm(tl):
                t = 4 * b + tl
                xs_ = t % 4
                hs_ = t % 3
                if t + 3 < NT:
                    xload(t + 3)
                HB = [(B1[:, 0:512], ("B1", 0)), (B1[:, 512:1024], ("B1", 1))] if tl % 2 == 0 else [(A0[:], "A0"), (A1[:], "A1")]
                for half in range(2):
                    hacc, hkey = HB[half]
                    for j in range(8):
                        P.op("tensor", lambda e, j=j, half=half, tl=tl, bb=bb, hacc=hacc: e.matmul(
                            hacc, lhsT=mT[bb][:, j, tl * 128:(tl + 1) * 128], rhs=wo[:, j, half * 512:(half + 1) * 512],
                            start=(j == 0), stop=(j == 7)),
                            reads=mkeys + wokeys, writes=[hkey])
                    P.op("vector", lambda e, half=half, xs_=xs_, hs_=hs_, hacc=hacc: e.scalar_tensor_tensor(
                        out=ht[hs_][:, half * 512:(half + 1) * 512], in0=hacc, scalar=0.5,
                        in1=xt[xs_][:, half * 512:(half + 1) * 512], op0=ALU.mult, op1=ALU.add),
                        reads=[hkey, ("xt", xs_)] + ([("ht", hs_)] if half == 1 else []), writes=[("ht", hs_)])
                P.op("sync", lambda e, t=t, hs_=hs_: e.dma_start(out=h_d.ap()[t * 128:(t + 1) * 128, :], in_=ht[hs_][:]),
                     reads=[("ht", hs_)], writes=[("h_d", t)], dma=True)

            def hnorm(tl):
                t = 4 * b + tl
                hs_ = t % 3
                rmsnorm_tile(ht[hs_][:], gffn[:], n2[bb][:, tl, :], ("ht", hs_), "gffn", ("n2", bb, tl),
                             pool=not (b == NB - 1 and 3 in phases))

            def htr(tl):
                t = 4 * b + tl
                z2 = t % 2
                transpose_tile(n2[bb][:, tl, :], 8, n2T[z2][:], ("n2", bb, tl), ("n2T", z2))
                for c in range(8):
                    P.op("tensor", lambda e, c=c, tl=tl, z2=z2: e.matmul(
                        B0[:, 512 + tl * 36:512 + (tl + 1) * 36], lhsT=n2T[z2][:, c, :], rhs=wrg[:, c, :], start=(c == 0), stop=(c == 7)),
                        reads=[("n2T", z2), "wrg"], writes=[("lgp", tl)])

            hmm(0)
            hnorm(0)
            for tl in range(4):
                if tl + 1 < 4:
                    hmm(tl + 1)
                htr(tl)
                if tl + 1 < 4:
                    hnorm(tl + 1)

        def rout2a(b):
            bb = b % 2
            tok0 = b * BT
            lgp = B0[:, 512:512 + 144].rearrange("p (t n) -> p t n", t=4)
            R = []

            def V(fn, reads, writes):
                P.op("vector", fn, reads=reads, writes=writes)

            V(lambda e: e.tensor_tensor(out=lg[:], in0=lgp, in1=bc_mid(brg[:], 4), op=ALU.add),
              [("lgp", tl) for tl in range(4)] + ["brg"], ["lg"])
            V(lambda e: e.tensor_reduce(out=gmax[:], in_=lg[:, :, 0:4], axis=AX.X, op=ALU.max), ["lg"], ["gmax"])
            V(lambda e: e.tensor_tensor(out=gmask[:], in0=lg[:, :, 0:4], in1=bc_last(gmax[:], 4), op=ALU.is_equal), ["lg", "gmax"], ["gmask"])
            V(lambda e: e.tensor_tensor(out=gex[:], in0=lg[:, :, 0:4], in1=bc_last(gmax[:], 4), op=ALU.subtract), ["lg", "gmax"], ["gex"])
            P.op("scalar", lambda e: e.activation(out=gex[:], in_=gex[:], func=AF.Exp), reads=["gex"], writes=["gex"])
            V(lambda e: e.tensor_reduce(out=gse[:], in_=gex[:], axis=AX.X, op=ALU.add), ["gex"], ["gse"])
            V(lambda e: e.reciprocal(out=gse[:], in_=gse[:]), ["gse"], ["gse"])
            V(lambda e: e.tensor_scalar(out=pen[:], in0=gmask[:], scalar1=1.0, scalar2=1e30, op0=ALU.subtract, op1=ALU.mult), ["gmask"], ["pen"])
            V(lambda e: e.tensor_tensor(out=elm[:].rearrange("p t (g k) -> p t g k", g=4),
                                        in0=lg[:, :, 4:36].rearrange("p t (g k) -> p t g k", g=4),
                                        in1=bc_last(pen[:], 8), op=ALU.add), ["lg", "pen"], ["elm"])
            V(lambda e: e.tensor_reduce(out=m1[:], in_=elm[:], axis=AX.X, op=ALU.max), ["elm"], ["m1"])
            V(lambda e: e.tensor_tensor(out=mk1[:], in0=elm[:], in1=bc_last(m1[:], 32), op=ALU.is_equal), ["elm", "m1"], ["mk1"])
            V(lambda e: e.scalar_tensor_tensor(out=elm2[:], in0=mk1[:], scalar=-1e30, in1=elm[:], op0=ALU.mult, op1=ALU.add), ["mk1", "elm"], ["elm2"])
            V(lambda e: e.tensor_reduce(out=m2[:], in_=elm2[:], axis=AX.X, op=ALU.max), ["elm2"], ["m2"])
            V(lambda e: e.tensor_tensor(out=mk2[:], in0=elm2[:], in1=bc_last(m2[:], 32), op=ALU.is_equal), ["elm2", "m2"], ["mk2"])
            V(lambda e: e.tensor_tensor(out=dd[:], in0=m2[:], in1=m1[:], op=ALU.subtract), ["m1", "m2"], ["dd"])
            P.op("scalar", lambda e: e.activation(out=ee[:], in_=dd[:], func=AF.Exp), reads=["dd"], writes=["ee"])
            V(lambda e: e.tensor_scalar(out=rr[:], in0=ee[:], scalar1=1.0, scalar2=None, op0=ALU.add), ["ee"], ["rr"])
            V(lambda e: e.reciprocal(out=rr[:], in_=rr[:]), ["rr"], ["rr"])
            V(lambda e: e.tensor_tensor(out=wA[:], in0=gse[:], in1=rr[:], op=ALU.mult), ["gse", "rr"], ["wA"])
            V(lambda e: e.tensor_tensor(out=wB[:], in0=wA[:], in1=ee[:], op=ALU.mult), ["wA", "ee"], ["wB"])
            V(lambda e: e.tensor_tensor(out=Mb[:], in0=mk1[:], in1=mk2[:], op=ALU.add), ["mk1", "mk2"], ["Mb"])

        def rout2b(b):
            bb = b % 2
            tok0 = b * BT

            def V(fn, reads, writes):
                P.op("vector", fn, reads=reads, writes=writes)

            for tl in range(4):
                P.op("tensor", lambda e, tl=tl: e.matmul(A0[:, tl * 32:(tl + 1) * 32], lhsT=utri[:], rhs=Mb[:, tl, :], start=True, stop=(tl == 0)),
                     reads=["utri", "Mb"], writes=["A0"])
                for t2 in range(tl):
                    P.op("tensor", lambda e, tl=tl, t2=t2: e.matmul(A0[:, tl * 32:(tl + 1) * 32], lhsT=ones[:], rhs=Mb[:, t2, :], start=False, stop=(t2 == tl - 1)),
                         reads=["ones", "Mb"], writes=["A0"])
            for tl in range(4):
                P.op("tensor", lambda e, tl=tl: e.matmul(A1[:, 0:32], lhsT=ones[:], rhs=Mb[:, tl, :], start=(tl == 0), stop=(tl == 3)),
                     reads=["ones", "Mb"], writes=["A1"])
            V(lambda e: e.tensor_tensor(out=pos[:], in0=A0[:, 0:128].rearrange("p (t n) -> p t n", t=4), in1=bc_mid(cnt[:], 4), op=ALU.add),
              ["A0", "cnt"], ["pos"])
            V(lambda e: e.tensor_tensor(out=cnt[:], in0=cnt[:], in1=A1[:, 0:32], op=ALU.add), ["A1", "cnt", "pos"], ["cnt"])
            V(lambda e: e.tensor_scalar(out=okm[:], in0=pos[:], scalar1=float(CAP), scalar2=None, op0=ALU.is_lt), ["pos"], ["okm"])
            V(lambda e: e.tensor_tensor(out=slot[:], in0=pos[:], in1=bc_mid(ecap[:], 4), op=ALU.add), ["pos", "ecap"], ["slot"])
            V(lambda e: e.tensor_scalar(out=tmp[:], in0=okm[:], scalar1=-1.0e6, scalar2=1.0e6, op0=ALU.mult, op1=ALU.add), ["okm"], ["tmp"])
            V(lambda e: e.tensor_tensor(out=slot[:], in0=slot[:], in1=tmp[:], op=ALU.add), ["slot", "tmp"], ["slot"])
            V(lambda e: e.tensor_scalar(out=slot[:], in0=slot[:], scalar1=float(NSLOT), scalar2=None, op0=ALU.min), ["slot"], ["slot"])
            for k, mk in ((0, mk1), (1, mk2)):
                V(lambda e, mk=mk: e.tensor_tensor(out=tmp[:], in0=mk[:], in1=slot[:], op=ALU.mult), ["mk1", "mk2", "slot"], ["tmp"])
                V(lambda e, k=k: e.tensor_reduce(out=dsel[:, :, k], in_=tmp[:], axis=AX.X, op=ALU.add), ["tmp"], [("dsel", k)])
                V(lambda e, mk=mk: e.tensor_tensor(out=tmp[:], in0=mk[:], in1=okm[:], op=ALU.mult), ["mk1", "mk2", "okm", ("dsel", k)], ["tmp"])
                V(lambda e, k=k: e.tensor_reduce(out=oksel[:, :, k], in_=tmp[:], axis=AX.X, op=ALU.add), ["tmp"], [("oksel", k)])
            tb = 4 * b
            V(lambda e, tb=tb: e.tensor_copy(out=dtab[:, tb:tb + 4, :], in_=dsel[:]), [("dsel", 0), ("dsel", 1)], [("dtab", b)])
            V(lambda e, tb=tb: e.tensor_tensor(out=wtab[:, tb:tb + 4, 0], in0=wA[:], in1=oksel[:, :, 0], op=ALU.mult), ["wA", ("oksel", 0)], [("wtab", b, 0)])
            V(lambda e, tb=tb: e.tensor_tensor(out=wtab[:, tb:tb + 4, 1], in0=wB[:], in1=oksel[:, :, 1], op=ALU.mult), ["wB", ("oksel", 1)], [("wtab", b, 1)])
            for tl in range(4):
                t = 4 * b + tl
                for k in range(2):
                    P.op("gpsimd", lambda e, t=t, k=k, tl=tl, bb=bb: e.indirect_dma_start(
                        out=xs_d[:, :], out_offset=bass.IndirectOffsetOnAxis(ap=dtab[:, t, k:k + 1], axis=0),
                        in_=n2[bb][:, tl, :], in_offset=None),
                        reads=[("n2", bb, tl), ("dtab", b)], writes=[("xs_d", t, k)], dma=True)

        gates2(0)
        for b in range(NB):
            hsec2(b)
            rout2a(b)
            if b + 1 < NB:
                gates2(b + 1)
            rout2b(b)
            if b == NB - 2 and 3 in phases:
                for i in range(2):
                    expert_weight_loads(i, pre3[i]["w1"], pre3[i]["w3"], pre3[i]["w2"], [("w1p", i), ("w3p", i), ("w2p", i)],
                                        extra_writes=WGKEYS + ["wpa", "wpc"])
        if debug:
            P.op("sync", lambda e: e.dma_start(out=dtab_o.ap(), in_=dtab[:].rearrange("p t k -> p (t k)")),
                 reads=[("dtab", b) for b in range(NB)], dma=True, is_out=True)
            P.op("sync", lambda e: e.dma_start(out=wtab_o.ap(), in_=wtab[:].rearrange("p t k -> p (t k)")),
                 reads=[("wtab", b, k) for b in range(NB) for k in range(2)], dma=True, is_out=True)
        P.barrier()
    if 2 in phases:
        phase2()
    sb.reset(base_mark)

    TOP = SBAlloc.HI - 20 * 1024
    p4w = {"wpg": nc.alloc_sbuf_tensor_at("wpg_top", [128, 8, D], BF16, offset=TOP),
           "wpp": nc.alloc_sbuf_tensor_at("wpp_top", [128, 2, D], BF16, offset=TOP + 16 * 1024)}

    def p4_weight_loads():
        wpg_v = wpg_d.ap().rearrange("(c p) n -> p c n", p=128)
        for c in range(0, 8, 4):
            P.op("gpsimd", lambda e, c=c: e.dma_start(out=p4w["wpg"][:, c:c + 4, :], in_=wpg_v[:, c:c + 4, :]), writes=[("wpg", c)], dma=True)
        P.op("gpsimd", lambda e: e.dma_start(out=p4w["wpp"][:], in_=wpp_d.ap().rearrange("(c p) n -> p c n", p=128)), writes=["wpp"], dma=True)

    def phase3():
        NWB = 3
        w1b, w3b, w2b = [], [], []
        for i in range(NWB):
            if i < 2:
                assert sb.reserve("wexp%d" % i, 24576) == BASE + i * 24576
                w1b.append(pre3[i]["w1"])
                w3b.append(pre3[i]["w3"])
                w2b.append(pre3[i]["w2"])
            else:
                w1b.append(sb.alloc("w1b%d" % i, [128, 8, 512], BF16))
                w3b.append(sb.alloc("w3b%d" % i, [128, 8, 512], BF16))
                w2b.append(sb.alloc("w2b%d" % i, [128, 4, D], BF16))
        preloaded = 2 if 2 in phases else 0
        xr = [sb.alloc("xr%d" % i, [128, 3, D], BF16) for i in range(3)]
        xsT = [sb.alloc("xsT%d" % i, [128, 8, CAP], BF16) for i in range(2)]
        s1 = [sb.alloc("s1%d" % i, [128, CAP], F32) for i in range(2)]
        hdn = [sb.alloc("hdn%d" % i, [128, 4, CAP], BF16) for i in range(2)]
        yb = [sb.alloc("yb%d" % i, [128, D], BF16) for i in range(3)]
        HACC = [(A0, "A0", A1, "A1"), (A2, "A2", B0, ("B0", 0))]
        yctr = [0]
        P.op("gpsimd", lambda e: e.memset(yb[0][:], 0.0), writes=[("yb", 0, 0), ("yb", 0, 1)])
        P.op("sync", lambda e: e.dma_start(out=ys_d.ap()[NSLOT:NSLOT + 128, :], in_=yb[0][:]),
             reads=[("yb", 0, 0), ("yb", 0, 1)], writes=["ys_trash"], dma=True)
        def wload(ex):
            wb_ = ex % NWB
            if ex < preloaded:
                return
            expert_weight_loads(ex, w1b[wb_], w3b[wb_], w2b[wb_], [("w1b", wb_), ("w3b", wb_), ("w2b", wb_)])

        def xsload(ex):
            e3 = ex % 3
            P.op("sync", lambda e, ex=ex, e3=e3: e.dma_start(
                out=xr[e3][:], in_=xs_d.ap()[ex * CAP:(ex + 1) * CAP, :].rearrange("(r p) d -> p r d", p=128)),
                writes=[("xr", e3)], dma=True)

        def xsT_group(ex, r):
            eb = ex % 2
            e3 = ex % 3
            transpose_tile(xr[e3][:, r, :], 8, xsT[eb][:, :, r * 128:(r + 1) * 128], ("xr", e3), ("xsT", eb, r),
                           evac=("scalar" if r % 2 == 0 else "vector"), interleave=8)

        xsload(0)
        wload(0)
        xsload(1)
        wload(1)
        for r in range(3):
            xsT_group(0, r)
        for ex in range(NE):
            eb = ex % 2
            wb_ = ex % NWB
            if ex + 2 < NE:
                wload(ex + 2)
                xsload(ex + 2)
            if ex == 2:
                p4_weight_loads()
            xk = [("xsT", eb, r) for r in range(3)]
            for f in range(4):
                a1, k1, a3, k3 = HACC[f % 2]
                z = f % 2
                for (dst, dkey, wsrc, wkey) in ((a1, k1, w1b, "w1b"), (a3, k3, w3b, "w3b")):
                    for c in range(8):
                        P.op("tensor", lambda e, c=c, dst=dst, wsrc=wsrc, f=f, eb=eb, wb_=wb_: e.matmul(
                            dst[:, 0:CAP], lhsT=wsrc[wb_][:, c, f * 128:(f + 1) * 128], rhs=xsT[eb][:, c, :], start=(c == 0), stop=(c == 7)),
                            reads=[(wkey, wb_)] + xk, writes=[dkey])
                P.op("scalar", lambda e, a1=a1, z=z: e.activation(out=s1[z][:], in_=a1[:, 0:CAP], func=AF.Silu), reads=[k1], writes=[("s1", z)])
                P.op("vector", lambda e, a3=a3, z=z, f=f, eb=eb: e.tensor_tensor(out=hdn[eb][:, f, :], in0=s1[z][:], in1=a3[:, 0:CAP], op=ALU.mult),
                     reads=[("s1", z), k3], writes=[("hdn", eb, f)])
            hk_ = [("hdn", eb, f) for f in range(4)]
            for r in range(3):
                if ex + 1 < NE:
                    xsT_group(ex + 1, r)
                ys_ = yctr[0] % 3
                yctr[0] += 1
                for half in range(2):
                    for f in range(4):
                        P.op("tensor", lambda e, f=f, half=half, r=r, eb=eb, wb_=wb_: e.matmul(
                            B1[:, half * 512:(half + 1) * 512], lhsT=hdn[eb][:, f, r * 128:(r + 1) * 128], rhs=w2b[wb_][:, f, half * 512:(half + 1) * 512],
                            start=(f == 0), stop=(f == 3)),
                            reads=hk_ + [("w2b", wb_)], writes=[("B1", half)])
                    if half == 0:
                        P.op("scalar", lambda e, ys_=ys_: e.activation(out=yb[ys_][:, 0:512], in_=B1[:, 0:512], func=AF.Copy),
                             reads=[("B1", 0)], writes=[("yb", ys_, 0)])
                    else:
                        P.op("vector", lambda e, ys_=ys_: e.tensor_copy(out=yb[ys_][:, 512:1024], in_=B1[:, 512:1024]),
                             reads=[("B1", 1)], writes=[("yb", ys_, 1)])
                row0 = ex * CAP + r * 128
                P.op("sync", lambda e, row0=row0, ys_=ys_: e.dma_start(out=ys_d.ap()[row0:row0 + 128, :], in_=yb[ys_][:]),
                     reads=[("yb", ys_, 0), ("yb", ys_, 1)], writes=[("ys_d", ex, r)], dma=True)
        P.barrier()
    if 3 in phases:
        phase3()
    sb.reset(base_mark)

    def phase4():
        wpg, wpp = p4w["wpg"], p4w["wpp"]
        gple = sb.alloc("gple", [128, D], F32)
        bpg_row = sb.alloc("bpg_row", [1, D], BF16)
        hb = [sb.alloc("hb%d" % i, [128, D], F32) for i in range(3)]
        y1 = [sb.alloc("y1%d" % i, [128, D], BF16) for i in range(3)]
        y2 = [sb.alloc("y2%d" % i, [128, D], BF16) for i in range(3)]
        pin = [sb.alloc("pin%d" % i, [128, 256], F32) for i in range(3)]
        pbf = [sb.alloc("pbf%d" % i, [128, 256], BF16) for i in range(2)]
        ppT = [sb.alloc("ppT%d" % i, [128, 2, 128], BF16) for i in range(2)]
        n3 = [sb.alloc("n3%d" % i, [128, D], BF16) for i in range(2)]
        n3T = [sb.alloc("n3T%d" % i, [128, 8, 128], BF16) for i in range(2)]
        gz = [sb.alloc("gz%d" % i, [128, D], F32) for i in range(2)]
        ob = [sb.alloc("ob%d" % i, [128, D], F32) for i in range(2)]
        ld("sync", gple[:], gple_d.ap().partition_broadcast(128), "gple")
        P.op("gpsimd", lambda e: e.dma_start(out=bpg_row[:], in_=bpg_d.ap()), writes=["bpg"], dma=True)
        def loads4(t):
            h3 = t % 3
            P.op("sync", lambda e, t=t, h3=h3: e.dma_start(out=hb[h3][:], in_=h_d.ap()[t * 128:(t + 1) * 128, :]), writes=[("hb", h3)], dma=True)
            P.op("sync", lambda e, t=t, h3=h3: e.dma_start(out=pin[h3][:], in_=p_d.ap()[t * 128:(t + 1) * 128, :]), writes=[("pin", h3)], dma=True)
            for (yy, ykey, k) in ((y1, "y1", 0), (y2, "y2", 1)):
                P.op("gpsimd", lambda e, yy=yy, k=k, t=t, h3=h3: e.indirect_dma_start(
                    out=yy[h3][:, :], out_offset=None, in_=ys_d[:, :], in_offset=bass.IndirectOffsetOnAxis(ap=dtab[:, t, k:k + 1], axis=0)), reads=["dtab_all"], writes=[(ykey, h3)], dma=True)

        def S1a(t):
            h3 = t % 3
            z = t % 2
            P.op("vector", lambda e, h3=h3, t=t: e.scalar_tensor_tensor(out=hb[h3][:], in0=y1[h3][:], scalar=wtab[:, t, 0:1], in1=hb[h3][:],
                                                                       op0=ALU.mult, op1=ALU.add), reads=[("y1", h3), ("hb", h3)], writes=[("hb", h3)])
            P.op("vector", lambda e, h3=h3, t=t: e.scalar_tensor_tensor(out=hb[h3][:], in0=y2[h3][:], scalar=wtab[:, t, 1:2], in1=hb[h3][:],
                                                                       op0=ALU.mult, op1=ALU.add), reads=[("y2", h3), ("hb", h3)], writes=[("hb", h3)])
            rmsnorm_tile(hb[h3][:], gple[:], n3[z][:], ("hb", h3), "gple", ("n3", z))
            P.op("scalar", lambda e, z=z, h3=h3: e.activation(out=pbf[z][:], in_=pin[h3][:], func=AF.Copy), reads=[("pin", h3)], writes=[("pbf", z)])

        def S1b(t):
            z = t % 2
            transpose_tile(n3[z], 8, n3T[z][:], ("n3", z), ("n3T", z))
            transpose_tile(pbf[z], 2, ppT[z][:], ("pbf", z), ("ppT", z))

        GACC = [((A0, "A0"), (A2, "A2")), ((A1, "A1"), (B0, ("B0", 0)))]

        def S2mm(t, half):
            z = t % 2
            (ga, gk), (pa_, pk) = GACC[half]
            for c in range(8):
                P.op("tensor", lambda e, c=c, half=half, ga=ga, z=z: e.matmul(
                    ga[:], lhsT=n3T[z][:, c, :], rhs=wpg[:, c, half * 512:(half + 1) * 512], start=(c == 0), stop=False),
                    reads=[("n3T", z), ("wpg", 0), ("wpg", 4)], writes=[gk])
            P.op("tensor", lambda e, half=half, ga=ga: e.matmul(
                ga[:], lhsT=ones[0:1, 0:128], rhs=bpg_row[0:1, half * 512:(half + 1) * 512], start=False, stop=True),
                reads=["ones", "bpg"], writes=[gk])
            for c in range(2):
                P.op("tensor", lambda e, c=c, half=half, pa_=pa_, z=z: e.matmul(
                    pa_[:, 0:512], lhsT=ppT[z][:, c, :], rhs=wpp[:, c, half * 512:(half + 1) * 512], start=(c == 0), stop=(c == 1)),
                    reads=[("ppT", z), "wpp"], writes=[pk])

        def S2tail(t):
            z = t % 2
            h3 = t % 3
            hsl = [slice(0, 512), slice(512, 1024)]
            for half in range(2):
                (ga, gk), (pa_, pk) = GACC[half]
                hs = hsl[half]
                P.op("scalar", lambda e, ga=ga, z=z, hs=hs: e.activation(out=gz[z][:, hs], in_=ga[:], func=AF.Tanh, scale=0.5),
                     reads=[gk], writes=[("gz", z, half)])
            for half in range(2):
                (ga, gk), (pa_, pk) = GACC[half]
                hs = hsl[half]
                P.op("vector", lambda e, pa_=pa_, z=z, hs=hs: e.scalar_tensor_tensor(out=gz[z][:, hs], in0=gz[z][:, hs], scalar=1.0, in1=pa_[:, 0:512],
                                                                                    op0=ALU.add, op1=ALU.mult), reads=[("gz", z, half), pk], writes=[("gz", z, half)])
                P.op("vector", lambda e, z=z, hs=hs, h3=h3: e.scalar_tensor_tensor(out=ob[z][:, hs], in0=gz[z][:, hs], scalar=0.5, in1=hb[h3][:, hs],
                                                                                  op0=ALU.mult, op1=ALU.add), reads=[("gz", z, half), ("hb", h3)], writes=[("ob", z, half)])

        loads4(0)
        loads4(1)
        S1a(0)
        S1b(0)
        for t in range(NT):
            z = t % 2
            if t + 2 < NT:
                loads4(t + 2)
            if t + 1 < NT:
                S1a(t + 1)
            S2mm(t, 0)
            S2mm(t, 1)
            S2tail(t)
            if t + 1 < NT:
                S1b(t + 1)
            P.op("sync", lambda e, t=t, z=z: e.dma_start(out=out_d.ap()[t * 128:(t + 1) * 128, :], in_=ob[z][:]),
                 reads=[("ob", z, 0), ("ob", z, 1)], dma=True, is_out=True)
    if 4 in phases:
        phase4()
    P.emit()
    return nc, P


def _sel_tables():
    f = np.arange(128)[:, None, None]
    j = np.arange(8)[None, :, None]
    m = np.arange(128)[None, None, :]
    hit = ((m // 8) == (2 * j + f // 64)).astype(np.float32)
    selA = (hit / 64.0).reshape(128, 8 * 128).astype(ml_dtypes.bfloat16)
    selB = (hit.transpose(2, 1, 0) / 8.0).reshape(128, 8 * 128).astype(ml_dtypes.bfloat16)
    return np.ascontiguousarray(selA), np.ascontiguousarray(selB)


def _host_layout(inp):
    f = lambda a: np.ascontiguousarray(np.asarray(a, dtype=np.float32))
    bf = ml_dtypes.bfloat16
    b_in = f(inp["b_in"])[0]
    rel = f(inp["rel_bias"])[0]
    jj = np.arange(5)[::-1][:, None, None]
    kk = np.arange(128)[None, :, None]
    qq = np.arange(128)[None, None, :]
    dist = qq - kk + 128 * (4 - jj)
    idx = np.clip(dist, -63, 256) + 63
    cdiff = (qq // 64) - (kk // 64) + 2 * (4 - jj)
    mask = ((cdiff >= 0) & (cdiff <= 8)).astype(np.float32)
    rbT = rel[:, idx]
    rbT = np.ascontiguousarray(rbT.transpose(2, 0, 1, 3)).reshape(128, NH * 5 * 128)
    maskT = np.ascontiguousarray(mask.transpose(1, 0, 2)).reshape(128, 5 * 128)
    cwv = f(inp["conv_w"])[0]
    cw = np.ascontiguousarray(cwv.reshape(3, 4, 128).transpose(2, 1, 0)).reshape(128, 12)
    cb = np.ascontiguousarray(f(inp["conv_b"])[0].reshape(4, 128).T)
    gq = f(inp["g_q"])[0]
    gk = f(inp["g_k"])[0]
    shared = {
        "g_mix": f(inp["g_mix"]),
        "w_in": f(inp["w_in"])[0],
        "bcol": np.ascontiguousarray(b_in.reshape(40, 128).T),
        "gqk": np.ascontiguousarray(np.stack([np.tile(gq, 2), np.tile(gk, 2)], axis=1)),
        "bv": np.ascontiguousarray(b_in[1024:1536].reshape(1, 512)),
        "rbT": rbT, "maskT": maskT, "cw": cw, "cb": cb,
        "w_pa": f(inp["w_pa"])[0], "w_pc": f(inp["w_pc"])[0], "w_o": f(inp["w_o"])[0],
        "g_ffn": f(inp["g_ffn"]),
        "w_rg": np.ascontiguousarray(np.concatenate([f(inp["w_group"])[0], f(inp["w_router"])[0]], axis=1)),
        "b_rg": np.ascontiguousarray(np.concatenate([f(inp["b_group"])[0], f(inp["b_router"])[0]])[None, :]),
        "w1": f(inp["w1"])[0], "w3": f(inp["w3"])[0], "w2": f(inp["w2"])[0],
        "g_ple": f(inp["g_ple"]), "w_pg": f(inp["w_ple_gate"])[0], "b_pg": f(inp["b_ple_gate"]),
        "w_pp": f(inp["w_ple_proj"])[0],
        "ident": np.eye(128, dtype=np.float32).astype(bf),
        "utri": np.triu(np.ones((128, 128), np.float32), 1).astype(bf),
        "ones": np.ones((128, 128), np.float32).astype(bf),
        "bdiag": (np.kron(np.eye(2, dtype=np.float32), np.ones((64, 64), np.float32)) / 64.0).astype(bf),
        "ecap": np.ascontiguousarray(np.broadcast_to((np.arange(NE, dtype=np.float32) * CAP)[None, :], (128, NE))),
        "selA": _sel_tables()[0], "selB": _sel_tables()[1],
    }
    x = f(inp["x"])
    p = f(inp["p"])[0]
    maps = []
    for c in range(NCORES):
        m = dict(shared)
        m["x"] = x[c]
        m["p"] = p[c]
        maps.append(m)
    return maps


_CACHE = {}


def kernel(**inputs):
    if "nc" not in _CACHE:
        _CACHE["nc"] = build(debug=False)[0]
    nc = _CACHE["nc"]
    maps = _host_layout(inputs)
    res = run_bass_kernel_spmd(nc, maps, core_ids=list(range(NCORES)))
    out = np.stack([np.asarray(res.results[c]["out"], dtype=np.float32) for c in range(NCORES)], axis=0)
    return out
```
